# Optimizing a Trainium2 kernel written in Bass

```python
import math
import jax, jax.numpy as jnp
from jax import lax
import numpy as np

D_MODEL = 1024
BATCH = 8
SEQ = 4096
DEPTH = 4

RET_HEADS = 4
RET_DK = 128
RET_DV = 128
RET_CHUNK = 128
ROPE_BASE = 10000.0
NSA_HEADS = 8
NSA_KV_GROUPS = 2
NSA_HPG = NSA_HEADS // NSA_KV_GROUPS
NSA_HD = 64
CMP_BLOCK = 32
CMP_STRIDE = 16
SLC_BLOCK = 64
N_SELECT = 8
FORCED_LOCAL = 2
FORCE_BONUS = 1.0e4
WINDOW = 512
Q_BLOCK = 128
HGRN_HEADS = 8
HGRN_DK = D_MODEL // HGRN_HEADS
HGRN_DV = D_MODEL // HGRN_HEADS
HGRN_CHUNK = 64
D_FF = -(-8 * D_MODEL // (3 * 256)) * 256
EPS = 1e-6

EVEN_SIZES = ([RET_HEADS * RET_DK] * 2 + [RET_HEADS * RET_DV] * 2 + [NSA_HEADS * NSA_HD]
              + [NSA_KV_GROUPS * NSA_HD] * 6 + [3 * NSA_HEADS])
EVEN_IN = sum(EVEN_SIZES)
EVEN_MIX = RET_HEADS * RET_DV + NSA_HEADS * NSA_HD
ODD_SIZES = [HGRN_HEADS * HGRN_DK] * 2 + [HGRN_HEADS * HGRN_DV] * 2
ODD_IN = sum(ODD_SIZES)
ODD_MIX = HGRN_HEADS * HGRN_DV
N_EVEN = (DEPTH + 1) // 2
N_ODD = DEPTH // 2

kernel_name = "retnet_nsa_hgrn2_hybrid_trunk"


def _split(a, sizes):
    return jnp.split(a, list(np.cumsum(sizes)[:-1]), axis=-1)


def rmsnorm(x, g):
    xf = x.astype(jnp.float32)
    y = xf * lax.rsqrt(jnp.mean(xf * xf, axis=-1, keepdims=True) + EPS)
    return (y * g.astype(jnp.float32)).astype(x.dtype)


def rotary(x):
    S, d = x.shape[-2], x.shape[-1]
    half = d // 2
    inv = ROPE_BASE ** (-jnp.arange(half, dtype=jnp.float32) / half)
    ang = jnp.arange(S, dtype=jnp.float32)[:, None] * inv[None, :]
    cos, sin = jnp.cos(ang), jnp.sin(ang)
    x1, x2 = x[..., :half], x[..., half:]
    return jnp.concatenate([x1 * cos - x2 * sin, x1 * sin + x2 * cos], axis=-1)


def masked_softmax(s, mask):
    s = jnp.where(mask, s, -jnp.inf)
    m = jnp.max(s, axis=-1, keepdims=True)
    m = jnp.where(jnp.isfinite(m), m, 0.0)
    p = jnp.exp(s - m)
    return p / jnp.maximum(jnp.sum(p, axis=-1, keepdims=True), 1e-30)


def retention_chunkwise(q, k, v):
    B, H, S, dk = q.shape
    dv = v.shape[-1]
    C = min(RET_CHUNK, S)
    NC = S // C
    log_gamma = jnp.log(1.0 - 2.0 ** (-5.0 - jnp.arange(H, dtype=jnp.float32)))
    q = q.reshape(B, H, NC, C, dk)
    k = k.reshape(B, H, NC, C, dk)
    v = v.reshape(B, H, NC, C, dv)
    idx = jnp.arange(C, dtype=jnp.float32)
    diff = idx[:, None] - idx[None, :]
    causal = diff >= 0
    dmat = jnp.where(causal, jnp.exp(log_gamma[:, None, None] * jnp.where(causal, diff, 0.0)), 0.0)
    scores = jnp.einsum('bhncd,bhnsd->bhncs', q, k) * dmat[None, :, None]
    o_inner = jnp.einsum('bhncs,bhnse->bhnce', scores, v)
    k_dec = k * jnp.exp(log_gamma[:, None] * (C - 1 - idx)[None, :])[None, :, None, :, None]
    chunk_kv = jnp.einsum('bhncd,bhnce->nbhde', k_dec, v)
    gamma_c = jnp.exp(log_gamma * C)[None, :, None, None]

    def step(state, kv):
        return gamma_c * state + kv, state

    _, r_prev = lax.scan(step, jnp.zeros((B, H, dk, dv), jnp.float32), chunk_kv)
    q_dec = q * jnp.exp(log_gamma[:, None] * (idx + 1.0)[None, :])[None, :, None, :, None]
    o_cross = jnp.einsum('bhncd,nbhde->bhnce', q_dec, r_prev)
    return (o_inner + o_cross).reshape(B, H, S, dv)


def nsa_compress(kv, pos_emb, w1, w2):
    B, G, S, d = kv.shape
    r = CMP_BLOCK // CMP_STRIDE
    parts = kv.reshape(B, G, S // CMP_STRIDE, CMP_STRIDE, d)
    n = S // CMP_STRIDE - r + 1
    blocks = jnp.concatenate([parts[:, :, j:j + n] for j in range(r)], axis=3)
    flat = (blocks + pos_emb.astype(jnp.float32)).reshape(B, G, n, CMP_BLOCK * d)
    return jax.nn.gelu(flat @ w1.astype(jnp.float32)) @ w2.astype(jnp.float32)


def nsa_attention(q, k_cmp, v_cmp, k_slc, v_slc, k_win, v_win, gates):
    B, G, HPG, S, d = q.shape
    scale = d ** -0.5
    n_cmp = k_cmp.shape[2]
    n_slc = S // SLC_BLOCK
    n_sel = min(N_SELECT, n_slc)
    cs = np.arange(n_cmp) * CMP_STRIDE
    ce = cs + CMP_BLOCK - 1
    ss = np.arange(n_slc) * SLC_BLOCK
    se = ss + SLC_BLOCK - 1
    overlap = jnp.asarray(((cs[:, None] <= se[None, :]) & (ce[:, None] >= ss[None, :])).astype(np.float32))
    cmp_end = jnp.asarray(ce.astype(np.int32))
    ks_blocks = k_slc.reshape(B, G, n_slc, SLC_BLOCK, d)
    vs_blocks = v_slc.reshape(B, G, n_slc, SLC_BLOCK, d)
    kw_pad = jnp.pad(k_win, ((0, 0), (0, 0), (WINDOW, 0), (0, 0)))
    vw_pad = jnp.pad(v_win, ((0, 0), (0, 0), (WINDOW, 0), (0, 0)))
    bi = jnp.arange(B)[:, None, None, None]
    gi = jnp.arange(G)[None, :, None, None]
    jblk = jnp.arange(n_slc)
    offs = jnp.arange(SLC_BLOCK)

    def block(i):
        q0 = i * Q_BLOCK
        t = q0 + jnp.arange(Q_BLOCK)
        qb = lax.dynamic_slice_in_dim(q, q0, Q_BLOCK, axis=3)
        s_c = jnp.einsum('bghqd,bgnd->bghqn', qb, k_cmp) * scale
        p_c = masked_softmax(s_c, cmp_end[None, :] <= t[:, None])
        o_c = jnp.einsum('bghqn,bgnd->bghqd', p_c, v_cmp)
        imp = jnp.einsum('bgqn,nj->bgqj', jnp.sum(p_c, axis=2), overlap)
        bt = (t // SLC_BLOCK)[:, None]
        valid_s = jblk[None, :] <= bt
        forced = (jblk[None, :] == 0) | ((bt - jblk[None, :] >= 0) & (bt - jblk[None, :] < FORCED_LOCAL))
        score = jnp.where(valid_s, imp + jnp.where(forced, FORCE_BONUS, 0.0), -1e30)
        _, idx = lax.top_k(score, n_sel)
        kg = ks_blocks[bi, gi, idx].reshape(B, G, Q_BLOCK, n_sel * SLC_BLOCK, d)
        vg = vs_blocks[bi, gi, idx].reshape(B, G, Q_BLOCK, n_sel * SLC_BLOCK, d)
        kpos = (idx[..., None] * SLC_BLOCK + offs).reshape(B, G, Q_BLOCK, n_sel * SLC_BLOCK)
        s_s = jnp.einsum('bghqd,bgqkd->bghqk', qb, kg) * scale
        p_s = masked_softmax(s_s, (kpos <= t[None, None, :, None])[:, :, None])
        o_s = jnp.einsum('bghqk,bgqkd->bghqd', p_s, vg)
        kw = lax.dynamic_slice_in_dim(kw_pad, q0, WINDOW + Q_BLOCK, axis=2)
        vw = lax.dynamic_slice_in_dim(vw_pad, q0, WINDOW + Q_BLOCK, axis=2)
        spos = q0 - WINDOW + jnp.arange(WINDOW + Q_BLOCK)
        dist = t[:, None] - spos[None, :]
        valid_w = (dist >= 0) & (dist < WINDOW) & (spos[None, :] >= 0)
        s_w = jnp.einsum('bghqd,bgkd->bghqk', qb, kw) * scale
        o_w = jnp.einsum('bghqk,bgkd->bghqd', masked_softmax(s_w, valid_w), vw)
        gb = lax.dynamic_slice_in_dim(gates, q0, Q_BLOCK, axis=4)[..., None]
        return gb[0] * o_c + gb[1] * o_s + gb[2] * o_w

    out = lax.map(block, jnp.arange(S // Q_BLOCK))
    return jnp.transpose(out, (1, 2, 3, 0, 4, 5)).reshape(B, G * HPG, S, d)


def retention_nsa_mixer(h, w_in, w_out, pos_k, w1_k, w2_k, pos_v, w1_v, w2_v):
    B, S, _ = h.shape
    proj = (h @ w_in).astype(jnp.float32)
    (rq, rk, rv, rg, nq, kc, vc, ks, vs, kw, vw, ng) = _split(proj, EVEN_SIZES)
    heads = lambda a, n: jnp.transpose(a.reshape(B, S, n, -1), (0, 2, 1, 3))
    q_r = rotary(heads(rq, RET_HEADS))
    k_r = rotary(heads(rk, RET_HEADS)) * RET_DK ** -0.5
    o_r = retention_chunkwise(q_r, k_r, heads(rv, RET_HEADS))
    mu = jnp.mean(o_r, axis=-1, keepdims=True)
    var = jnp.mean(jnp.square(o_r - mu), axis=-1, keepdims=True)
    o_r = (o_r - mu) * lax.rsqrt(var + 1e-5)
    o_r = jnp.transpose(o_r, (0, 2, 1, 3)).reshape(B, S, RET_HEADS * RET_DV) * jax.nn.silu(rg)
    q_n = jnp.transpose(nq.reshape(B, S, NSA_KV_GROUPS, NSA_HPG, NSA_HD), (0, 2, 3, 1, 4))
    kvh = lambda a: jnp.transpose(a.reshape(B, S, NSA_KV_GROUPS, NSA_HD), (0, 2, 1, 3))
    k_cmp = nsa_compress(kvh(kc), pos_k, w1_k, w2_k)
    v_cmp = nsa_compress(kvh(vc), pos_v, w1_v, w2_v)
    gates = jnp.transpose(jax.nn.sigmoid(ng).reshape(B, S, 3, NSA_KV_GROUPS, NSA_HPG), (2, 0, 3, 4, 1))
    o_n = nsa_attention(q_n, k_cmp, v_cmp, kvh(ks), kvh(vs), kvh(kw), kvh(vw), gates)
    o_n = jnp.transpose(o_n, (0, 2, 1, 3)).reshape(B, S, NSA_HEADS * NSA_HD)
    mixed = jnp.concatenate([o_r, o_n], axis=-1).astype(h.dtype)
    return mixed @ w_out


def hgrn2_chunkwise(q, k, v, log_f):
    B, H, S, dk = q.shape
    dv = v.shape[-1]
    C = min(HGRN_CHUNK, S)
    NC = S // C
    to_chunks = lambda a: jnp.moveaxis(a.reshape(B, H, NC, C, a.shape[-1]), 2, 0)
    causal = jnp.tril(jnp.ones((C, C), dtype=bool))[:, :, None]

    def step(state, xs):
        qc, kc, vc, gc = xs
        b = jnp.cumsum(gc, axis=2)
        diff = b[:, :, :, None, :] - b[:, :, None, :, :]
        decay = jnp.exp(jnp.where(causal, diff, -jnp.inf))
        attn = jnp.sum(qc[:, :, :, None, :] * decay * kc[:, :, None, :, :], axis=-1)
        o = (jnp.einsum('bhnm,bhme->bhne', attn, vc)
             + jnp.einsum('bhnd,bhde->bhne', qc * jnp.exp(b), state))
        b_last = b[:, :, -1:, :]
        new_state = (jnp.exp(b_last[:, :, 0, :])[..., None] * state
                     + jnp.einsum('bhmd,bhme->bhde', kc * jnp.exp(b_last - b), vc))
        return new_state, o

    _, o = lax.scan(step, jnp.zeros((B, H, dk, dv), jnp.float32),
                    (to_chunks(q), to_chunks(k), to_chunks(v), to_chunks(log_f)))
    return jnp.moveaxis(o, 0, 2).reshape(B, H, S, dv)


def hgrn2_mixer(h, w_in, w_out, norm_g, lb):
    B, S, _ = h.shape
    proj = (h @ w_in).astype(jnp.float32)
    qp, fp, ip, gp = _split(proj, ODD_SIZES)
    heads = lambda a: jnp.transpose(a.reshape(B, S, HGRN_HEADS, -1), (0, 2, 1, 3))
    lb = jnp.maximum(lb, 0.0)
    log_f = jnp.logaddexp(jnp.log(lb), jnp.log1p(-lb) + jax.nn.log_sigmoid(fp))
    k = (1.0 - lb) * jax.nn.sigmoid(-fp)
    q = jax.nn.silu(qp) * HGRN_DK ** -0.5
    o = hgrn2_chunkwise(heads(q), heads(k), heads(ip), heads(log_f))
    o = o * lax.rsqrt(jnp.mean(o * o, axis=-1, keepdims=True) + EPS) * norm_g.astype(jnp.float32)
    o = jnp.transpose(o, (0, 2, 1, 3)).reshape(B, S, ODD_MIX) * jax.nn.sigmoid(gp)
    return o.astype(h.dtype) @ w_out


def swiglu(h, w1, w3, w2):
    return (jax.nn.silu(h @ w1) * (h @ w3)) @ w2


def setup_inputs(seed: int = 0) -> dict:
    key = jax.random.key(seed)
    ks = jax.random.split(key, 20)
    nrm = lambda k, shape, s: jax.random.normal(k, shape, jnp.float32) * s
    return {
        "x": nrm(ks[0], (BATCH, SEQ, D_MODEL), 1.0),
        "norm_mix_g": 1.0 + nrm(ks[1], (DEPTH, D_MODEL), 0.05),
        "norm_ffn_g": 1.0 + nrm(ks[2], (DEPTH, D_MODEL), 0.05),
        "final_norm_g": 1.0 + nrm(ks[3], (D_MODEL,), 0.05),
        "even_w_in": nrm(ks[4], (N_EVEN, D_MODEL, EVEN_IN), D_MODEL ** -0.5),
        "even_w_out": nrm(ks[5], (N_EVEN, EVEN_MIX, D_MODEL), EVEN_MIX ** -0.5),
        "cmp_pos_k": nrm(ks[6], (N_EVEN, CMP_BLOCK, NSA_HD), 0.1),
        "cmp_w1_k": nrm(ks[7], (N_EVEN, CMP_BLOCK * NSA_HD, NSA_HD), (CMP_BLOCK * NSA_HD) ** -0.5),
        "cmp_w2_k": nrm(ks[8], (N_EVEN, NSA_HD, NSA_HD), NSA_HD ** -0.5),
        "cmp_pos_v": nrm(ks[9], (N_EVEN, CMP_BLOCK, NSA_HD), 0.1),
        "cmp_w1_v": nrm(ks[10], (N_EVEN, CMP_BLOCK * NSA_HD, NSA_HD), (CMP_BLOCK * NSA_HD) ** -0.5),
        "cmp_w2_v": nrm(ks[11], (N_EVEN, NSA_HD, NSA_HD), NSA_HD ** -0.5),
        "odd_w_in": nrm(ks[12], (N_ODD, D_MODEL, ODD_IN), D_MODEL ** -0.5),
        "odd_w_out": nrm(ks[13], (N_ODD, ODD_MIX, D_MODEL), ODD_MIX ** -0.5),
        "hgrn_norm_g": 1.0 + nrm(ks[14], (N_ODD, HGRN_DV), 0.05),
        "hgrn_lb_logits": nrm(ks[15], (N_ODD, HGRN_HEADS * HGRN_DK), 1.0),
        "ffn_w1": nrm(ks[16], (DEPTH, D_MODEL, D_FF), D_MODEL ** -0.5),
        "ffn_w3": nrm(ks[17], (DEPTH, D_MODEL, D_FF), D_MODEL ** -0.5),
        "ffn_w2": nrm(ks[18], (DEPTH, D_FF, D_MODEL), D_FF ** -0.5),
    }


def reference(x, norm_mix_g, norm_ffn_g, final_norm_g, even_w_in, even_w_out,
              cmp_pos_k, cmp_w1_k, cmp_w2_k, cmp_pos_v, cmp_w1_v, cmp_w2_v,
              odd_w_in, odd_w_out, hgrn_norm_g, hgrn_lb_logits,
              ffn_w1, ffn_w3, ffn_w2):
    lb_sm = jax.nn.softmax(hgrn_lb_logits.astype(jnp.float32), axis=0)
    lb_all = jnp.cumsum(lb_sm, axis=0) - lb_sm[0:1]
    for layer in range(DEPTH):
        h = rmsnorm(x, norm_mix_g[layer])
        j = layer // 2
        if layer % 2 == 0:
            mix = retention_nsa_mixer(h, even_w_in[j], even_w_out[j],
                                      cmp_pos_k[j], cmp_w1_k[j], cmp_w2_k[j],
                                      cmp_pos_v[j], cmp_w1_v[j], cmp_w2_v[j])
        else:
            mix = hgrn2_mixer(h, odd_w_in[j], odd_w_out[j], hgrn_norm_g[j], lb_all[j])
        x = x + mix.astype(x.dtype)
        h = rmsnorm(x, norm_ffn_g[layer])
        x = x + swiglu(h, ffn_w1[layer], ffn_w3[layer], ffn_w2[layer]).astype(x.dtype)
    return rmsnorm(x, final_norm_g)
```

```python
import math
from contextlib import ExitStack

import numpy as np
import concourse.bass as bass
import concourse.mybir as mybir
from concourse.bass_utils import run_bass_kernel_spmd

F32 = mybir.dt.float32
BF16 = mybir.dt.bfloat16
AF = mybir.ActivationFunctionType
ALU = mybir.AluOpType
AX = mybir.AxisListType

D = 1024
DFF = 2816
NFF = DFF // 128
EPS = 1e-6
EVEN_IN = 3352


class Buf:
    def __init__(self, t, key):
        self.t = t
        self.key = key

    def __getitem__(self, k):
        return self.t[k]


class Ctx:
    NDMA = 8
    ENG = ("pe", "act", "dve", "pool", "sp")

    def __init__(self, nc):
        self.nc = nc
        self.streams = {e: [] for e in self.ENG}
        self.cnt = {e: 0 for e in ("pe", "act", "dve", "pool")}
        self.dman = {q: 0 for q in ("sp", "act", "pool")}
        self.lastw = {}
        self.readers = {}
        self.known = {e: {} for e in self.ENG}
        self.all_tokens = {}
        self.nbuf = 0

    def sb(self, es, name, shape, dtype):
        self.nbuf += 1
        nm = "%s_%d" % (name, self.nbuf)
        t = es.enter_context(self.nc.sbuf_tensor(nm, list(shape), dtype))
        return Buf(t, nm)

    def ps(self, es, name, shape, dtype):
        self.nbuf += 1
        nm = "%s_%d" % (name, self.nbuf)
        t = es.enter_context(self.nc.psum_tensor(nm, list(shape), dtype))
        return Buf(t, nm)

    @staticmethod
    def _k(x):
        return x.key if isinstance(x, Buf) else x

    def _collect(self, eng, reads, writes):
        deps = []
        for r in reads:
            k = self._k(r)
            if k in self.lastw:
                deps.append(self.lastw[k])
        for w in writes:
            k = self._k(w)
            if k in self.lastw:
                deps.append(self.lastw[k])
            deps.extend(self.readers.get(k, ()))
        return deps

    def _record(self, tok, reads, writes):
        for r in reads:
            self.readers.setdefault(self._k(r), []).append(tok)
        for w in writes:
            k = self._k(w)
            self.lastw[k] = tok
            self.readers[k] = []

    def _waits(self, eng, deps, is_pe_compute):
        waits = {}
        kn = self.known[eng]
        for (sk, v, src) in deps:
            if is_pe_compute and src == "pe":
                continue
            if kn.get(sk, 0) >= v:
                continue
            if waits.get(sk, 0) < v:
                waits[sk] = v
        for sk, v in waits.items():
            kn[sk] = v
        return list(waits.items())

    def op(self, eng, fn, reads=(), writes=()):
        deps = self._collect(eng, reads, writes)
        waits = self._waits(eng, deps, eng == "pe")
        self.cnt[eng] += 1
        tok = (eng, self.cnt[eng], eng)
        self.streams[eng].append((waits, fn, (eng, 1)))
        self.all_tokens[eng] = tok
        self._record(tok, reads, writes)

    def dma(self, out, in_, reads=(), writes=(), q="sp", **kw):
        deps = self._collect(q, reads, writes)
        n = self.dman[q]
        slot = n % self.NDMA
        sk = ("dma", q, slot)
        if n >= self.NDMA:
            deps.append((sk, 16 * (n // self.NDMA), "dma"))
        waits = self._waits(q, deps, False)
        self.dman[q] += 1
        tok = (sk, 16 * (n // self.NDMA + 1), "dma")
        self.streams[q].append((waits, lambda e: e.dma_start(out=out, in_=in_, **kw), (sk, 16)))
        self.all_tokens[sk] = tok
        self._record(tok, reads, writes)

    def barrier(self):
        toks = list(self.all_tokens.values())
        for e in self.ENG:
            waits = self._waits(e, toks, False)
            if waits:
                self.streams[e].append((waits, None, None))

    def pe(self, fn, r=(), w=()):
        self.op("pe", fn, r, w)

    def act(self, fn, r=(), w=()):
        self.op("act", fn, r, w)

    def dve(self, fn, r=(), w=()):
        self.op("dve", fn, r, w)

    def pool(self, fn, r=(), w=()):
        self.op("pool", fn, r, w)

    def emit(self):
        nc = self.nc
        self.barrier()
        with ExitStack() as es:
            sems = {}
            for e in ("pe", "act", "dve", "pool"):
                sems[e] = es.enter_context(nc.semaphore("s_" + e))
            for q in ("sp", "act", "pool"):
                for s in range(self.NDMA):
                    sems[("dma", q, s)] = es.enter_context(nc.semaphore("d_%s%d" % (q, s)))
            block = es.enter_context(nc.Block())
            streams = self.streams

            def replay(name, eng):
                for waits, fn, inc in streams[name]:
                    for sk, v in waits:
                        eng.wait_ge(sems[sk], v)
                    if fn is not None:
                        fn(eng).then_inc(sems[inc[0]], inc[1])

            if streams["sp"]:
                @block.sync
                def _(eng):
                    replay("sp", eng)
            if streams["pe"]:
                @block.tensor
                def _(eng):
                    replay("pe", eng)
            if streams["dve"]:
                @block.vector
                def _(eng):
                    replay("dve", eng)
            if streams["act"]:
                @block.scalar
                def _(eng):
                    replay("act", eng)
            if streams["pool"]:
                @block.gpsimd
                def _(eng):
                    replay("pool", eng)


def load_weight_bf16(cx, wb, w_ap, kchunks, ncols, stage):
    step = stage[0].t.shape[1]
    i = 0
    for c in range(kchunks):
        for c0 in range(0, ncols, step):
            n = min(step, ncols - c0)
            st = stage[i % len(stage)]
            cx.dma(st[:, 0:n], w_ap[c * 128:(c + 1) * 128, c0:c0 + n], writes=[st])
            if i % 2 == 0:
                cx.dve(lambda e, st=st, c=c, c0=c0, n=n: e.tensor_copy(wb[:, c, c0:c0 + n], st[:, 0:n]),
                       r=[st], w=[(wb.key, c, c0)])
            else:
                cx.act(lambda e, st=st, c=c, c0=c0, n=n: e.copy(wb[:, c, c0:c0 + n], st[:, 0:n]),
                       r=[st], w=[(wb.key, c, c0)])
            i += 1
    return wb


class NormTools:
    def __init__(self, cx, es, g_ap):
        self.cx = cx
        self.ident = cx.sb(es, "ident", [128, 128], BF16)
        self.gT = cx.sb(es, "gT", [128, 8], F32)
        self.ss = [cx.sb(es, "ss%d" % i, [128, 1], F32) for i in range(2)]
        self.rstd = [cx.sb(es, "rstd%d" % i, [128, 1], F32) for i in range(2)]
        self.junk = cx.sb(es, "junk", [128, D], BF16)
        self.hb = [cx.sb(es, "hb%d" % i, [128, D], BF16) for i in range(2)]
        self.tp = [cx.ps(es, "tp%d" % i, [128, 8, 128], BF16) for i in range(2)]
        self.i = 0
        cx.dma(self.gT[:], g_ap.rearrange("(c p) -> p c", p=128), writes=[self.gT],
               allow_slow_non_contiguous=True)

    def load_ident(self, ident_ap):
        self.cx.dma(self.ident[:], ident_ap, writes=[self.ident])

    def stats(self, xt):
        cx = self.cx
        i = self.i
        self.i += 1
        ss, rstd = self.ss[i % 2], self.rstd[i % 2]
        junk = self.junk
        cx.act(lambda e: e.activation(junk[:], xt[:], AF.Square, scale=1.0 / 32.0, accum_out=ss[:]),
               r=[xt], w=[junk, ss])
        cx.act(lambda e: e.activation(ss[:], ss[:], AF.Sqrt, bias=EPS, scale=1.0), r=[ss], w=[ss])
        cx.dve(lambda e: e.reciprocal(rstd[:], ss[:]), r=[ss], w=[rstd])
        return rstd

    def run(self, xt, hT, col0, scale_by_g=True):
        cx = self.cx
        i = self.i
        self.i += 1
        ss, rstd, hb, tp = self.ss[i % 2], self.rstd[i % 2], self.hb[i % 2], self.tp[i % 2]
        junk = self.junk
        cx.act(lambda e: e.activation(junk[:], xt[:], AF.Square, scale=1.0 / 32.0, accum_out=ss[:]),
               r=[xt], w=[junk, ss])
        cx.act(lambda e: e.activation(ss[:], ss[:], AF.Sqrt, bias=EPS, scale=1.0), r=[ss], w=[ss])
        cx.dve(lambda e: e.reciprocal(rstd[:], ss[:]), r=[ss], w=[rstd])
        cx.act(lambda e: e.activation(hb[:], xt[:], AF.Copy, scale=rstd[:]), r=[xt, rstd], w=[hb])
        for c in range(8):
            cx.pe(lambda e, c=c: e.transpose(tp[:, c, :], hb[:, c * 128:(c + 1) * 128], self.ident[:]),
                  r=[hb, self.ident], w=[tp])
        gT = self.gT
        cx.dve(lambda e: e.tensor_tensor(hT[:, :, col0:col0 + 128], tp[:],
                                         gT[:].unsqueeze(2).to_broadcast([128, 8, 128]), ALU.mult),
               r=[tp, gT], w=[hT])
        return rstd


def dram_fm(ap, c0, c1, s0, s1):
    return ap[c0:c1, :, s0:s1].rearrange("c p s -> p c s")


def stage_norm0(cx, x_ap, hT_ap, g_ap, ident_ap, S):
    with ExitStack() as es:
        nt = NormTools(cx, es, g_ap)
        nt.load_ident(ident_ap)
        xts = [cx.sb(es, "xt%d" % i, [128, D], F32) for i in range(2)]
        hTs = [cx.sb(es, "hTt%d" % i, [128, 8, 512], BF16) for i in range(2)]
        for m in range(S // 512):
            hT = hTs[m % 2]
            for j in range(4):
                t = m * 4 + j
                xt = xts[t % 2]
                cx.dma(xt[:], x_ap[t * 128:(t + 1) * 128, :], writes=[xt])
                nt.run(xt, hT, j * 128)
            cx.dma(dram_fm(hT_ap, 0, 8, m * 512, (m + 1) * 512), hT[:], reads=[hT], q="pool")
    cx.barrier()


def stage_out(cx, mixT_ap, w_ap, xin_ap, xout_ap, hT_ap, g_ap, ident_ap, S):
    with ExitStack() as es:
        wb = cx.sb(es, "woutb", [128, 8, D], BF16)
        with ExitStack() as es2:
            stg = [cx.sb(es2, "wstg%d" % i, [128, 1024], F32) for i in range(2)]
            load_weight_bf16(cx, wb, w_ap, 8, D, stg)
            cx.barrier()
        nt = NormTools(cx, es, g_ap)
        nt.load_ident(ident_ap)
        mts = [cx.sb(es, "mixt%d" % i, [128, 8, 512], BF16) for i in range(2)]
        xts = [cx.sb(es, "xt%d" % i, [128, D], F32) for i in range(2)]
        x1s = [cx.sb(es, "x1t%d" % i, [128, D], F32) for i in range(2)]
        hTs = [cx.sb(es, "hTt%d" % i, [128, 8, 512], BF16) for i in range(2)]
        yps = [cx.ps(es, "yps%d" % i, [128, 512], F32) for i in range(4)]
        for m in range(S // 512):
            mt = mts[m % 2]
            hT = hTs[m % 2]
            cx.dma(mt[:], dram_fm(mixT_ap, 0, 8, m * 512, (m + 1) * 512), writes=[mt])
            for j in range(4):
                t = m * 4 + j
                xt = xts[t % 2]
                x1 = x1s[t % 2]
                cx.dma(xt[:], xin_ap[t * 128:(t + 1) * 128, :], writes=[xt])
                for half in range(2):
                    yp = yps[(t * 2 + half) % 4]
                    for c in range(8):
                        cx.pe(lambda e, yp=yp, c=c, j=j, half=half, mt=mt: e.matmul(
                            yp[:], mt[:, c, j * 128:(j + 1) * 128], wb[:, c, half * 512:(half + 1) * 512],
                            start=(c == 0), stop=(c == 7)), r=[mt, wb], w=[yp])
                    cx.dve(lambda e, yp=yp, half=half, xt=xt, x1=x1: e.tensor_tensor(
                        x1[:, half * 512:(half + 1) * 512], yp[:], xt[:, half * 512:(half + 1) * 512], ALU.add),
                        r=[yp, xt], w=[x1])
                cx.dma(xout_ap[t * 128:(t + 1) * 128, :], x1[:], reads=[x1], q="pool")
                nt.run(x1, hT, j * 128)
            cx.dma(dram_fm(hT_ap, 0, 8, m * 512, (m + 1) * 512), hT[:], reads=[hT], q="pool")
    cx.barrier()


def stage_ffn(cx, hT_ap, w1_ap, w3_ap, w2_ap, xin_ap, xout_ap, hTout_ap, g_ap, ident_ap, S, final,
              gfin_ap=None, y_ap=None):
    MT = 256
    with ExitStack() as es:
        w1b = cx.sb(es, "w1b", [128, 8, DFF], BF16)
        w3b = cx.sb(es, "w3b", [128, 8, DFF], BF16)
        w2b = cx.sb(es, "w2b", [128, NFF, D], BF16)
        with ExitStack() as es2:
            stg = [cx.sb(es2, "wstg%d" % i, [128, 1408], F32) for i in range(2)]
            load_weight_bf16(cx, w1b, w1_ap, 8, DFF, stg)
            load_weight_bf16(cx, w3b, w3_ap, 8, DFF, stg)
            load_weight_bf16(cx, w2b, w2_ap, NFF, D, stg)
            cx.barrier()
        nt = NormTools(cx, es, g_ap)
        nt.load_ident(ident_ap)
        if final:
            gfin = cx.sb(es, "gfin", [128, D], F32)
            cx.dma(gfin[:], gfin_ap.partition_broadcast(128), writes=[gfin])
        hins = [cx.sb(es, "hin%d" % i, [128, 8, MT], BF16) for i in range(2)]
        gT = cx.sb(es, "gTff", [128, NFF, MT], BF16)
        sil = [cx.sb(es, "sil%d" % i, [128, MT], F32) for i in range(2)]
        xts = [cx.sb(es, "xt%d" % i, [128, D], F32) for i in range(2)]
        x2s = [cx.sb(es, "x2t%d" % i, [128, D], F32) for i in range(2)]
        hTs = [cx.sb(es, "hTt%d" % i, [128, 8, MT], BF16) for i in range(2)]
        ups = [cx.ps(es, "ups%d" % i, [128, 512], F32) for i in range(4)]
        yps = [cx.ps(es, "yps%d" % i, [128, 512], F32) for i in range(2)]
        nsub = MT // 128
        for m in range(S // MT):
            hin = hins[m % 2]
            hT = hTs[m % 2]
            cx.dma(hin[:], dram_fm(hT_ap, 0, 8, m * MT, (m + 1) * MT), writes=[hin])
            for f in range(NFF):
                u1 = ups[(f % 2) * 2]
                u3 = ups[(f % 2) * 2 + 1]
                sl = sil[f % 2]
                for (wb, up) in ((w1b, u1), (w3b, u3)):
                    for c in range(8):
                        cx.pe(lambda e, wb=wb, up=up, c=c, f=f, hin=hin: e.matmul(
                            up[:, 0:MT], wb[:, c, f * 128:(f + 1) * 128], hin[:, c, :],
                            start=(c == 0), stop=(c == 7)), r=[wb, hin], w=[up])
                cx.act(lambda e, u1=u1, sl=sl: e.activation(sl[:], u1[:, 0:MT], AF.Silu), r=[u1], w=[sl])
                cx.dve(lambda e, u3=u3, sl=sl, f=f: e.tensor_tensor(gT[:, f, :], u3[:, 0:MT], sl[:], ALU.mult),
                       r=[u3, sl], w=[gT])
            for j in range(nsub):
                t = m * nsub + j
                xt = xts[t % 2]
                x2 = x2s[t % 2]
                cx.dma(xt[:], xin_ap[t * 128:(t + 1) * 128, :], writes=[xt])
                for half in range(2):
                    yp = yps[half]
                    for f in range(NFF):
                        cx.pe(lambda e, yp=yp, f=f, j=j, half=half: e.matmul(
                            yp[:], gT[:, f, j * 128:(j + 1) * 128], w2b[:, f, half * 512:(half + 1) * 512],
                            start=(f == 0), stop=(f == NFF - 1)), r=[gT, w2b], w=[yp])
                    cx.dve(lambda e, yp=yp, half=half, xt=xt, x2=x2: e.tensor_tensor(
                        x2[:, half * 512:(half + 1) * 512], yp[:], xt[:, half * 512:(half + 1) * 512], ALU.add),
                        r=[yp, xt], w=[x2])
                if not final:
                    cx.dma(xout_ap[t * 128:(t + 1) * 128, :], x2[:], reads=[x2], q="pool")
                    nt.run(x2, hT, j * 128)
                else:
                    rstd = nt.stats(x2)
                    ot = xt
                    cx.dve(lambda e, x2=x2, rstd=rstd, ot=ot: e.scalar_tensor_tensor(
                        ot[:], x2[:], rstd[:], gfin[:], ALU.mult, ALU.mult), r=[x2, rstd, gfin], w=[ot])
                    cx.dma(y_ap[t * 128:(t + 1) * 128, :], ot[:], reads=[ot], q="pool")
            if not final:
                cx.dma(dram_fm(hTout_ap, 0, 8, m * MT, (m + 1) * MT), hT[:], reads=[hT], q="pool")
    cx.barrier()


DEBUG = {}
CH = 64
MTK = 512
NCH = MTK // CH


class GLACore:
    def __init__(self, cx, es, nheads, ident, maskT_ap):
        self.cx = cx
        self.ident = ident
        self.maskT = cx.sb(es, "maskT", [64, 64], F32)
        cx.dma(self.maskT[:], maskT_ap, writes=[self.maskT])
        self.S = [cx.sb(es, "S%d" % h, [128, 128], F32) for h in range(nheads)]
        for h in range(nheads):
            cx.dve(lambda e, h=h: e.memset(self.S[h][:], 0.0), w=[self.S[h]])
        self.ATb = cx.sb(es, "ATb", [64, NCH, 64], BF16)
        self.ktm = cx.sb(es, "ktm", [64, NCH, 128], BF16)
        self.KVd = cx.sb(es, "KVd", [128, NCH, 128], F32)
        self.spb = [cx.sb(es, "spb%d" % i, [128, 128], BF16) for i in range(2)]
        self.AT = cx.ps(es, "ATp", [64, NCH, 64], F32)
        self.KTt = cx.ps(es, "KTt", [64, NCH, 128], BF16)
        self.OT = cx.ps(es, "OTp", [128, MTK], F32)
        self.KV = [cx.ps(es, "KVp%d" % i, [128, 4, 128], F32) for i in range(2)]

    def run(self, h, qt, kt, v, vcol0, ebm, e2, dlast, oT_sb):
        cx = self.cx
        AT, ATb, KTt, ktm, KV, KVd, OT, S = self.AT, self.ATb, self.KTt, self.ktm, self.KV, self.KVd, self.OT, self.S[h]
        maskT, ident = self.maskT, self.ident

        def sc(x, c):
            return (x, []) if isinstance(x, float) else (x[1][:, c:c + 1], [x[0]])

        for c in range(NCH):
            cs = slice(c * CH, (c + 1) * CH)
            cx.pe(lambda e, c=c, cs=cs: e.matmul(AT[:, c, :], kt[:, cs], qt[:, cs], start=True, stop=True),
                  r=[kt, qt], w=[AT])
        cx.dve(lambda e: e.tensor_tensor(ATb[:], AT[:], maskT[:].unsqueeze(1).to_broadcast([64, NCH, 64]), ALU.mult),
               r=[AT, maskT], w=[ATb])
        for c in range(NCH):
            cs = slice(c * CH, (c + 1) * CH)
            cx.pe(lambda e, c=c, cs=cs: e.transpose(KTt[:, c, :], kt[:, cs], ident[:]), r=[kt, ident], w=[KTt])
        cx.act(lambda e: e.copy(ktm[:], KTt[:]), r=[KTt], w=[ktm])
        for c in range(NCH):
            cx.pe(lambda e, c=c: e.matmul(KV[c // 4][:, c % 4, :], ktm[:, c, :], v[:, c, vcol0:vcol0 + 128],
                                          start=True, stop=True), r=[ktm, v], w=[KV[c // 4]])
        for b in range(2):
            if isinstance(dlast, float):
                cx.dve(lambda e, b=b: e.tensor_scalar_mul(KVd[:, 4 * b:4 * b + 4, :], KV[b][:], dlast),
                       r=[KV[b]], w=[KVd])
            else:
                cx.dve(lambda e, b=b: e.tensor_tensor(
                    KVd[:, 4 * b:4 * b + 4, :], KV[b][:],
                    dlast[1][:, 4 * b:4 * b + 4].unsqueeze(2).to_broadcast([128, 4, 128]), ALU.mult),
                    r=[KV[b], dlast[0]], w=[KVd])
        for c in range(NCH):
            cs = slice(c * CH, (c + 1) * CH)
            spb = self.spb[c % 2]
            s_ebm, r_ebm = sc(ebm, c)
            s_e2, r_e2 = sc(e2, c)
            cx.act(lambda e, spb=spb, s_ebm=s_ebm: e.activation(spb[:], S[:], AF.Copy, scale=s_ebm),
                   r=[S] + r_ebm, w=[spb])
            cx.pe(lambda e, c=c, cs=cs: e.matmul(OT[:, cs], v[:, c, vcol0:vcol0 + 128], ATb[:, c, :],
                                                 start=True, stop=False), r=[v, ATb], w=[OT])
            cx.pe(lambda e, cs=cs, spb=spb: e.matmul(OT[:, cs], spb[:], qt[:, cs], start=False, stop=True),
                  r=[spb, qt], w=[OT])
            cx.dve(lambda e, c=c, s_e2=s_e2: e.scalar_tensor_tensor(S[:], S[:], s_e2, KVd[:, c, :], ALU.mult, ALU.add),
                   r=[S, KVd] + r_e2, w=[S])
        cx.act(lambda e: e.copy(oT_sb[:], OT[:]), r=[OT], w=[oT_sb])


def proj_fm(cx, ps, wb, col0, ncols, hin, n):
    for c in range(8):
        cx.pe(lambda e, c=c: e.matmul(ps[0:ncols, 0:n], wb[:, c, col0:col0 + ncols], hin[:, c, 0:n],
                                      start=(c == 0), stop=(c == 7)), r=[wb, hin], w=[ps])


def stage_hgrn(cx, hT_ap, w_ap, normg_ap, lb_ap, mixT_ap, ident_ap, maskT_ap, scanm_ap, S, layer_j):
    with ExitStack() as es:
        wb = cx.sb(es, "winb", [128, 8, 4096], BF16)
        with ExitStack() as es2:
            stg = [cx.sb(es2, "wstg%d" % i, [128, 2048], F32) for i in range(2)]
            load_weight_bf16(cx, wb, w_ap, 8, 4096, stg)
            cx.barrier()
        ident = cx.sb(es, "ident", [128, 128], BF16)
        cx.dma(ident[:], ident_ap, writes=[ident])
        onesb = cx.sb(es, "onesb", [128, 128], BF16)
        cx.dve(lambda e: e.memset(onesb[:], 1.0), w=[onesb])
        scanm = cx.sb(es, "scanm", [128, MTK], F32)
        cx.dma(scanm[:], scanm_ap, writes=[scanm])
        normg = cx.sb(es, "normg", [128, 1], F32)
        cx.dma(normg[:], normg_ap.rearrange("(p o) -> p o", o=1), writes=[normg])
        lbT = cx.sb(es, "lbT", [128, 8], F32)
        omlT = cx.sb(es, "omlT", [128, 8], F32)
        if layer_j == 0:
            cx.dve(lambda e: e.memset(lbT[:], 0.0), w=[lbT])
        else:
            l0 = cx.sb(es, "l0", [128, 8], F32)
            l1 = cx.sb(es, "l1", [128, 8], F32)
            cx.dma(l0[:], lb_ap[0, :].rearrange("(h p) -> p h", p=128), writes=[l0], allow_slow_non_contiguous=True)
            cx.dma(l1[:], lb_ap[1, :].rearrange("(h p) -> p h", p=128), writes=[l1], allow_slow_non_contiguous=True)
            cx.dve(lambda e: e.tensor_tensor(l1[:], l1[:], l0[:], ALU.subtract), r=[l0, l1], w=[l1])
            cx.act(lambda e: e.activation(lbT[:], l1[:], AF.Sigmoid), r=[l1], w=[lbT])
        cx.dve(lambda e: e.tensor_scalar(omlT[:], lbT[:], -1.0, 1.0, ALU.mult, ALU.add), r=[lbT], w=[omlT])
        core = GLACore(cx, es, 8, ident, maskT_ap)
        hins = [cx.sb(es, "hin%d" % i, [128, 8, MTK], BF16) for i in range(2)]
        v = cx.sb(es, "vtm", [64, NCH, 1024], BF16)
        f32t = lambda n: cx.sb(es, n, [128, MTK], F32)
        sq, sg, gl, bb, dd, eq, ek = [f32t(n) for n in ("sq", "sg", "gl", "bb", "dd", "eq", "ek")]
        ebm = cx.sb(es, "ebm", [128, NCH], F32)
        e2 = cx.sb(es, "e2", [128, NCH], F32)
        qt = cx.sb(es, "qt", [128, MTK], BF16)
        kt = cx.sb(es, "kt", [128, MTK], BF16)
        oT = f32t("oT")
        sqo = cx.sb(es, "sqo", [128, MTK], BF16)
        rt = f32t("rt")
        sgp = f32t("sgp")
        mts = [cx.sb(es, "mixt%d" % i, [128, 8, MTK], BF16) for i in range(2)]
        P = [cx.ps(es, "P%d" % i, [128, 512], F32) for i in range(3)]
        pi = [0]

        def nextP():
            pi[0] += 1
            return P[pi[0] % 3]

        def macro(m, hin, mt):
            cx.dma(hin[:], dram_fm(hT_ap, 0, 8, m * MTK, (m + 1) * MTK), writes=[hin])
            k = 0
            for c in range(NCH):
                for half in range(2):
                    ps = nextP()
                    for kc in range(8):
                        cx.pe(lambda e, ps=ps, kc=kc, c=c, half=half: e.matmul(
                            ps[0:64, :], hin[:, kc, c * CH:(c + 1) * CH],
                            wb[:, kc, 2048 + half * 512:2048 + (half + 1) * 512],
                            start=(kc == 0), stop=(kc == 7)), r=[hin, wb], w=[ps])
                    if k % 2 == 0:
                        cx.act(lambda e, ps=ps, c=c, half=half: e.copy(v[:, c, half * 512:(half + 1) * 512], ps[0:64, :]),
                               r=[ps], w=[v])
                    else:
                        cx.dve(lambda e, ps=ps, c=c, half=half: e.tensor_copy(v[:, c, half * 512:(half + 1) * 512], ps[0:64, :]),
                               r=[ps], w=[v])
                    k += 1
            for h in range(8):
                pq = nextP()
                proj_fm(cx, pq, wb, h * 128, 128, hin, MTK)
                cx.act(lambda e, pq=pq: e.activation(sq[:], pq[:], AF.Silu), r=[pq], w=[sq])
                pf = nextP()
                proj_fm(cx, pf, wb, 1024 + h * 128, 128, hin, MTK)
                cx.act(lambda e, pf=pf: e.activation(sg[:], pf[:], AF.Sigmoid), r=[pf], w=[sg])
                cx.dve(lambda e, h=h: e.tensor_scalar(sg[:], sg[:], omlT[:, h:h + 1], lbT[:, h:h + 1], ALU.mult, ALU.add),
                       r=[sg, omlT, lbT], w=[sg])
                cx.act(lambda e: e.activation(gl[:], sg[:], AF.Ln), r=[sg], w=[gl])
                cx.dve(lambda e: e.tensor_tensor_scan(bb[:], scanm[:], gl[:], 0.0, ALU.mult, ALU.add),
                       r=[scanm, gl], w=[bb])
                b3 = bb[:].rearrange("p (c n) -> p c n", n=CH)
                cx.dve(lambda e, b3=b3: e.tensor_tensor(
                    dd[:].rearrange("p (c n) -> p c n", n=CH), b3,
                    b3[:, :, 31:32].to_broadcast([128, NCH, CH]), ALU.subtract), r=[bb], w=[dd])
                cx.act(lambda e: e.activation(eq[:], dd[:], AF.Exp), r=[dd], w=[eq])
                cx.act(lambda e: e.activation(ek[:], dd[:], AF.Exp, scale=-1.0), r=[dd], w=[ek])
                cx.act(lambda e, b3=b3: e.activation(ebm[:], b3[:, :, 31], AF.Exp), r=[bb], w=[ebm])
                eq3 = eq[:].rearrange("p (c n) -> p c n", n=CH)
                cx.dve(lambda e, eq3=eq3: e.tensor_tensor(e2[:], ebm[:], eq3[:, :, CH - 1], ALU.mult),
                       r=[ebm, eq], w=[e2])
                cx.dve(lambda e: e.scalar_tensor_tensor(qt[:], sq[:], 128.0 ** -0.5, eq[:], ALU.mult, ALU.mult),
                       r=[sq, eq], w=[qt])
                cx.dve(lambda e: e.tensor_scalar(sg[:], sg[:], -1.0, 1.0, ALU.mult, ALU.add), r=[sg], w=[sg])
                cx.dve(lambda e: e.tensor_tensor(kt[:], sg[:], ek[:], ALU.mult), r=[sg, ek], w=[kt])
                core.run(h, qt, kt, v, h * 128, (ebm, ebm.t), (e2, e2.t), (eq, eq3[:, :, CH - 1]), oT)
                if DEBUG and m == DEBUG.get("m", 0) and h == 0:
                    for nm, bf in (("sq", sq), ("sg", sg), ("bb", bb), ("eq", eq), ("qt", qt), ("kt", kt), ("oT", oT), ("gl", gl)):
                        if nm in DEBUG:
                            cx.dma(DEBUG[nm], bf[:], reads=[bf], q="pool")
                    if "v" in DEBUG:
                        cx.dma(DEBUG["v"], v[:, :, 0:128], reads=[v], q="pool")
                cx.act(lambda e: e.activation(sqo[:], oT[:], AF.Square), r=[oT], w=[sqo])
                pss = nextP()
                cx.pe(lambda e, pss=pss: e.matmul(pss[:], onesb[:], sqo[:], start=True, stop=True),
                      r=[onesb, sqo], w=[pss])
                cx.act(lambda e, pss=pss: e.activation(rt[:], pss[:], AF.Sqrt, bias=EPS, scale=1.0 / 128.0),
                       r=[pss], w=[rt])
                cx.dve(lambda e: e.reciprocal(rt[:], rt[:]), r=[rt], w=[rt])
                pg = nextP()
                proj_fm(cx, pg, wb, 3072 + h * 128, 128, hin, MTK)
                cx.act(lambda e, pg=pg: e.activation(sgp[:], pg[:], AF.Sigmoid), r=[pg], w=[sgp])
                cx.dve(lambda e: e.scalar_tensor_tensor(rt[:], oT[:], normg[:, 0:1], rt[:], ALU.mult, ALU.mult),
                       r=[oT, normg, rt], w=[rt])
                cx.dve(lambda e, h=h, mt=mt: e.tensor_tensor(mt[:, h, :], rt[:], sgp[:], ALU.mult),
                       r=[rt, sgp], w=[mt])
            cx.dma(dram_fm(mixT_ap, 0, 8, m * MTK, (m + 1) * MTK), mt[:], reads=[mt], q="pool")

        for m in range(S // MTK):
            macro(m, hins[m % 2], mts[m % 2])
    cx.barrier()


RET_GAMMA = [1.0 - 2.0 ** (-5.0 - h) for h in range(4)]


def stage_ret(cx, hT_ap, w_ap, cos_ap, sin_ap, dec_ap, mixT_ap, ident_ap, maskT_ap, S):
    with ExitStack() as es:
        wb = cx.sb(es, "winb", [128, 8, 3072], BF16)
        with ExitStack() as es2:
            stg = [cx.sb(es2, "wstg%d" % i, [128, 1536], F32) for i in range(2)]
            load_weight_bf16(cx, wb, w_ap, 8, 3072, stg)
            cx.barrier()
        ident = cx.sb(es, "ident", [128, 128], BF16)
        cx.dma(ident[:], ident_ap, writes=[ident])
        onesb = cx.sb(es, "onesb", [128, 128], BF16)
        cx.dve(lambda e: e.memset(onesb[:], 1.0), w=[onesb])
        dec = cx.sb(es, "dec", [128, 8, MTK], F32)
        cx.dma(dec[:], dec_ap.rearrange("h t p n -> p (h t) n"), writes=[dec])
        core = GLACore(cx, es, 4, ident, maskT_ap)
        hins = [cx.sb(es, "hin%d" % i, [128, 8, MTK], BF16) for i in range(2)]
        coss = [cx.sb(es, "cos%d" % i, [128, MTK], F32) for i in range(2)]
        sins = [cx.sb(es, "sin%d" % i, [128, MTK], F32) for i in range(2)]
        v = cx.sb(es, "vtm", [64, NCH, 512], BF16)
        f32t = lambda n: cx.sb(es, n, [128, MTK], F32)
        t1, t2, oT, mean, var, sgp = [f32t(n) for n in ("t1", "t2", "oT", "mean", "var", "sgp")]
        qt = cx.sb(es, "qt", [128, MTK], BF16)
        kt = cx.sb(es, "kt", [128, MTK], BF16)
        ob = cx.sb(es, "ob", [128, MTK], BF16)
        sqo = cx.sb(es, "sqo", [128, MTK], BF16)
        mts = [cx.sb(es, "mixt%d" % i, [128, 4, MTK], BF16) for i in range(2)]
        P = [cx.ps(es, "P%d" % i, [128, 512], F32) for i in range(3)]
        pi = [0]

        def nextP():
            pi[0] += 1
            return P[pi[0] % 3]

        def rot(h, col0, tab, out, hin, cs, sn):
            pa = nextP()
            proj_fm(cx, pa, wb, col0 + h * 128, 128, hin, MTK)
            cx.dve(lambda e: e.tensor_tensor(t1[:], pa[:], cs[:], ALU.mult), r=[pa, cs], w=[t1])
            pb = nextP()
            proj_fm(cx, pb, wb, 1024 + col0 + h * 128, 128, hin, MTK)
            cx.dve(lambda e: e.tensor_tensor(t2[:], pb[:], sn[:], ALU.mult), r=[pb, sn], w=[t2])
            cx.dve(lambda e: e.tensor_tensor(t1[:], t1[:], t2[:], ALU.add), r=[t1, t2], w=[t1])
            cx.dve(lambda e: e.tensor_tensor(out[:], t1[:], dec[:, tab, :], ALU.mult), r=[t1, dec], w=[out])

        def macro(m, hin, mt, cs, sn):
            cx.dma(hin[:], dram_fm(hT_ap, 0, 8, m * MTK, (m + 1) * MTK), writes=[hin])
            cx.dma(cs[:], cos_ap[:, m * MTK:(m + 1) * MTK], writes=[cs])
            cx.dma(sn[:], sin_ap[:, m * MTK:(m + 1) * MTK], writes=[sn])
            for c in range(NCH):
                ps = nextP()
                for kc in range(8):
                    cx.pe(lambda e, ps=ps, kc=kc, c=c: e.matmul(
                        ps[0:64, :], hin[:, kc, c * CH:(c + 1) * CH], wb[:, kc, 2048:2560],
                        start=(kc == 0), stop=(kc == 7)), r=[hin, wb], w=[ps])
                if c % 2 == 0:
                    cx.act(lambda e, ps=ps, c=c: e.copy(v[:, c, :], ps[0:64, :]), r=[ps], w=[v])
                else:
                    cx.dve(lambda e, ps=ps, c=c: e.tensor_copy(v[:, c, :], ps[0:64, :]), r=[ps], w=[v])
            for h in range(4):
                g = RET_GAMMA[h]
                rot(h, 0, 2 * h, qt, hin, cs, sn)
                rot(h, 512, 2 * h + 1, kt, hin, cs, sn)
                core.run(h, qt, kt, v, h * 128, float(g ** 32), float(g ** 64), float(g ** 32), oT)
                cx.act(lambda e: e.copy(ob[:], oT[:]), r=[oT], w=[ob])
                cx.act(lambda e: e.activation(sqo[:], oT[:], AF.Square), r=[oT], w=[sqo])
                p1 = nextP()
                cx.pe(lambda e, p1=p1: e.matmul(p1[:], onesb[:], ob[:], start=True, stop=True), r=[onesb, ob], w=[p1])
                p2 = nextP()
                cx.pe(lambda e, p2=p2: e.matmul(p2[:], onesb[:], sqo[:], start=True, stop=True), r=[onesb, sqo], w=[p2])
                cx.act(lambda e, p1=p1: e.activation(mean[:], p1[:], AF.Copy, scale=1.0 / 128.0), r=[p1], w=[mean])
                cx.dve(lambda e: e.tensor_tensor(var[:], mean[:], mean[:], ALU.mult), r=[mean], w=[var])
                cx.dve(lambda e, p2=p2: e.scalar_tensor_tensor(var[:], p2[:], 1.0 / 128.0, var[:], ALU.mult, ALU.subtract),
                       r=[p2, var], w=[var])
                cx.act(lambda e: e.activation(var[:], var[:], AF.Sqrt, bias=1e-5, scale=1.0), r=[var], w=[var])
                cx.dve(lambda e: e.reciprocal(var[:], var[:]), r=[var], w=[var])
                cx.dve(lambda e: e.tensor_tensor(oT[:], oT[:], mean[:], ALU.subtract), r=[oT, mean], w=[oT])
                cx.dve(lambda e: e.tensor_tensor(oT[:], oT[:], var[:], ALU.mult), r=[oT, var], w=[oT])
                pg = nextP()
                proj_fm(cx, pg, wb, 2560 + h * 128, 128, hin, MTK)
                cx.act(lambda e, pg=pg: e.activation(sgp[:], pg[:], AF.Silu), r=[pg], w=[sgp])
                cx.dve(lambda e, h=h: e.tensor_tensor(mt[:, h, :], oT[:], sgp[:], ALU.mult), r=[oT, sgp], w=[mt])
            cx.dma(dram_fm(mixT_ap, 0, 4, m * MTK, (m + 1) * MTK), mt[:], reads=[mt], q="pool")

        for m in range(S // MTK):
            macro(m, hins[m % 2], mts[m % 2], coss[m % 2], sins[m % 2])
    cx.barrier()


def host_consts(S):
    import ml_dtypes
    c = {}
    c["ident"] = np.eye(128, dtype=np.float32).astype(ml_dtypes.bfloat16)
    m = np.arange(64)
    c["maskT"] = (m[:, None] <= m[None, :]).astype(np.float32)
    sm = np.ones((128, MTK), np.float32)
    sm[:, ::CH] = 0
    c["scanm"] = sm
    half = 64
    inv = (10000.0 ** (-np.arange(half, dtype=np.float32) / half)).astype(np.float32)
    ang = (np.arange(S, dtype=np.float32)[:, None] * inv[None, :]).astype(np.float32)
    cos = np.cos(ang).T.astype(np.float32)
    sin = np.sin(ang).T.astype(np.float32)
    c["cos"] = np.ascontiguousarray(np.concatenate([cos, cos], 0))
    c["sin"] = np.ascontiguousarray(np.concatenate([-sin, sin], 0))
    dec = np.zeros((4, 2, 128, MTK), np.float32)
    n = (np.arange(MTK) % CH).astype(np.float64)
    for h in range(4):
        g = RET_GAMMA[h]
        dec[h, 0] = (g ** (n - 31.0))[None, :]
        dec[h, 1] = (g ** (31.0 - n) * 128.0 ** -0.5)[None, :]
    c["dec"] = dec
    return c


NEG = -30000.0
NSA_PIPE = True
NSA_HOLD = True
NEG8 = NEG * 8.0


def nsa_consts(S):
    import ml_dtypes
    bf = ml_dtypes.bfloat16
    nb = S // 128
    c = {}
    tl = np.arange(128)
    n = np.arange(256)
    cm = np.full((nb, 128, 256), NEG8, np.float32)
    for i in range(nb):
        t = i * 128 + tl
        ok = (16 * n[None, :] + 31 <= t[:, None]) & (n[None, :] < S // 16 - 1)
        cm[i][ok] = 0.0
    c["cmask"] = cm.astype(bf)
    j = np.arange(64)
    fb = np.zeros((nb, 128, 64), np.float32)
    for i in range(nb):
        bt = (i * 128 + tl) // 64
        d = bt[:, None] - j[None, :]
        forced = (j[None, :] == 0) | ((d >= 0) & (d < 2))
        fb[i] = np.where(d >= 0, np.where(forced, 1.0e4, 0.0), -1.0e30)
    c["fbias"] = fb
    cs = np.arange(256) * 16
    ce = cs + 31
    ss = np.arange(64) * 64
    se = ss + 63
    ov = ((cs[:, None] <= se[None, :]) & (ce[:, None] >= ss[None, :])).astype(np.float32)
    ov[S // 16 - 1:, :] = 0
    c["ovl"] = ov.astype(bf)
    c["causal"] = np.where(tl[None, :] <= tl[:, None], 0.0, NEG8).astype(np.float32).astype(bf)
    kr = np.arange(640) - 512
    dist = tl[:, None] - kr[None, :]
    c["wmask"] = np.where((dist >= 0) & (dist < 512), 0.0, NEG8).astype(np.float32).astype(bf)
    c["rvalid"] = (tl >= 31).astype(np.float32).reshape(128, 1)
    kk = np.arange(S)
    c["blockE"] = (kk[None, :] // 64 == np.arange(64)[:, None]).astype(np.float32).astype(bf)
    return c


def stage_nsa(cx, hT_ap, w_ap, cw, mixT_ap, ident_ap, cn, S):
    NB = S // 128
    NT = S // 128
    with ExitStack() as es:
        wb = cx.sb(es, "wnsa", [128, 8, 1304], BF16)
        ksE = [cx.sb(es, "ksE%d" % g, [128, S], BF16) for g in range(2)]
        kwT = [cx.sb(es, "kwT%d" % g, [64, S], BF16) for g in range(2)]
        vsw = cx.sb(es, "vsw", [128, NT, 256], BF16)
        kcmpT = [cx.sb(es, "kcmpT%d" % g, [64, 256], BF16) for g in range(2)]
        vcmp = cx.sb(es, "vcmp", [128, 2, 2, 64], BF16)
        ident = cx.sb(es, "ident", [128, 128], BF16)
        P = [cx.ps(es, "P%d" % i, [128, 512], F32) for i in range(4)]
        TP = [cx.ps(es, "TP%d" % i, [128, 8, 128], BF16) for i in range(2)]
        PV = [cx.ps(es, "PV%d" % i, [128, 64], F32) for i in range(1)]
        IMP = cx.ps(es, "IMP", [128, 64], F32)
        cnt = {"p": 0, "tp": 0, "pv": 0, "cp": 0, "sp": 0}

        def nP():
            cnt["p"] += 1
            return P[cnt["p"] % 2]

        def nH():
            cnt["h"] = cnt.get("h", 0) + 1
            return P[2 + cnt["h"] % 2]

        def nTP():
            cnt["tp"] += 1
            return TP[cnt["tp"] % 2]

        def nPV():
            return PV[0]

        def cp(out_ap, in_ap, r, w):
            cnt["cp"] += 1
            if cnt["cp"] % 2:
                cx.act(lambda e: e.copy(out_ap, in_ap), r=r, w=w)
            else:
                cx.dve(lambda e: e.tensor_copy(out_ap, in_ap), r=r, w=w)

        with ExitStack() as es2:
            stg = [cx.sb(es2, "wstg%d" % i, [128, 1304], F32) for i in range(2)]
            load_weight_bf16(cx, wb, w_ap, 8, 1304, stg)
            cx.dma(ident[:], ident_ap, writes=[ident])
            for g in range(2):
                cx.dma(ksE[g][64:128, :], cn["blockE"], writes=[(ksE[g].key, "E")])
            cx.barrier()
            kcT = [cx.sb(es2, "kcT%d" % g, [64, S], BF16) for g in range(2)]
            vcT = [cx.sb(es2, "vcT%d" % g, [64, S], BF16) for g in range(2)]
            hins = [cx.sb(es2, "hin%d" % i, [128, 8, MTK], BF16) for i in range(2)]

            def phaseA(m, hin):
                cx.dma(hin[:], dram_fm(hT_ap, 0, 8, m * MTK, (m + 1) * MTK), writes=[hin])
                for (dst, col) in ((kcT, 512), (vcT, 640), (ksE, 768), (kwT, 896)):
                    for g in range(2):
                        ps = nP()
                        proj_fm(cx, ps, wb, col + g * 64, 64, hin, MTK)
                        cp(dst[g][0:64, m * MTK:(m + 1) * MTK], ps[0:64, :], [ps], [dst[g]])
                for j in range(4):
                    ps = nP()
                    for kc in range(8):
                        cx.pe(lambda e, ps=ps, kc=kc, j=j: e.matmul(
                            ps[:, 0:256], hin[:, kc, j * 128:(j + 1) * 128], wb[:, kc, 1024:1280],
                            start=(kc == 0), stop=(kc == 7)), r=[hin, wb], w=[ps])
                    cp(vsw[:, m * 4 + j, :], ps[:, 0:256], [ps], [vsw])

            for m in range(S // MTK):
                phaseA(m, hins[m % 2])

            w1s = cx.sb(es2, "w1s", [64, 32, 64], F32)
            w1b = cx.sb(es2, "w1b", [64, 32, 64], BF16)
            w2s = cx.sb(es2, "w2s", [64, 64], F32)
            w2b = cx.sb(es2, "w2b", [64, 64], BF16)
            poss = cx.sb(es2, "poss", [64, 32], F32)
            posb = cx.sb(es2, "posb", [64, 32], BF16)
            cb = cx.sb(es2, "cb", [64, 1], F32)
            tt = [cx.sb(es2, "gt%d" % i, [64, 256], F32) for i in range(3)]
            glb = cx.sb(es2, "glb", [64, 256], BF16)
            for g in range(2):
                cx.dve(lambda e, g=g: e.memset(kcmpT[g][:], 0.0), w=[kcmpT[g]])
            cx.dve(lambda e: e.memset(vcmp[:], 0.0), w=[vcmp])
            cx.dve(lambda e: e.memset(glb[:], 0.0), w=[glb])
            NCMP = S // 16 - 1

            def phaseB(kind, g, src):
                pos_ap, w1_ap, w2_ap = cw["pos_" + kind], cw["w1_" + kind], cw["w2_" + kind]
                if g == 0:
                    cx.dma(w1s[:], w1_ap.rearrange("(p d) o -> d p o", d=64), writes=[w1s])
                    cx.dma(w2s[:], w2_ap, writes=[w2s])
                    cx.dma(poss[:], pos_ap.rearrange("p d -> d p"), writes=[poss], allow_slow_non_contiguous=True)
                    cx.dve(lambda e: e.tensor_copy(w1b[:], w1s[:]), r=[w1s], w=[w1b])
                    cx.dve(lambda e: e.tensor_copy(w2b[:], w2s[:]), r=[w2s], w=[w2b])
                    cx.dve(lambda e: e.tensor_copy(posb[:], poss[:]), r=[poss], w=[posb])
                    pc = nPV()
                    for p in range(32):
                        cx.pe(lambda e, p=p, pc=pc: e.matmul(pc[0:64, 0:1], w1b[:, p, :], posb[:, p:p + 1],
                                                             start=(p == 0), stop=(p == 31)), r=[w1b, posb], w=[pc])
                    cx.act(lambda e, pc=pc: e.copy(cb[:], pc[0:64, 0:1]), r=[pc], w=[cb])
                ps = nP()
                x3 = src[0:64, :].rearrange("d (n s) -> d n s", s=16)
                for p in range(32):
                    n0, r_ = (0, p) if p < 16 else (1, p - 16)
                    cx.pe(lambda e, p=p, n0=n0, r_=r_, ps=ps: e.matmul(
                        ps[0:64, 0:NCMP], w1b[:, p, :], x3[:, n0:n0 + NCMP, r_],
                        start=(p == 0), stop=(p == 31)), r=[w1b, src], w=[ps])
                t0, t1_, t2_ = tt
                N = NCMP
                cx.act(lambda e, ps=ps: e.activation(t0[:, 0:N], ps[0:64, 0:N], AF.Identity, bias=cb[:], scale=1.0),
                       r=[ps, cb], w=[t0])
                cx.dve(lambda e: e.tensor_tensor(t1_[:, 0:N], t0[:, 0:N], t0[:, 0:N], ALU.mult), r=[t0], w=[t1_])
                cx.dve(lambda e: e.tensor_scalar(t1_[:, 0:N], t1_[:, 0:N], 0.044715, 1.0, ALU.mult, ALU.add), r=[t1_], w=[t1_])
                cx.dve(lambda e: e.tensor_tensor(t1_[:, 0:N], t1_[:, 0:N], t0[:, 0:N], ALU.mult), r=[t1_, t0], w=[t1_])
                cx.act(lambda e: e.activation(t2_[:, 0:N], t1_[:, 0:N], AF.Sigmoid, scale=2.0 * math.sqrt(2.0 / math.pi)),
                       r=[t1_], w=[t2_])
                cx.dve(lambda e: e.tensor_tensor(glb[:, 0:N], t0[:, 0:N], t2_[:, 0:N], ALU.mult), r=[t0, t2_], w=[glb])
                if kind == "k":
                    po = nP()
                    cx.pe(lambda e, po=po: e.matmul(po[0:64, 0:N], w2b[:], glb[:, 0:N], start=True, stop=True),
                          r=[w2b, glb], w=[po])
                    cp(kcmpT[g][:, 0:N], po[0:64, 0:N], [po], [kcmpT[g]])
                else:
                    for kc2 in range(2):
                        po = nPV()
                        n1 = min(128, N - kc2 * 128)
                        if n1 <= 0:
                            continue
                        cx.pe(lambda e, po=po, kc2=kc2, n1=n1: e.matmul(
                            po[0:n1, :], glb[:, kc2 * 128:kc2 * 128 + n1], w2b[:], start=True, stop=True),
                            r=[glb, w2b], w=[po])
                        cp(vcmp[0:n1, kc2, g, :], po[0:n1, :], [po], [vcmp])

            for kind, srcs in (("k", kcT), ("v", vcT)):
                for g in range(2):
                    phaseB(kind, g, srcs[g])
            cx.barrier()

        ovl = cx.sb(es, "ovl", [128, 2, 64], BF16)
        cx.dma(ovl[:], cn["ovl"].rearrange("(c p) j -> p c j", p=128), writes=[ovl])
        causal = cx.sb(es, "causal", [128, 128], BF16)
        cx.dma(causal[:], cn["causal"], writes=[causal])
        wmask = cx.sb(es, "wmask", [128, 640], BF16)
        cx.dma(wmask[:], cn["wmask"], writes=[wmask])
        rvalid = cx.sb(es, "rvalid", [128, 1], F32)
        cx.dma(rvalid[:], cn["rvalid"], writes=[rvalid])
        hqs = [cx.sb(es, "hq%d" % i, [128, 8, 128], BF16) for i in range(2)]
        cms = [cx.sb(es, "cm%d" % i, [128, 256], BF16) for i in range(2)]
        fbs = [cx.sb(es, "fb%d" % i, [128, 64], F32) for i in range(2)]
        qsel = [cx.sb(es, "qsel%d" % i, [128, 4, 128], BF16) for i in range(2)]
        selw = cx.sb(es, "selw", [128, 128], BF16)
        cx.dve(lambda e: e.memset(selw[:], 0.0), w=[selw])
        pcT = [cx.sb(es, "pcT%d" % i, [128, 2, 128], BF16) for i in range(4)]
        pc32 = [cx.sb(es, "pc32_%d" % i, [128, 256], F32) for i in range(2)]
        pbs = [cx.sb(es, "pb%d" % i, [128, S], BF16) for i in range(2)]
        pTs = [cx.sb(es, "pT%d" % i, [128, NT, 128], BF16) for i in range(2)]
        acc = cx.sb(es, "acc", [128, 512], F32)
        accb = cx.sb(es, "accb", [128, 512], BF16)
        mixt = [cx.sb(es, "mixt%d" % i, [128, 4, 128], BF16) for i in range(2)]
        sms = [{n_: cx.sb(es, n_ + str(i), [128, 8 if n_[0] == "c" else 1], F32)
                for n_ in ("cmax", "crs", "mx", "rs", "rinv", "fac")} for i in range(2)]
        sc64 = cx.sb(es, "sc64", [128, 64], F32)
        top8 = cx.sb(es, "top8", [128, 8], F32)
        sel01 = cx.sb(es, "sel01", [128, 64], F32)
        SCALE = 0.125

        def softmax_item(q_ap, q_r, KT, k0, nk, maskfn, vfn, gate_ap, gate_r, acc_ap, first, normalize, keepT=None, i0=False):
            cnt["sp"] += 1
            b = cnt["sp"] % 2
            sm, pb, pT = sms[b], pbs[b], pTs[b]
            cmax, crs, mx, rs, rinv, fac = sm["cmax"], sm["crs"], sm["mx"], sm["rs"], sm["rinv"], sm["fac"]
            chunks = [(c0, min(512, nk - c0)) for c0 in range(0, nk, 512)]
            ncn = len(chunks)
            dst32 = pc32[b] if normalize else None
            held = []

            def scores(ps, c0, n_):
                mm = maskfn(c0, n_)
                cx.pe(lambda e: e.matmul(ps[:, 0:n_], q_ap, KT[0:q_ap.shape[0], k0 + c0:k0 + c0 + n_],
                                         start=True, stop=(len(mm) == 0)), r=q_r + [KT], w=[ps])
                for idx, (l_ap, r_ap, lo, hi, rd) in enumerate(mm):
                    cx.pe(lambda e, l_ap=l_ap, r_ap=r_ap, lo=lo, hi=hi, idx=idx: e.matmul(
                        ps[:, lo:hi], l_ap, r_ap, start=False, stop=(idx == len(mm) - 1)), r=rd, w=[ps])

            def expo(ps, ci, c0, n_):
                out_ap = dst32[:, c0:c0 + n_] if normalize else pb[:, c0:c0 + n_]
                wr = [dst32] if normalize else [pb]
                cx.act(lambda e: e.activation(out_ap, ps[:, 0:n_], AF.Exp, bias=mx[:], scale=SCALE,
                                              accum_out=crs[:, ci:ci + 1]), r=[ps, mx], w=wr + [crs])

            def p1():
                for ci, (c0, n_) in enumerate(chunks):
                    ps = nH() if (ncn == 1 and NSA_HOLD) else nP()
                    scores(ps, c0, n_)
                    cx.dve(lambda e, ps=ps, ci=ci, n_=n_: e.reduce_max(cmax[:, ci:ci + 1], ps[:, 0:n_], AX.X),
                           r=[ps], w=[cmax])
                    if ncn == 1 and NSA_HOLD:
                        held.append(ps)
                if ncn == 1:
                    cx.dve(lambda e: e.tensor_scalar_mul(mx[:], cmax[:, 0:1], -SCALE), r=[cmax], w=[mx])
                else:
                    cx.dve(lambda e: e.tensor_reduce(mx[:], cmax[:, 0:ncn], AX.X, ALU.max), r=[cmax], w=[mx])
                    cx.dve(lambda e: e.tensor_scalar_mul(mx[:], mx[:], -SCALE), r=[mx], w=[mx])

            def p2():
                if ncn == 1 and NSA_HOLD:
                    expo(held[0], 0, chunks[0][0], chunks[0][1])
                    cx.dve(lambda e: e.reciprocal(rinv[:], crs[:, 0:1]), r=[crs], w=[rinv])
                elif ncn == 1:
                    ps = nP()
                    scores(ps, chunks[0][0], chunks[0][1])
                    expo(ps, 0, chunks[0][0], chunks[0][1])
                    cx.dve(lambda e: e.reciprocal(rinv[:], crs[:, 0:1]), r=[crs], w=[rinv])
                else:
                    for ci, (c0, n_) in enumerate(chunks):
                        ps = nP()
                        scores(ps, c0, n_)
                        expo(ps, ci, c0, n_)
                    cx.dve(lambda e: e.reduce_sum(rs[:], crs[:, 0:ncn], AX.X), r=[crs], w=[rs])
                    cx.dve(lambda e: e.reciprocal(rinv[:], rs[:]), r=[rs], w=[rinv])
                if normalize:
                    if i0:
                        cx.dve(lambda e: e.tensor_tensor(rinv[:], rinv[:], rvalid[:], ALU.mult), r=[rinv, rvalid], w=[rinv])
                    cx.dve(lambda e: e.tensor_scalar_mul(pb[:, 0:nk], dst32[:, 0:nk], rinv[:, 0:1]), r=[dst32, rinv], w=[pb])
                dstT = keepT if keepT is not None else pT
                nkt = nk // 128
                for t0 in range(0, nkt, 8):
                    n8 = min(8, nkt - t0)
                    tp = nTP()
                    for t in range(n8):
                        cx.pe(lambda e, tp=tp, t=t, t0=t0: e.transpose(tp[:, t, :], pb[:, (t0 + t) * 128:(t0 + t + 1) * 128], ident[:]),
                              r=[pb, ident], w=[tp])
                    cp(dstT[:, t0:t0 + n8, :], tp[:, 0:n8, :], [tp], [dstT])
                po = nPV()
                for t in range(nkt):
                    v_ap, v_r = vfn(t)
                    cx.pe(lambda e, po=po, t=t, v_ap=v_ap: e.matmul(po[:], dstT[:, t, :], v_ap, start=(t == 0), stop=(t == nkt - 1)),
                          r=[dstT] + v_r, w=[po])
                if normalize:
                    sc_ap, sc_r = gate_ap, [gate_r]
                else:
                    cx.dve(lambda e: e.tensor_tensor(fac[:], rinv[:], gate_ap, ALU.mult), r=[rinv, gate_r], w=[fac])
                    sc_ap, sc_r = fac[:, 0:1], [fac]
                if first:
                    cx.dve(lambda e, po=po: e.tensor_scalar_mul(acc_ap, po[:], sc_ap), r=[po] + sc_r, w=[acc])
                else:
                    cx.dve(lambda e, po=po: e.scalar_tensor_tensor(acc_ap, po[:], sc_ap, acc_ap, ALU.mult, ALU.add),
                           r=[po, acc] + sc_r, w=[acc])

            return p1, p2

        items = []

        def block(i, hq, cm, fb, mt, gates):
            nk = 128 * (i + 1)
            kt0 = max(0, i - 4)
            nkw = 128 * (i - kt0 + 1)

            def blk_pre():
                cx.dma(hq[:], dram_fm(hT_ap, 0, 8, i * 128, (i + 1) * 128), writes=[hq])
                cx.dma(cm[:], cn["cmask"][i], writes=[cm])
                cx.dma(fb[:], cn["fbias"][i], writes=[fb])
                pg = nPV()
                for kc in range(8):
                    cx.pe(lambda e, kc=kc, pg=pg: e.matmul(pg[:, 0:24], hq[:, kc, :], wb[:, kc, 1280:1304],
                                                           start=(kc == 0), stop=(kc == 7)), r=[hq, wb], w=[pg])
                cx.act(lambda e, pg=pg: e.activation(gates[:], pg[:, 0:24], AF.Sigmoid), r=[pg], w=[gates])

            def blk_post():
                cx.act(lambda e: e.copy(accb[:], acc[:]), r=[acc], w=[accb])
                tp = nTP()
                for c in range(4):
                    cx.pe(lambda e, c=c, tp=tp: e.transpose(tp[:, c, :], accb[:, c * 128:(c + 1) * 128], ident[:]),
                          r=[accb, ident], w=[tp])
                cp(mt[:], tp[:, 0:4, :], [tp], [mt])
                cx.dma(dram_fm(mixT_ap, 4, 8, i * 128, (i + 1) * 128), mt[:], reads=[mt], q="pool")

            def group(g):
                qs = qsel[g]
                qk = (qs.key, "q")
                sk = (qs.key, "s")

                def q_pre():
                    for hp in range(4):
                        hd = g * 4 + hp
                        ps = nP()
                        proj_fm(cx, ps, wb, hd * 64, 64, hq, 128)
                        cp(qs[0:64, hp, :], ps[0:64, 0:128], [ps], [qk])

                def sel_pre():
                    for hp in range(4):
                        for kc2 in range(2):
                            cx.pe(lambda e, hp=hp, kc2=kc2: e.matmul(IMP[:], pcT[hp][:, kc2, :], ovl[:, kc2, :],
                                                                     start=(hp == 0 and kc2 == 0), stop=(hp == 3 and kc2 == 1)),
                                  r=[pcT[hp], ovl], w=[IMP])
                    cx.dve(lambda e: e.tensor_tensor(sc64[:], IMP[:], fb[:], ALU.add), r=[IMP, fb], w=[sc64])
                    cx.dve(lambda e: e.max(top8[:], sc64[:]), r=[sc64], w=[top8])
                    cx.dve(lambda e: e.tensor_scalar(sel01[:], sc64[:], top8[:, 7:8], None, ALU.is_ge), r=[sc64, top8], w=[sel01])
                    cx.dve(lambda e: e.tensor_scalar(selw[:, 64:128], sel01[:], -NEG8, NEG8, ALU.mult, ALU.add), r=[sel01], w=[selw])
                    tps = nTP()
                    cx.pe(lambda e: e.transpose(tps[:, 0, :], selw[:], ident[:]), r=[selw, ident], w=[tps])
                    cx.act(lambda e: e.copy(qs[64:128, :, :], tps[64:128, 0:1, :].to_broadcast([64, 4, 128])),
                           r=[tps], w=[sk])

                def mk_cmp(hp):
                    hd = g * 4 + hp
                    return lambda: softmax_item(
                        qs[0:64, hp, :], [qk], kcmpT[g], 0, 256,
                        lambda c0, n_: [(ident[:], cm[:, c0:c0 + n_], 0, n_, [ident, cm])],
                        lambda t: (vcmp[:, t, g, :], [vcmp]),
                        gates[:, hd:hd + 1], gates, acc[:, hd * 64:(hd + 1) * 64], True, True, keepT=pcT[hp], i0=(i == 0))

                def mk_win(hp):
                    hd = g * 4 + hp
                    return lambda: softmax_item(
                        qs[0:64, hp, :], [qk], kwT[g], kt0 * 128, nkw,
                        lambda c0, n_: [(ident[:], wmask[:, 640 - nkw + c0:640 - nkw + c0 + n_], 0, n_, [ident, wmask])],
                        lambda t: (vsw[:, kt0 + t, 128 + g * 64:128 + (g + 1) * 64], [vsw]),
                        gates[:, 16 + hd:17 + hd], gates, acc[:, hd * 64:(hd + 1) * 64], False, False)

                def mk_slc(hp):
                    hd = g * 4 + hp
                    return lambda: softmax_item(
                        qs[:, hp, :], [qk, sk], ksE[g], 0, nk,
                        lambda c0, n_: ([(ident[:], causal[:], n_ - 128, n_, [ident, causal])] if c0 + n_ == nk else []),
                        lambda t: (vsw[:, t, g * 64:(g + 1) * 64], [vsw]),
                        gates[:, 8 + hd:9 + hd], gates, acc[:, hd * 64:(hd + 1) * 64], False, False)

                lst = []
                for hp in range(4):
                    lst.append([q_pre if hp == 0 else None, mk_cmp(hp), None])
                for hp in range(4):
                    lst.append([sel_pre if hp == 1 else None, mk_win(hp), None])
                for hp in range(4):
                    lst.append([None, mk_slc(hp), None])
                return lst

            lst = group(0) + group(1)
            first_pre = lst[0][0]
            lst[0][0] = lambda: (blk_pre(), first_pre())
            lst[-1][2] = blk_post
            items.extend(lst)

        gates2 = [cx.sb(es, "gates%d" % i_, [128, 24], F32) for i_ in range(2)]
        for i in range(NB):
            block(i, hqs[i % 2], cms[i % 2], fbs[i % 2], mixt[i % 2], gates2[i % 2])
        prev = None
        for pre, mk, post in items:
            if pre is not None:
                pre()
            p1, p2 = mk()
            p1()
            if not NSA_PIPE:
                p2()
                if post is not None:
                    post()
                continue
            if prev is not None:
                prev[0]()
                if prev[1] is not None:
                    prev[1]()
            prev = (p2, post)
        if NSA_PIPE:
            prev[0]()
            if prev[1] is not None:
                prev[1]()
    cx.barrier()


SEQ = 4096
NCORES = 8
DEPTH = 4


def build_full(S=SEQ, depth=DEPTH):
    nc = bass.Bass("TRN2", target_bir_lowering=False)

    def din(n, s, d=F32):
        return nc.dram_tensor(n, list(s), d, kind="ExternalInput").ap()

    x = din("x", [S, D])
    norm_mix_g = din("norm_mix_g", [4, D])
    norm_ffn_g = din("norm_ffn_g", [4, D])
    final_norm_g = din("final_norm_g", [D])
    w_ret = din("w_ret", [2, D, 3072])
    w_nsa = din("w_nsa", [2, D, 1304])
    even_w_out = din("even_w_out", [2, D, D])
    cws = {}
    for kind in "kv":
        cws["pos_" + kind] = din("cmp_pos_" + kind, [2, 32, 64])
        cws["w1_" + kind] = din("cmp_w1_" + kind, [2, 2048, 64])
        cws["w2_" + kind] = din("cmp_w2_" + kind, [2, 64, 64])
    odd_w_in = din("odd_w_in", [2, D, 4096])
    odd_w_out = din("odd_w_out", [2, D, D])
    hgrn_norm_g = din("hgrn_norm_g", [2, 128])
    hgrn_lb = din("hgrn_lb_logits", [2, 1024])
    ffn_w1 = din("ffn_w1", [4, D, DFF])
    ffn_w3 = din("ffn_w3", [4, D, DFF])
    ffn_w2 = din("ffn_w2", [4, DFF, D])
    ident = din("c_ident", [128, 128], BF16)
    maskT = din("c_maskT", [64, 64])
    scanm = din("c_scanm", [128, MTK])
    cos = din("c_cos", [128, S])
    sin = din("c_sin", [128, S])
    dec = din("c_dec", [4, 2, 128, MTK])
    NB = S // 128
    cn = {
        "cmask": din("c_cmask", [NB, 128, 256], BF16),
        "fbias": din("c_fbias", [NB, 128, 64]),
        "ovl": din("c_ovl", [256, 64], BF16),
        "causal": din("c_causal", [128, 128], BF16),
        "wmask": din("c_wmask", [128, 640], BF16),
        "blockE": din("c_blockE", [64, S], BF16),
        "rvalid": din("c_rvalid", [128, 1]),
    }
    y = nc.dram_tensor("y", [S, D], F32, kind="ExternalOutput").ap()
    xs = nc.dram_tensor("xs", [S, D], F32).ap()
    hTa = nc.dram_tensor("hTa", [8, 128, S], BF16).ap()
    hTb = nc.dram_tensor("hTb", [8, 128, S], BF16).ap()
    mixT = nc.dram_tensor("mixT", [8, 128, S], BF16).ap()

    cx = Ctx(nc)
    stage_norm0(cx, x, hTb, norm_mix_g[0], ident, S)
    for layer in range(depth):
        j = layer // 2
        if layer % 2 == 0:
            stage_ret(cx, hTb, w_ret[j], cos, sin, dec, mixT, ident, maskT, S)
            cw = {k_: v_[j] for k_, v_ in cws.items()}
            stage_nsa(cx, hTb, w_nsa[j], cw, mixT, ident, cn, S)
            w_out = even_w_out[j]
        else:
            stage_hgrn(cx, hTb, odd_w_in[j], hgrn_norm_g[j], hgrn_lb, mixT, ident, maskT, scanm, S, j)
            w_out = odd_w_out[j]
        stage_out(cx, mixT, w_out, x if layer == 0 else xs, xs, hTa, norm_ffn_g[layer], ident, S)
        last = layer == depth - 1
        stage_ffn(cx, hTa, ffn_w1[layer], ffn_w3[layer], ffn_w2[layer], xs, xs, hTb,
                  norm_mix_g[min(layer + 1, 3)], ident, S, last, gfin_ap=final_norm_g, y_ap=y)
    cx.emit()
    return nc


def host_layout(inputs, S=SEQ):
    f32 = lambda a: np.ascontiguousarray(np.asarray(a, dtype=np.float32))
    ew = f32(inputs["even_w_in"])

    def swap(w):
        return w.reshape(w.shape[0], D, 4, 2, 64)[:, :, :, ::-1, :].reshape(w.shape[0], D, 512)

    rq, rk, rv, rg = ew[:, :, 0:512], ew[:, :, 512:1024], ew[:, :, 1024:1536], ew[:, :, 1536:2048]
    nq = ew[:, :, 2048:2560]
    kc, vc, ks, vs, kw, vw = [ew[:, :, 2560 + 128 * i:2560 + 128 * (i + 1)] for i in range(6)]
    ng = ew[:, :, 3328:3352]
    shared = {
        "w_ret": np.ascontiguousarray(np.concatenate([rq, rk, swap(rq), swap(rk), rv, rg], axis=2)),
        "w_nsa": np.ascontiguousarray(np.concatenate([nq, kc, vc, ks, kw, vs, vw, ng], axis=2)),
    }
    for k_ in ("norm_mix_g", "norm_ffn_g", "final_norm_g", "even_w_out", "cmp_pos_k", "cmp_w1_k", "cmp_w2_k",
               "cmp_pos_v", "cmp_w1_v", "cmp_w2_v", "odd_w_in", "odd_w_out", "hgrn_norm_g", "hgrn_lb_logits",
               "ffn_w1", "ffn_w3", "ffn_w2"):
        shared[k_] = f32(inputs[k_])
    for k_, v_ in host_consts(S).items():
        shared["c_" + k_] = v_
    for k_, v_ in nsa_consts(S).items():
        shared["c_" + k_] = v_
    return shared


def kernel(**inputs):
    x = np.ascontiguousarray(np.asarray(inputs["x"], dtype=np.float32))
    B, S, _ = x.shape
    shared = host_layout(inputs, S)
    nc = build_full(S)
    in_maps = []
    for b in range(B):
        m = dict(shared)
        m["x"] = np.ascontiguousarray(x[b])
        in_maps.append(m)
    res = run_bass_kernel_spmd(nc, in_maps, core_ids=list(range(B)))
    return np.stack([np.asarray(r["y"], dtype=np.float32) for r in res.results], axis=0)
```

```python
import math
from contextlib import ExitStack

import numpy as np
import concourse.bass as bass
import concourse.mybir as mybir
from concourse.bass_utils import run_bass_kernel_spmd

F32 = mybir.dt.float32
BF16 = mybir.dt.bfloat16
AF = mybir.ActivationFunctionType
ALU = mybir.AluOpType
AX = mybir.AxisListType

D = 1024
DFF = 2816
NFF = DFF // 128
EPS = 1e-6
EVEN_IN = 3352


class Buf:
    def __init__(self, t, key, psum=False):
        self.t = t
        self.key = key
        self.psum = psum

    def __getitem__(self, k):
        return self.t[k]


class Ctx:
    NDMA = 8
    ENG = ("pe", "act", "dve", "pool", "sp")

    def __init__(self, nc):
        self.nc = nc
        self.streams = {e: [] for e in self.ENG}
        self.cnt = {e: 0 for e in ("pe", "act", "dve", "pool")}
        self.dman = {q: 0 for q in ("sp", "act", "pool")}
        self.lastw = {}
        self.readers = {}
        self.known = {e: {} for e in self.ENG}
        self.all_tokens = {}
        self.nbuf = 0

    def sb(self, es, name, shape, dtype):
        self.nbuf += 1
        nm = "%s_%d" % (name, self.nbuf)
        t = es.enter_context(self.nc.sbuf_tensor(nm, list(shape), dtype))
        return Buf(t, nm)

    def ps(self, es, name, shape, dtype):
        self.nbuf += 1
        nm = "%s_%d" % (name, self.nbuf)
        t = es.enter_context(self.nc.psum_tensor(nm, list(shape), dtype))
        return Buf(t, nm, psum=True)

    @staticmethod
    def _k(x):
        return x.key if isinstance(x, Buf) else x

    def _collect(self, eng, reads, writes):
        deps = []
        for r in reads:
            k = self._k(r)
            if k in self.lastw:
                deps.append(self.lastw[k])
        for w in writes:
            k = self._k(w)
            if k in self.lastw:
                deps.append(self.lastw[k])
            deps.extend(self.readers.get(k, ()))
        return deps

    def _record(self, tok, reads, writes):
        for r in reads:
            self.readers.setdefault(self._k(r), []).append(tok)
        for w in writes:
            k = self._k(w)
            self.lastw[k] = tok
            self.readers[k] = []

    def _waits(self, eng, deps, is_pe_compute):
        waits = {}
        kn = self.known[eng]
        for (sk, v, src) in deps:
            if is_pe_compute and src == "pe":
                continue
            if kn.get(sk, 0) >= v:
                continue
            if waits.get(sk, 0) < v:
                waits[sk] = v
        for sk, v in waits.items():
            kn[sk] = v
        return list(waits.items())

    def op(self, eng, fn, reads=(), writes=()):
        ex = [r for r in reads if isinstance(r, Buf) and r.psum]
        if ex:
            writes = list(writes) + ex
        deps = self._collect(eng, reads, writes)
        waits = self._waits(eng, deps, eng == "pe")
        self.cnt[eng] += 1
        tok = (eng, self.cnt[eng], eng)
        self.streams[eng].append((waits, fn, (eng, 1)))
        self.all_tokens[eng] = tok
        self._record(tok, reads, writes)

    def dma(self, out, in_, reads=(), writes=(), q="sp", **kw):
        deps = self._collect(q, reads, writes)
        n = self.dman[q]
        slot = n % self.NDMA
        sk = ("dma", q, slot)
        if n >= self.NDMA:
            deps.append((sk, 16 * (n // self.NDMA), "dma"))
        waits = self._waits(q, deps, False)
        self.dman[q] += 1
        tok = (sk, 16 * (n // self.NDMA + 1), "dma")
        self.streams[q].append((waits, lambda e: e.dma_start(out=out, in_=in_, **kw), (sk, 16)))
        self.all_tokens[sk] = tok
        self._record(tok, reads, writes)

    def barrier(self):
        toks = list(self.all_tokens.values())
        for e in self.ENG:
            waits = self._waits(e, toks, False)
            if waits:
                self.streams[e].append((waits, None, None))

    def pe(self, fn, r=(), w=()):
        self.op("pe", fn, r, w)

    def act(self, fn, r=(), w=()):
        self.op("act", fn, r, w)

    def dve(self, fn, r=(), w=()):
        self.op("dve", fn, r, w)

    def pool(self, fn, r=(), w=()):
        self.op("pool", fn, r, w)

    def emit(self):
        nc = self.nc
        self.barrier()
        with ExitStack() as es:
            sems = {}
            for e in ("pe", "act", "dve", "pool"):
                sems[e] = es.enter_context(nc.semaphore("s_" + e))
            for q in ("sp", "act", "pool"):
                for s in range(self.NDMA):
                    sems[("dma", q, s)] = es.enter_context(nc.semaphore("d_%s%d" % (q, s)))
            block = es.enter_context(nc.Block())
            streams = self.streams

            def replay(name, eng):
                for waits, fn, inc in streams[name]:
                    for sk, v in waits:
                        eng.wait_ge(sems[sk], v)
                    if fn is not None:
                        fn(eng).then_inc(sems[inc[0]], inc[1])

            if streams["sp"]:
                @block.sync
                def _(eng):
                    replay("sp", eng)
            if streams["pe"]:
                @block.tensor
                def _(eng):
                    replay("pe", eng)
            if streams["dve"]:
                @block.vector
                def _(eng):
                    replay("dve", eng)
            if streams["act"]:
                @block.scalar
                def _(eng):
                    replay("act", eng)
            if streams["pool"]:
                @block.gpsimd
                def _(eng):
                    replay("pool", eng)


def load_weight_bf16(cx, wb, w_ap, kchunks, ncols, stage):
    step = stage[0].t.shape[1]
    i = 0
    for c in range(kchunks):
        for c0 in range(0, ncols, step):
            n = min(step, ncols - c0)
            st = stage[i % len(stage)]
            cx.dma(st[:, 0:n], w_ap[c * 128:(c + 1) * 128, c0:c0 + n], writes=[st])
            if i % 2 == 0:
                cx.dve(lambda e, st=st, c=c, c0=c0, n=n: e.tensor_copy(wb[:, c, c0:c0 + n], st[:, 0:n]),
                       r=[st], w=[(wb.key, c, c0)])
            else:
                cx.act(lambda e, st=st, c=c, c0=c0, n=n: e.copy(wb[:, c, c0:c0 + n], st[:, 0:n]),
                       r=[st], w=[(wb.key, c, c0)])
            i += 1
    return wb


class NormTools:
    def __init__(self, cx, es, g_ap):
        self.cx = cx
        self.ident = cx.sb(es, "ident", [128, 128], BF16)
        self.gT = cx.sb(es, "gT", [128, 8], F32)
        self.ss = [cx.sb(es, "ss%d" % i, [128, 1], F32) for i in range(2)]
        self.rstd = [cx.sb(es, "rstd%d" % i, [128, 1], F32) for i in range(2)]
        self.junk = cx.sb(es, "junk", [128, D], BF16)
        self.hb = [cx.sb(es, "hb%d" % i, [128, D], BF16) for i in range(2)]
        self.tp = [cx.ps(es, "tp%d" % i, [128, 8, 128], BF16) for i in range(2)]
        self.i = 0
        cx.dma(self.gT[:], g_ap.rearrange("(c p) -> p c", p=128), writes=[self.gT],
               allow_slow_non_contiguous=True)

    def load_ident(self, ident_ap):
        self.cx.dma(self.ident[:], ident_ap, writes=[self.ident])

    def stats(self, xt):
        cx = self.cx
        i = self.i
        self.i += 1
        ss, rstd = self.ss[i % 2], self.rstd[i % 2]
        junk = self.junk
        cx.act(lambda e: e.activation(junk[:], xt[:], AF.Square, scale=1.0 / 32.0, accum_out=ss[:]),
               r=[xt], w=[junk, ss])
        cx.act(lambda e: e.activation(ss[:], ss[:], AF.Sqrt, bias=EPS, scale=1.0), r=[ss], w=[ss])
        cx.dve(lambda e: e.reciprocal(rstd[:], ss[:]), r=[ss], w=[rstd])
        return rstd

    def run(self, xt, hT, col0, scale_by_g=True):
        cx = self.cx
        i = self.i
        self.i += 1
        ss, rstd, hb, tp = self.ss[i % 2], self.rstd[i % 2], self.hb[i % 2], self.tp[i % 2]
        junk = self.junk
        cx.act(lambda e: e.activation(junk[:], xt[:], AF.Square, scale=1.0 / 32.0, accum_out=ss[:]),
               r=[xt], w=[junk, ss])
        cx.act(lambda e: e.activation(ss[:], ss[:], AF.Sqrt, bias=EPS, scale=1.0), r=[ss], w=[ss])
        cx.dve(lambda e: e.reciprocal(rstd[:], ss[:]), r=[ss], w=[rstd])
        cx.act(lambda e: e.activation(hb[:], xt[:], AF.Copy, scale=rstd[:]), r=[xt, rstd], w=[hb])
        for c in range(8):
            cx.pe(lambda e, c=c: e.transpose(tp[:, c, :], hb[:, c * 128:(c + 1) * 128], self.ident[:]),
                  r=[hb, self.ident], w=[tp])
        gT = self.gT
        cx.dve(lambda e: e.tensor_tensor(hT[:, :, col0:col0 + 128], tp[:],
                                         gT[:].unsqueeze(2).to_broadcast([128, 8, 128]), ALU.mult),
               r=[tp, gT], w=[hT])
        return rstd


def dram_fm(ap, c0, c1, s0, s1):
    return ap[c0:c1, :, s0:s1].rearrange("c p s -> p c s")


def stage_norm0(cx, x_ap, hT_ap, g_ap, ident_ap, S):
    with ExitStack() as es:
        nt = NormTools(cx, es, g_ap)
        nt.load_ident(ident_ap)
        xts = [cx.sb(es, "xt%d" % i, [128, D], F32) for i in range(2)]
        hTs = [cx.sb(es, "hTt%d" % i, [128, 8, 512], BF16) for i in range(2)]
        for m in range(S // 512):
            hT = hTs[m % 2]
            for j in range(4):
                t = m * 4 + j
                xt = xts[t % 2]
                cx.dma(xt[:], x_ap[t * 128:(t + 1) * 128, :], writes=[xt])
                nt.run(xt, hT, j * 128)
            cx.dma(dram_fm(hT_ap, 0, 8, m * 512, (m + 1) * 512), hT[:], reads=[hT], q="pool")
    cx.barrier()


def stage_out(cx, mixT_ap, w_ap, xin_ap, xout_ap, hT_ap, g_ap, ident_ap, S):
    with ExitStack() as es:
        wb = cx.sb(es, "woutb", [128, 8, D], BF16)
        with ExitStack() as es2:
            stg = [cx.sb(es2, "wstg%d" % i, [128, 1024], F32) for i in range(2)]
            load_weight_bf16(cx, wb, w_ap, 8, D, stg)
            cx.barrier()
        nt = NormTools(cx, es, g_ap)
        nt.load_ident(ident_ap)
        mts = [cx.sb(es, "mixt%d" % i, [128, 8, 512], BF16) for i in range(2)]
        xts = [cx.sb(es, "xt%d" % i, [128, D], F32) for i in range(2)]
        x1s = [cx.sb(es, "x1t%d" % i, [128, D], F32) for i in range(2)]
        hTs = [cx.sb(es, "hTt%d" % i, [128, 8, 512], BF16) for i in range(2)]
        yps = [cx.ps(es, "yps%d" % i, [128, 512], F32) for i in range(4)]
        for m in range(S // 512):
            mt = mts[m % 2]
            hT = hTs[m % 2]
            cx.dma(mt[:], dram_fm(mixT_ap, 0, 8, m * 512, (m + 1) * 512), writes=[mt])
            for j in range(4):
                t = m * 4 + j
                xt = xts[t % 2]
                x1 = x1s[t % 2]
                cx.dma(xt[:], xin_ap[t * 128:(t + 1) * 128, :], writes=[xt])
                for half in range(2):
                    yp = yps[(t * 2 + half) % 4]
                    for c in range(8):
                        cx.pe(lambda e, yp=yp, c=c, j=j, half=half, mt=mt: e.matmul(
                            yp[:], mt[:, c, j * 128:(j + 1) * 128], wb[:, c, half * 512:(half + 1) * 512],
                            start=(c == 0), stop=(c == 7)), r=[mt, wb], w=[yp])
                    cx.dve(lambda e, yp=yp, half=half, xt=xt, x1=x1: e.tensor_tensor(
                        x1[:, half * 512:(half + 1) * 512], yp[:], xt[:, half * 512:(half + 1) * 512], ALU.add),
                        r=[yp, xt], w=[x1])
                cx.dma(xout_ap[t * 128:(t + 1) * 128, :], x1[:], reads=[x1], q="pool")
                nt.run(x1, hT, j * 128)
            cx.dma(dram_fm(hT_ap, 0, 8, m * 512, (m + 1) * 512), hT[:], reads=[hT], q="pool")
    cx.barrier()


def stage_ffn(cx, hT_ap, w1_ap, w3_ap, w2_ap, xin_ap, xout_ap, hTout_ap, g_ap, ident_ap, S, final,
              gfin_ap=None, y_ap=None):
    MT = 256
    with ExitStack() as es:
        w1b = cx.sb(es, "w1b", [128, 8, DFF], BF16)
        w3b = cx.sb(es, "w3b", [128, 8, DFF], BF16)
        w2b = cx.sb(es, "w2b", [128, NFF, D], BF16)
        with ExitStack() as es2:
            stg = [cx.sb(es2, "wstg%d" % i, [128, 1408], F32) for i in range(2)]
            load_weight_bf16(cx, w1b, w1_ap, 8, DFF, stg)
            load_weight_bf16(cx, w3b, w3_ap, 8, DFF, stg)
            load_weight_bf16(cx, w2b, w2_ap, NFF, D, stg)
            cx.barrier()
        nt = NormTools(cx, es, g_ap)
        nt.load_ident(ident_ap)
        if final:
            gfin = cx.sb(es, "gfin", [128, D], F32)
            cx.dma(gfin[:], gfin_ap.partition_broadcast(128), writes=[gfin])
        hins = [cx.sb(es, "hin%d" % i, [128, 8, MT], BF16) for i in range(2)]
        gT = cx.sb(es, "gTff", [128, NFF, MT], BF16)
        sil = [cx.sb(es, "sil%d" % i, [128, MT], F32) for i in range(2)]
        xts = [cx.sb(es, "xt%d" % i, [128, D], F32) for i in range(2)]
        x2s = [cx.sb(es, "x2t%d" % i, [128, D], F32) for i in range(2)]
        hTs = [cx.sb(es, "hTt%d" % i, [128, 8, MT], BF16) for i in range(2)]
        ups = [cx.ps(es, "ups%d" % i, [128, 512], F32) for i in range(4)]
        yps = [cx.ps(es, "yps%d" % i, [128, 512], F32) for i in range(2)]
        nsub = MT // 128
        for m in range(S // MT):
            hin = hins[m % 2]
            hT = hTs[m % 2]
            cx.dma(hin[:], dram_fm(hT_ap, 0, 8, m * MT, (m + 1) * MT), writes=[hin])
            for f in range(NFF):
                u1 = ups[(f % 2) * 2]
                u3 = ups[(f % 2) * 2 + 1]
                sl = sil[f % 2]
                for (wb, up) in ((w1b, u1), (w3b, u3)):
                    for c in range(8):
                        cx.pe(lambda e, wb=wb, up=up, c=c, f=f, hin=hin: e.matmul(
                            up[:, 0:MT], wb[:, c, f * 128:(f + 1) * 128], hin[:, c, :],
                            start=(c == 0), stop=(c == 7)), r=[wb, hin], w=[up])
                cx.act(lambda e, u1=u1, sl=sl: e.activation(sl[:], u1[:, 0:MT], AF.Silu), r=[u1], w=[sl])
                cx.dve(lambda e, u3=u3, sl=sl, f=f: e.tensor_tensor(gT[:, f, :], u3[:, 0:MT], sl[:], ALU.mult),
                       r=[u3, sl], w=[gT])
            for j in range(nsub):
                t = m * nsub + j
                xt = xts[t % 2]
                x2 = x2s[t % 2]
                cx.dma(xt[:], xin_ap[t * 128:(t + 1) * 128, :], writes=[xt])
                for half in range(2):
                    yp = yps[half]
                    for f in range(NFF):
                        cx.pe(lambda e, yp=yp, f=f, j=j, half=half: e.matmul(
                            yp[:], gT[:, f, j * 128:(j + 1) * 128], w2b[:, f, half * 512:(half + 1) * 512],
                            start=(f == 0), stop=(f == NFF - 1)), r=[gT, w2b], w=[yp])
                    cx.dve(lambda e, yp=yp, half=half, xt=xt, x2=x2: e.tensor_tensor(
                        x2[:, half * 512:(half + 1) * 512], yp[:], xt[:, half * 512:(half + 1) * 512], ALU.add),
                        r=[yp, xt], w=[x2])
                if not final:
                    cx.dma(xout_ap[t * 128:(t + 1) * 128, :], x2[:], reads=[x2], q="pool")
                    nt.run(x2, hT, j * 128)
                else:
                    rstd = nt.stats(x2)
                    ot = xt
                    cx.dve(lambda e, x2=x2, rstd=rstd, ot=ot: e.scalar_tensor_tensor(
                        ot[:], x2[:], rstd[:], gfin[:], ALU.mult, ALU.mult), r=[x2, rstd, gfin], w=[ot])
                    cx.dma(y_ap[t * 128:(t + 1) * 128, :], ot[:], reads=[ot], q="pool")
            if not final:
                cx.dma(dram_fm(hTout_ap, 0, 8, m * MT, (m + 1) * MT), hT[:], reads=[hT], q="pool")
    cx.barrier()


DEBUG = {}
CH = 64
MTK = 512
NCH = MTK // CH


class GLACore:
    def __init__(self, cx, es, nheads, ident, maskT_ap):
        self.cx = cx
        self.ident = ident
        self.maskT = cx.sb(es, "maskT", [64, 64], F32)
        cx.dma(self.maskT[:], maskT_ap, writes=[self.maskT])
        self.S = [cx.sb(es, "S%d" % h, [128, 128], F32) for h in range(nheads)]
        for h in range(nheads):
            cx.dve(lambda e, h=h: e.memset(self.S[h][:], 0.0), w=[self.S[h]])
        self.ATb = cx.sb(es, "ATb", [64, NCH, 64], BF16)
        self.ktm = cx.sb(es, "ktm", [64, NCH, 128], BF16)
        self.KVd = cx.sb(es, "KVd", [128, NCH, 128], F32)
        self.spb = [cx.sb(es, "spb%d" % i, [128, 128], BF16) for i in range(2)]
        self.AT = cx.ps(es, "ATp", [64, NCH, 64], F32)
        self.KTt = cx.ps(es, "KTt", [64, NCH, 128], BF16)
        self.OT = cx.ps(es, "OTp", [128, MTK], F32)
        self.KV = [cx.ps(es, "KVp%d" % i, [128, 4, 128], F32) for i in range(2)]

    def run(self, h, qt, kt, v, vcol0, ebm, e2, dlast, oT_sb):
        cx = self.cx
        AT, ATb, KTt, ktm, KV, KVd, OT, S = self.AT, self.ATb, self.KTt, self.ktm, self.KV, self.KVd, self.OT, self.S[h]
        maskT, ident = self.maskT, self.ident

        def sc(x, c):
            return (x, []) if isinstance(x, float) else (x[1][:, c:c + 1], [x[0]])

        for c in range(NCH):
            cs = slice(c * CH, (c + 1) * CH)
            cx.pe(lambda e, c=c, cs=cs: e.matmul(AT[:, c, :], kt[:, cs], qt[:, cs], start=True, stop=True),
                  r=[kt, qt], w=[AT])
        cx.dve(lambda e: e.tensor_tensor(ATb[:], AT[:], maskT[:].unsqueeze(1).to_broadcast([64, NCH, 64]), ALU.mult),
               r=[AT, maskT], w=[ATb])
        for c in range(NCH):
            cs = slice(c * CH, (c + 1) * CH)
            cx.pe(lambda e, c=c, cs=cs: e.transpose(KTt[:, c, :], kt[:, cs], ident[:]), r=[kt, ident], w=[KTt])
        cx.act(lambda e: e.copy(ktm[:], KTt[:]), r=[KTt], w=[ktm])
        for c in range(NCH):
            cx.pe(lambda e, c=c: e.matmul(KV[c // 4][:, c % 4, :], ktm[:, c, :], v[:, c, vcol0:vcol0 + 128],
                                          start=True, stop=True), r=[ktm, v], w=[KV[c // 4]])
        for b in range(2):
            if isinstance(dlast, float):
                cx.dve(lambda e, b=b: e.tensor_scalar_mul(KVd[:, 4 * b:4 * b + 4, :], KV[b][:], dlast),
                       r=[KV[b]], w=[KVd])
            else:
                cx.dve(lambda e, b=b: e.tensor_tensor(
                    KVd[:, 4 * b:4 * b + 4, :], KV[b][:],
                    dlast[1][:, 4 * b:4 * b + 4].unsqueeze(2).to_broadcast([128, 4, 128]), ALU.mult),
                    r=[KV[b], dlast[0]], w=[KVd])
        for c in range(NCH):
            cs = slice(c * CH, (c + 1) * CH)
            spb = self.spb[c % 2]
            s_ebm, r_ebm = sc(ebm, c)
            s_e2, r_e2 = sc(e2, c)
            cx.act(lambda e, spb=spb, s_ebm=s_ebm: e.activation(spb[:], S[:], AF.Copy, scale=s_ebm),
                   r=[S] + r_ebm, w=[spb])
            cx.pe(lambda e, c=c, cs=cs: e.matmul(OT[:, cs], v[:, c, vcol0:vcol0 + 128], ATb[:, c, :],
                                                 start=True, stop=False), r=[v, ATb], w=[OT])
            cx.pe(lambda e, cs=cs, spb=spb: e.matmul(OT[:, cs], spb[:], qt[:, cs], start=False, stop=True),
                  r=[spb, qt], w=[OT])
            cx.dve(lambda e, c=c, s_e2=s_e2: e.scalar_tensor_tensor(S[:], S[:], s_e2, KVd[:, c, :], ALU.mult, ALU.add),
                   r=[S, KVd] + r_e2, w=[S])
        cx.act(lambda e: e.copy(oT_sb[:], OT[:]), r=[OT], w=[oT_sb])


def proj_fm(cx, ps, wb, col0, ncols, hin, n):
    for c in range(8):
        cx.pe(lambda e, c=c: e.matmul(ps[0:ncols, 0:n], wb[:, c, col0:col0 + ncols], hin[:, c, 0:n],
                                      start=(c == 0), stop=(c == 7)), r=[wb, hin], w=[ps])


def stage_hgrn(cx, hT_ap, w_ap, normg_ap, lb_ap, mixT_ap, ident_ap, maskT_ap, scanm_ap, S, layer_j):
    with ExitStack() as es:
        wb = cx.sb(es, "winb", [128, 8, 4096], BF16)
        with ExitStack() as es2:
            stg = [cx.sb(es2, "wstg%d" % i, [128, 2048], F32) for i in range(2)]
            load_weight_bf16(cx, wb, w_ap, 8, 4096, stg)
            cx.barrier()
        ident = cx.sb(es, "ident", [128, 128], BF16)
        cx.dma(ident[:], ident_ap, writes=[ident])
        onesb = cx.sb(es, "onesb", [128, 128], BF16)
        cx.dve(lambda e: e.memset(onesb[:], 1.0), w=[onesb])
        scanm = cx.sb(es, "scanm", [128, MTK], F32)
        cx.dma(scanm[:], scanm_ap, writes=[scanm])
        normg = cx.sb(es, "normg", [128, 1], F32)
        cx.dma(normg[:], normg_ap.rearrange("(p o) -> p o", o=1), writes=[normg])
        lbT = cx.sb(es, "lbT", [128, 8], F32)
        omlT = cx.sb(es, "omlT", [128, 8], F32)
        if layer_j == 0:
            cx.dve(lambda e: e.memset(lbT[:], 0.0), w=[lbT])
        else:
            l0 = cx.sb(es, "l0", [128, 8], F32)
            l1 = cx.sb(es, "l1", [128, 8], F32)
            cx.dma(l0[:], lb_ap[0, :].rearrange("(h p) -> p h", p=128), writes=[l0], allow_slow_non_contiguous=True)
            cx.dma(l1[:], lb_ap[1, :].rearrange("(h p) -> p h", p=128), writes=[l1], allow_slow_non_contiguous=True)
            cx.dve(lambda e: e.tensor_tensor(l1[:], l1[:], l0[:], ALU.subtract), r=[l0, l1], w=[l1])
            cx.act(lambda e: e.activation(lbT[:], l1[:], AF.Sigmoid), r=[l1], w=[lbT])
        cx.dve(lambda e: e.tensor_scalar(omlT[:], lbT[:], -1.0, 1.0, ALU.mult, ALU.add), r=[lbT], w=[omlT])
        core = GLACore(cx, es, 8, ident, maskT_ap)
        hins = [cx.sb(es, "hin%d" % i, [128, 8, MTK], BF16) for i in range(2)]
        v = cx.sb(es, "vtm", [64, NCH, 1024], BF16)
        f32t = lambda n: cx.sb(es, n, [128, MTK], F32)
        sq, sg, gl, bb, dd, eq, ek = [f32t(n) for n in ("sq", "sg", "gl", "bb", "dd", "eq", "ek")]
        ebm = cx.sb(es, "ebm", [128, NCH], F32)
        e2 = cx.sb(es, "e2", [128, NCH], F32)
        qt = cx.sb(es, "qt", [128, MTK], BF16)
        kt = cx.sb(es, "kt", [128, MTK], BF16)
        oT = f32t("oT")
        sqo = cx.sb(es, "sqo", [128, MTK], BF16)
        rt = f32t("rt")
        sgp = f32t("sgp")
        mts = [cx.sb(es, "mixt%d" % i, [128, 8, MTK], BF16) for i in range(2)]
        P = [cx.ps(es, "P%d" % i, [128, 512], F32) for i in range(3)]
        pi = [0]

        def nextP():
            pi[0] += 1
            return P[pi[0] % 3]

        def macro(m, hin, mt):
            cx.dma(hin[:], dram_fm(hT_ap, 0, 8, m * MTK, (m + 1) * MTK), writes=[hin])
            k = 0
            for c in range(NCH):
                for half in range(2):
                    ps = nextP()
                    for kc in range(8):
                        cx.pe(lambda e, ps=ps, kc=kc, c=c, half=half: e.matmul(
                            ps[0:64, :], hin[:, kc, c * CH:(c + 1) * CH],
                            wb[:, kc, 2048 + half * 512:2048 + (half + 1) * 512],
                            start=(kc == 0), stop=(kc == 7)), r=[hin, wb], w=[ps])
                    if k % 2 == 0:
                        cx.act(lambda e, ps=ps, c=c, half=half: e.copy(v[:, c, half * 512:(half + 1) * 512], ps[0:64, :]),
                               r=[ps], w=[v])
                    else:
                        cx.dve(lambda e, ps=ps, c=c, half=half: e.tensor_copy(v[:, c, half * 512:(half + 1) * 512], ps[0:64, :]),
                               r=[ps], w=[v])
                    k += 1
            for h in range(8):
                pq = nextP()
                proj_fm(cx, pq, wb, h * 128, 128, hin, MTK)
                cx.act(lambda e, pq=pq: e.activation(sq[:], pq[:], AF.Silu), r=[pq], w=[sq])
                pf = nextP()
                proj_fm(cx, pf, wb, 1024 + h * 128, 128, hin, MTK)
                cx.act(lambda e, pf=pf: e.activation(sg[:], pf[:], AF.Sigmoid), r=[pf], w=[sg])
                cx.dve(lambda e, h=h: e.tensor_scalar(sg[:], sg[:], omlT[:, h:h + 1], lbT[:, h:h + 1], ALU.mult, ALU.add),
                       r=[sg, omlT, lbT], w=[sg])
                cx.act(lambda e: e.activation(gl[:], sg[:], AF.Ln), r=[sg], w=[gl])
                cx.dve(lambda e: e.tensor_tensor_scan(bb[:], scanm[:], gl[:], 0.0, ALU.mult, ALU.add),
                       r=[scanm, gl], w=[bb])
                b3 = bb[:].rearrange("p (c n) -> p c n", n=CH)
                cx.dve(lambda e, b3=b3: e.tensor_tensor(
                    dd[:].rearrange("p (c n) -> p c n", n=CH), b3,
                    b3[:, :, 31:32].to_broadcast([128, NCH, CH]), ALU.subtract), r=[bb], w=[dd])
                cx.act(lambda e: e.activation(eq[:], dd[:], AF.Exp), r=[dd], w=[eq])
                cx.act(lambda e: e.activation(ek[:], dd[:], AF.Exp, scale=-1.0), r=[dd], w=[ek])
                cx.act(lambda e, b3=b3: e.activation(ebm[:], b3[:, :, 31], AF.Exp), r=[bb], w=[ebm])
                eq3 = eq[:].rearrange("p (c n) -> p c n", n=CH)
                cx.dve(lambda e, eq3=eq3: e.tensor_tensor(e2[:], ebm[:], eq3[:, :, CH - 1], ALU.mult),
                       r=[ebm, eq], w=[e2])
                cx.dve(lambda e: e.scalar_tensor_tensor(qt[:], sq[:], 128.0 ** -0.5, eq[:], ALU.mult, ALU.mult),
                       r=[sq, eq], w=[qt])
                cx.dve(lambda e: e.tensor_scalar(sg[:], sg[:], -1.0, 1.0, ALU.mult, ALU.add), r=[sg], w=[sg])
                cx.dve(lambda e: e.tensor_tensor(kt[:], sg[:], ek[:], ALU.mult), r=[sg, ek], w=[kt])
                core.run(h, qt, kt, v, h * 128, (ebm, ebm.t), (e2, e2.t), (eq, eq3[:, :, CH - 1]), oT)
                if DEBUG and m == DEBUG.get("m", 0) and h == 0:
                    for nm, bf in (("sq", sq), ("sg", sg), ("bb", bb), ("eq", eq), ("qt", qt), ("kt", kt), ("oT", oT), ("gl", gl)):
                        if nm in DEBUG:
                            cx.dma(DEBUG[nm], bf[:], reads=[bf], q="pool")
                    if "v" in DEBUG:
                        cx.dma(DEBUG["v"], v[:, :, 0:128], reads=[v], q="pool")
                cx.act(lambda e: e.activation(sqo[:], oT[:], AF.Square), r=[oT], w=[sqo])
                pss = nextP()
                cx.pe(lambda e, pss=pss: e.matmul(pss[:], onesb[:], sqo[:], start=True, stop=True),
                      r=[onesb, sqo], w=[pss])
                cx.act(lambda e, pss=pss: e.activation(rt[:], pss[:], AF.Sqrt, bias=EPS, scale=1.0 / 128.0),
                       r=[pss], w=[rt])
                cx.dve(lambda e: e.reciprocal(rt[:], rt[:]), r=[rt], w=[rt])
                pg = nextP()
                proj_fm(cx, pg, wb, 3072 + h * 128, 128, hin, MTK)
                cx.act(lambda e, pg=pg: e.activation(sgp[:], pg[:], AF.Sigmoid), r=[pg], w=[sgp])
                cx.dve(lambda e: e.scalar_tensor_tensor(rt[:], oT[:], normg[:, 0:1], rt[:], ALU.mult, ALU.mult),
                       r=[oT, normg, rt], w=[rt])
                cx.dve(lambda e, h=h, mt=mt: e.tensor_tensor(mt[:, h, :], rt[:], sgp[:], ALU.mult),
                       r=[rt, sgp], w=[mt])
            cx.dma(dram_fm(mixT_ap, 0, 8, m * MTK, (m + 1) * MTK), mt[:], reads=[mt], q="pool")

        for m in range(S // MTK):
            macro(m, hins[m % 2], mts[m % 2])
    cx.barrier()


RET_GAMMA = [1.0 - 2.0 ** (-5.0 - h) for h in range(4)]


def stage_ret(cx, hT_ap, w_ap, cos_ap, sin_ap, dec_ap, mixT_ap, ident_ap, maskT_ap, S):
    with ExitStack() as es:
        wb = cx.sb(es, "winb", [128, 8, 3072], BF16)
        with ExitStack() as es2:
            stg = [cx.sb(es2, "wstg%d" % i, [128, 1536], F32) for i in range(2)]
            load_weight_bf16(cx, wb, w_ap, 8, 3072, stg)
            cx.barrier()
        ident = cx.sb(es, "ident", [128, 128], BF16)
        cx.dma(ident[:], ident_ap, writes=[ident])
        onesb = cx.sb(es, "onesb", [128, 128], BF16)
        cx.dve(lambda e: e.memset(onesb[:], 1.0), w=[onesb])
        dec = cx.sb(es, "dec", [128, 8, MTK], F32)
        cx.dma(dec[:], dec_ap.rearrange("h t p n -> p (h t) n"), writes=[dec])
        core = GLACore(cx, es, 4, ident, maskT_ap)
        hins = [cx.sb(es, "hin%d" % i, [128, 8, MTK], BF16) for i in range(2)]
        coss = [cx.sb(es, "cos%d" % i, [128, MTK], F32) for i in range(2)]
        sins = [cx.sb(es, "sin%d" % i, [128, MTK], F32) for i in range(2)]
        v = cx.sb(es, "vtm", [64, NCH, 512], BF16)
        f32t = lambda n: cx.sb(es, n, [128, MTK], F32)
        t1, t2, oT, mean, var, sgp = [f32t(n) for n in ("t1", "t2", "oT", "mean", "var", "sgp")]
        qt = cx.sb(es, "qt", [128, MTK], BF16)
        kt = cx.sb(es, "kt", [128, MTK], BF16)
        ob = cx.sb(es, "ob", [128, MTK], BF16)
        sqo = cx.sb(es, "sqo", [128, MTK], BF16)
        mts = [cx.sb(es, "mixt%d" % i, [128, 4, MTK], BF16) for i in range(2)]
        P = [cx.ps(es, "P%d" % i, [128, 512], F32) for i in range(3)]
        pi = [0]

        def nextP():
            pi[0] += 1
            return P[pi[0] % 3]

        def rot(h, col0, tab, out, hin, cs, sn):
            pa = nextP()
            proj_fm(cx, pa, wb, col0 + h * 128, 128, hin, MTK)
            cx.dve(lambda e: e.tensor_tensor(t1[:], pa[:], cs[:], ALU.mult), r=[pa, cs], w=[t1])
            pb = nextP()
            proj_fm(cx, pb, wb, 1024 + col0 + h * 128, 128, hin, MTK)
            cx.dve(lambda e: e.tensor_tensor(t2[:], pb[:], sn[:], ALU.mult), r=[pb, sn], w=[t2])
            cx.dve(lambda e: e.tensor_tensor(t1[:], t1[:], t2[:], ALU.add), r=[t1, t2], w=[t1])
            cx.dve(lambda e: e.tensor_tensor(out[:], t1[:], dec[:, tab, :], ALU.mult), r=[t1, dec], w=[out])

        def macro(m, hin, mt, cs, sn):
            cx.dma(hin[:], dram_fm(hT_ap, 0, 8, m * MTK, (m + 1) * MTK), writes=[hin])
            cx.dma(cs[:], cos_ap[:, m * MTK:(m + 1) * MTK], writes=[cs])
            cx.dma(sn[:], sin_ap[:, m * MTK:(m + 1) * MTK], writes=[sn])
            for c in range(NCH):
                ps = nextP()
                for kc in range(8):
                    cx.pe(lambda e, ps=ps, kc=kc, c=c: e.matmul(
                        ps[0:64, :], hin[:, kc, c * CH:(c + 1) * CH], wb[:, kc, 2048:2560],
                        start=(kc == 0), stop=(kc == 7)), r=[hin, wb], w=[ps])
                if c % 2 == 0:
                    cx.act(lambda e, ps=ps, c=c: e.copy(v[:, c, :], ps[0:64, :]), r=[ps], w=[v])
                else:
                    cx.dve(lambda e, ps=ps, c=c: e.tensor_copy(v[:, c, :], ps[0:64, :]), r=[ps], w=[v])
            for h in range(4):
                g = RET_GAMMA[h]
                rot(h, 0, 2 * h, qt, hin, cs, sn)
                rot(h, 512, 2 * h + 1, kt, hin, cs, sn)
                core.run(h, qt, kt, v, h * 128, float(g ** 32), float(g ** 64), float(g ** 32), oT)
                cx.act(lambda e: e.copy(ob[:], oT[:]), r=[oT], w=[ob])
                cx.act(lambda e: e.activation(sqo[:], oT[:], AF.Square), r=[oT], w=[sqo])
                p1 = nextP()
                cx.pe(lambda e, p1=p1: e.matmul(p1[:], onesb[:], ob[:], start=True, stop=True), r=[onesb, ob], w=[p1])
                p2 = nextP()
                cx.pe(lambda e, p2=p2: e.matmul(p2[:], onesb[:], sqo[:], start=True, stop=True), r=[onesb, sqo], w=[p2])
                cx.act(lambda e, p1=p1: e.activation(mean[:], p1[:], AF.Copy, scale=1.0 / 128.0), r=[p1], w=[mean])
                cx.dve(lambda e: e.tensor_tensor(var[:], mean[:], mean[:], ALU.mult), r=[mean], w=[var])
                cx.dve(lambda e, p2=p2: e.scalar_tensor_tensor(var[:], p2[:], 1.0 / 128.0, var[:], ALU.mult, ALU.subtract),
                       r=[p2, var], w=[var])
                cx.act(lambda e: e.activation(var[:], var[:], AF.Sqrt, bias=1e-5, scale=1.0), r=[var], w=[var])
                cx.dve(lambda e: e.reciprocal(var[:], var[:]), r=[var], w=[var])
                cx.dve(lambda e: e.tensor_tensor(oT[:], oT[:], mean[:], ALU.subtract), r=[oT, mean], w=[oT])
                cx.dve(lambda e: e.tensor_tensor(oT[:], oT[:], var[:], ALU.mult), r=[oT, var], w=[oT])
                pg = nextP()
                proj_fm(cx, pg, wb, 2560 + h * 128, 128, hin, MTK)
                cx.act(lambda e, pg=pg: e.activation(sgp[:], pg[:], AF.Silu), r=[pg], w=[sgp])
                cx.dve(lambda e, h=h: e.tensor_tensor(mt[:, h, :], oT[:], sgp[:], ALU.mult), r=[oT, sgp], w=[mt])
            cx.dma(dram_fm(mixT_ap, 0, 4, m * MTK, (m + 1) * MTK), mt[:], reads=[mt], q="pool")

        for m in range(S // MTK):
            macro(m, hins[m % 2], mts[m % 2], coss[m % 2], sins[m % 2])
    cx.barrier()


def host_consts(S):
    import ml_dtypes
    c = {}
    c["ident"] = np.eye(128, dtype=np.float32).astype(ml_dtypes.bfloat16)
    m = np.arange(64)
    c["maskT"] = (m[:, None] <= m[None, :]).astype(np.float32)
    sm = np.ones((128, MTK), np.float32)
    sm[:, ::CH] = 0
    c["scanm"] = sm
    half = 64
    inv = (10000.0 ** (-np.arange(half, dtype=np.float32) / half)).astype(np.float32)
    ang = (np.arange(S, dtype=np.float32)[:, None] * inv[None, :]).astype(np.float32)
    cos = np.cos(ang).T.astype(np.float32)
    sin = np.sin(ang).T.astype(np.float32)
    c["cos"] = np.ascontiguousarray(np.concatenate([cos, cos], 0))
    c["sin"] = np.ascontiguousarray(np.concatenate([-sin, sin], 0))
    dec = np.zeros((4, 2, 128, MTK), np.float32)
    n = (np.arange(MTK) % CH).astype(np.float64)
    for h in range(4):
        g = RET_GAMMA[h]
        dec[h, 0] = (g ** (n - 31.0))[None, :]
        dec[h, 1] = (g ** (31.0 - n) * 128.0 ** -0.5)[None, :]
    c["dec"] = dec
    return c


NEG = -30000.0
NSA_PIPE = True
NSA_HOLD = True
NEG8 = NEG * 8.0


def nsa_consts(S):
    import ml_dtypes
    bf = ml_dtypes.bfloat16
    nb = S // 128
    c = {}
    tl = np.arange(128)
    n = np.arange(256)
    cm = np.full((nb, 128, 256), NEG8, np.float32)
    for i in range(nb):
        t = i * 128 + tl
        ok = (16 * n[None, :] + 31 <= t[:, None]) & (n[None, :] < S // 16 - 1)
        cm[i][ok] = 0.0
    c["cmask"] = cm.astype(bf)
    j = np.arange(64)
    fb = np.zeros((nb, 128, 64), np.float32)
    for i in range(nb):
        bt = (i * 128 + tl) // 64
        d = bt[:, None] - j[None, :]
        forced = (j[None, :] == 0) | ((d >= 0) & (d < 2))
        fb[i] = np.where(d >= 0, np.where(forced, 1.0e4, 0.0), -1.0e30)
    c["fbias"] = fb
    cs = np.arange(256) * 16
    ce = cs + 31
    ss = np.arange(64) * 64
    se = ss + 63
    ov = ((cs[:, None] <= se[None, :]) & (ce[:, None] >= ss[None, :])).astype(np.float32)
    ov[S // 16 - 1:, :] = 0
    c["ovl"] = ov.astype(bf)
    c["causal"] = np.where(tl[None, :] <= tl[:, None], 0.0, NEG8).astype(np.float32).astype(bf)
    kr = np.arange(640) - 512
    dist = tl[:, None] - kr[None, :]
    c["wmask"] = np.where((dist >= 0) & (dist < 512), 0.0, NEG8).astype(np.float32).astype(bf)
    c["rvalid"] = (tl >= 31).astype(np.float32).reshape(128, 1)
    kk = np.arange(S)
    c["blockE"] = (kk[None, :] // 64 == np.arange(64)[:, None]).astype(np.float32).astype(bf)
    return c


def stage_nsa(cx, hT_ap, w_ap, cw, mixT_ap, ident_ap, cn, S):
    NB = S // 128
    NT = S // 128
    with ExitStack() as es:
        wb = cx.sb(es, "wnsa", [128, 8, 1304], BF16)
        ksE = [cx.sb(es, "ksE%d" % g, [128, S], BF16) for g in range(2)]
        kwT = [cx.sb(es, "kwT%d" % g, [64, S], BF16) for g in range(2)]
        vsw = cx.sb(es, "vsw", [128, NT, 256], BF16)
        kcmpT = [cx.sb(es, "kcmpT%d" % g, [64, 256], BF16) for g in range(2)]
        vcmp = cx.sb(es, "vcmp", [128, 2, 2, 64], BF16)
        ident = cx.sb(es, "ident", [128, 128], BF16)
        P = [cx.ps(es, "P%d" % i, [128, 512], F32) for i in range(4)]
        TP = [cx.ps(es, "TP%d" % i, [128, 8, 128], BF16) for i in range(2)]
        PV = [cx.ps(es, "PV%d" % i, [128, 64], F32) for i in range(1)]
        IMP = cx.ps(es, "IMP", [128, 64], F32)
        cnt = {"p": 0, "tp": 0, "pv": 0, "cp": 0, "sp": 0}

        def nP():
            cnt["p"] += 1
            return P[cnt["p"] % 2]

        def nH():
            cnt["h"] = cnt.get("h", 0) + 1
            return P[2 + cnt["h"] % 2]

        def nTP():
            cnt["tp"] += 1
            return TP[cnt["tp"] % 2]

        def nPV():
            return PV[0]

        def cp(out_ap, in_ap, r, w):
            cnt["cp"] += 1
            if cnt["cp"] % 2:
                cx.act(lambda e: e.copy(out_ap, in_ap), r=r, w=w)
            else:
                cx.dve(lambda e: e.tensor_copy(out_ap, in_ap), r=r, w=w)

        with ExitStack() as es2:
            stg = [cx.sb(es2, "wstg%d" % i, [128, 1304], F32) for i in range(2)]
            load_weight_bf16(cx, wb, w_ap, 8, 1304, stg)
            cx.dma(ident[:], ident_ap, writes=[ident])
            for g in range(2):
                cx.dma(ksE[g][64:128, :], cn["blockE"], writes=[(ksE[g].key, "E")])
            cx.barrier()
            kcT = [cx.sb(es2, "kcT%d" % g, [64, S], BF16) for g in range(2)]
            vcT = [cx.sb(es2, "vcT%d" % g, [64, S], BF16) for g in range(2)]
            hins = [cx.sb(es2, "hin%d" % i, [128, 8, MTK], BF16) for i in range(2)]

            def phaseA(m, hin):
                cx.dma(hin[:], dram_fm(hT_ap, 0, 8, m * MTK, (m + 1) * MTK), writes=[hin])
                for (dst, col) in ((kcT, 512), (vcT, 640), (ksE, 768), (kwT, 896)):
                    for g in range(2):
                        ps = nP()
                        proj_fm(cx, ps, wb, col + g * 64, 64, hin, MTK)
                        cp(dst[g][0:64, m * MTK:(m + 1) * MTK], ps[0:64, :], [ps], [dst[g]])
                for j in range(4):
                    ps = nP()
                    for kc in range(8):
                        cx.pe(lambda e, ps=ps, kc=kc, j=j: e.matmul(
                            ps[:, 0:256], hin[:, kc, j * 128:(j + 1) * 128], wb[:, kc, 1024:1280],
                            start=(kc == 0), stop=(kc == 7)), r=[hin, wb], w=[ps])
                    cp(vsw[:, m * 4 + j, :], ps[:, 0:256], [ps], [vsw])

            for m in range(S // MTK):
                phaseA(m, hins[m % 2])

            w1s = cx.sb(es2, "w1s", [64, 32, 64], F32)
            w1b = cx.sb(es2, "w1b", [64, 32, 64], BF16)
            w2s = cx.sb(es2, "w2s", [64, 64], F32)
            w2b = cx.sb(es2, "w2b", [64, 64], BF16)
            poss = cx.sb(es2, "poss", [64, 32], F32)
            posb = cx.sb(es2, "posb", [64, 32], BF16)
            cb = cx.sb(es2, "cb", [64, 1], F32)
            tt = [cx.sb(es2, "gt%d" % i, [64, 256], F32) for i in range(3)]
            glb = cx.sb(es2, "glb", [64, 256], BF16)
            for g in range(2):
                cx.dve(lambda e, g=g: e.memset(kcmpT[g][:], 0.0), w=[kcmpT[g]])
            cx.dve(lambda e: e.memset(vcmp[:], 0.0), w=[vcmp])
            cx.dve(lambda e: e.memset(glb[:], 0.0), w=[glb])
            NCMP = S // 16 - 1

            def phaseB(kind, g, src):
                pos_ap, w1_ap, w2_ap = cw["pos_" + kind], cw["w1_" + kind], cw["w2_" + kind]
                if g == 0:
                    cx.dma(w1s[:], w1_ap.rearrange("(p d) o -> d p o", d=64), writes=[w1s])
                    cx.dma(w2s[:], w2_ap, writes=[w2s])
                    cx.dma(poss[:], pos_ap.rearrange("p d -> d p"), writes=[poss], allow_slow_non_contiguous=True)
                    cx.dve(lambda e: e.tensor_copy(w1b[:], w1s[:]), r=[w1s], w=[w1b])
                    cx.dve(lambda e: e.tensor_copy(w2b[:], w2s[:]), r=[w2s], w=[w2b])
                    cx.dve(lambda e: e.tensor_copy(posb[:], poss[:]), r=[poss], w=[posb])
                    pc = nPV()
                    for p in range(32):
                        cx.pe(lambda e, p=p, pc=pc: e.matmul(pc[0:64, 0:1], w1b[:, p, :], posb[:, p:p + 1],
                                                             start=(p == 0), stop=(p == 31)), r=[w1b, posb], w=[pc])
                    cx.act(lambda e, pc=pc: e.copy(cb[:], pc[0:64, 0:1]), r=[pc], w=[cb])
                ps = nP()
                x3 = src[0:64, :].rearrange("d (n s) -> d n s", s=16)
                for p in range(32):
                    n0, r_ = (0, p) if p < 16 else (1, p - 16)
                    cx.pe(lambda e, p=p, n0=n0, r_=r_, ps=ps: e.matmul(
                        ps[0:64, 0:NCMP], w1b[:, p, :], x3[:, n0:n0 + NCMP, r_],
                        start=(p == 0), stop=(p == 31)), r=[w1b, src], w=[ps])
                t0, t1_, t2_ = tt
                N = NCMP
                cx.act(lambda e, ps=ps: e.activation(t0[:, 0:N], ps[0:64, 0:N], AF.Identity, bias=cb[:], scale=1.0),
                       r=[ps, cb], w=[t0])
                cx.dve(lambda e: e.tensor_tensor(t1_[:, 0:N], t0[:, 0:N], t0[:, 0:N], ALU.mult), r=[t0], w=[t1_])
                cx.dve(lambda e: e.tensor_scalar(t1_[:, 0:N], t1_[:, 0:N], 0.044715, 1.0, ALU.mult, ALU.add), r=[t1_], w=[t1_])
                cx.dve(lambda e: e.tensor_tensor(t1_[:, 0:N], t1_[:, 0:N], t0[:, 0:N], ALU.mult), r=[t1_, t0], w=[t1_])
                cx.act(lambda e: e.activation(t2_[:, 0:N], t1_[:, 0:N], AF.Sigmoid, scale=2.0 * math.sqrt(2.0 / math.pi)),
                       r=[t1_], w=[t2_])
                cx.dve(lambda e: e.tensor_tensor(glb[:, 0:N], t0[:, 0:N], t2_[:, 0:N], ALU.mult), r=[t0, t2_], w=[glb])
                if kind == "k":
                    po = nP()
                    cx.pe(lambda e, po=po: e.matmul(po[0:64, 0:N], w2b[:], glb[:, 0:N], start=True, stop=True),
                          r=[w2b, glb], w=[po])
                    cp(kcmpT[g][:, 0:N], po[0:64, 0:N], [po], [kcmpT[g]])
                else:
                    for kc2 in range(2):
                        po = nPV()
                        n1 = min(128, N - kc2 * 128)
                        if n1 <= 0:
                            continue
                        cx.pe(lambda e, po=po, kc2=kc2, n1=n1: e.matmul(
                            po[0:n1, :], glb[:, kc2 * 128:kc2 * 128 + n1], w2b[:], start=True, stop=True),
                            r=[glb, w2b], w=[po])
                        cp(vcmp[0:n1, kc2, g, :], po[0:n1, :], [po], [vcmp])

            for kind, srcs in (("k", kcT), ("v", vcT)):
                for g in range(2):
                    phaseB(kind, g, srcs[g])
            cx.barrier()

        ovl = cx.sb(es, "ovl", [128, 2, 64], BF16)
        cx.dma(ovl[:], cn["ovl"].rearrange("(c p) j -> p c j", p=128), writes=[ovl])
        causal = cx.sb(es, "causal", [128, 128], BF16)
        cx.dma(causal[:], cn["causal"], writes=[causal])
        wmask = cx.sb(es, "wmask", [128, 640], BF16)
        cx.dma(wmask[:], cn["wmask"], writes=[wmask])
        rvalid = cx.sb(es, "rvalid", [128, 1], F32)
        cx.dma(rvalid[:], cn["rvalid"], writes=[rvalid])
        hqs = [cx.sb(es, "hq%d" % i, [128, 8, 128], BF16) for i in range(2)]
        cms = [cx.sb(es, "cm%d" % i, [128, 256], BF16) for i in range(2)]
        fbs = [cx.sb(es, "fb%d" % i, [128, 64], F32) for i in range(2)]
        qsel = [cx.sb(es, "qsel%d" % i, [128, 4, 128], BF16) for i in range(2)]
        selw = cx.sb(es, "selw", [128, 128], BF16)
        cx.dve(lambda e: e.memset(selw[:], 0.0), w=[selw])
        pcT = [cx.sb(es, "pcT%d" % i, [128, 2, 128], BF16) for i in range(4)]
        pc32 = [cx.sb(es, "pc32_%d" % i, [128, 256], F32) for i in range(2)]
        pbs = [cx.sb(es, "pb%d" % i, [128, S], BF16) for i in range(2)]
        pTs = [cx.sb(es, "pT%d" % i, [128, NT, 128], BF16) for i in range(2)]
        acc = cx.sb(es, "acc", [128, 512], F32)
        accb = cx.sb(es, "accb", [128, 512], BF16)
        mixt = [cx.sb(es, "mixt%d" % i, [128, 4, 128], BF16) for i in range(2)]
        sms = [{n_: cx.sb(es, n_ + str(i), [128, 8 if n_[0] == "c" else 1], F32)
                for n_ in ("cmax", "crs", "mx", "rs", "rinv", "fac")} for i in range(2)]
        sc64 = cx.sb(es, "sc64", [128, 64], F32)
        top8 = cx.sb(es, "top8", [128, 8], F32)
        sel01 = cx.sb(es, "sel01", [128, 64], F32)
        SCALE = 0.125

        def softmax_item(q_ap, q_r, KT, k0, nk, maskfn, vfn, gate_ap, gate_r, acc_ap, first, normalize, keepT=None, i0=False):
            cnt["sp"] += 1
            b = cnt["sp"] % 2
            sm, pb, pT = sms[b], pbs[b], pTs[b]
            cmax, crs, mx, rs, rinv, fac = sm["cmax"], sm["crs"], sm["mx"], sm["rs"], sm["rinv"], sm["fac"]
            chunks = [(c0, min(512, nk - c0)) for c0 in range(0, nk, 512)]
            ncn = len(chunks)
            dst32 = pc32[b] if normalize else None
            held = []

            def scores(ps, c0, n_):
                mm = maskfn(c0, n_)
                cx.pe(lambda e: e.matmul(ps[:, 0:n_], q_ap, KT[0:q_ap.shape[0], k0 + c0:k0 + c0 + n_],
                                         start=True, stop=(len(mm) == 0)), r=q_r + [KT], w=[ps])
                for idx, (l_ap, r_ap, lo, hi, rd) in enumerate(mm):
                    cx.pe(lambda e, l_ap=l_ap, r_ap=r_ap, lo=lo, hi=hi, idx=idx: e.matmul(
                        ps[:, lo:hi], l_ap, r_ap, start=False, stop=(idx == len(mm) - 1)), r=rd, w=[ps])

            def expo(ps, ci, c0, n_):
                out_ap = dst32[:, c0:c0 + n_] if normalize else pb[:, c0:c0 + n_]
                wr = [dst32] if normalize else [pb]
                cx.act(lambda e: e.activation(out_ap, ps[:, 0:n_], AF.Exp, bias=mx[:], scale=SCALE,
                                              accum_out=crs[:, ci:ci + 1]), r=[ps, mx], w=wr + [crs])

            def p1():
                for ci, (c0, n_) in enumerate(chunks):
                    ps = nH() if (ncn == 1 and NSA_HOLD) else nP()
                    scores(ps, c0, n_)
                    cx.dve(lambda e, ps=ps, ci=ci, n_=n_: e.reduce_max(cmax[:, ci:ci + 1], ps[:, 0:n_], AX.X),
                           r=[ps], w=[cmax])
                    if ncn == 1 and NSA_HOLD:
                        held.append(ps)
                if ncn == 1:
                    cx.dve(lambda e: e.tensor_scalar_mul(mx[:], cmax[:, 0:1], -SCALE), r=[cmax], w=[mx])
                else:
                    cx.dve(lambda e: e.tensor_reduce(mx[:], cmax[:, 0:ncn], AX.X, ALU.max), r=[cmax], w=[mx])
                    cx.dve(lambda e: e.tensor_scalar_mul(mx[:], mx[:], -SCALE), r=[mx], w=[mx])

            def p2():
                if ncn == 1 and NSA_HOLD:
                    expo(held[0], 0, chunks[0][0], chunks[0][1])
                    cx.dve(lambda e: e.reciprocal(rinv[:], crs[:, 0:1]), r=[crs], w=[rinv])
                elif ncn == 1:
                    ps = nP()
                    scores(ps, chunks[0][0], chunks[0][1])
                    expo(ps, 0, chunks[0][0], chunks[0][1])
                    cx.dve(lambda e: e.reciprocal(rinv[:], crs[:, 0:1]), r=[crs], w=[rinv])
                else:
                    for ci, (c0, n_) in enumerate(chunks):
                        ps = nP()
                        scores(ps, c0, n_)
                        expo(ps, ci, c0, n_)
                    cx.dve(lambda e: e.reduce_sum(rs[:], crs[:, 0:ncn], AX.X), r=[crs], w=[rs])
                    cx.dve(lambda e: e.reciprocal(rinv[:], rs[:]), r=[rs], w=[rinv])
                if normalize:
                    if i0:
                        cx.dve(lambda e: e.tensor_tensor(rinv[:], rinv[:], rvalid[:], ALU.mult), r=[rinv, rvalid], w=[rinv])
                    cx.dve(lambda e: e.tensor_scalar_mul(pb[:, 0:nk], dst32[:, 0:nk], rinv[:, 0:1]), r=[dst32, rinv], w=[pb])
                dstT = keepT if keepT is not None else pT
                nkt = nk // 128
                for t0 in range(0, nkt, 8):
                    n8 = min(8, nkt - t0)
                    tp = nTP()
                    for t in range(n8):
                        cx.pe(lambda e, tp=tp, t=t, t0=t0: e.transpose(tp[:, t, :], pb[:, (t0 + t) * 128:(t0 + t + 1) * 128], ident[:]),
                              r=[pb, ident], w=[tp])
                    cp(dstT[:, t0:t0 + n8, :], tp[:, 0:n8, :], [tp], [dstT])
                po = nPV()
                for t in range(nkt):
                    v_ap, v_r = vfn(t)
                    cx.pe(lambda e, po=po, t=t, v_ap=v_ap: e.matmul(po[:], dstT[:, t, :], v_ap, start=(t == 0), stop=(t == nkt - 1)),
                          r=[dstT] + v_r, w=[po])
                if normalize:
                    sc_ap, sc_r = gate_ap, [gate_r]
                else:
                    cx.dve(lambda e: e.tensor_tensor(fac[:], rinv[:], gate_ap, ALU.mult), r=[rinv, gate_r], w=[fac])
                    sc_ap, sc_r = fac[:, 0:1], [fac]
                if first:
                    cx.dve(lambda e, po=po: e.tensor_scalar_mul(acc_ap, po[:], sc_ap), r=[po] + sc_r, w=[acc])
                else:
                    cx.dve(lambda e, po=po: e.scalar_tensor_tensor(acc_ap, po[:], sc_ap, acc_ap, ALU.mult, ALU.add),
                           r=[po, acc] + sc_r, w=[acc])

            return p1, p2

        items = []

        def block(i, hq, cm, fb, mt, gates):
            nk = 128 * (i + 1)
            kt0 = max(0, i - 4)
            nkw = 128 * (i - kt0 + 1)

            def blk_pre():
                cx.dma(hq[:], dram_fm(hT_ap, 0, 8, i * 128, (i + 1) * 128), writes=[hq])
                cx.dma(cm[:], cn["cmask"][i], writes=[cm])
                cx.dma(fb[:], cn["fbias"][i], writes=[fb])
                pg = nPV()
                for kc in range(8):
                    cx.pe(lambda e, kc=kc, pg=pg: e.matmul(pg[:, 0:24], hq[:, kc, :], wb[:, kc, 1280:1304],
                                                           start=(kc == 0), stop=(kc == 7)), r=[hq, wb], w=[pg])
                cx.act(lambda e, pg=pg: e.activation(gates[:], pg[:, 0:24], AF.Sigmoid), r=[pg], w=[gates])

            def blk_post():
                cx.act(lambda e: e.copy(accb[:], acc[:]), r=[acc], w=[accb])
                tp = nTP()
                for c in range(4):
                    cx.pe(lambda e, c=c, tp=tp: e.transpose(tp[:, c, :], accb[:, c * 128:(c + 1) * 128], ident[:]),
                          r=[accb, ident], w=[tp])
                cp(mt[:], tp[:, 0:4, :], [tp], [mt])
                cx.dma(dram_fm(mixT_ap, 4, 8, i * 128, (i + 1) * 128), mt[:], reads=[mt], q="pool")

            def group(g):
                qs = qsel[g]
                qk = (qs.key, "q")
                sk = (qs.key, "s")

                def q_pre():
                    for hp in range(4):
                        hd = g * 4 + hp
                        ps = nP()
                        proj_fm(cx, ps, wb, hd * 64, 64, hq, 128)
                        cp(qs[0:64, hp, :], ps[0:64, 0:128], [ps], [qk])

                def sel_pre():
                    for hp in range(4):
                        for kc2 in range(2):
                            cx.pe(lambda e, hp=hp, kc2=kc2: e.matmul(IMP[:], pcT[hp][:, kc2, :], ovl[:, kc2, :],
                                                                     start=(hp == 0 and kc2 == 0), stop=(hp == 3 and kc2 == 1)),
                                  r=[pcT[hp], ovl], w=[IMP])
                    cx.dve(lambda e: e.tensor_tensor(sc64[:], IMP[:], fb[:], ALU.add), r=[IMP, fb], w=[sc64])
                    cx.dve(lambda e: e.max(top8[:], sc64[:]), r=[sc64], w=[top8])
                    cx.dve(lambda e: e.tensor_scalar(sel01[:], sc64[:], top8[:, 7:8], None, ALU.is_ge), r=[sc64, top8], w=[sel01])
                    cx.dve(lambda e: e.tensor_scalar(selw[:, 64:128], sel01[:], -NEG8, NEG8, ALU.mult, ALU.add), r=[sel01], w=[selw])
                    tps = nTP()
                    cx.pe(lambda e: e.transpose(tps[:, 0, :], selw[:], ident[:]), r=[selw, ident], w=[tps])
                    cx.act(lambda e: e.copy(qs[64:128, :, :], tps[64:128, 0:1, :].to_broadcast([64, 4, 128])),
                           r=[tps], w=[sk])

                def mk_cmp(hp):
                    hd = g * 4 + hp
                    return lambda: softmax_item(
                        qs[0:64, hp, :], [qk], kcmpT[g], 0, 256,
                        lambda c0, n_: [(ident[:], cm[:, c0:c0 + n_], 0, n_, [ident, cm])],
                        lambda t: (vcmp[:, t, g, :], [vcmp]),
                        gates[:, hd:hd + 1], gates, acc[:, hd * 64:(hd + 1) * 64], True, True, keepT=pcT[hp], i0=(i == 0))

                def mk_win(hp):
                    hd = g * 4 + hp
                    return lambda: softmax_item(
                        qs[0:64, hp, :], [qk], kwT[g], kt0 * 128, nkw,
                        lambda c0, n_: [(ident[:], wmask[:, 640 - nkw + c0:640 - nkw + c0 + n_], 0, n_, [ident, wmask])],
                        lambda t: (vsw[:, kt0 + t, 128 + g * 64:128 + (g + 1) * 64], [vsw]),
                        gates[:, 16 + hd:17 + hd], gates, acc[:, hd * 64:(hd + 1) * 64], False, False)

                def mk_slc(hp):
                    hd = g * 4 + hp
                    return lambda: softmax_item(
                        qs[:, hp, :], [qk, sk], ksE[g], 0, nk,
                        lambda c0, n_: ([(ident[:], causal[:], n_ - 128, n_, [ident, causal])] if c0 + n_ == nk else []),
                        lambda t: (vsw[:, t, g * 64:(g + 1) * 64], [vsw]),
                        gates[:, 8 + hd:9 + hd], gates, acc[:, hd * 64:(hd + 1) * 64], False, False)

                lst = []
                for hp in range(4):
                    lst.append([q_pre if hp == 0 else None, mk_cmp(hp), None])
                for hp in range(4):
                    lst.append([sel_pre if hp == 1 else None, mk_win(hp), None])
                for hp in range(4):
                    lst.append([None, mk_slc(hp), None])
                return lst

            lst = group(0) + group(1)
            first_pre = lst[0][0]
            lst[0][0] = lambda: (blk_pre(), first_pre())
            lst[-1][2] = blk_post
            items.extend(lst)

        gates2 = [cx.sb(es, "gates%d" % i_, [128, 24], F32) for i_ in range(2)]
        for i in range(NB):
            block(i, hqs[i % 2], cms[i % 2], fbs[i % 2], mixt[i % 2], gates2[i % 2])
        prev = None
        for pre, mk, post in items:
            if pre is not None:
                pre()
            p1, p2 = mk()
            p1()
            if not NSA_PIPE:
                p2()
                if post is not None:
                    post()
                continue
            if prev is not None:
                prev[0]()
                if prev[1] is not None:
                    prev[1]()
            prev = (p2, post)
        if NSA_PIPE:
            prev[0]()
            if prev[1] is not None:
                prev[1]()
    cx.barrier()


def nsa2_consts(S):
    import ml_dtypes
    bf = ml_dtypes.bfloat16
    nb = S // 128
    base = nsa_consts(S)
    c = {"fbias": base["fbias"], "blockE": base["blockE"]}
    cm = base["cmask"].astype(np.float32)
    cmT = cm.transpose(0, 2, 1).reshape(nb, 2, 128, 128)
    cmT = np.broadcast_to(cmT.transpose(0, 2, 1, 3)[:, :, :, None, :], (nb, 128, 2, 4, 128))
    c["cmaskT"] = np.ascontiguousarray(cmT).astype(bf)
    kl = np.arange(128)[:, None]
    ql = np.arange(128)[None, :]
    cz = np.where(kl <= ql, 0.0, NEG8).astype(np.float32)
    w0 = np.where(kl > ql, 0.0, NEG8).astype(np.float32)
    c["causalT4"] = np.ascontiguousarray(np.broadcast_to(cz[:, None, :], (128, 4, 128))).astype(bf)
    c["wm0T4"] = np.ascontiguousarray(np.broadcast_to(w0[:, None, :], (128, 4, 128))).astype(bf)
    ov = np.ones((256, 80), np.float32)
    ov[:, 0:64] = base["ovl"].astype(np.float32)
    c["ovla"] = ov.astype(bf)
    sr = np.zeros((24, 24, 64), np.float32)
    for r in range(24):
        sr[r, r, :] = 1.0
    c["selrows"] = sr
    return c


NSA2_STOP = ""


def stage_nsa2(cx, hT_ap, w_ap, cw, mixT_ap, ident_ap, cn, S):
    NB = S // 128
    NT = S // 128
    with ExitStack() as es:
        wb = cx.sb(es, "wnsa", [128, 8, 1312], BF16)
        cx.dve(lambda e: e.memset(wb[:, :, 1304:1312], 0.0), w=[(wb.key, "pad")])
        ksE = [cx.sb(es, "ksE%d" % g, [128, S], BF16) for g in range(2)]
        kwT = [cx.sb(es, "kwT%d" % g, [64, S], BF16) for g in range(2)]
        vaug = cx.sb(es, "vaug", [128, NT, 4, 80], BF16)
        kcmpT = [cx.sb(es, "kcmpT%d" % g, [64, 256], BF16) for g in range(2)]
        vcmp = cx.sb(es, "vcmp", [128, 2, 2, 80], BF16)
        ident = cx.sb(es, "ident", [128, 128], BF16)
        onesb = cx.sb(es, "onesb", [128, 128], BF16)
        ones32 = cx.sb(es, "ones32", [128, 64], F32)
        kmx = cx.sb(es, "kmx", [128, 8], F32)
        P = [cx.ps(es, "P%d" % i, [128, 512], F32) for i in range(3)]
        OTs = [cx.ps(es, "OT%d" % i, [128, 512], F32) for i in range(2)]
        RP = cx.ps(es, "RP", [128, 4, 128], F32)
        M1 = cx.ps(es, "M1", [128, 512], F32)
        M2 = cx.ps(es, "M2", [128, 8, 128], BF16)
        cnt = {"p": 0, "cp": 0, "o": 0, "pt": 0}

        def nP():
            cnt["p"] += 1
            return P[cnt["p"] % 3]

        def nO():
            cnt["o"] += 1
            return OTs[cnt["o"] % 2]

        def cp(out_ap, in_ap, r, w):
            cnt["cp"] += 1
            if cnt["cp"] % 2:
                cx.act(lambda e: e.copy(out_ap, in_ap), r=r, w=w)
            else:
                cx.dve(lambda e: e.tensor_copy(out_ap, in_ap), r=r, w=w)

        cx.dve(lambda e: e.memset(onesb[:], 1.0), w=[onesb])
        cx.dve(lambda e: e.memset(ones32[:], 1.0), w=[ones32])
        cx.dve(lambda e: e.memset(vaug[:], 1.0), w=[vaug])
        with ExitStack() as es2:
            stg = [cx.sb(es2, "wstg%d" % i, [128, 1304], F32) for i in range(2)]
            load_weight_bf16(cx, wb, w_ap, 8, 1304, stg)
            cx.dma(ident[:], ident_ap, writes=[ident])
            for g in range(2):
                cx.dma(ksE[g][64:128, :], cn["blockE"], writes=[(ksE[g].key, "E")])
            cx.barrier()
            kcT = [cx.sb(es2, "kcT%d" % g, [64, S], BF16) for g in range(2)]
            vcT = [cx.sb(es2, "vcT%d" % g, [64, S], BF16) for g in range(2)]
            hins = [cx.sb(es2, "hin%d" % i, [128, 8, MTK], BF16) for i in range(2)]

            def phaseA(m, hin):
                cx.dma(hin[:], dram_fm(hT_ap, 0, 8, m * MTK, (m + 1) * MTK), writes=[hin])
                for (dst, col) in ((kcT, 512), (vcT, 640), (ksE, 768), (kwT, 896)):
                    for g in range(2):
                        ps = nP()
                        proj_fm(cx, ps, wb, col + g * 64, 64, hin, MTK)
                        cp(dst[g][0:64, m * MTK:(m + 1) * MTK], ps[0:64, :], [ps], [dst[g]])
                for j in range(4):
                    ps = nP()
                    for kc in range(8):
                        cx.pe(lambda e, ps=ps, kc=kc, j=j: e.matmul(
                            ps[:, 0:256], hin[:, kc, j * 128:(j + 1) * 128], wb[:, kc, 1024:1280],
                            start=(kc == 0), stop=(kc == 7)), r=[hin, wb], w=[ps])
                    cp(vaug[:, m * 4 + j, :, 0:64], ps[:, 0:256].rearrange("p (v d) -> p v d", d=64), [ps], [vaug])

            for m in range(S // MTK):
                phaseA(m, hins[m % 2])

            w1s = cx.sb(es2, "w1s", [64, 32, 64], F32)
            w1b = cx.sb(es2, "w1b", [64, 32, 64], BF16)
            w2s = cx.sb(es2, "w2s", [64, 64], F32)
            w2b = cx.sb(es2, "w2b", [64, 64], BF16)
            poss = cx.sb(es2, "poss", [64, 32], F32)
            posb = cx.sb(es2, "posb", [64, 32], BF16)
            cb = cx.sb(es2, "cb", [64, 1], F32)
            tt = [cx.sb(es2, "gt%d" % i, [64, 256], F32) for i in range(3)]
            glb = cx.sb(es2, "glb", [64, 256], BF16)
            for g in range(2):
                cx.dve(lambda e, g=g: e.memset(kcmpT[g][:], 0.0), w=[kcmpT[g]])
            cx.dve(lambda e: e.memset(vcmp[:], 0.0), w=[vcmp])
            cx.dve(lambda e: e.memset(vcmp[:, :, :, 64:80], 1.0), r=[vcmp], w=[vcmp])
            cx.dve(lambda e: e.memset(glb[:], 0.0), w=[glb])
            NCMP = S // 16 - 1

            def phaseB(kind, g, src):
                pos_ap, w1_ap, w2_ap = cw["pos_" + kind], cw["w1_" + kind], cw["w2_" + kind]
                if g == 0:
                    cx.dma(w1s[:], w1_ap.rearrange("(p d) o -> d p o", d=64), writes=[w1s])
                    cx.dma(w2s[:], w2_ap, writes=[w2s])
                    cx.dma(poss[:], pos_ap.rearrange("p d -> d p"), writes=[poss], allow_slow_non_contiguous=True)
                    cx.dve(lambda e: e.tensor_copy(w1b[:], w1s[:]), r=[w1s], w=[w1b])
                    cx.dve(lambda e: e.tensor_copy(w2b[:], w2s[:]), r=[w2s], w=[w2b])
                    cx.dve(lambda e: e.tensor_copy(posb[:], poss[:]), r=[poss], w=[posb])
                    for p in range(32):
                        cx.pe(lambda e, p=p: e.matmul(M1[0:64, 0:1], w1b[:, p, :], posb[:, p:p + 1],
                                                      start=(p == 0), stop=(p == 31)), r=[w1b, posb], w=[M1])
                    cx.act(lambda e: e.copy(cb[:], M1[0:64, 0:1]), r=[M1], w=[cb])
                ps = nP()
                x3 = src[0:64, :].rearrange("d (n s) -> d n s", s=16)
                for p in range(32):
                    n0, r_ = (0, p) if p < 16 else (1, p - 16)
                    cx.pe(lambda e, p=p, n0=n0, r_=r_, ps=ps: e.matmul(
                        ps[0:64, 0:NCMP], w1b[:, p, :], x3[:, n0:n0 + NCMP, r_],
                        start=(p == 0), stop=(p == 31)), r=[w1b, src], w=[ps])
                t0, t1_, t2_ = tt
                N = NCMP
                cx.act(lambda e, ps=ps: e.activation(t0[:, 0:N], ps[0:64, 0:N], AF.Identity, bias=cb[:], scale=1.0),
                       r=[ps, cb], w=[t0])
                cx.dve(lambda e: e.tensor_tensor(t1_[:, 0:N], t0[:, 0:N], t0[:, 0:N], ALU.mult), r=[t0], w=[t1_])
                cx.dve(lambda e: e.tensor_scalar(t1_[:, 0:N], t1_[:, 0:N], 0.044715, 1.0, ALU.mult, ALU.add), r=[t1_], w=[t1_])
                cx.dve(lambda e: e.tensor_tensor(t1_[:, 0:N], t1_[:, 0:N], t0[:, 0:N], ALU.mult), r=[t1_, t0], w=[t1_])
                cx.act(lambda e: e.activation(t2_[:, 0:N], t1_[:, 0:N], AF.Sigmoid, scale=2.0 * math.sqrt(2.0 / math.pi)),
                       r=[t1_], w=[t2_])
                cx.dve(lambda e: e.tensor_tensor(glb[:, 0:N], t0[:, 0:N], t2_[:, 0:N], ALU.mult), r=[t0, t2_], w=[glb])
                if kind == "k":
                    po = nP()
                    cx.pe(lambda e, po=po: e.matmul(po[0:64, 0:N], w2b[:], glb[:, 0:N], start=True, stop=True),
                          r=[w2b, glb], w=[po])
                    cp(kcmpT[g][:, 0:N], po[0:64, 0:N], [po], [kcmpT[g]])
                else:
                    for kc2 in range(2):
                        n1 = min(128, N - kc2 * 128)
                        if n1 <= 0:
                            continue
                        po = nP()
                        cx.pe(lambda e, po=po, kc2=kc2, n1=n1: e.matmul(
                            po[0:n1, 0:64], glb[:, kc2 * 128:kc2 * 128 + n1], w2b[:], start=True, stop=True),
                            r=[glb, w2b], w=[po])
                        cp(vcmp[0:n1, kc2, g, 0:64], po[0:n1, 0:64], [po], [vcmp])

            for kind, srcs in (("k", kcT), ("v", vcT)):
                for g in range(2):
                    phaseB(kind, g, srcs[g])

            sqk = [cx.sb(es2, "sqk%d" % i, [64, 512], BF16) for i in range(2)]
            kcm = cx.sb(es2, "kcm", [128, 8], F32)
            qi = [0]

            def kmax(src, ncols, col):
                nchunk = (ncols + 511) // 512
                for ci in range(nchunk):
                    c0 = ci * 512
                    n_ = min(512, ncols - c0)
                    sq = sqk[qi[0] % 2]
                    qi[0] += 1
                    cx.act(lambda e, sq=sq, c0=c0, n_=n_: e.activation(sq[:, 0:n_], src[0:64, c0:c0 + n_], AF.Square),
                           r=[src], w=[sq])
                    ps = nP()
                    cx.pe(lambda e, ps=ps, sq=sq, n_=n_: e.matmul(ps[:, 0:n_], onesb[0:64, :], sq[:, 0:n_], start=True, stop=True),
                          r=[onesb, sq], w=[ps])
                    cx.dve(lambda e, ps=ps, ci=ci, n_=n_: e.reduce_max(kcm[:, ci:ci + 1], ps[:, 0:n_], AX.X), r=[ps], w=[kcm])
                cx.dve(lambda e: e.tensor_reduce(kmx[:, col:col + 1], kcm[:, 0:nchunk], AX.X, ALU.max), r=[kcm], w=[kmx])

            for g in range(2):
                kmax(kcmpT[g], 256, 0 + g)
                kmax(kwT[g], S, 2 + g)
                kmax(ksE[g], S, 4 + g)
            cx.barrier()

        def cload(name, shape, dtype, src):
            t = cx.sb(es, name, shape, dtype)
            cx.dma(t[:], src, writes=[t])
            return t

        ovla = cload("ovla", [128, 2, 80], BF16, cn["ovla"].rearrange("(c p) j -> p c j", p=128))
        causalT4 = cload("causalT4", [128, 4, 128], BF16, cn["causalT4"])
        wm0T4 = cload("wm0T4", [128, 4, 128], BF16, cn["wm0T4"])
        selrows = cload("selrows", [24, 24, 64], F32, cn["selrows"])
        hqs = [cx.sb(es, "hq%d" % i, [128, 8, 128], BF16) for i in range(2)]
        cmTs = [cx.sb(es, "cmT%d" % i, [128, 2, 4, 128], BF16) for i in range(2)]
        fbs = [cx.sb(es, "fb%d" % i, [128, 64], F32) for i in range(2)]
        gatesT = [cx.sb(es, "gatesT%d" % i, [32, 128], F32) for i in range(2)]
        qsel = [cx.sb(es, "qsel%d" % i, [128, 4, 128], BF16) for i in range(2)]
        sqq = cx.sb(es, "sqq", [64, 4, 128], BF16)
        qm = cx.sb(es, "qm", [128, 1], F32)
        negc = [cx.sb(es, "negc%d" % i, [128, 1], F32) for i in range(3)]
        selw = cx.sb(es, "selw", [128, 128], BF16)
        cx.dve(lambda e: e.memset(selw[:], 0.0), w=[selw])
        PcT = cx.sb(es, "PcT", [128, 2, 512], BF16)
        NPT = 6
        PTs = [cx.sb(es, "PT%d" % i, [128, 512], BF16) for i in range(NPT)]
        rsr = cx.sb(es, "rsr", [128, 512], F32)
        bcs = cx.sb(es, "bcs", [64, 512], F32)
        tmpo = cx.sb(es, "tmpo", [64, 512], F32)
        accT = [cx.sb(es, "accT%d" % i, [64, 8, 128], F32) for i in range(2)]
        accTb = [cx.sb(es, "accTb%d" % i, [64, 8, 128], BF16) for i in range(2)]
        rs4 = cx.sb(es, "rs4", [128, 4], F32)
        impb = cx.sb(es, "impb", [128, 64], F32)
        sc64 = cx.sb(es, "sc64", [128, 64], F32)
        top8 = cx.sb(es, "top8", [128, 8], F32)
        sel01 = cx.sb(es, "sel01", [128, 64], F32)
        SCALE = 0.125
        mix_dst = mixT_ap[4:8].rearrange("c (two d) s -> d (c two) s", two=2)

        def block(i, hq, cmT, fb, gT, acc, accb):
            kt0 = max(0, i - 4)
            cx.dma(hq[:], dram_fm(hT_ap, 0, 8, i * 128, (i + 1) * 128), writes=[hq])
            cx.dma(cmT[:], cn["cmaskT"][i], writes=[cmT])
            cx.dma(fb[:], cn["fbias"][i], writes=[fb])
            for kc in range(8):
                cx.pe(lambda e, kc=kc: e.matmul(M1[0:32, 0:128], wb[:, kc, 1280:1312], hq[:, kc, :],
                                                start=(kc == 0), stop=(kc == 7)), r=[hq, wb], w=[M1])
            cx.act(lambda e: e.activation(gT[:], M1[0:32, 0:128], AF.Sigmoid), r=[M1], w=[gT])

            def finalize(OT, g, br, first):
                accg = acc[:, g * 4:(g + 1) * 4, :]
                cx.dve(lambda e: e.tensor_scalar_max(rsr[64:65, :], OT[64:65, :], 1e-30), r=[OT], w=[rsr])
                cx.dve(lambda e: e.reciprocal(rsr[64:65, :], rsr[64:65, :]), r=[rsr], w=[rsr])
                cx.pe(lambda e: e.matmul(M1[0:64, :], ones32[64:65, 0:64], rsr[64:65, :], start=True, stop=True),
                      r=[ones32, rsr], w=[M1])
                cx.act(lambda e: e.copy(bcs[:], M1[0:64, :]), r=[M1], w=[bcs])
                cx.dve(lambda e: e.tensor_tensor(tmpo[:], OT[0:64, :], bcs[:], ALU.mult), r=[OT, bcs], w=[tmpo])
                for hp in range(4):
                    r_ = br * 8 + g * 4 + hp
                    cx.pe(lambda e, hp=hp, r_=r_: e.matmul(M1[0:64, hp * 128:(hp + 1) * 128], selrows[:, r_, :], gT[0:24, :],
                                                           start=True, stop=True), r=[selrows, gT], w=[M1])
                t3 = tmpo[:].rearrange("p (h q) -> p h q", q=128)
                m3 = M1[0:64, :].rearrange("p (h q) -> p h q", q=128)
                if first:
                    cx.dve(lambda e: e.tensor_tensor(accg, t3, m3, ALU.mult), r=[tmpo, M1], w=[acc])
                else:
                    cx.dve(lambda e: e.tensor_tensor(t3, t3, m3, ALU.mult), r=[tmpo, M1], w=[tmpo])
                    cx.dve(lambda e: e.tensor_tensor(accg, accg, t3, ALU.add), r=[acc, tmpo], w=[acc])

            def run_tiles(tiles, qrows, ncg, vfn, mfn, qsel_key):
                OT = nO()
                pend = None
                n = len(tiles)
                for t, (l_ap, l_r) in enumerate(tiles):
                    ps = nP()
                    mm = mfn(t)
                    cx.pe(lambda e, ps=ps, l_ap=l_ap, stp=(mm is None): e.matmul(ps[:], l_ap, qrows, start=True, stop=stp),
                          r=l_r + [qsel_key], w=[ps])
                    if mm is not None:
                        cx.pe(lambda e, ps=ps, mm=mm: e.matmul(ps[:], ident[:], mm[0], start=False, stop=True),
                              r=[ident] + mm[1], w=[ps])
                    cnt["pt"] += 1
                    pt = PTs[cnt["pt"] % NPT]
                    cx.act(lambda e, ps=ps, pt=pt: e.activation(pt[:], ps[:], AF.Exp, bias=ncg[:], scale=SCALE),
                           r=[ps, ncg], w=[pt])
                    if pend is not None:
                        pend()
                    v_ap, v_r = vfn(t)
                    pend = (lambda t=t, pt=pt, v_ap=v_ap, v_r=v_r: cx.pe(
                        lambda e: e.matmul(OT[0:80, :], v_ap[:, 0:80], pt[:], start=(t == 0), stop=(t == n - 1)),
                        r=[pt] + v_r, w=[OT]))
                pend()
                return OT

            def group(g):
                qs = qsel[g]
                qk = (qs.key, "q")
                sk = (qs.key, "s")
                global_q = qs[0:64, :, :].rearrange("p h q -> p (h q)")
                if NSA2_STOP == "q0":
                    return
                for hp in range(4):
                    hd = g * 4 + hp
                    for kc in range(8):
                        cx.pe(lambda e, kc=kc, hp=hp, hd=hd: e.matmul(M1[0:64, hp * 128:(hp + 1) * 128], wb[:, kc, hd * 64:(hd + 1) * 64],
                                                                     hq[:, kc, :], start=(kc == 0), stop=(kc == 7)),
                              r=[wb, hq], w=[M1])
                cx.dve(lambda e, qs=qs: e.tensor_copy(qs[0:64, :, :].rearrange("p h q -> p (h q)"), M1[0:64, :]), r=[M1], w=[qk])
                cx.act(lambda e: e.activation(sqq[:].rearrange("p h q -> p (h q)"), M1[0:64, :], AF.Square), r=[M1], w=[sqq])
                ps = nP()
                cx.pe(lambda e, ps=ps: e.matmul(ps[:], onesb[0:64, :], sqq[:].rearrange("p h q -> p (h q)"), start=True, stop=True),
                      r=[onesb, sqq], w=[ps])
                cx.dve(lambda e, ps=ps: e.reduce_max(qm[:], ps[:], AX.X), r=[ps], w=[qm])
                for br in range(3):
                    nb_ = negc[br]
                    cx.dve(lambda e, nb_=nb_, br=br, g=g: e.tensor_tensor(nb_[:], qm[:], kmx[:, 2 * br + g:2 * br + g + 1], ALU.mult),
                           r=[qm, kmx], w=[nb_])
                    cx.act(lambda e, nb_=nb_: e.activation(nb_[:], nb_[:], AF.Sqrt), r=[nb_], w=[nb_])
                    cx.dve(lambda e, nb_=nb_: e.tensor_scalar_mul(nb_[:], nb_[:], -SCALE * 1.05), r=[nb_], w=[nb_])
                if NSA2_STOP in ("q", "q1"):
                    return
                OT = nO()
                for kc2 in range(2):
                    ps = nP()
                    cx.pe(lambda e, ps=ps, kc2=kc2: e.matmul(ps[:], kcmpT[g][:, kc2 * 128:(kc2 + 1) * 128], global_q,
                                                            start=True, stop=False), r=[kcmpT[g], qk], w=[ps])
                    cx.pe(lambda e, ps=ps, kc2=kc2: e.matmul(ps[:], ident[:], cmT[:, kc2, :, :].rearrange("p h q -> p (h q)"),
                                                            start=False, stop=True), r=[ident, cmT], w=[ps])
                    cx.act(lambda e, ps=ps, kc2=kc2: e.activation(PcT[:, kc2, :], ps[:], AF.Exp, bias=negc[0][:], scale=SCALE),
                           r=[ps, negc[0]], w=[PcT])
                if NSA2_STOP == "exp":
                    return
                for kc2 in range(2):
                    cx.pe(lambda e, kc2=kc2, OT=OT: e.matmul(OT[0:80, :], vcmp[:, kc2, g, 0:80], PcT[:, kc2, :],
                                                            start=(kc2 == 0), stop=(kc2 == 1)), r=[vcmp, PcT], w=[OT])
                if NSA2_STOP == "pv":
                    return
                for hp in range(4):
                    for kc2 in range(2):
                        cx.pe(lambda e, hp=hp, kc2=kc2: e.matmul(RP[:, hp, 0:80], PcT[:, kc2, hp * 128:(hp + 1) * 128], ovla[:, kc2, :],
                                                                 start=(kc2 == 0), stop=(kc2 == 1)), r=[PcT, ovla], w=[RP])
                cx.dve(lambda e: e.tensor_scalar_max(rs4[:], RP[:, :, 64], 1e-30), r=[RP], w=[rs4])
                cx.dve(lambda e: e.reciprocal(rs4[:], rs4[:]), r=[rs4], w=[rs4])
                cx.dve(lambda e: e.scalar_tensor_tensor(impb[:], RP[:, 0, 0:64], rs4[:, 0:1], fb[:], ALU.mult, ALU.add),
                       r=[RP, rs4, fb], w=[impb])
                for hp in range(1, 4):
                    cx.dve(lambda e, hp=hp: e.scalar_tensor_tensor(impb[:], RP[:, hp, 0:64], rs4[:, hp:hp + 1], impb[:], ALU.mult, ALU.add),
                           r=[RP, rs4, impb], w=[impb])
                cx.dve(lambda e: e.max(top8[:], impb[:]), r=[impb], w=[top8])
                cx.dve(lambda e: e.tensor_scalar(sel01[:], impb[:], top8[:, 7:8], None, ALU.is_ge), r=[impb, top8], w=[sel01])
                cx.dve(lambda e: e.tensor_scalar(selw[:, 64:128], sel01[:], -NEG8, NEG8, ALU.mult, ALU.add), r=[sel01], w=[selw])
                cx.pe(lambda e: e.transpose(M2[:, 0, :], selw[:], ident[:]), r=[selw, ident], w=[M2])
                cx.act(lambda e, qs=qs: e.copy(qs[64:128, :, :], M2[64:128, 0:1, :].to_broadcast([64, 4, 128])), r=[M2], w=[sk])
                if NSA2_STOP == "sel":
                    return
                finalize(OT, g, 0, True)
                if NSA2_STOP == "cmp":
                    return
                wt = list(range(kt0, i + 1))

                def wmask(t):
                    r_ = wt[t] - (i - 4)
                    if r_ == 0:
                        return (wm0T4[:].rearrange("p h q -> p (h q)"), [wm0T4])
                    if r_ == 4:
                        return (causalT4[:].rearrange("p h q -> p (h q)"), [causalT4])
                    return None

                OT = run_tiles([(kwT[g][:, kt * 128:(kt + 1) * 128], [kwT[g]]) for kt in wt], global_q, negc[1],
                               lambda t: (vaug[:, wt[t], 2 + g, :], [vaug]), wmask, qk)
                finalize(OT, g, 2, False)
                if NSA2_STOP == "win":
                    return
                qfull = qs[:, :, :].rearrange("p h q -> p (h q)")
                OT = run_tiles([(ksE[g][:, kt * 128:(kt + 1) * 128], [ksE[g], (ksE[g].key, "E"), qk]) for kt in range(i + 1)],
                               qfull, negc[2], lambda t: (vaug[:, t, g, :], [vaug]),
                               lambda t: ((causalT4[:].rearrange("p h q -> p (h q)"), [causalT4]) if t == i else None), sk)
                finalize(OT, g, 1, False)

            for g in range(2):
                group(g)
            if NSA2_STOP in ("q0", "q1"):
                return
            cx.act(lambda e: e.copy(accb[:], acc[:]), r=[acc], w=[accb])
            cx.dma(mix_dst[:, :, i * 128:(i + 1) * 128], accb[:], reads=[accb], q="pool")

        for i in range(NB if NSA2_STOP != "AB" else 0):
            block(i, hqs[i % 2], cmTs[i % 2], fbs[i % 2], gatesT[i % 2], accT[i % 2], accTb[i % 2])
    cx.barrier()


SEQ = 4096
NCORES = 8
DEPTH = 4


def build_full(S=SEQ, depth=DEPTH):
    nc = bass.Bass("TRN2", target_bir_lowering=False)

    def din(n, s, d=F32):
        return nc.dram_tensor(n, list(s), d, kind="ExternalInput").ap()

    x = din("x", [S, D])
    norm_mix_g = din("norm_mix_g", [4, D])
    norm_ffn_g = din("norm_ffn_g", [4, D])
    final_norm_g = din("final_norm_g", [D])
    w_ret = din("w_ret", [2, D, 3072])
    w_nsa = din("w_nsa", [2, D, 1304])
    even_w_out = din("even_w_out", [2, D, D])
    cws = {}
    for kind in "kv":
        cws["pos_" + kind] = din("cmp_pos_" + kind, [2, 32, 64])
        cws["w1_" + kind] = din("cmp_w1_" + kind, [2, 2048, 64])
        cws["w2_" + kind] = din("cmp_w2_" + kind, [2, 64, 64])
    odd_w_in = din("odd_w_in", [2, D, 4096])
    odd_w_out = din("odd_w_out", [2, D, D])
    hgrn_norm_g = din("hgrn_norm_g", [2, 128])
    hgrn_lb = din("hgrn_lb_logits", [2, 1024])
    ffn_w1 = din("ffn_w1", [4, D, DFF])
    ffn_w3 = din("ffn_w3", [4, D, DFF])
    ffn_w2 = din("ffn_w2", [4, DFF, D])
    ident = din("c_ident", [128, 128], BF16)
    maskT = din("c_maskT", [64, 64])
    scanm = din("c_scanm", [128, MTK])
    cos = din("c_cos", [128, S])
    sin = din("c_sin", [128, S])
    dec = din("c_dec", [4, 2, 128, MTK])
    NB = S // 128
    cn = {
        "cmaskT": din("c_cmaskT", [NB, 128, 2, 4, 128], BF16),
        "fbias": din("c_fbias", [NB, 128, 64]),
        "blockE": din("c_blockE", [64, S], BF16),
        "causalT4": din("c_causalT4", [128, 4, 128], BF16),
        "wm0T4": din("c_wm0T4", [128, 4, 128], BF16),
        "ovla": din("c_ovla", [256, 80], BF16),
        "selrows": din("c_selrows", [24, 24, 64]),
    }
    y = nc.dram_tensor("y", [S, D], F32, kind="ExternalOutput").ap()
    xs = nc.dram_tensor("xs", [S, D], F32).ap()
    hTa = nc.dram_tensor("hTa", [8, 128, S], BF16).ap()
    hTb = nc.dram_tensor("hTb", [8, 128, S], BF16).ap()
    mixT = nc.dram_tensor("mixT", [8, 128, S], BF16).ap()

    cx = Ctx(nc)
    stage_norm0(cx, x, hTb, norm_mix_g[0], ident, S)
    for layer in range(depth):
        j = layer // 2
        if layer % 2 == 0:
            stage_ret(cx, hTb, w_ret[j], cos, sin, dec, mixT, ident, maskT, S)
            cw = {k_: v_[j] for k_, v_ in cws.items()}
            stage_nsa2(cx, hTb, w_nsa[j], cw, mixT, ident, cn, S)
            w_out = even_w_out[j]
        else:
            stage_hgrn(cx, hTb, odd_w_in[j], hgrn_norm_g[j], hgrn_lb, mixT, ident, maskT, scanm, S, j)
            w_out = odd_w_out[j]
        stage_out(cx, mixT, w_out, x if layer == 0 else xs, xs, hTa, norm_ffn_g[layer], ident, S)
        last = layer == depth - 1
        stage_ffn(cx, hTa, ffn_w1[layer], ffn_w3[layer], ffn_w2[layer], xs, xs, hTb,
                  norm_mix_g[min(layer + 1, 3)], ident, S, last, gfin_ap=final_norm_g, y_ap=y)
    cx.emit()
    return nc


def host_layout(inputs, S=SEQ):
    f32 = lambda a: np.ascontiguousarray(np.asarray(a, dtype=np.float32))
    ew = f32(inputs["even_w_in"])

    def swap(w):
        return w.reshape(w.shape[0], D, 4, 2, 64)[:, :, :, ::-1, :].reshape(w.shape[0], D, 512)

    rq, rk, rv, rg = ew[:, :, 0:512], ew[:, :, 512:1024], ew[:, :, 1024:1536], ew[:, :, 1536:2048]
    nq = ew[:, :, 2048:2560]
    kc, vc, ks, vs, kw, vw = [ew[:, :, 2560 + 128 * i:2560 + 128 * (i + 1)] for i in range(6)]
    ng = ew[:, :, 3328:3352]
    shared = {
        "w_ret": np.ascontiguousarray(np.concatenate([rq, rk, swap(rq), swap(rk), rv, rg], axis=2)),
        "w_nsa": np.ascontiguousarray(np.concatenate([nq, kc, vc, ks, kw, vs, vw, ng], axis=2)),
    }
    for k_ in ("norm_mix_g", "norm_ffn_g", "final_norm_g", "even_w_out", "cmp_pos_k", "cmp_w1_k", "cmp_w2_k",
               "cmp_pos_v", "cmp_w1_v", "cmp_w2_v", "odd_w_in", "odd_w_out", "hgrn_norm_g", "hgrn_lb_logits",
               "ffn_w1", "ffn_w3", "ffn_w2"):
        shared[k_] = f32(inputs[k_])
    for k_, v_ in host_consts(S).items():
        shared["c_" + k_] = v_
    for k_, v_ in nsa2_consts(S).items():
        shared["c_" + k_] = v_
    return shared


def kernel(**inputs):
    x = np.ascontiguousarray(np.asarray(inputs["x"], dtype=np.float32))
    B, S, _ = x.shape
    shared = host_layout(inputs, S)
    nc = build_full(S)
    in_maps = []
    for b in range(B):
        m = dict(shared)
        m["x"] = np.ascontiguousarray(x[b])
        in_maps.append(m)
    res = run_bass_kernel_spmd(nc, in_maps, core_ids=list(range(B)))
    return np.stack([np.asarray(r["y"], dtype=np.float32) for r in res.results], axis=0)
```

```python
import math
from contextlib import ExitStack

import numpy as np
import concourse.bass as bass
import concourse.mybir as mybir
from concourse.bass_utils import run_bass_kernel_spmd

F32 = mybir.dt.float32
BF16 = mybir.dt.bfloat16
AF = mybir.ActivationFunctionType
ALU = mybir.AluOpType
AX = mybir.AxisListType

D = 1024
DFF = 2816
NFF = DFF // 128
EPS = 1e-6
EVEN_IN = 3352


class Buf:
    def __init__(self, t, key, psum=False):
        self.t = t
        self.key = key
        self.psum = psum

    def __getitem__(self, k):
        return self.t[k]


class Ctx:
    NDMA = 8
    ENG = ("pe", "act", "dve", "pool", "sp")

    def __init__(self, nc):
        self.nc = nc
        self.streams = {e: [] for e in self.ENG}
        self.cnt = {e: 0 for e in ("pe", "act", "dve", "pool")}
        self.dman = {q: 0 for q in ("sp", "act", "pool")}
        self.lastw = {}
        self.readers = {}
        self.known = {e: {} for e in self.ENG}
        self.all_tokens = {}
        self.nbuf = 0

    def sb(self, es, name, shape, dtype):
        self.nbuf += 1
        nm = "%s_%d" % (name, self.nbuf)
        t = es.enter_context(self.nc.sbuf_tensor(nm, list(shape), dtype))
        return Buf(t, nm)

    def ps(self, es, name, shape, dtype):
        self.nbuf += 1
        nm = "%s_%d" % (name, self.nbuf)
        t = es.enter_context(self.nc.psum_tensor(nm, list(shape), dtype))
        return Buf(t, nm, psum=True)

    @staticmethod
    def _k(x):
        return x.key if isinstance(x, Buf) else x

    def _collect(self, eng, reads, writes):
        deps = []
        for r in reads:
            k = self._k(r)
            if k in self.lastw:
                deps.append(self.lastw[k])
        for w in writes:
            k = self._k(w)
            if k in self.lastw:
                deps.append(self.lastw[k])
            deps.extend(self.readers.get(k, ()))
        return deps

    def _record(self, tok, reads, writes):
        for r in reads:
            self.readers.setdefault(self._k(r), []).append(tok)
        for w in writes:
            k = self._k(w)
            self.lastw[k] = tok
            self.readers[k] = []

    def _waits(self, eng, deps, is_pe_compute):
        waits = {}
        kn = self.known[eng]
        for (sk, v, src) in deps:
            if is_pe_compute and src == "pe":
                continue
            if kn.get(sk, 0) >= v:
                continue
            if waits.get(sk, 0) < v:
                waits[sk] = v
        for sk, v in waits.items():
            kn[sk] = v
        return list(waits.items())

    def op(self, eng, fn, reads=(), writes=()):
        ex = [r for r in reads if isinstance(r, Buf) and r.psum]
        if ex:
            writes = list(writes) + ex
        deps = self._collect(eng, reads, writes)
        waits = self._waits(eng, deps, eng == "pe")
        self.cnt[eng] += 1
        tok = (eng, self.cnt[eng], eng)
        self.streams[eng].append((waits, fn, (eng, 1)))
        self.all_tokens[eng] = tok
        self._record(tok, reads, writes)

    def dma(self, out, in_, reads=(), writes=(), q="sp", **kw):
        deps = self._collect(q, reads, writes)
        n = self.dman[q]
        slot = n % self.NDMA
        sk = ("dma", q, slot)
        if n >= self.NDMA:
            deps.append((sk, 16 * (n // self.NDMA), "dma"))
        waits = self._waits(q, deps, False)
        self.dman[q] += 1
        tok = (sk, 16 * (n // self.NDMA + 1), "dma")
        self.streams[q].append((waits, lambda e: e.dma_start(out=out, in_=in_, **kw), (sk, 16)))
        self.all_tokens[sk] = tok
        self._record(tok, reads, writes)

    def barrier(self):
        toks = list(self.all_tokens.values())
        for e in self.ENG:
            waits = self._waits(e, toks, False)
            if waits:
                self.streams[e].append((waits, None, None))

    def pe(self, fn, r=(), w=()):
        self.op("pe", fn, r, w)

    def act(self, fn, r=(), w=()):
        self.op("act", fn, r, w)

    def dve(self, fn, r=(), w=()):
        self.op("dve", fn, r, w)

    def pool(self, fn, r=(), w=()):
        self.op("pool", fn, r, w)

    def emit(self):
        nc = self.nc
        self.barrier()
        with ExitStack() as es:
            sems = {}
            for e in ("pe", "act", "dve", "pool"):
                sems[e] = es.enter_context(nc.semaphore("s_" + e))
            for q in ("sp", "act", "pool"):
                for s in range(self.NDMA):
                    sems[("dma", q, s)] = es.enter_context(nc.semaphore("d_%s%d" % (q, s)))
            block = es.enter_context(nc.Block())
            streams = self.streams

            def replay(name, eng):
                for waits, fn, inc in streams[name]:
                    for sk, v in waits:
                        eng.wait_ge(sems[sk], v)
                    if fn is not None:
                        fn(eng).then_inc(sems[inc[0]], inc[1])

            if streams["sp"]:
                @block.sync
                def _(eng):
                    replay("sp", eng)
            if streams["pe"]:
                @block.tensor
                def _(eng):
                    replay("pe", eng)
            if streams["dve"]:
                @block.vector
                def _(eng):
                    replay("dve", eng)
            if streams["act"]:
                @block.scalar
                def _(eng):
                    replay("act", eng)
            if streams["pool"]:
                @block.gpsimd
                def _(eng):
                    replay("pool", eng)


def load_weight_bf16(cx, wb, w_ap, kchunks, ncols, stage):
    step = stage[0].t.shape[1]
    i = 0
    for c in range(kchunks):
        for c0 in range(0, ncols, step):
            n = min(step, ncols - c0)
            st = stage[i % len(stage)]
            cx.dma(st[:, 0:n], w_ap[c * 128:(c + 1) * 128, c0:c0 + n], writes=[st])
            if i % 2 == 0:
                cx.dve(lambda e, st=st, c=c, c0=c0, n=n: e.tensor_copy(wb[:, c, c0:c0 + n], st[:, 0:n]),
                       r=[st], w=[(wb.key, c, c0)])
            else:
                cx.act(lambda e, st=st, c=c, c0=c0, n=n: e.copy(wb[:, c, c0:c0 + n], st[:, 0:n]),
                       r=[st], w=[(wb.key, c, c0)])
            i += 1
    return wb


class NormTools:
    def __init__(self, cx, es, g_ap):
        self.cx = cx
        self.ident = cx.sb(es, "ident", [128, 128], BF16)
        self.gT = cx.sb(es, "gT", [128, 8], F32)
        self.ss = [cx.sb(es, "ss%d" % i, [128, 1], F32) for i in range(2)]
        self.rstd = [cx.sb(es, "rstd%d" % i, [128, 1], F32) for i in range(2)]
        self.junk = cx.sb(es, "junk", [128, D], BF16)
        self.hb = [cx.sb(es, "hb%d" % i, [128, D], BF16) for i in range(2)]
        self.tp = [cx.ps(es, "tp%d" % i, [128, 8, 128], BF16) for i in range(2)]
        self.i = 0
        cx.dma(self.gT[:], g_ap.rearrange("(c p) -> p c", p=128), writes=[self.gT],
               allow_slow_non_contiguous=True)

    def load_ident(self, ident_ap):
        self.cx.dma(self.ident[:], ident_ap, writes=[self.ident])

    def stats(self, xt):
        cx = self.cx
        i = self.i
        self.i += 1
        ss, rstd = self.ss[i % 2], self.rstd[i % 2]
        junk = self.junk
        cx.act(lambda e: e.activation(junk[:], xt[:], AF.Square, scale=1.0 / 32.0, accum_out=ss[:]),
               r=[xt], w=[junk, ss])
        cx.act(lambda e: e.activation(ss[:], ss[:], AF.Sqrt, bias=EPS, scale=1.0), r=[ss], w=[ss])
        cx.dve(lambda e: e.reciprocal(rstd[:], ss[:]), r=[ss], w=[rstd])
        return rstd

    def run(self, xt, hT, col0, scale_by_g=True):
        cx = self.cx
        i = self.i
        self.i += 1
        ss, rstd, hb, tp = self.ss[i % 2], self.rstd[i % 2], self.hb[i % 2], self.tp[i % 2]
        junk = self.junk
        cx.act(lambda e: e.activation(junk[:], xt[:], AF.Square, scale=1.0 / 32.0, accum_out=ss[:]),
               r=[xt], w=[junk, ss])
        cx.act(lambda e: e.activation(ss[:], ss[:], AF.Sqrt, bias=EPS, scale=1.0), r=[ss], w=[ss])
        cx.dve(lambda e: e.reciprocal(rstd[:], ss[:]), r=[ss], w=[rstd])
        cx.act(lambda e: e.activation(hb[:], xt[:], AF.Copy, scale=rstd[:]), r=[xt, rstd], w=[hb])
        for c in range(8):
            cx.pe(lambda e, c=c: e.transpose(tp[:, c, :], hb[:, c * 128:(c + 1) * 128], self.ident[:]),
                  r=[hb, self.ident], w=[tp])
        gT = self.gT
        cx.dve(lambda e: e.tensor_tensor(hT[:, :, col0:col0 + 128], tp[:],
                                         gT[:].unsqueeze(2).to_broadcast([128, 8, 128]), ALU.mult),
               r=[tp, gT], w=[hT])
        return rstd


def dram_fm(ap, c0, c1, s0, s1):
    return ap[c0:c1, :, s0:s1].rearrange("c p s -> p c s")


def stage_norm0(cx, x_ap, hT_ap, g_ap, ident_ap, S):
    with ExitStack() as es:
        nt = NormTools(cx, es, g_ap)
        nt.load_ident(ident_ap)
        xts = [cx.sb(es, "xt%d" % i, [128, D], F32) for i in range(2)]
        hTs = [cx.sb(es, "hTt%d" % i, [128, 8, 512], BF16) for i in range(2)]
        for m in range(S // 512):
            hT = hTs[m % 2]
            for j in range(4):
                t = m * 4 + j
                xt = xts[t % 2]
                cx.dma(xt[:], x_ap[t * 128:(t + 1) * 128, :], writes=[xt])
                nt.run(xt, hT, j * 128)
            cx.dma(dram_fm(hT_ap, 0, 8, m * 512, (m + 1) * 512), hT[:], reads=[hT], q="pool")
    cx.barrier()


def stage_out(cx, mixT_ap, w_ap, xin_ap, xout_ap, hT_ap, g_ap, ident_ap, S):
    with ExitStack() as es:
        wb = cx.sb(es, "woutb", [128, 8, D], BF16)
        with ExitStack() as es2:
            stg = [cx.sb(es2, "wstg%d" % i, [128, 1024], F32) for i in range(2)]
            load_weight_bf16(cx, wb, w_ap, 8, D, stg)
            cx.barrier()
        nt = NormTools(cx, es, g_ap)
        nt.load_ident(ident_ap)
        mts = [cx.sb(es, "mixt%d" % i, [128, 8, 512], BF16) for i in range(2)]
        xts = [cx.sb(es, "xt%d" % i, [128, D], F32) for i in range(2)]
        x1s = [cx.sb(es, "x1t%d" % i, [128, D], F32) for i in range(2)]
        hTs = [cx.sb(es, "hTt%d" % i, [128, 8, 512], BF16) for i in range(2)]
        yps = [cx.ps(es, "yps%d" % i, [128, 512], F32) for i in range(4)]
        for m in range(S // 512):
            mt = mts[m % 2]
            hT = hTs[m % 2]
            cx.dma(mt[:], dram_fm(mixT_ap, 0, 8, m * 512, (m + 1) * 512), writes=[mt])
            for j in range(4):
                t = m * 4 + j
                xt = xts[t % 2]
                x1 = x1s[t % 2]
                cx.dma(xt[:], xin_ap[t * 128:(t + 1) * 128, :], writes=[xt])
                for half in range(2):
                    yp = yps[(t * 2 + half) % 4]
                    for c in range(8):
                        cx.pe(lambda e, yp=yp, c=c, j=j, half=half, mt=mt: e.matmul(
                            yp[:], mt[:, c, j * 128:(j + 1) * 128], wb[:, c, half * 512:(half + 1) * 512],
                            start=(c == 0), stop=(c == 7)), r=[mt, wb], w=[yp])
                    cx.dve(lambda e, yp=yp, half=half, xt=xt, x1=x1: e.tensor_tensor(
                        x1[:, half * 512:(half + 1) * 512], yp[:], xt[:, half * 512:(half + 1) * 512], ALU.add),
                        r=[yp, xt], w=[x1])
                cx.dma(xout_ap[t * 128:(t + 1) * 128, :], x1[:], reads=[x1], q="pool")
                nt.run(x1, hT, j * 128)
            cx.dma(dram_fm(hT_ap, 0, 8, m * 512, (m + 1) * 512), hT[:], reads=[hT], q="pool")
    cx.barrier()


def stage_ffn(cx, hT_ap, w1_ap, w3_ap, w2_ap, xin_ap, xout_ap, hTout_ap, g_ap, ident_ap, S, final,
              gfin_ap=None, y_ap=None):
    MT = 256
    with ExitStack() as es:
        w1b = cx.sb(es, "w1b", [128, 8, DFF], BF16)
        w3b = cx.sb(es, "w3b", [128, 8, DFF], BF16)
        w2b = cx.sb(es, "w2b", [128, NFF, D], BF16)
        with ExitStack() as es2:
            stg = [cx.sb(es2, "wstg%d" % i, [128, 1408], F32) for i in range(2)]
            load_weight_bf16(cx, w1b, w1_ap, 8, DFF, stg)
            load_weight_bf16(cx, w3b, w3_ap, 8, DFF, stg)
            load_weight_bf16(cx, w2b, w2_ap, NFF, D, stg)
            cx.barrier()
        nt = NormTools(cx, es, g_ap)
        nt.load_ident(ident_ap)
        if final:
            gfin = cx.sb(es, "gfin", [128, D], F32)
            cx.dma(gfin[:], gfin_ap.partition_broadcast(128), writes=[gfin])
        hins = [cx.sb(es, "hin%d" % i, [128, 8, MT], BF16) for i in range(2)]
        gT = cx.sb(es, "gTff", [128, NFF, MT], BF16)
        sil = [cx.sb(es, "sil%d" % i, [128, MT], F32) for i in range(2)]
        xts = [cx.sb(es, "xt%d" % i, [128, D], F32) for i in range(2)]
        x2s = [cx.sb(es, "x2t%d" % i, [128, D], F32) for i in range(2)]
        hTs = [cx.sb(es, "hTt%d" % i, [128, 8, MT], BF16) for i in range(2)]
        ups = [cx.ps(es, "ups%d" % i, [128, 512], F32) for i in range(4)]
        yps = [cx.ps(es, "yps%d" % i, [128, 512], F32) for i in range(2)]
        nsub = MT // 128
        for m in range(S // MT):
            hin = hins[m % 2]
            hT = hTs[m % 2]
            cx.dma(hin[:], dram_fm(hT_ap, 0, 8, m * MT, (m + 1) * MT), writes=[hin])
            for f in range(NFF):
                u1 = ups[(f % 2) * 2]
                u3 = ups[(f % 2) * 2 + 1]
                sl = sil[f % 2]
                for (wb, up) in ((w1b, u1), (w3b, u3)):
                    for c in range(8):
                        cx.pe(lambda e, wb=wb, up=up, c=c, f=f, hin=hin: e.matmul(
                            up[:, 0:MT], wb[:, c, f * 128:(f + 1) * 128], hin[:, c, :],
                            start=(c == 0), stop=(c == 7)), r=[wb, hin], w=[up])
                cx.act(lambda e, u1=u1, sl=sl: e.activation(sl[:], u1[:, 0:MT], AF.Silu), r=[u1], w=[sl])
                cx.dve(lambda e, u3=u3, sl=sl, f=f: e.tensor_tensor(gT[:, f, :], u3[:, 0:MT], sl[:], ALU.mult),
                       r=[u3, sl], w=[gT])
            for j in range(nsub):
                t = m * nsub + j
                xt = xts[t % 2]
                x2 = x2s[t % 2]
                cx.dma(xt[:], xin_ap[t * 128:(t + 1) * 128, :], writes=[xt])
                for half in range(2):
                    yp = yps[half]
                    for f in range(NFF):
                        cx.pe(lambda e, yp=yp, f=f, j=j, half=half: e.matmul(
                            yp[:], gT[:, f, j * 128:(j + 1) * 128], w2b[:, f, half * 512:(half + 1) * 512],
                            start=(f == 0), stop=(f == NFF - 1)), r=[gT, w2b], w=[yp])
                    cx.dve(lambda e, yp=yp, half=half, xt=xt, x2=x2: e.tensor_tensor(
                        x2[:, half * 512:(half + 1) * 512], yp[:], xt[:, half * 512:(half + 1) * 512], ALU.add),
                        r=[yp, xt], w=[x2])
                if not final:
                    cx.dma(xout_ap[t * 128:(t + 1) * 128, :], x2[:], reads=[x2], q="pool")
                    nt.run(x2, hT, j * 128)
                else:
                    rstd = nt.stats(x2)
                    ot = xt
                    cx.dve(lambda e, x2=x2, rstd=rstd, ot=ot: e.scalar_tensor_tensor(
                        ot[:], x2[:], rstd[:], gfin[:], ALU.mult, ALU.mult), r=[x2, rstd, gfin], w=[ot])
                    cx.dma(y_ap[t * 128:(t + 1) * 128, :], ot[:], reads=[ot], q="pool")
            if not final:
                cx.dma(dram_fm(hTout_ap, 0, 8, m * MT, (m + 1) * MT), hT[:], reads=[hT], q="pool")
    cx.barrier()


DEBUG = {}
CH = 64
MTK = 512
NCH = MTK // CH


class GLACore:
    def __init__(self, cx, es, nheads, ident, maskT_ap):
        self.cx = cx
        self.ident = ident
        self.maskT = cx.sb(es, "maskT", [64, 64], F32)
        cx.dma(self.maskT[:], maskT_ap, writes=[self.maskT])
        self.S = [cx.sb(es, "S%d" % h, [128, 128], F32) for h in range(nheads)]
        for h in range(nheads):
            cx.dve(lambda e, h=h: e.memset(self.S[h][:], 0.0), w=[self.S[h]])
        self.ATbs = [cx.sb(es, "ATb%d" % i, [64, NCH, 64], BF16) for i in range(2)]
        self.ktms = [cx.sb(es, "ktm%d" % i, [64, NCH, 128], BF16) for i in range(2)]
        self.KVds = [cx.sb(es, "KVd%d" % i, [128, NCH, 128], F32) for i in range(2)]
        self.spb = [cx.sb(es, "spb%d" % i, [128, 128], BF16) for i in range(2)]
        self.AT = cx.ps(es, "ATp", [64, NCH, 64], F32)
        self.KTt = cx.ps(es, "KTt", [64, NCH, 128], BF16)
        self.OT = cx.ps(es, "OTp", [128, MTK], F32)
        self.KV = [cx.ps(es, "KVp%d" % i, [128, 4, 128], F32) for i in range(2)]

    def pre(self, par, qt, kt, v, vcol0, dlast):
        cx = self.cx
        AT, ATb, KTt, ktm, KV, KVd = self.AT, self.ATbs[par], self.KTt, self.ktms[par], self.KV, self.KVds[par]
        maskT, ident = self.maskT, self.ident
        for c in range(NCH):
            cs = slice(c * CH, (c + 1) * CH)
            cx.pe(lambda e, c=c, cs=cs: e.matmul(AT[:, c, :], kt[:, cs], qt[:, cs], start=True, stop=True),
                  r=[kt, qt], w=[AT])
        cx.dve(lambda e: e.tensor_tensor(ATb[:], AT[:], maskT[:].unsqueeze(1).to_broadcast([64, NCH, 64]), ALU.mult),
               r=[AT, maskT], w=[ATb])
        for c in range(NCH):
            cs = slice(c * CH, (c + 1) * CH)
            cx.pe(lambda e, c=c, cs=cs: e.transpose(KTt[:, c, :], kt[:, cs], ident[:]), r=[kt, ident], w=[KTt])
        cx.act(lambda e: e.copy(ktm[:], KTt[:]), r=[KTt], w=[ktm])
        for c in range(NCH):
            cx.pe(lambda e, c=c: e.matmul(KV[c // 4][:, c % 4, :], ktm[:, c, :], v[:, c, vcol0:vcol0 + 128],
                                          start=True, stop=True), r=[ktm, v], w=[KV[c // 4]])
        for b in range(2):
            if isinstance(dlast, float):
                cx.dve(lambda e, b=b: e.tensor_scalar_mul(KVd[:, 4 * b:4 * b + 4, :], KV[b][:], dlast),
                       r=[KV[b]], w=[KVd])
            else:
                cx.dve(lambda e, b=b: e.tensor_tensor(
                    KVd[:, 4 * b:4 * b + 4, :], KV[b][:],
                    dlast[1][:, 4 * b:4 * b + 4].unsqueeze(2).to_broadcast([128, 4, 128]), ALU.mult),
                    r=[KV[b], dlast[0]], w=[KVd])

    def chain(self, par, h, qt, v, vcol0, ebm, e2, oT_sb):
        cx = self.cx
        ATb, KVd, OT, S = self.ATbs[par], self.KVds[par], self.OT, self.S[h]

        def sc(x, c):
            return (x, []) if isinstance(x, float) else (x[1][:, c:c + 1], [x[0]])

        for c in range(NCH):
            cs = slice(c * CH, (c + 1) * CH)
            spb = self.spb[c % 2]
            s_ebm, r_ebm = sc(ebm, c)
            s_e2, r_e2 = sc(e2, c)
            cx.act(lambda e, spb=spb, s_ebm=s_ebm: e.activation(spb[:], S[:], AF.Copy, scale=s_ebm),
                   r=[S] + r_ebm, w=[spb])
            cx.pe(lambda e, c=c, cs=cs: e.matmul(OT[:, cs], v[:, c, vcol0:vcol0 + 128], ATb[:, c, :],
                                                 start=True, stop=False), r=[v, ATb], w=[OT])
            cx.pe(lambda e, cs=cs, spb=spb: e.matmul(OT[:, cs], spb[:], qt[:, cs], start=False, stop=True),
                  r=[spb, qt], w=[OT])
            cx.dve(lambda e, c=c, s_e2=s_e2: e.scalar_tensor_tensor(S[:], S[:], s_e2, KVd[:, c, :], ALU.mult, ALU.add),
                   r=[S, KVd] + r_e2, w=[S])
        cx.act(lambda e: e.copy(oT_sb[:], OT[:]), r=[OT], w=[oT_sb])

    def run(self, h, qt, kt, v, vcol0, ebm, e2, dlast, oT_sb):
        self.pre(0, qt, kt, v, vcol0, dlast)
        self.chain(0, h, qt, v, vcol0, ebm, e2, oT_sb)


def proj_fm(cx, ps, wb, col0, ncols, hin, n):
    for c in range(8):
        cx.pe(lambda e, c=c: e.matmul(ps[0:ncols, 0:n], wb[:, c, col0:col0 + ncols], hin[:, c, 0:n],
                                      start=(c == 0), stop=(c == 7)), r=[wb, hin], w=[ps])


def stage_hgrn(cx, hT_ap, w_ap, normg_ap, lb_ap, mixT_ap, ident_ap, maskT_ap, scanm_ap, S, layer_j):
    with ExitStack() as es:
        wb = cx.sb(es, "winb", [128, 8, 4096], BF16)
        with ExitStack() as es2:
            stg = [cx.sb(es2, "wstg%d" % i, [128, 2048], F32) for i in range(2)]
            load_weight_bf16(cx, wb, w_ap, 8, 4096, stg)
            cx.barrier()
        ident = cx.sb(es, "ident", [128, 128], BF16)
        cx.dma(ident[:], ident_ap, writes=[ident])
        onesb = cx.sb(es, "onesb", [128, 128], BF16)
        cx.dve(lambda e: e.memset(onesb[:], 1.0), w=[onesb])
        scanm = cx.sb(es, "scanm", [128, MTK], F32)
        cx.dma(scanm[:], scanm_ap, writes=[scanm])
        normg = cx.sb(es, "normg", [128, 1], F32)
        cx.dma(normg[:], normg_ap.rearrange("(p o) -> p o", o=1), writes=[normg])
        lbT = cx.sb(es, "lbT", [128, 8], F32)
        omlT = cx.sb(es, "omlT", [128, 8], F32)
        if layer_j == 0:
            cx.dve(lambda e: e.memset(lbT[:], 0.0), w=[lbT])
        else:
            l0 = cx.sb(es, "l0", [128, 8], F32)
            l1 = cx.sb(es, "l1", [128, 8], F32)
            cx.dma(l0[:], lb_ap[0, :].rearrange("(h p) -> p h", p=128), writes=[l0], allow_slow_non_contiguous=True)
            cx.dma(l1[:], lb_ap[1, :].rearrange("(h p) -> p h", p=128), writes=[l1], allow_slow_non_contiguous=True)
            cx.dve(lambda e: e.tensor_tensor(l1[:], l1[:], l0[:], ALU.subtract), r=[l0, l1], w=[l1])
            cx.act(lambda e: e.activation(lbT[:], l1[:], AF.Sigmoid), r=[l1], w=[lbT])
        cx.dve(lambda e: e.tensor_scalar(omlT[:], lbT[:], -1.0, 1.0, ALU.mult, ALU.add), r=[lbT], w=[omlT])
        core = GLACore(cx, es, 8, ident, maskT_ap)
        hins = [cx.sb(es, "hin%d" % i, [128, 8, MTK], BF16) for i in range(2)]
        v = cx.sb(es, "vtm", [64, NCH, 1024], BF16)
        f32t = lambda n: cx.sb(es, n, [128, MTK], F32)
        TT = [{n: f32t(n + str(i)) for n in ("sq", "sg", "gl", "bb", "dd", "eq", "ek")} for i in range(2)]
        for i in range(2):
            TT[i]["ebm"] = cx.sb(es, "ebm%d" % i, [128, NCH], F32)
            TT[i]["e2"] = cx.sb(es, "e2%d" % i, [128, NCH], F32)
            TT[i]["qt"] = cx.sb(es, "qt%d" % i, [128, MTK], BF16)
            TT[i]["kt"] = cx.sb(es, "kt%d" % i, [128, MTK], BF16)
        oT = f32t("oT")
        sqo = cx.sb(es, "sqo", [128, MTK], BF16)
        rt = f32t("rt")
        sgp = f32t("sgp")
        mts = [cx.sb(es, "mixt%d" % i, [128, 8, MTK], BF16) for i in range(1)]
        P = [cx.ps(es, "P%d" % i, [128, 512], F32) for i in range(3)]
        pi = [0]

        def nextP():
            pi[0] += 1
            return P[pi[0] % 3]

        def macro(m, hin, mt):
            cx.dma(hin[:], dram_fm(hT_ap, 0, 8, m * MTK, (m + 1) * MTK), writes=[hin])
            k = 0
            for c in range(NCH):
                for half in range(2):
                    ps = nextP()
                    for kc in range(8):
                        cx.pe(lambda e, ps=ps, kc=kc, c=c, half=half: e.matmul(
                            ps[0:64, :], hin[:, kc, c * CH:(c + 1) * CH],
                            wb[:, kc, 2048 + half * 512:2048 + (half + 1) * 512],
                            start=(kc == 0), stop=(kc == 7)), r=[hin, wb], w=[ps])
                    if k % 2 == 0:
                        cx.act(lambda e, ps=ps, c=c, half=half: e.copy(v[:, c, half * 512:(half + 1) * 512], ps[0:64, :]),
                               r=[ps], w=[v])
                    else:
                        cx.dve(lambda e, ps=ps, c=c, half=half: e.tensor_copy(v[:, c, half * 512:(half + 1) * 512], ps[0:64, :]),
                               r=[ps], w=[v])
                    k += 1
            def A(h, par):
                T = TT[par]
                sq, sg, gl, bb, dd, eq, ek, ebm, e2, qt, kt = (T[n] for n in ("sq", "sg", "gl", "bb", "dd", "eq", "ek", "ebm", "e2", "qt", "kt"))
                pq = nextP()
                proj_fm(cx, pq, wb, h * 128, 128, hin, MTK)
                cx.act(lambda e: e.activation(sq[:], pq[:], AF.Silu), r=[pq], w=[sq])
                pf = nextP()
                proj_fm(cx, pf, wb, 1024 + h * 128, 128, hin, MTK)
                cx.act(lambda e: e.activation(sg[:], pf[:], AF.Sigmoid), r=[pf], w=[sg])
                cx.dve(lambda e: e.tensor_scalar(sg[:], sg[:], omlT[:, h:h + 1], lbT[:, h:h + 1], ALU.mult, ALU.add),
                       r=[sg, omlT, lbT], w=[sg])
                cx.act(lambda e: e.activation(gl[:], sg[:], AF.Ln), r=[sg], w=[gl])
                cx.dve(lambda e: e.tensor_tensor_scan(bb[:], scanm[:], gl[:], 0.0, ALU.mult, ALU.add),
                       r=[scanm, gl], w=[bb])
                b3 = bb[:].rearrange("p (c n) -> p c n", n=CH)
                cx.dve(lambda e: e.tensor_tensor(
                    dd[:].rearrange("p (c n) -> p c n", n=CH), b3,
                    b3[:, :, 31:32].to_broadcast([128, NCH, CH]), ALU.subtract), r=[bb], w=[dd])
                cx.act(lambda e: e.activation(eq[:], dd[:], AF.Exp), r=[dd], w=[eq])
                cx.act(lambda e: e.activation(ek[:], dd[:], AF.Exp, scale=-1.0), r=[dd], w=[ek])
                cx.act(lambda e: e.activation(ebm[:], b3[:, :, 31], AF.Exp), r=[bb], w=[ebm])
                eq3 = eq[:].rearrange("p (c n) -> p c n", n=CH)
                cx.dve(lambda e: e.tensor_tensor(e2[:], ebm[:], eq3[:, :, CH - 1], ALU.mult),
                       r=[ebm, eq], w=[e2])
                cx.dve(lambda e: e.scalar_tensor_tensor(qt[:], sq[:], 128.0 ** -0.5, eq[:], ALU.mult, ALU.mult),
                       r=[sq, eq], w=[qt])
                cx.dve(lambda e: e.tensor_scalar(sg[:], sg[:], -1.0, 1.0, ALU.mult, ALU.add), r=[sg], w=[sg])
                cx.dve(lambda e: e.tensor_tensor(kt[:], sg[:], ek[:], ALU.mult), r=[sg, ek], w=[kt])
                core.pre(par, qt, kt, v, h * 128, (eq, eq3[:, :, CH - 1]))

            def B(h, par):
                T = TT[par]
                ebm, e2, qt = T["ebm"], T["e2"], T["qt"]
                core.chain(par, h, qt, v, h * 128, (ebm, ebm.t), (e2, e2.t), oT)
                cx.act(lambda e: e.activation(sqo[:], oT[:], AF.Square), r=[oT], w=[sqo])
                pss = nextP()
                cx.pe(lambda e: e.matmul(pss[:], onesb[:], sqo[:], start=True, stop=True),
                      r=[onesb, sqo], w=[pss])
                cx.act(lambda e: e.activation(rt[:], pss[:], AF.Sqrt, bias=EPS, scale=1.0 / 128.0),
                       r=[pss], w=[rt])
                cx.dve(lambda e: e.reciprocal(rt[:], rt[:]), r=[rt], w=[rt])
                pg = nextP()
                proj_fm(cx, pg, wb, 3072 + h * 128, 128, hin, MTK)
                cx.act(lambda e: e.activation(sgp[:], pg[:], AF.Sigmoid), r=[pg], w=[sgp])
                cx.dve(lambda e: e.scalar_tensor_tensor(rt[:], oT[:], normg[:, 0:1], rt[:], ALU.mult, ALU.mult),
                       r=[oT, normg, rt], w=[rt])
                cx.dve(lambda e: e.tensor_tensor(mt[:, h, :], rt[:], sgp[:], ALU.mult),
                       r=[rt, sgp], w=[mt])

            A(0, 0)
            for h in range(8):
                if h + 1 < 8:
                    A(h + 1, (h + 1) % 2)
                B(h, h % 2)
            cx.dma(dram_fm(mixT_ap, 0, 8, m * MTK, (m + 1) * MTK), mt[:], reads=[mt], q="pool")

        for m in range(S // MTK):
            macro(m, hins[m % 2], mts[0])
    cx.barrier()


RET_GAMMA = [1.0 - 2.0 ** (-5.0 - h) for h in range(4)]


def stage_ret(cx, hT_ap, w_ap, cos_ap, sin_ap, dec_ap, mixT_ap, ident_ap, maskT_ap, S):
    with ExitStack() as es:
        wb = cx.sb(es, "winb", [128, 8, 3072], BF16)
        with ExitStack() as es2:
            stg = [cx.sb(es2, "wstg%d" % i, [128, 1536], F32) for i in range(2)]
            load_weight_bf16(cx, wb, w_ap, 8, 3072, stg)
            cx.barrier()
        ident = cx.sb(es, "ident", [128, 128], BF16)
        cx.dma(ident[:], ident_ap, writes=[ident])
        onesb = cx.sb(es, "onesb", [128, 128], BF16)
        cx.dve(lambda e: e.memset(onesb[:], 1.0), w=[onesb])
        dec = cx.sb(es, "dec", [128, 8, MTK], F32)
        cx.dma(dec[:], dec_ap.rearrange("h t p n -> p (h t) n"), writes=[dec])
        core = GLACore(cx, es, 4, ident, maskT_ap)
        hins = [cx.sb(es, "hin%d" % i, [128, 8, MTK], BF16) for i in range(2)]
        coss = [cx.sb(es, "cos%d" % i, [128, MTK], F32) for i in range(2)]
        sins = [cx.sb(es, "sin%d" % i, [128, MTK], F32) for i in range(2)]
        v = cx.sb(es, "vtm", [64, NCH, 512], BF16)
        f32t = lambda n: cx.sb(es, n, [128, MTK], F32)
        oT, mean, var, sgp = [f32t(n) for n in ("oT", "mean", "var", "sgp")]
        TT = [{"t1": f32t("t1_%d" % i), "t2": f32t("t2_%d" % i),
               "qt": cx.sb(es, "qt%d" % i, [128, MTK], BF16), "kt": cx.sb(es, "kt%d" % i, [128, MTK], BF16)} for i in range(2)]
        ob = cx.sb(es, "ob", [128, MTK], BF16)
        sqo = cx.sb(es, "sqo", [128, MTK], BF16)
        mts = [cx.sb(es, "mixt%d" % i, [128, 4, MTK], BF16) for i in range(2)]
        P = [cx.ps(es, "P%d" % i, [128, 512], F32) for i in range(3)]
        pi = [0]

        def nextP():
            pi[0] += 1
            return P[pi[0] % 3]

        def rot(h, col0, tab, out, hin, cs, sn, t1, t2):
            pa = nextP()
            proj_fm(cx, pa, wb, col0 + h * 128, 128, hin, MTK)
            cx.dve(lambda e: e.tensor_tensor(t1[:], pa[:], cs[:], ALU.mult), r=[pa, cs], w=[t1])
            pb = nextP()
            proj_fm(cx, pb, wb, 1024 + col0 + h * 128, 128, hin, MTK)
            cx.dve(lambda e: e.tensor_tensor(t2[:], pb[:], sn[:], ALU.mult), r=[pb, sn], w=[t2])
            cx.dve(lambda e: e.tensor_tensor(t1[:], t1[:], t2[:], ALU.add), r=[t1, t2], w=[t1])
            cx.dve(lambda e: e.tensor_tensor(out[:], t1[:], dec[:, tab, :], ALU.mult), r=[t1, dec], w=[out])

        def macro(m, hin, mt, cs, sn):
            cx.dma(hin[:], dram_fm(hT_ap, 0, 8, m * MTK, (m + 1) * MTK), writes=[hin])
            cx.dma(cs[:], cos_ap[:, m * MTK:(m + 1) * MTK], writes=[cs])
            cx.dma(sn[:], sin_ap[:, m * MTK:(m + 1) * MTK], writes=[sn])
            for c in range(NCH):
                ps = nextP()
                for kc in range(8):
                    cx.pe(lambda e, ps=ps, kc=kc, c=c: e.matmul(
                        ps[0:64, :], hin[:, kc, c * CH:(c + 1) * CH], wb[:, kc, 2048:2560],
                        start=(kc == 0), stop=(kc == 7)), r=[hin, wb], w=[ps])
                if c % 2 == 0:
                    cx.act(lambda e, ps=ps, c=c: e.copy(v[:, c, :], ps[0:64, :]), r=[ps], w=[v])
                else:
                    cx.dve(lambda e, ps=ps, c=c: e.tensor_copy(v[:, c, :], ps[0:64, :]), r=[ps], w=[v])
            def A(h, par):
                T = TT[par]
                g = RET_GAMMA[h]
                rot(h, 0, 2 * h, T["qt"], hin, cs, sn, T["t1"], T["t2"])
                rot(h, 512, 2 * h + 1, T["kt"], hin, cs, sn, T["t1"], T["t2"])
                core.pre(par, T["qt"], T["kt"], v, h * 128, float(g ** 32))

            def B(h, par):
                T = TT[par]
                g = RET_GAMMA[h]
                core.chain(par, h, T["qt"], v, h * 128, float(g ** 32), float(g ** 64), oT)
                cx.act(lambda e: e.copy(ob[:], oT[:]), r=[oT], w=[ob])
                cx.act(lambda e: e.activation(sqo[:], oT[:], AF.Square), r=[oT], w=[sqo])
                p1 = nextP()
                cx.pe(lambda e: e.matmul(p1[:], onesb[:], ob[:], start=True, stop=True), r=[onesb, ob], w=[p1])
                p2 = nextP()
                cx.pe(lambda e: e.matmul(p2[:], onesb[:], sqo[:], start=True, stop=True), r=[onesb, sqo], w=[p2])
                cx.act(lambda e: e.activation(mean[:], p1[:], AF.Copy, scale=1.0 / 128.0), r=[p1], w=[mean])
                cx.dve(lambda e: e.tensor_tensor(var[:], mean[:], mean[:], ALU.mult), r=[mean], w=[var])
                cx.dve(lambda e: e.scalar_tensor_tensor(var[:], p2[:], 1.0 / 128.0, var[:], ALU.mult, ALU.subtract),
                       r=[p2, var], w=[var])
                cx.act(lambda e: e.activation(var[:], var[:], AF.Sqrt, bias=1e-5, scale=1.0), r=[var], w=[var])
                cx.dve(lambda e: e.reciprocal(var[:], var[:]), r=[var], w=[var])
                cx.dve(lambda e: e.tensor_tensor(oT[:], oT[:], mean[:], ALU.subtract), r=[oT, mean], w=[oT])
                cx.dve(lambda e: e.tensor_tensor(oT[:], oT[:], var[:], ALU.mult), r=[oT, var], w=[oT])
                pg = nextP()
                proj_fm(cx, pg, wb, 2560 + h * 128, 128, hin, MTK)
                cx.act(lambda e: e.activation(sgp[:], pg[:], AF.Silu), r=[pg], w=[sgp])
                cx.dve(lambda e: e.tensor_tensor(mt[:, h, :], oT[:], sgp[:], ALU.mult), r=[oT, sgp], w=[mt])

            A(0, 0)
            for h in range(4):
                if h + 1 < 4:
                    A(h + 1, (h + 1) % 2)
                B(h, h % 2)
            cx.dma(dram_fm(mixT_ap, 0, 4, m * MTK, (m + 1) * MTK), mt[:], reads=[mt], q="pool")

        for m in range(S // MTK):
            macro(m, hins[m % 2], mts[m % 2], coss[m % 2], sins[m % 2])
    cx.barrier()


def host_consts(S):
    import ml_dtypes
    c = {}
    c["ident"] = np.eye(128, dtype=np.float32).astype(ml_dtypes.bfloat16)
    m = np.arange(64)
    c["maskT"] = (m[:, None] <= m[None, :]).astype(np.float32)
    sm = np.ones((128, MTK), np.float32)
    sm[:, ::CH] = 0
    c["scanm"] = sm
    half = 64
    inv = (10000.0 ** (-np.arange(half, dtype=np.float32) / half)).astype(np.float32)
    ang = (np.arange(S, dtype=np.float32)[:, None] * inv[None, :]).astype(np.float32)
    cos = np.cos(ang).T.astype(np.float32)
    sin = np.sin(ang).T.astype(np.float32)
    c["cos"] = np.ascontiguousarray(np.concatenate([cos, cos], 0))
    c["sin"] = np.ascontiguousarray(np.concatenate([-sin, sin], 0))
    dec = np.zeros((4, 2, 128, MTK), np.float32)
    n = (np.arange(MTK) % CH).astype(np.float64)
    for h in range(4):
        g = RET_GAMMA[h]
        dec[h, 0] = (g ** (n - 31.0))[None, :]
        dec[h, 1] = (g ** (31.0 - n) * 128.0 ** -0.5)[None, :]
    c["dec"] = dec
    return c


NEG = -30000.0
NSA_PIPE = True
NSA_HOLD = True
NEG8 = NEG * 8.0


def nsa_consts(S):
    import ml_dtypes
    bf = ml_dtypes.bfloat16
    nb = S // 128
    c = {}
    tl = np.arange(128)
    n = np.arange(256)
    cm = np.full((nb, 128, 256), NEG8, np.float32)
    for i in range(nb):
        t = i * 128 + tl
        ok = (16 * n[None, :] + 31 <= t[:, None]) & (n[None, :] < S // 16 - 1)
        cm[i][ok] = 0.0
    c["cmask"] = cm.astype(bf)
    j = np.arange(64)
    fb = np.zeros((nb, 128, 64), np.float32)
    for i in range(nb):
        bt = (i * 128 + tl) // 64
        d = bt[:, None] - j[None, :]
        forced = (j[None, :] == 0) | ((d >= 0) & (d < 2))
        fb[i] = np.where(d >= 0, np.where(forced, 1.0e4, 0.0), -1.0e30)
    c["fbias"] = fb
    cs = np.arange(256) * 16
    ce = cs + 31
    ss = np.arange(64) * 64
    se = ss + 63
    ov = ((cs[:, None] <= se[None, :]) & (ce[:, None] >= ss[None, :])).astype(np.float32)
    ov[S // 16 - 1:, :] = 0
    c["ovl"] = ov.astype(bf)
    c["causal"] = np.where(tl[None, :] <= tl[:, None], 0.0, NEG8).astype(np.float32).astype(bf)
    kr = np.arange(640) - 512
    dist = tl[:, None] - kr[None, :]
    c["wmask"] = np.where((dist >= 0) & (dist < 512), 0.0, NEG8).astype(np.float32).astype(bf)
    c["rvalid"] = (tl >= 31).astype(np.float32).reshape(128, 1)
    kk = np.arange(S)
    c["blockE"] = (kk[None, :] // 64 == np.arange(64)[:, None]).astype(np.float32).astype(bf)
    return c


def stage_nsa(cx, hT_ap, w_ap, cw, mixT_ap, ident_ap, cn, S):
    NB = S // 128
    NT = S // 128
    with ExitStack() as es:
        wb = cx.sb(es, "wnsa", [128, 8, 1304], BF16)
        ksE = [cx.sb(es, "ksE%d" % g, [128, S], BF16) for g in range(2)]
        kwT = [cx.sb(es, "kwT%d" % g, [64, S], BF16) for g in range(2)]
        vsw = cx.sb(es, "vsw", [128, NT, 256], BF16)
        kcmpT = [cx.sb(es, "kcmpT%d" % g, [64, 256], BF16) for g in range(2)]
        vcmp = cx.sb(es, "vcmp", [128, 2, 2, 64], BF16)
        ident = cx.sb(es, "ident", [128, 128], BF16)
        P = [cx.ps(es, "P%d" % i, [128, 512], F32) for i in range(4)]
        TP = [cx.ps(es, "TP%d" % i, [128, 8, 128], BF16) for i in range(2)]
        PV = [cx.ps(es, "PV%d" % i, [128, 64], F32) for i in range(1)]
        IMP = cx.ps(es, "IMP", [128, 64], F32)
        cnt = {"p": 0, "tp": 0, "pv": 0, "cp": 0, "sp": 0}

        def nP():
            cnt["p"] += 1
            return P[cnt["p"] % 2]

        def nH():
            cnt["h"] = cnt.get("h", 0) + 1
            return P[2 + cnt["h"] % 2]

        def nTP():
            cnt["tp"] += 1
            return TP[cnt["tp"] % 2]

        def nPV():
            return PV[0]

        def cp(out_ap, in_ap, r, w):
            cnt["cp"] += 1
            if cnt["cp"] % 2:
                cx.act(lambda e: e.copy(out_ap, in_ap), r=r, w=w)
            else:
                cx.dve(lambda e: e.tensor_copy(out_ap, in_ap), r=r, w=w)

        with ExitStack() as es2:
            stg = [cx.sb(es2, "wstg%d" % i, [128, 1304], F32) for i in range(2)]
            load_weight_bf16(cx, wb, w_ap, 8, 1304, stg)
            cx.dma(ident[:], ident_ap, writes=[ident])
            for g in range(2):
                cx.dma(ksE[g][64:128, :], cn["blockE"], writes=[(ksE[g].key, "E")])
            cx.barrier()
            kcT = [cx.sb(es2, "kcT%d" % g, [64, S], BF16) for g in range(2)]
            vcT = [cx.sb(es2, "vcT%d" % g, [64, S], BF16) for g in range(2)]
            hins = [cx.sb(es2, "hin%d" % i, [128, 8, MTK], BF16) for i in range(2)]

            def phaseA(m, hin):
                cx.dma(hin[:], dram_fm(hT_ap, 0, 8, m * MTK, (m + 1) * MTK), writes=[hin])
                for (dst, col) in ((kcT, 512), (vcT, 640), (ksE, 768), (kwT, 896)):
                    for g in range(2):
                        ps = nP()
                        proj_fm(cx, ps, wb, col + g * 64, 64, hin, MTK)
                        cp(dst[g][0:64, m * MTK:(m + 1) * MTK], ps[0:64, :], [ps], [dst[g]])
                for j in range(4):
                    ps = nP()
                    for kc in range(8):
                        cx.pe(lambda e, ps=ps, kc=kc, j=j: e.matmul(
                            ps[:, 0:256], hin[:, kc, j * 128:(j + 1) * 128], wb[:, kc, 1024:1280],
                            start=(kc == 0), stop=(kc == 7)), r=[hin, wb], w=[ps])
                    cp(vsw[:, m * 4 + j, :], ps[:, 0:256], [ps], [vsw])

            for m in range(S // MTK):
                phaseA(m, hins[m % 2])

            w1s = cx.sb(es2, "w1s", [64, 32, 64], F32)
            w1b = cx.sb(es2, "w1b", [64, 32, 64], BF16)
            w2s = cx.sb(es2, "w2s", [64, 64], F32)
            w2b = cx.sb(es2, "w2b", [64, 64], BF16)
            poss = cx.sb(es2, "poss", [64, 32], F32)
            posb = cx.sb(es2, "posb", [64, 32], BF16)
            cb = cx.sb(es2, "cb", [64, 1], F32)
            tt = [cx.sb(es2, "gt%d" % i, [64, 256], F32) for i in range(3)]
            glb = cx.sb(es2, "glb", [64, 256], BF16)
            for g in range(2):
                cx.dve(lambda e, g=g: e.memset(kcmpT[g][:], 0.0), w=[kcmpT[g]])
            cx.dve(lambda e: e.memset(vcmp[:], 0.0), w=[vcmp])
            cx.dve(lambda e: e.memset(glb[:], 0.0), w=[glb])
            NCMP = S // 16 - 1

            def phaseB(kind, g, src):
                pos_ap, w1_ap, w2_ap = cw["pos_" + kind], cw["w1_" + kind], cw["w2_" + kind]
                if g == 0:
                    cx.dma(w1s[:], w1_ap.rearrange("(p d) o -> d p o", d=64), writes=[w1s])
                    cx.dma(w2s[:], w2_ap, writes=[w2s])
                    cx.dma(poss[:], pos_ap.rearrange("p d -> d p"), writes=[poss], allow_slow_non_contiguous=True)
                    cx.dve(lambda e: e.tensor_copy(w1b[:], w1s[:]), r=[w1s], w=[w1b])
                    cx.dve(lambda e: e.tensor_copy(w2b[:], w2s[:]), r=[w2s], w=[w2b])
                    cx.dve(lambda e: e.tensor_copy(posb[:], poss[:]), r=[poss], w=[posb])
                    pc = nPV()
                    for p in range(32):
                        cx.pe(lambda e, p=p, pc=pc: e.matmul(pc[0:64, 0:1], w1b[:, p, :], posb[:, p:p + 1],
                                                             start=(p == 0), stop=(p == 31)), r=[w1b, posb], w=[pc])
                    cx.act(lambda e, pc=pc: e.copy(cb[:], pc[0:64, 0:1]), r=[pc], w=[cb])
                ps = nP()
                x3 = src[0:64, :].rearrange("d (n s) -> d n s", s=16)
                for p in range(32):
                    n0, r_ = (0, p) if p < 16 else (1, p - 16)
                    cx.pe(lambda e, p=p, n0=n0, r_=r_, ps=ps: e.matmul(
                        ps[0:64, 0:NCMP], w1b[:, p, :], x3[:, n0:n0 + NCMP, r_],
                        start=(p == 0), stop=(p == 31)), r=[w1b, src], w=[ps])
                t0, t1_, t2_ = tt
                N = NCMP
                cx.act(lambda e, ps=ps: e.activation(t0[:, 0:N], ps[0:64, 0:N], AF.Identity, bias=cb[:], scale=1.0),
                       r=[ps, cb], w=[t0])
                cx.dve(lambda e: e.tensor_tensor(t1_[:, 0:N], t0[:, 0:N], t0[:, 0:N], ALU.mult), r=[t0], w=[t1_])
                cx.dve(lambda e: e.tensor_scalar(t1_[:, 0:N], t1_[:, 0:N], 0.044715, 1.0, ALU.mult, ALU.add), r=[t1_], w=[t1_])
                cx.dve(lambda e: e.tensor_tensor(t1_[:, 0:N], t1_[:, 0:N], t0[:, 0:N], ALU.mult), r=[t1_, t0], w=[t1_])
                cx.act(lambda e: e.activation(t2_[:, 0:N], t1_[:, 0:N], AF.Sigmoid, scale=2.0 * math.sqrt(2.0 / math.pi)),
                       r=[t1_], w=[t2_])
                cx.dve(lambda e: e.tensor_tensor(glb[:, 0:N], t0[:, 0:N], t2_[:, 0:N], ALU.mult), r=[t0, t2_], w=[glb])
                if kind == "k":
                    po = nP()
                    cx.pe(lambda e, po=po: e.matmul(po[0:64, 0:N], w2b[:], glb[:, 0:N], start=True, stop=True),
                          r=[w2b, glb], w=[po])
                    cp(kcmpT[g][:, 0:N], po[0:64, 0:N], [po], [kcmpT[g]])
                else:
                    for kc2 in range(2):
                        po = nPV()
                        n1 = min(128, N - kc2 * 128)
                        if n1 <= 0:
                            continue
                        cx.pe(lambda e, po=po, kc2=kc2, n1=n1: e.matmul(
                            po[0:n1, :], glb[:, kc2 * 128:kc2 * 128 + n1], w2b[:], start=True, stop=True),
                            r=[glb, w2b], w=[po])
                        cp(vcmp[0:n1, kc2, g, :], po[0:n1, :], [po], [vcmp])

            for kind, srcs in (("k", kcT), ("v", vcT)):
                for g in range(2):
                    phaseB(kind, g, srcs[g])
            cx.barrier()

        ovl = cx.sb(es, "ovl", [128, 2, 64], BF16)
        cx.dma(ovl[:], cn["ovl"].rearrange("(c p) j -> p c j", p=128), writes=[ovl])
        causal = cx.sb(es, "causal", [128, 128], BF16)
        cx.dma(causal[:], cn["causal"], writes=[causal])
        wmask = cx.sb(es, "wmask", [128, 640], BF16)
        cx.dma(wmask[:], cn["wmask"], writes=[wmask])
        rvalid = cx.sb(es, "rvalid", [128, 1], F32)
        cx.dma(rvalid[:], cn["rvalid"], writes=[rvalid])
        hqs = [cx.sb(es, "hq%d" % i, [128, 8, 128], BF16) for i in range(2)]
        cms = [cx.sb(es, "cm%d" % i, [128, 256], BF16) for i in range(2)]
        fbs = [cx.sb(es, "fb%d" % i, [128, 64], F32) for i in range(2)]
        qsel = [cx.sb(es, "qsel%d" % i, [128, 4, 128], BF16) for i in range(2)]
        selw = cx.sb(es, "selw", [128, 128], BF16)
        cx.dve(lambda e: e.memset(selw[:], 0.0), w=[selw])
        pcT = [cx.sb(es, "pcT%d" % i, [128, 2, 128], BF16) for i in range(4)]
        pc32 = [cx.sb(es, "pc32_%d" % i, [128, 256], F32) for i in range(2)]
        pbs = [cx.sb(es, "pb%d" % i, [128, S], BF16) for i in range(2)]
        pTs = [cx.sb(es, "pT%d" % i, [128, NT, 128], BF16) for i in range(2)]
        acc = cx.sb(es, "acc", [128, 512], F32)
        accb = cx.sb(es, "accb", [128, 512], BF16)
        mixt = [cx.sb(es, "mixt%d" % i, [128, 4, 128], BF16) for i in range(2)]
        sms = [{n_: cx.sb(es, n_ + str(i), [128, 8 if n_[0] == "c" else 1], F32)
                for n_ in ("cmax", "crs", "mx", "rs", "rinv", "fac")} for i in range(2)]
        sc64 = cx.sb(es, "sc64", [128, 64], F32)
        top8 = cx.sb(es, "top8", [128, 8], F32)
        sel01 = cx.sb(es, "sel01", [128, 64], F32)
        SCALE = 0.125

        def softmax_item(q_ap, q_r, KT, k0, nk, maskfn, vfn, gate_ap, gate_r, acc_ap, first, normalize, keepT=None, i0=False):
            cnt["sp"] += 1
            b = cnt["sp"] % 2
            sm, pb, pT = sms[b], pbs[b], pTs[b]
            cmax, crs, mx, rs, rinv, fac = sm["cmax"], sm["crs"], sm["mx"], sm["rs"], sm["rinv"], sm["fac"]
            chunks = [(c0, min(512, nk - c0)) for c0 in range(0, nk, 512)]
            ncn = len(chunks)
            dst32 = pc32[b] if normalize else None
            held = []

            def scores(ps, c0, n_):
                mm = maskfn(c0, n_)
                cx.pe(lambda e: e.matmul(ps[:, 0:n_], q_ap, KT[0:q_ap.shape[0], k0 + c0:k0 + c0 + n_],
                                         start=True, stop=(len(mm) == 0)), r=q_r + [KT], w=[ps])
                for idx, (l_ap, r_ap, lo, hi, rd) in enumerate(mm):
                    cx.pe(lambda e, l_ap=l_ap, r_ap=r_ap, lo=lo, hi=hi, idx=idx: e.matmul(
                        ps[:, lo:hi], l_ap, r_ap, start=False, stop=(idx == len(mm) - 1)), r=rd, w=[ps])

            def expo(ps, ci, c0, n_):
                out_ap = dst32[:, c0:c0 + n_] if normalize else pb[:, c0:c0 + n_]
                wr = [dst32] if normalize else [pb]
                cx.act(lambda e: e.activation(out_ap, ps[:, 0:n_], AF.Exp, bias=mx[:], scale=SCALE,
                                              accum_out=crs[:, ci:ci + 1]), r=[ps, mx], w=wr + [crs])

            def p1():
                for ci, (c0, n_) in enumerate(chunks):
                    ps = nH() if (ncn == 1 and NSA_HOLD) else nP()
                    scores(ps, c0, n_)
                    cx.dve(lambda e, ps=ps, ci=ci, n_=n_: e.reduce_max(cmax[:, ci:ci + 1], ps[:, 0:n_], AX.X),
                           r=[ps], w=[cmax])
                    if ncn == 1 and NSA_HOLD:
                        held.append(ps)
                if ncn == 1:
                    cx.dve(lambda e: e.tensor_scalar_mul(mx[:], cmax[:, 0:1], -SCALE), r=[cmax], w=[mx])
                else:
                    cx.dve(lambda e: e.tensor_reduce(mx[:], cmax[:, 0:ncn], AX.X, ALU.max), r=[cmax], w=[mx])
                    cx.dve(lambda e: e.tensor_scalar_mul(mx[:], mx[:], -SCALE), r=[mx], w=[mx])

            def p2():
                if ncn == 1 and NSA_HOLD:
                    expo(held[0], 0, chunks[0][0], chunks[0][1])
                    cx.dve(lambda e: e.reciprocal(rinv[:], crs[:, 0:1]), r=[crs], w=[rinv])
                elif ncn == 1:
                    ps = nP()
                    scores(ps, chunks[0][0], chunks[0][1])
                    expo(ps, 0, chunks[0][0], chunks[0][1])
                    cx.dve(lambda e: e.reciprocal(rinv[:], crs[:, 0:1]), r=[crs], w=[rinv])
                else:
                    for ci, (c0, n_) in enumerate(chunks):
                        ps = nP()
                        scores(ps, c0, n_)
                        expo(ps, ci, c0, n_)
                    cx.dve(lambda e: e.reduce_sum(rs[:], crs[:, 0:ncn], AX.X), r=[crs], w=[rs])
                    cx.dve(lambda e: e.reciprocal(rinv[:], rs[:]), r=[rs], w=[rinv])
                if normalize:
                    if i0:
                        cx.dve(lambda e: e.tensor_tensor(rinv[:], rinv[:], rvalid[:], ALU.mult), r=[rinv, rvalid], w=[rinv])
                    cx.dve(lambda e: e.tensor_scalar_mul(pb[:, 0:nk], dst32[:, 0:nk], rinv[:, 0:1]), r=[dst32, rinv], w=[pb])
                dstT = keepT if keepT is not None else pT
                nkt = nk // 128
                for t0 in range(0, nkt, 8):
                    n8 = min(8, nkt - t0)
                    tp = nTP()
                    for t in range(n8):
                        cx.pe(lambda e, tp=tp, t=t, t0=t0: e.transpose(tp[:, t, :], pb[:, (t0 + t) * 128:(t0 + t + 1) * 128], ident[:]),
                              r=[pb, ident], w=[tp])
                    cp(dstT[:, t0:t0 + n8, :], tp[:, 0:n8, :], [tp], [dstT])
                po = nPV()
                for t in range(nkt):
                    v_ap, v_r = vfn(t)
                    cx.pe(lambda e, po=po, t=t, v_ap=v_ap: e.matmul(po[:], dstT[:, t, :], v_ap, start=(t == 0), stop=(t == nkt - 1)),
                          r=[dstT] + v_r, w=[po])
                if normalize:
                    sc_ap, sc_r = gate_ap, [gate_r]
                else:
                    cx.dve(lambda e: e.tensor_tensor(fac[:], rinv[:], gate_ap, ALU.mult), r=[rinv, gate_r], w=[fac])
                    sc_ap, sc_r = fac[:, 0:1], [fac]
                if first:
                    cx.dve(lambda e, po=po: e.tensor_scalar_mul(acc_ap, po[:], sc_ap), r=[po] + sc_r, w=[acc])
                else:
                    cx.dve(lambda e, po=po: e.scalar_tensor_tensor(acc_ap, po[:], sc_ap, acc_ap, ALU.mult, ALU.add),
                           r=[po, acc] + sc_r, w=[acc])

            return p1, p2

        items = []

        def block(i, hq, cm, fb, mt, gates):
            nk = 128 * (i + 1)
            kt0 = max(0, i - 4)
            nkw = 128 * (i - kt0 + 1)

            def blk_pre():
                cx.dma(hq[:], dram_fm(hT_ap, 0, 8, i * 128, (i + 1) * 128), writes=[hq])
                cx.dma(cm[:], cn["cmask"][i], writes=[cm])
                cx.dma(fb[:], cn["fbias"][i], writes=[fb])
                pg = nPV()
                for kc in range(8):
                    cx.pe(lambda e, kc=kc, pg=pg: e.matmul(pg[:, 0:24], hq[:, kc, :], wb[:, kc, 1280:1304],
                                                           start=(kc == 0), stop=(kc == 7)), r=[hq, wb], w=[pg])
                cx.act(lambda e, pg=pg: e.activation(gates[:], pg[:, 0:24], AF.Sigmoid), r=[pg], w=[gates])

            def blk_post():
                cx.act(lambda e: e.copy(accb[:], acc[:]), r=[acc], w=[accb])
                tp = nTP()
                for c in range(4):
                    cx.pe(lambda e, c=c, tp=tp: e.transpose(tp[:, c, :], accb[:, c * 128:(c + 1) * 128], ident[:]),
                          r=[accb, ident], w=[tp])
                cp(mt[:], tp[:, 0:4, :], [tp], [mt])
                cx.dma(dram_fm(mixT_ap, 4, 8, i * 128, (i + 1) * 128), mt[:], reads=[mt], q="pool")

            def group(g):
                qs = qsel[g]
                qk = (qs.key, "q")
                sk = (qs.key, "s")

                def q_pre():
                    for hp in range(4):
                        hd = g * 4 + hp
                        ps = nP()
                        proj_fm(cx, ps, wb, hd * 64, 64, hq, 128)
                        cp(qs[0:64, hp, :], ps[0:64, 0:128], [ps], [qk])

                def sel_pre():
                    for hp in range(4):
                        for kc2 in range(2):
                            cx.pe(lambda e, hp=hp, kc2=kc2: e.matmul(IMP[:], pcT[hp][:, kc2, :], ovl[:, kc2, :],
                                                                     start=(hp == 0 and kc2 == 0), stop=(hp == 3 and kc2 == 1)),
                                  r=[pcT[hp], ovl], w=[IMP])
                    cx.dve(lambda e: e.tensor_tensor(sc64[:], IMP[:], fb[:], ALU.add), r=[IMP, fb], w=[sc64])
                    cx.dve(lambda e: e.max(top8[:], sc64[:]), r=[sc64], w=[top8])
                    cx.dve(lambda e: e.tensor_scalar(sel01[:], sc64[:], top8[:, 7:8], None, ALU.is_ge), r=[sc64, top8], w=[sel01])
                    cx.dve(lambda e: e.tensor_scalar(selw[:, 64:128], sel01[:], -NEG8, NEG8, ALU.mult, ALU.add), r=[sel01], w=[selw])
                    tps = nTP()
                    cx.pe(lambda e: e.transpose(tps[:, 0, :], selw[:], ident[:]), r=[selw, ident], w=[tps])
                    cx.act(lambda e: e.copy(qs[64:128, :, :], tps[64:128, 0:1, :].to_broadcast([64, 4, 128])),
                           r=[tps], w=[sk])

                def mk_cmp(hp):
                    hd = g * 4 + hp
                    return lambda: softmax_item(
                        qs[0:64, hp, :], [qk], kcmpT[g], 0, 256,
                        lambda c0, n_: [(ident[:], cm[:, c0:c0 + n_], 0, n_, [ident, cm])],
                        lambda t: (vcmp[:, t, g, :], [vcmp]),
                        gates[:, hd:hd + 1], gates, acc[:, hd * 64:(hd + 1) * 64], True, True, keepT=pcT[hp], i0=(i == 0))

                def mk_win(hp):
                    hd = g * 4 + hp
                    return lambda: softmax_item(
                        qs[0:64, hp, :], [qk], kwT[g], kt0 * 128, nkw,
                        lambda c0, n_: [(ident[:], wmask[:, 640 - nkw + c0:640 - nkw + c0 + n_], 0, n_, [ident, wmask])],
                        lambda t: (vsw[:, kt0 + t, 128 + g * 64:128 + (g + 1) * 64], [vsw]),
                        gates[:, 16 + hd:17 + hd], gates, acc[:, hd * 64:(hd + 1) * 64], False, False)

                def mk_slc(hp):
                    hd = g * 4 + hp
                    return lambda: softmax_item(
                        qs[:, hp, :], [qk, sk], ksE[g], 0, nk,
                        lambda c0, n_: ([(ident[:], causal[:], n_ - 128, n_, [ident, causal])] if c0 + n_ == nk else []),
                        lambda t: (vsw[:, t, g * 64:(g + 1) * 64], [vsw]),
                        gates[:, 8 + hd:9 + hd], gates, acc[:, hd * 64:(hd + 1) * 64], False, False)

                lst = []
                for hp in range(4):
                    lst.append([q_pre if hp == 0 else None, mk_cmp(hp), None])
                for hp in range(4):
                    lst.append([sel_pre if hp == 1 else None, mk_win(hp), None])
                for hp in range(4):
                    lst.append([None, mk_slc(hp), None])
                return lst

            lst = group(0) + group(1)
            first_pre = lst[0][0]
            lst[0][0] = lambda: (blk_pre(), first_pre())
            lst[-1][2] = blk_post
            items.extend(lst)

        gates2 = [cx.sb(es, "gates%d" % i_, [128, 24], F32) for i_ in range(2)]
        for i in range(NB):
            block(i, hqs[i % 2], cms[i % 2], fbs[i % 2], mixt[i % 2], gates2[i % 2])
        prev = None
        for pre, mk, post in items:
            if pre is not None:
                pre()
            p1, p2 = mk()
            p1()
            if not NSA_PIPE:
                p2()
                if post is not None:
                    post()
                continue
            if prev is not None:
                prev[0]()
                if prev[1] is not None:
                    prev[1]()
            prev = (p2, post)
        if NSA_PIPE:
            prev[0]()
            if prev[1] is not None:
                prev[1]()
    cx.barrier()


def nsa2_consts(S):
    import ml_dtypes
    bf = ml_dtypes.bfloat16
    nb = S // 128
    base = nsa_consts(S)
    c = {"fbias": base["fbias"], "blockE": base["blockE"]}
    cm = base["cmask"].astype(np.float32)
    cmT = cm.transpose(0, 2, 1).reshape(nb, 2, 128, 128)
    cmT = np.broadcast_to(cmT.transpose(0, 2, 1, 3)[:, :, :, None, :], (nb, 128, 2, 4, 128))
    c["cmaskT"] = np.ascontiguousarray(cmT).astype(bf)
    kl = np.arange(128)[:, None]
    ql = np.arange(128)[None, :]
    cz = np.where(kl <= ql, 0.0, NEG8).astype(np.float32)
    w0 = np.where(kl > ql, 0.0, NEG8).astype(np.float32)
    c["causalT4"] = np.ascontiguousarray(np.broadcast_to(cz[:, None, :], (128, 4, 128))).astype(bf)
    c["wm0T4"] = np.ascontiguousarray(np.broadcast_to(w0[:, None, :], (128, 4, 128))).astype(bf)
    ov = np.ones((256, 80), np.float32)
    ov[:, 0:64] = base["ovl"].astype(np.float32)
    c["ovla"] = ov.astype(bf)
    sr = np.zeros((24, 24, 64), np.float32)
    for r in range(24):
        sr[r, r, :] = 1.0
    c["selrows"] = sr
    return c


NSA2_STOP = ""


def stage_nsa2(cx, hT_ap, w_ap, cw, mixT_ap, ident_ap, cn, S):
    NB = S // 128
    NT = S // 128
    with ExitStack() as es:
        wb = cx.sb(es, "wnsa", [128, 8, 1312], BF16)
        cx.dve(lambda e: e.memset(wb[:, :, 1304:1312], 0.0), w=[(wb.key, "pad")])
        ksE = [cx.sb(es, "ksE%d" % g, [128, S], BF16) for g in range(2)]
        kwT = [cx.sb(es, "kwT%d" % g, [64, S], BF16) for g in range(2)]
        vaug = cx.sb(es, "vaug", [128, NT, 4, 80], BF16)
        kcmpT = [cx.sb(es, "kcmpT%d" % g, [64, 256], BF16) for g in range(2)]
        vcmp = cx.sb(es, "vcmp", [128, 2, 2, 80], BF16)
        ident = cx.sb(es, "ident", [128, 128], BF16)
        onesb = cx.sb(es, "onesb", [128, 128], BF16)
        ones32 = cx.sb(es, "ones32", [128, 64], F32)
        kmx = cx.sb(es, "kmx", [128, 8], F32)
        P = [cx.ps(es, "P%d" % i, [128, 512], F32) for i in range(3)]
        OTs = [cx.ps(es, "OT%d" % i, [128, 512], F32) for i in range(2)]
        RP = cx.ps(es, "RP", [128, 4, 128], F32)
        M1 = cx.ps(es, "M1", [128, 512], F32)
        M2 = cx.ps(es, "M2", [128, 8, 128], BF16)
        cnt = {"p": 0, "cp": 0, "o": 0, "pt": 0}

        def nP():
            cnt["p"] += 1
            return P[cnt["p"] % 3]

        def nO():
            cnt["o"] += 1
            return OTs[cnt["o"] % 2]

        def cp(out_ap, in_ap, r, w):
            cnt["cp"] += 1
            if cnt["cp"] % 2:
                cx.act(lambda e: e.copy(out_ap, in_ap), r=r, w=w)
            else:
                cx.dve(lambda e: e.tensor_copy(out_ap, in_ap), r=r, w=w)

        cx.dve(lambda e: e.memset(onesb[:], 1.0), w=[onesb])
        cx.dve(lambda e: e.memset(ones32[:], 1.0), w=[ones32])
        cx.dve(lambda e: e.memset(vaug[:], 1.0), w=[vaug])
        with ExitStack() as es2:
            stg = [cx.sb(es2, "wstg%d" % i, [128, 1304], F32) for i in range(2)]
            load_weight_bf16(cx, wb, w_ap, 8, 1304, stg)
            cx.dma(ident[:], ident_ap, writes=[ident])
            for g in range(2):
                cx.dma(ksE[g][64:128, :], cn["blockE"], writes=[(ksE[g].key, "E")])
            cx.barrier()
            kcT = [cx.sb(es2, "kcT%d" % g, [64, S], BF16) for g in range(2)]
            vcT = [cx.sb(es2, "vcT%d" % g, [64, S], BF16) for g in range(2)]
            hins = [cx.sb(es2, "hin%d" % i, [128, 8, MTK], BF16) for i in range(2)]

            def phaseA(m, hin):
                cx.dma(hin[:], dram_fm(hT_ap, 0, 8, m * MTK, (m + 1) * MTK), writes=[hin])
                for (dst, col) in ((kcT, 512), (vcT, 640), (ksE, 768), (kwT, 896)):
                    for g in range(2):
                        ps = nP()
                        proj_fm(cx, ps, wb, col + g * 64, 64, hin, MTK)
                        cp(dst[g][0:64, m * MTK:(m + 1) * MTK], ps[0:64, :], [ps], [dst[g]])
                for j in range(4):
                    ps = nP()
                    for kc in range(8):
                        cx.pe(lambda e, ps=ps, kc=kc, j=j: e.matmul(
                            ps[:, 0:256], hin[:, kc, j * 128:(j + 1) * 128], wb[:, kc, 1024:1280],
                            start=(kc == 0), stop=(kc == 7)), r=[hin, wb], w=[ps])
                    cp(vaug[:, m * 4 + j, :, 0:64], ps[:, 0:256].rearrange("p (v d) -> p v d", d=64), [ps], [vaug])

            for m in range(S // MTK):
                phaseA(m, hins[m % 2])

            w1s = cx.sb(es2, "w1s", [64, 32, 64], F32)
            w1b = cx.sb(es2, "w1b", [64, 32, 64], BF16)
            w2s = cx.sb(es2, "w2s", [64, 64], F32)
            w2b = cx.sb(es2, "w2b", [64, 64], BF16)
            poss = cx.sb(es2, "poss", [64, 32], F32)
            posb = cx.sb(es2, "posb", [64, 32], BF16)
            cb = cx.sb(es2, "cb", [64, 1], F32)
            tt = [cx.sb(es2, "gt%d" % i, [64, 256], F32) for i in range(3)]
            glb = cx.sb(es2, "glb", [64, 256], BF16)
            for g in range(2):
                cx.dve(lambda e, g=g: e.memset(kcmpT[g][:], 0.0), w=[kcmpT[g]])
            cx.dve(lambda e: e.memset(vcmp[:], 0.0), w=[vcmp])
            cx.dve(lambda e: e.memset(vcmp[:, :, :, 64:80], 1.0), r=[vcmp], w=[vcmp])
            cx.dve(lambda e: e.memset(glb[:], 0.0), w=[glb])
            NCMP = S // 16 - 1

            def phaseB(kind, g, src):
                pos_ap, w1_ap, w2_ap = cw["pos_" + kind], cw["w1_" + kind], cw["w2_" + kind]
                if g == 0:
                    cx.dma(w1s[:], w1_ap.rearrange("(p d) o -> d p o", d=64), writes=[w1s])
                    cx.dma(w2s[:], w2_ap, writes=[w2s])
                    cx.dma(poss[:], pos_ap.rearrange("p d -> d p"), writes=[poss], allow_slow_non_contiguous=True)
                    cx.dve(lambda e: e.tensor_copy(w1b[:], w1s[:]), r=[w1s], w=[w1b])
                    cx.dve(lambda e: e.tensor_copy(w2b[:], w2s[:]), r=[w2s], w=[w2b])
                    cx.dve(lambda e: e.tensor_copy(posb[:], poss[:]), r=[poss], w=[posb])
                    for p in range(32):
                        cx.pe(lambda e, p=p: e.matmul(M1[0:64, 0:1], w1b[:, p, :], posb[:, p:p + 1],
                                                      start=(p == 0), stop=(p == 31)), r=[w1b, posb], w=[M1])
                    cx.act(lambda e: e.copy(cb[:], M1[0:64, 0:1]), r=[M1], w=[cb])
                ps = nP()
                x3 = src[0:64, :].rearrange("d (n s) -> d n s", s=16)
                for p in range(32):
                    n0, r_ = (0, p) if p < 16 else (1, p - 16)
                    cx.pe(lambda e, p=p, n0=n0, r_=r_, ps=ps: e.matmul(
                        ps[0:64, 0:NCMP], w1b[:, p, :], x3[:, n0:n0 + NCMP, r_],
                        start=(p == 0), stop=(p == 31)), r=[w1b, src], w=[ps])
                t0, t1_, t2_ = tt
                N = NCMP
                cx.act(lambda e, ps=ps: e.activation(t0[:, 0:N], ps[0:64, 0:N], AF.Identity, bias=cb[:], scale=1.0),
                       r=[ps, cb], w=[t0])
                cx.dve(lambda e: e.tensor_tensor(t1_[:, 0:N], t0[:, 0:N], t0[:, 0:N], ALU.mult), r=[t0], w=[t1_])
                cx.dve(lambda e: e.tensor_scalar(t1_[:, 0:N], t1_[:, 0:N], 0.044715, 1.0, ALU.mult, ALU.add), r=[t1_], w=[t1_])
                cx.dve(lambda e: e.tensor_tensor(t1_[:, 0:N], t1_[:, 0:N], t0[:, 0:N], ALU.mult), r=[t1_, t0], w=[t1_])
                cx.act(lambda e: e.activation(t2_[:, 0:N], t1_[:, 0:N], AF.Sigmoid, scale=2.0 * math.sqrt(2.0 / math.pi)),
                       r=[t1_], w=[t2_])
                cx.dve(lambda e: e.tensor_tensor(glb[:, 0:N], t0[:, 0:N], t2_[:, 0:N], ALU.mult), r=[t0, t2_], w=[glb])
                if kind == "k":
                    po = nP()
                    cx.pe(lambda e, po=po: e.matmul(po[0:64, 0:N], w2b[:], glb[:, 0:N], start=True, stop=True),
                          r=[w2b, glb], w=[po])
                    cp(kcmpT[g][:, 0:N], po[0:64, 0:N], [po], [kcmpT[g]])
                else:
                    for kc2 in range(2):
                        n1 = min(128, N - kc2 * 128)
                        if n1 <= 0:
                            continue
                        po = nP()
                        cx.pe(lambda e, po=po, kc2=kc2, n1=n1: e.matmul(
                            po[0:n1, 0:64], glb[:, kc2 * 128:kc2 * 128 + n1], w2b[:], start=True, stop=True),
                            r=[glb, w2b], w=[po])
                        cp(vcmp[0:n1, kc2, g, 0:64], po[0:n1, 0:64], [po], [vcmp])

            for kind, srcs in (("k", kcT), ("v", vcT)):
                for g in range(2):
                    phaseB(kind, g, srcs[g])

            sqk = [cx.sb(es2, "sqk%d" % i, [64, 512], BF16) for i in range(2)]
            kcm = cx.sb(es2, "kcm", [128, 8], F32)
            qi = [0]

            def kmax(src, ncols, col):
                nchunk = (ncols + 511) // 512
                for ci in range(nchunk):
                    c0 = ci * 512
                    n_ = min(512, ncols - c0)
                    sq = sqk[qi[0] % 2]
                    qi[0] += 1
                    cx.act(lambda e, sq=sq, c0=c0, n_=n_: e.activation(sq[:, 0:n_], src[0:64, c0:c0 + n_], AF.Square),
                           r=[src], w=[sq])
                    ps = nP()
                    cx.pe(lambda e, ps=ps, sq=sq, n_=n_: e.matmul(ps[:, 0:n_], onesb[0:64, :], sq[:, 0:n_], start=True, stop=True),
                          r=[onesb, sq], w=[ps])
                    cx.dve(lambda e, ps=ps, ci=ci, n_=n_: e.reduce_max(kcm[:, ci:ci + 1], ps[:, 0:n_], AX.X), r=[ps], w=[kcm])
                cx.dve(lambda e: e.tensor_reduce(kmx[:, col:col + 1], kcm[:, 0:nchunk], AX.X, ALU.max), r=[kcm], w=[kmx])

            for g in range(2):
                kmax(kcmpT[g], 256, 0 + g)
                kmax(kwT[g], S, 2 + g)
                kmax(ksE[g], S, 4 + g)
            cx.barrier()

        def cload(name, shape, dtype, src):
            t = cx.sb(es, name, shape, dtype)
            cx.dma(t[:], src, writes=[t])
            return t

        ovla = cload("ovla", [128, 2, 80], BF16, cn["ovla"].rearrange("(c p) j -> p c j", p=128))
        causalT4 = cload("causalT4", [128, 4, 128], BF16, cn["causalT4"])
        wm0T4 = cload("wm0T4", [128, 4, 128], BF16, cn["wm0T4"])
        selrows = cload("selrows", [24, 24, 64], F32, cn["selrows"])
        hqs = [cx.sb(es, "hq%d" % i, [128, 8, 128], BF16) for i in range(2)]
        cmTs = [cx.sb(es, "cmT%d" % i, [128, 2, 4, 128], BF16) for i in range(2)]
        fbs = [cx.sb(es, "fb%d" % i, [128, 64], F32) for i in range(2)]
        gatesT = [cx.sb(es, "gatesT%d" % i, [32, 128], F32) for i in range(2)]
        qsel = [cx.sb(es, "qsel%d" % i, [128, 4, 128], BF16) for i in range(2)]
        sqqs = [cx.sb(es, "sqq%d" % i, [64, 4, 128], BF16) for i in range(2)]
        qms = [cx.sb(es, "qm%d" % i, [128, 1], F32) for i in range(2)]
        negcs = [[cx.sb(es, "negc%d_%d" % (g_, i), [128, 1], F32) for i in range(3)] for g_ in range(2)]
        pending = []

        def flush():
            while pending:
                pending.pop(0)()
        selw = cx.sb(es, "selw", [128, 128], BF16)
        cx.dve(lambda e: e.memset(selw[:], 0.0), w=[selw])
        PcT = cx.sb(es, "PcT", [128, 2, 512], BF16)
        NPT = 6
        PTs = [cx.sb(es, "PT%d" % i, [128, 512], BF16) for i in range(NPT)]
        rsr = cx.sb(es, "rsr", [128, 512], F32)
        bcs = cx.sb(es, "bcs", [64, 512], F32)
        tmpo = cx.sb(es, "tmpo", [64, 512], F32)
        accT = [cx.sb(es, "accT%d" % i, [64, 8, 128], F32) for i in range(2)]
        accTb = [cx.sb(es, "accTb%d" % i, [64, 8, 128], BF16) for i in range(2)]
        rs4 = cx.sb(es, "rs4", [128, 4], F32)
        impb = cx.sb(es, "impb", [128, 64], F32)
        sc64 = cx.sb(es, "sc64", [128, 64], F32)
        top8 = cx.sb(es, "top8", [128, 8], F32)
        sel01 = cx.sb(es, "sel01", [128, 64], F32)
        SCALE = 0.125
        mix_dst = mixT_ap[4:8].rearrange("c (two d) s -> d (c two) s", two=2)

        def block(i, hq, cmT, fb, gT, acc, accb):
            kt0 = max(0, i - 4)
            cx.dma(hq[:], dram_fm(hT_ap, 0, 8, i * 128, (i + 1) * 128), writes=[hq])
            cx.dma(cmT[:], cn["cmaskT"][i], writes=[cmT])
            cx.dma(fb[:], cn["fbias"][i], writes=[fb])
            for kc in range(8):
                cx.pe(lambda e, kc=kc: e.matmul(M1[0:32, 0:128], wb[:, kc, 1280:1312], hq[:, kc, :],
                                                start=(kc == 0), stop=(kc == 7)), r=[hq, wb], w=[M1])
            cx.act(lambda e: e.activation(gT[:], M1[0:32, 0:128], AF.Sigmoid), r=[M1], w=[gT])

            def finalize(OT, g, br, first):
                accg = acc[:, g * 4:(g + 1) * 4, :]
                cx.dve(lambda e: e.tensor_scalar_max(rsr[64:65, :], OT[64:65, :], 1e-30), r=[OT], w=[rsr])
                cx.dve(lambda e: e.reciprocal(rsr[64:65, :], rsr[64:65, :]), r=[rsr], w=[rsr])
                cx.pe(lambda e: e.matmul(M1[0:64, :], ones32[64:65, 0:64], rsr[64:65, :], start=True, stop=True),
                      r=[ones32, rsr], w=[M1])
                cx.act(lambda e: e.copy(bcs[:], M1[0:64, :]), r=[M1], w=[bcs])
                cx.dve(lambda e: e.tensor_tensor(tmpo[:], OT[0:64, :], bcs[:], ALU.mult), r=[OT, bcs], w=[tmpo])
                for hp in range(4):
                    r_ = br * 8 + g * 4 + hp
                    cx.pe(lambda e, hp=hp, r_=r_: e.matmul(M1[0:64, hp * 128:(hp + 1) * 128], selrows[:, r_, :], gT[0:24, :],
                                                           start=True, stop=True), r=[selrows, gT], w=[M1])
                t3 = tmpo[:].rearrange("p (h q) -> p h q", q=128)
                m3 = M1[0:64, :].rearrange("p (h q) -> p h q", q=128)
                if first:
                    cx.dve(lambda e: e.tensor_tensor(accg, t3, m3, ALU.mult), r=[tmpo, M1], w=[acc])
                else:
                    cx.dve(lambda e: e.tensor_tensor(t3, t3, m3, ALU.mult), r=[tmpo, M1], w=[tmpo])
                    cx.dve(lambda e: e.tensor_tensor(accg, accg, t3, ALU.add), r=[acc, tmpo], w=[acc])

            def run_tiles(tiles, qrows, ncg, vfn, mfn, qsel_key):
                OT = nO()
                pend = None
                n = len(tiles)
                for t, (l_ap, l_r) in enumerate(tiles):
                    ps = nP()
                    mm = mfn(t)
                    cx.pe(lambda e, ps=ps, l_ap=l_ap, stp=(mm is None): e.matmul(ps[:], l_ap, qrows, start=True, stop=stp),
                          r=l_r + [qsel_key], w=[ps])
                    if mm is not None:
                        cx.pe(lambda e, ps=ps, mm=mm: e.matmul(ps[:], ident[:], mm[0], start=False, stop=True),
                              r=[ident] + mm[1], w=[ps])
                    cnt["pt"] += 1
                    pt = PTs[cnt["pt"] % NPT]
                    cx.act(lambda e, ps=ps, pt=pt: e.activation(pt[:], ps[:], AF.Exp, bias=ncg[:], scale=SCALE),
                           r=[ps, ncg], w=[pt])
                    if pend is not None:
                        pend()
                    v_ap, v_r = vfn(t)
                    pend = (lambda t=t, pt=pt, v_ap=v_ap, v_r=v_r: cx.pe(
                        lambda e: e.matmul(OT[0:80, :], v_ap[:, 0:80], pt[:], start=(t == 0), stop=(t == n - 1)),
                        r=[pt] + v_r, w=[OT]))
                pend()
                return OT

            def prelude(g):
                qs = qsel[g]
                qk = (qs.key, "q")
                sqq, qm, negc = sqqs[g], qms[g], negcs[g]
                for hp in range(4):
                    hd = g * 4 + hp
                    for kc in range(8):
                        cx.pe(lambda e, kc=kc, hp=hp, hd=hd: e.matmul(M1[0:64, hp * 128:(hp + 1) * 128], wb[:, kc, hd * 64:(hd + 1) * 64],
                                                                     hq[:, kc, :], start=(kc == 0), stop=(kc == 7)),
                              r=[wb, hq], w=[M1])
                cx.dve(lambda e: e.tensor_copy(qs[0:64, :, :].rearrange("p h q -> p (h q)"), M1[0:64, :]), r=[M1], w=[qk])
                cx.act(lambda e: e.activation(sqq[:].rearrange("p h q -> p (h q)"), M1[0:64, :], AF.Square), r=[M1], w=[sqq])
                ps = nP()
                cx.pe(lambda e: e.matmul(ps[:], onesb[0:64, :], sqq[:].rearrange("p h q -> p (h q)"), start=True, stop=True),
                      r=[onesb, sqq], w=[ps])
                cx.dve(lambda e: e.reduce_max(qm[:], ps[:], AX.X), r=[ps], w=[qm])
                for br in range(3):
                    nb_ = negc[br]
                    cx.dve(lambda e, nb_=nb_, br=br: e.tensor_tensor(nb_[:], qm[:], kmx[:, 2 * br + g:2 * br + g + 1], ALU.mult),
                           r=[qm, kmx], w=[nb_])
                    cx.act(lambda e, nb_=nb_: e.activation(nb_[:], nb_[:], AF.Sqrt), r=[nb_], w=[nb_])
                    cx.dve(lambda e, nb_=nb_: e.tensor_scalar_mul(nb_[:], nb_[:], -SCALE * 1.05), r=[nb_], w=[nb_])

            def group(g):
                qs = qsel[g]
                qk = (qs.key, "q")
                sk = (qs.key, "s")
                negc = negcs[g]
                global_q = qs[0:64, :, :].rearrange("p h q -> p (h q)")
                OTc = nO()
                for kc2 in range(2):
                    ps = nP()
                    cx.pe(lambda e, ps=ps, kc2=kc2: e.matmul(ps[:], kcmpT[g][:, kc2 * 128:(kc2 + 1) * 128], global_q,
                                                            start=True, stop=False), r=[kcmpT[g], qk], w=[ps])
                    cx.pe(lambda e, ps=ps, kc2=kc2: e.matmul(ps[:], ident[:], cmT[:, kc2, :, :].rearrange("p h q -> p (h q)"),
                                                            start=False, stop=True), r=[ident, cmT], w=[ps])
                    cx.act(lambda e, ps=ps, kc2=kc2: e.activation(PcT[:, kc2, :], ps[:], AF.Exp, bias=negc[0][:], scale=SCALE),
                           r=[ps, negc[0]], w=[PcT])
                for kc2 in range(2):
                    cx.pe(lambda e, kc2=kc2: e.matmul(OTc[0:80, :], vcmp[:, kc2, g, 0:80], PcT[:, kc2, :],
                                                      start=(kc2 == 0), stop=(kc2 == 1)), r=[vcmp, PcT], w=[OTc])
                for hp in range(4):
                    for kc2 in range(2):
                        cx.pe(lambda e, hp=hp, kc2=kc2: e.matmul(RP[:, hp, 0:80], PcT[:, kc2, hp * 128:(hp + 1) * 128], ovla[:, kc2, :],
                                                                 start=(kc2 == 0), stop=(kc2 == 1)), r=[PcT, ovla], w=[RP])
                cx.dve(lambda e: e.tensor_scalar_max(rs4[:], RP[:, :, 64], 1e-30), r=[RP], w=[rs4])
                cx.dve(lambda e: e.reciprocal(rs4[:], rs4[:]), r=[rs4], w=[rs4])
                cx.dve(lambda e: e.scalar_tensor_tensor(impb[:], RP[:, 0, 0:64], rs4[:, 0:1], fb[:], ALU.mult, ALU.add),
                       r=[RP, rs4, fb], w=[impb])
                for hp in range(1, 4):
                    cx.dve(lambda e, hp=hp: e.scalar_tensor_tensor(impb[:], RP[:, hp, 0:64], rs4[:, hp:hp + 1], impb[:], ALU.mult, ALU.add),
                           r=[RP, rs4, impb], w=[impb])
                cx.dve(lambda e: e.max(top8[:], impb[:]), r=[impb], w=[top8])
                cx.dve(lambda e: e.tensor_scalar(sel01[:], impb[:], top8[:, 7:8], None, ALU.is_ge), r=[impb, top8], w=[sel01])
                cx.dve(lambda e: e.tensor_scalar(selw[:, 64:128], sel01[:], -NEG8, NEG8, ALU.mult, ALU.add), r=[sel01], w=[selw])
                flush()
                wt = list(range(kt0, i + 1))

                def wmask(t):
                    r_ = wt[t] - (i - 4)
                    if r_ == 0:
                        return (wm0T4[:].rearrange("p h q -> p (h q)"), [wm0T4])
                    if r_ == 4:
                        return (causalT4[:].rearrange("p h q -> p (h q)"), [causalT4])
                    return None

                OTw = run_tiles([(kwT[g][:, kt * 128:(kt + 1) * 128], [kwT[g]]) for kt in wt], global_q, negc[1],
                                lambda t: (vaug[:, wt[t], 2 + g, :], [vaug]), wmask, qk)
                cx.pe(lambda e: e.transpose(M2[:, 0, :], selw[:], ident[:]), r=[selw, ident], w=[M2])
                cx.act(lambda e: e.copy(qs[64:128, :, :], M2[64:128, 0:1, :].to_broadcast([64, 4, 128])), r=[M2], w=[sk])
                finalize(OTc, g, 0, True)
                qfull = qs[:, :, :].rearrange("p h q -> p (h q)")
                OTs = run_tiles([(ksE[g][:, kt * 128:(kt + 1) * 128], [ksE[g], (ksE[g].key, "E"), qk]) for kt in range(i + 1)],
                                qfull, negc[2], lambda t: (vaug[:, t, g, :], [vaug]),
                                lambda t: ((causalT4[:].rearrange("p h q -> p (h q)"), [causalT4]) if t == i else None), sk)
                finalize(OTw, g, 2, False)
                pending.append(lambda: finalize(OTs, g, 1, False))

            def blk_post():
                cx.act(lambda e: e.copy(accb[:], acc[:]), r=[acc], w=[accb])
                cx.dma(mix_dst[:, :, i * 128:(i + 1) * 128], accb[:], reads=[accb], q="pool")

            prelude(0)
            prelude(1)
            group(0)
            group(1)
            pending.append(blk_post)

        for i in range(NB if NSA2_STOP != "AB" else 0):
            block(i, hqs[i % 2], cmTs[i % 2], fbs[i % 2], gatesT[i % 2], accT[i % 2], accTb[i % 2])
        flush()
    cx.barrier()


SEQ = 4096
NCORES = 8
DEPTH = 4


def build_full(S=SEQ, depth=DEPTH):
    nc = bass.Bass("TRN2", target_bir_lowering=False)

    def din(n, s, d=F32):
        return nc.dram_tensor(n, list(s), d, kind="ExternalInput").ap()

    x = din("x", [S, D])
    norm_mix_g = din("norm_mix_g", [4, D])
    norm_ffn_g = din("norm_ffn_g", [4, D])
    final_norm_g = din("final_norm_g", [D])
    w_ret = din("w_ret", [2, D, 3072])
    w_nsa = din("w_nsa", [2, D, 1304])
    even_w_out = din("even_w_out", [2, D, D])
    cws = {}
    for kind in "kv":
        cws["pos_" + kind] = din("cmp_pos_" + kind, [2, 32, 64])
        cws["w1_" + kind] = din("cmp_w1_" + kind, [2, 2048, 64])
        cws["w2_" + kind] = din("cmp_w2_" + kind, [2, 64, 64])
    odd_w_in = din("odd_w_in", [2, D, 4096])
    odd_w_out = din("odd_w_out", [2, D, D])
    hgrn_norm_g = din("hgrn_norm_g", [2, 128])
    hgrn_lb = din("hgrn_lb_logits", [2, 1024])
    ffn_w1 = din("ffn_w1", [4, D, DFF])
    ffn_w3 = din("ffn_w3", [4, D, DFF])
    ffn_w2 = din("ffn_w2", [4, DFF, D])
    ident = din("c_ident", [128, 128], BF16)
    maskT = din("c_maskT", [64, 64])
    scanm = din("c_scanm", [128, MTK])
    cos = din("c_cos", [128, S])
    sin = din("c_sin", [128, S])
    dec = din("c_dec", [4, 2, 128, MTK])
    NB = S // 128
    cn = {
        "cmaskT": din("c_cmaskT", [NB, 128, 2, 4, 128], BF16),
        "fbias": din("c_fbias", [NB, 128, 64]),
        "blockE": din("c_blockE", [64, S], BF16),
        "causalT4": din("c_causalT4", [128, 4, 128], BF16),
        "wm0T4": din("c_wm0T4", [128, 4, 128], BF16),
        "ovla": din("c_ovla", [256, 80], BF16),
        "selrows": din("c_selrows", [24, 24, 64]),
    }
    y = nc.dram_tensor("y", [S, D], F32, kind="ExternalOutput").ap()
    xs = nc.dram_tensor("xs", [S, D], F32).ap()
    hTa = nc.dram_tensor("hTa", [8, 128, S], BF16).ap()
    hTb = nc.dram_tensor("hTb", [8, 128, S], BF16).ap()
    mixT = nc.dram_tensor("mixT", [8, 128, S], BF16).ap()

    cx = Ctx(nc)
    stage_norm0(cx, x, hTb, norm_mix_g[0], ident, S)
    for layer in range(depth):
        j = layer // 2
        if layer % 2 == 0:
            stage_ret(cx, hTb, w_ret[j], cos, sin, dec, mixT, ident, maskT, S)
            cw = {k_: v_[j] for k_, v_ in cws.items()}
            stage_nsa2(cx, hTb, w_nsa[j], cw, mixT, ident, cn, S)
            w_out = even_w_out[j]
        else:
            stage_hgrn(cx, hTb, odd_w_in[j], hgrn_norm_g[j], hgrn_lb, mixT, ident, maskT, scanm, S, j)
            w_out = odd_w_out[j]
        stage_out(cx, mixT, w_out, x if layer == 0 else xs, xs, hTa, norm_ffn_g[layer], ident, S)
        last = layer == depth - 1
        stage_ffn(cx, hTa, ffn_w1[layer], ffn_w3[layer], ffn_w2[layer], xs, xs, hTb,
                  norm_mix_g[min(layer + 1, 3)], ident, S, last, gfin_ap=final_norm_g, y_ap=y)
    cx.emit()
    return nc


def host_layout(inputs, S=SEQ):
    f32 = lambda a: np.ascontiguousarray(np.asarray(a, dtype=np.float32))
    ew = f32(inputs["even_w_in"])

    def swap(w):
        return w.reshape(w.shape[0], D, 4, 2, 64)[:, :, :, ::-1, :].reshape(w.shape[0], D, 512)

    rq, rk, rv, rg = ew[:, :, 0:512], ew[:, :, 512:1024], ew[:, :, 1024:1536], ew[:, :, 1536:2048]
    nq = ew[:, :, 2048:2560]
    kc, vc, ks, vs, kw, vw = [ew[:, :, 2560 + 128 * i:2560 + 128 * (i + 1)] for i in range(6)]
    ng = ew[:, :, 3328:3352]
    shared = {
        "w_ret": np.ascontiguousarray(np.concatenate([rq, rk, swap(rq), swap(rk), rv, rg], axis=2)),
        "w_nsa": np.ascontiguousarray(np.concatenate([nq, kc, vc, ks, kw, vs, vw, ng], axis=2)),
    }
    for k_ in ("norm_mix_g", "norm_ffn_g", "final_norm_g", "even_w_out", "cmp_pos_k", "cmp_w1_k", "cmp_w2_k",
               "cmp_pos_v", "cmp_w1_v", "cmp_w2_v", "odd_w_in", "odd_w_out", "hgrn_norm_g", "hgrn_lb_logits",
               "ffn_w1", "ffn_w3", "ffn_w2"):
        shared[k_] = f32(inputs[k_])
    for k_, v_ in host_consts(S).items():
        shared["c_" + k_] = v_
    for k_, v_ in nsa2_consts(S).items():
        shared["c_" + k_] = v_
    return shared


def kernel(**inputs):
    x = np.ascontiguousarray(np.asarray(inputs["x"], dtype=np.float32))
    B, S, _ = x.shape
    shared = host_layout(inputs, S)
    nc = build_full(S)
    in_maps = []
    for b in range(B):
        m = dict(shared)
        m["x"] = np.ascontiguousarray(x[b])
        in_maps.append(m)
    res = run_bass_kernel_spmd(nc, in_maps, core_ids=list(range(B)))
    return np.stack([np.asarray(r["y"], dtype=np.float32) for r in res.results], axis=0)
```

```python
import math
from contextlib import ExitStack

import numpy as np
import concourse.bass as bass
import concourse.mybir as mybir
from concourse.bass_utils import run_bass_kernel_spmd

F32 = mybir.dt.float32
BF16 = mybir.dt.bfloat16
AF = mybir.ActivationFunctionType
ALU = mybir.AluOpType
AX = mybir.AxisListType

D = 1024
DFF = 2816
NFF = DFF // 128
EPS = 1e-6
EVEN_IN = 3352


class Buf:
    def __init__(self, t, key, psum=False):
        self.t = t
        self.key = key
        self.psum = psum

    def __getitem__(self, k):
        return self.t[k]


class Ctx:
    NDMA = 8
    ENG = ("pe", "act", "dve", "pool", "sp")

    def __init__(self, nc):
        self.nc = nc
        self.streams = {e: [] for e in self.ENG}
        self.cnt = {e: 0 for e in ("pe", "act", "dve", "pool")}
        self.dman = {q: 0 for q in ("sp", "act", "pool")}
        self.lastw = {}
        self.readers = {}
        self.known = {e: {} for e in self.ENG}
        self.all_tokens = {}
        self.nbuf = 0

    def sb(self, es, name, shape, dtype):
        self.nbuf += 1
        nm = "%s_%d" % (name, self.nbuf)
        t = es.enter_context(self.nc.sbuf_tensor(nm, list(shape), dtype))
        return Buf(t, nm)

    def ps(self, es, name, shape, dtype):
        self.nbuf += 1
        nm = "%s_%d" % (name, self.nbuf)
        t = es.enter_context(self.nc.psum_tensor(nm, list(shape), dtype))
        return Buf(t, nm, psum=True)

    @staticmethod
    def _k(x):
        return x.key if isinstance(x, Buf) else x

    def _collect(self, eng, reads, writes):
        deps = []
        for r in reads:
            k = self._k(r)
            if k in self.lastw:
                deps.append(self.lastw[k])
        for w in writes:
            k = self._k(w)
            if k in self.lastw:
                deps.append(self.lastw[k])
            deps.extend(self.readers.get(k, ()))
        return deps

    def _record(self, tok, reads, writes):
        for r in reads:
            self.readers.setdefault(self._k(r), []).append(tok)
        for w in writes:
            k = self._k(w)
            self.lastw[k] = tok
            self.readers[k] = []

    def _waits(self, eng, deps, is_pe_compute):
        waits = {}
        kn = self.known[eng]
        for (sk, v, src) in deps:
            if is_pe_compute and src == "pe":
                continue
            if kn.get(sk, 0) >= v:
                continue
            if waits.get(sk, 0) < v:
                waits[sk] = v
        for sk, v in waits.items():
            kn[sk] = v
        return list(waits.items())

    def op(self, eng, fn, reads=(), writes=()):
        ex = [r for r in reads if isinstance(r, Buf) and r.psum]
        if ex:
            writes = list(writes) + ex
        deps = self._collect(eng, reads, writes)
        waits = self._waits(eng, deps, eng == "pe")
        self.cnt[eng] += 1
        tok = (eng, self.cnt[eng], eng)
        self.streams[eng].append((waits, fn, (eng, 1)))
        self.all_tokens[eng] = tok
        self._record(tok, reads, writes)

    def dma(self, out, in_, reads=(), writes=(), q="sp", **kw):
        deps = self._collect(q, reads, writes)
        n = self.dman[q]
        slot = n % self.NDMA
        sk = ("dma", q, slot)
        if n >= self.NDMA:
            deps.append((sk, 16 * (n // self.NDMA), "dma"))
        waits = self._waits(q, deps, False)
        self.dman[q] += 1
        tok = (sk, 16 * (n // self.NDMA + 1), "dma")
        self.streams[q].append((waits, lambda e: e.dma_start(out=out, in_=in_, **kw), (sk, 16)))
        self.all_tokens[sk] = tok
        self._record(tok, reads, writes)

    def barrier(self):
        toks = list(self.all_tokens.values())
        for e in self.ENG:
            waits = self._waits(e, toks, False)
            if waits:
                self.streams[e].append((waits, None, None))

    def pe(self, fn, r=(), w=()):
        self.op("pe", fn, r, w)

    def act(self, fn, r=(), w=()):
        self.op("act", fn, r, w)

    def dve(self, fn, r=(), w=()):
        self.op("dve", fn, r, w)

    def pool(self, fn, r=(), w=()):
        self.op("pool", fn, r, w)

    def emit(self):
        nc = self.nc
        self.barrier()
        with ExitStack() as es:
            sems = {}
            for e in ("pe", "act", "dve", "pool"):
                sems[e] = es.enter_context(nc.semaphore("s_" + e))
            for q in ("sp", "act", "pool"):
                for s in range(self.NDMA):
                    sems[("dma", q, s)] = es.enter_context(nc.semaphore("d_%s%d" % (q, s)))
            block = es.enter_context(nc.Block())
            streams = self.streams

            def replay(name, eng):
                for waits, fn, inc in streams[name]:
                    for sk, v in waits:
                        eng.wait_ge(sems[sk], v)
                    if fn is not None:
                        fn(eng).then_inc(sems[inc[0]], inc[1])

            if streams["sp"]:
                @block.sync
                def _(eng):
                    replay("sp", eng)
            if streams["pe"]:
                @block.tensor
                def _(eng):
                    replay("pe", eng)
            if streams["dve"]:
                @block.vector
                def _(eng):
                    replay("dve", eng)
            if streams["act"]:
                @block.scalar
                def _(eng):
                    replay("act", eng)
            if streams["pool"]:
                @block.gpsimd
                def _(eng):
                    replay("pool", eng)


def load_weight_bf16(cx, wb, w_ap, kchunks, ncols, stage):
    step = stage[0].t.shape[1]
    i = 0
    for c in range(kchunks):
        for c0 in range(0, ncols, step):
            n = min(step, ncols - c0)
            st = stage[i % len(stage)]
            cx.dma(st[:, 0:n], w_ap[c * 128:(c + 1) * 128, c0:c0 + n], writes=[st])
            if i % 2 == 0:
                cx.dve(lambda e, st=st, c=c, c0=c0, n=n: e.tensor_copy(wb[:, c, c0:c0 + n], st[:, 0:n]),
                       r=[st], w=[(wb.key, c, c0)])
            else:
                cx.act(lambda e, st=st, c=c, c0=c0, n=n: e.copy(wb[:, c, c0:c0 + n], st[:, 0:n]),
                       r=[st], w=[(wb.key, c, c0)])
            i += 1
    return wb


class NormTools:
    def __init__(self, cx, es, g_ap):
        self.cx = cx
        self.ident = cx.sb(es, "ident", [128, 128], BF16)
        self.gT = cx.sb(es, "gT", [128, 8], F32)
        self.ss = [cx.sb(es, "ss%d" % i, [128, 1], F32) for i in range(2)]
        self.rstd = [cx.sb(es, "rstd%d" % i, [128, 1], F32) for i in range(2)]
        self.junk = cx.sb(es, "junk", [128, D], BF16)
        self.hb = [cx.sb(es, "hb%d" % i, [128, D], BF16) for i in range(2)]
        self.tp = [cx.ps(es, "tp%d" % i, [128, 8, 128], BF16) for i in range(2)]
        self.i = 0
        cx.dma(self.gT[:], g_ap.rearrange("(c p) -> p c", p=128), writes=[self.gT],
               allow_slow_non_contiguous=True)

    def load_ident(self, ident_ap):
        self.cx.dma(self.ident[:], ident_ap, writes=[self.ident])

    def stats(self, xt):
        cx = self.cx
        i = self.i
        self.i += 1
        ss, rstd = self.ss[i % 2], self.rstd[i % 2]
        junk = self.junk
        cx.act(lambda e: e.activation(junk[:], xt[:], AF.Square, scale=1.0 / 32.0, accum_out=ss[:]),
               r=[xt], w=[junk, ss])
        cx.act(lambda e: e.activation(ss[:], ss[:], AF.Sqrt, bias=EPS, scale=1.0), r=[ss], w=[ss])
        cx.dve(lambda e: e.reciprocal(rstd[:], ss[:]), r=[ss], w=[rstd])
        return rstd

    def run(self, xt, hT, col0, scale_by_g=True):
        cx = self.cx
        i = self.i
        self.i += 1
        ss, rstd, hb, tp = self.ss[i % 2], self.rstd[i % 2], self.hb[i % 2], self.tp[i % 2]
        junk = self.junk
        cx.act(lambda e: e.activation(junk[:], xt[:], AF.Square, scale=1.0 / 32.0, accum_out=ss[:]),
               r=[xt], w=[junk, ss])
        cx.act(lambda e: e.activation(ss[:], ss[:], AF.Sqrt, bias=EPS, scale=1.0), r=[ss], w=[ss])
        cx.dve(lambda e: e.reciprocal(rstd[:], ss[:]), r=[ss], w=[rstd])
        cx.act(lambda e: e.activation(hb[:], xt[:], AF.Copy, scale=rstd[:]), r=[xt, rstd], w=[hb])
        for c in range(8):
            cx.pe(lambda e, c=c: e.transpose(tp[:, c, :], hb[:, c * 128:(c + 1) * 128], self.ident[:]),
                  r=[hb, self.ident], w=[tp])
        gT = self.gT
        cx.dve(lambda e: e.tensor_tensor(hT[:, :, col0:col0 + 128], tp[:],
                                         gT[:].unsqueeze(2).to_broadcast([128, 8, 128]), ALU.mult),
               r=[tp, gT], w=[hT])
        return rstd


def dram_fm(ap, c0, c1, s0, s1):
    return ap[c0:c1, :, s0:s1].rearrange("c p s -> p c s")


def stage_norm0(cx, x_ap, hT_ap, g_ap, ident_ap, S):
    with ExitStack() as es:
        nt = NormTools(cx, es, g_ap)
        nt.load_ident(ident_ap)
        xts = [cx.sb(es, "xt%d" % i, [128, D], F32) for i in range(2)]
        hTs = [cx.sb(es, "hTt%d" % i, [128, 8, 512], BF16) for i in range(2)]
        for m in range(S // 512):
            hT = hTs[m % 2]
            for j in range(4):
                t = m * 4 + j
                xt = xts[t % 2]
                cx.dma(xt[:], x_ap[t * 128:(t + 1) * 128, :], writes=[xt])
                nt.run(xt, hT, j * 128)
            cx.dma(dram_fm(hT_ap, 0, 8, m * 512, (m + 1) * 512), hT[:], reads=[hT], q="pool")
    cx.barrier()


def stage_out(cx, mixT_ap, w_ap, xin_ap, xout_ap, hT_ap, g_ap, ident_ap, S):
    with ExitStack() as es:
        wb = cx.sb(es, "woutb", [128, 8, D], BF16)
        with ExitStack() as es2:
            stg = [cx.sb(es2, "wstg%d" % i, [128, 1024], F32) for i in range(2)]
            load_weight_bf16(cx, wb, w_ap, 8, D, stg)
            cx.barrier()
        nt = NormTools(cx, es, g_ap)
        nt.load_ident(ident_ap)
        mts = [cx.sb(es, "mixt%d" % i, [128, 8, 512], BF16) for i in range(2)]
        xts = [cx.sb(es, "xt%d" % i, [128, D], F32) for i in range(2)]
        x1s = [cx.sb(es, "x1t%d" % i, [128, D], F32) for i in range(2)]
        hTs = [cx.sb(es, "hTt%d" % i, [128, 8, 512], BF16) for i in range(2)]
        yps = [cx.ps(es, "yps%d" % i, [128, 512], F32) for i in range(4)]
        for m in range(S // 512):
            mt = mts[m % 2]
            hT = hTs[m % 2]
            cx.dma(mt[:], dram_fm(mixT_ap, 0, 8, m * 512, (m + 1) * 512), writes=[mt])
            for j in range(4):
                t = m * 4 + j
                xt = xts[t % 2]
                x1 = x1s[t % 2]
                cx.dma(xt[:], xin_ap[t * 128:(t + 1) * 128, :], writes=[xt])
                for half in range(2):
                    yp = yps[(t * 2 + half) % 4]
                    for c in range(8):
                        cx.pe(lambda e, yp=yp, c=c, j=j, half=half, mt=mt: e.matmul(
                            yp[:], mt[:, c, j * 128:(j + 1) * 128], wb[:, c, half * 512:(half + 1) * 512],
                            start=(c == 0), stop=(c == 7)), r=[mt, wb], w=[yp])
                    cx.dve(lambda e, yp=yp, half=half, xt=xt, x1=x1: e.tensor_tensor(
                        x1[:, half * 512:(half + 1) * 512], yp[:], xt[:, half * 512:(half + 1) * 512], ALU.add),
                        r=[yp, xt], w=[x1])
                cx.dma(xout_ap[t * 128:(t + 1) * 128, :], x1[:], reads=[x1], q="pool")
                nt.run(x1, hT, j * 128)
            cx.dma(dram_fm(hT_ap, 0, 8, m * 512, (m + 1) * 512), hT[:], reads=[hT], q="pool")
    cx.barrier()


def stage_ffn(cx, hT_ap, w1_ap, w3_ap, w2_ap, xin_ap, xout_ap, hTout_ap, g_ap, ident_ap, S, final,
              gfin_ap=None, y_ap=None):
    MT = 256
    with ExitStack() as es:
        w1b = cx.sb(es, "w1b", [128, 8, DFF], BF16)
        w3b = cx.sb(es, "w3b", [128, 8, DFF], BF16)
        w2b = cx.sb(es, "w2b", [128, NFF, D], BF16)
        with ExitStack() as es2:
            stg = [cx.sb(es2, "wstg%d" % i, [128, 1408], F32) for i in range(2)]
            load_weight_bf16(cx, w1b, w1_ap, 8, DFF, stg)
            load_weight_bf16(cx, w3b, w3_ap, 8, DFF, stg)
            load_weight_bf16(cx, w2b, w2_ap, NFF, D, stg)
            cx.barrier()
        nt = NormTools(cx, es, g_ap)
        nt.load_ident(ident_ap)
        if final:
            gfin = cx.sb(es, "gfin", [128, D], F32)
            cx.dma(gfin[:], gfin_ap.partition_broadcast(128), writes=[gfin])
        hins = [cx.sb(es, "hin%d" % i, [128, 8, MT], BF16) for i in range(2)]
        gT = cx.sb(es, "gTff", [128, NFF, MT], BF16)
        sil = [cx.sb(es, "sil%d" % i, [128, MT], F32) for i in range(2)]
        xts = [cx.sb(es, "xt%d" % i, [128, D], F32) for i in range(2)]
        x2s = [cx.sb(es, "x2t%d" % i, [128, D], F32) for i in range(2)]
        hTs = [cx.sb(es, "hTt%d" % i, [128, 8, MT], BF16) for i in range(2)]
        ups = [cx.ps(es, "ups%d" % i, [128, 512], F32) for i in range(4)]
        yps = [cx.ps(es, "yps%d" % i, [128, 512], F32) for i in range(2)]
        nsub = MT // 128
        for m in range(S // MT):
            hin = hins[m % 2]
            hT = hTs[m % 2]
            cx.dma(hin[:], dram_fm(hT_ap, 0, 8, m * MT, (m + 1) * MT), writes=[hin])
            for f in range(NFF):
                u1 = ups[(f % 2) * 2]
                u3 = ups[(f % 2) * 2 + 1]
                sl = sil[f % 2]
                for (wb, up) in ((w1b, u1), (w3b, u3)):
                    for c in range(8):
                        cx.pe(lambda e, wb=wb, up=up, c=c, f=f, hin=hin: e.matmul(
                            up[:, 0:MT], wb[:, c, f * 128:(f + 1) * 128], hin[:, c, :],
                            start=(c == 0), stop=(c == 7)), r=[wb, hin], w=[up])
                cx.act(lambda e, u1=u1, sl=sl: e.activation(sl[:], u1[:, 0:MT], AF.Silu), r=[u1], w=[sl])
                cx.dve(lambda e, u3=u3, sl=sl, f=f: e.tensor_tensor(gT[:, f, :], u3[:, 0:MT], sl[:], ALU.mult),
                       r=[u3, sl], w=[gT])
            for j in range(nsub):
                t = m * nsub + j
                xt = xts[t % 2]
                x2 = x2s[t % 2]
                cx.dma(xt[:], xin_ap[t * 128:(t + 1) * 128, :], writes=[xt])
                for half in range(2):
                    yp = yps[half]
                    for f in range(NFF):
                        cx.pe(lambda e, yp=yp, f=f, j=j, half=half: e.matmul(
                            yp[:], gT[:, f, j * 128:(j + 1) * 128], w2b[:, f, half * 512:(half + 1) * 512],
                            start=(f == 0), stop=(f == NFF - 1)), r=[gT, w2b], w=[yp])
                    cx.dve(lambda e, yp=yp, half=half, xt=xt, x2=x2: e.tensor_tensor(
                        x2[:, half * 512:(half + 1) * 512], yp[:], xt[:, half * 512:(half + 1) * 512], ALU.add),
                        r=[yp, xt], w=[x2])
                if not final:
                    cx.dma(xout_ap[t * 128:(t + 1) * 128, :], x2[:], reads=[x2], q="pool")
                    nt.run(x2, hT, j * 128)
                else:
                    rstd = nt.stats(x2)
                    ot = xt
                    cx.dve(lambda e, x2=x2, rstd=rstd, ot=ot: e.scalar_tensor_tensor(
                        ot[:], x2[:], rstd[:], gfin[:], ALU.mult, ALU.mult), r=[x2, rstd, gfin], w=[ot])
                    cx.dma(y_ap[t * 128:(t + 1) * 128, :], ot[:], reads=[ot], q="pool")
            if not final:
                cx.dma(dram_fm(hTout_ap, 0, 8, m * MT, (m + 1) * MT), hT[:], reads=[hT], q="pool")
    cx.barrier()


DEBUG = {}
CH = 64
MTK = 512
NCH = MTK // CH


class GLACore:
    def __init__(self, cx, es, nheads, ident, maskT_ap):
        self.cx = cx
        self.ident = ident
        self.maskT = cx.sb(es, "maskT", [64, 64], F32)
        cx.dma(self.maskT[:], maskT_ap, writes=[self.maskT])
        self.S = [cx.sb(es, "S%d" % h, [128, 128], F32) for h in range(nheads)]
        self.Sx = [cx.sb(es, "Sx%d" % h, [128, 128], F32) for h in range(nheads)]
        for h in range(nheads):
            cx.dve(lambda e, h=h: e.memset(self.S[h][:], 0.0), w=[self.S[h]])
        self.ATbs = [cx.sb(es, "ATb%d" % i, [64, NCH, 64], BF16) for i in range(2)]
        self.ktms = [cx.sb(es, "ktm%d" % i, [64, NCH, 128], BF16) for i in range(2)]
        self.KVds = [cx.sb(es, "KVd%d" % i, [128, NCH, 128], F32) for i in range(2)]
        self.spb = [cx.sb(es, "spb%d" % i, [128, 128], BF16) for i in range(2)]
        self.AT = cx.ps(es, "ATp", [64, NCH, 64], F32)
        self.KTt = cx.ps(es, "KTt", [64, NCH, 128], BF16)
        self.OT = cx.ps(es, "OTp", [128, MTK], F32)
        self.KV = [cx.ps(es, "KVp%d" % i, [128, 4, 128], F32) for i in range(2)]

    def pre(self, par, qt, kt, v, vcol0, dlast):
        cx = self.cx
        AT, ATb, KTt, ktm, KV, KVd = self.AT, self.ATbs[par], self.KTt, self.ktms[par], self.KV, self.KVds[par]
        maskT, ident = self.maskT, self.ident
        for c in range(NCH):
            cs = slice(c * CH, (c + 1) * CH)
            cx.pe(lambda e, c=c, cs=cs: e.matmul(AT[:, c, :], kt[:, cs], qt[:, cs], start=True, stop=True),
                  r=[kt, qt], w=[AT])
        cx.dve(lambda e: e.tensor_tensor(ATb[:], AT[:], maskT[:].unsqueeze(1).to_broadcast([64, NCH, 64]), ALU.mult),
               r=[AT, maskT], w=[ATb])
        for c in range(NCH):
            cs = slice(c * CH, (c + 1) * CH)
            cx.pe(lambda e, c=c, cs=cs: e.transpose(KTt[:, c, :], kt[:, cs], ident[:]), r=[kt, ident], w=[KTt])
        cx.act(lambda e: e.copy(ktm[:], KTt[:]), r=[KTt], w=[ktm])
        for c in range(NCH):
            cx.pe(lambda e, c=c: e.matmul(KV[c // 4][:, c % 4, :], ktm[:, c, :], v[:, c, vcol0:vcol0 + 128],
                                          start=True, stop=True), r=[ktm, v], w=[KV[c // 4]])
        for b in range(2):
            if isinstance(dlast, float):
                cx.dve(lambda e, b=b: e.tensor_scalar_mul(KVd[:, 4 * b:4 * b + 4, :], KV[b][:], dlast),
                       r=[KV[b]], w=[KVd])
            else:
                cx.dve(lambda e, b=b: e.tensor_tensor(
                    KVd[:, 4 * b:4 * b + 4, :], KV[b][:],
                    dlast[1][:, 4 * b:4 * b + 4].unsqueeze(2).to_broadcast([128, 4, 128]), ALU.mult),
                    r=[KV[b], dlast[0]], w=[KVd])

    def chain(self, par, h, qt, v, vcol0, ebm, e2, oT_sb):
        cx = self.cx
        ATb, KVd, OT = self.ATbs[par], self.KVds[par], self.OT
        SS = (self.S[h], self.Sx[h])

        def sc(x, c):
            return (x, []) if isinstance(x, float) else (x[1][:, c:c + 1], [x[0]])

        for c in range(NCH):
            cs = slice(c * CH, (c + 1) * CH)
            spb = self.spb[c % 2]
            S, Sn = SS[c % 2], SS[(c + 1) % 2]
            s_ebm, r_ebm = sc(ebm, c)
            s_e2, r_e2 = sc(e2, c)
            cx.act(lambda e, spb=spb, s_ebm=s_ebm, S=S: e.activation(spb[:], S[:], AF.Copy, scale=s_ebm),
                   r=[S] + r_ebm, w=[spb])
            cx.pe(lambda e, c=c, cs=cs: e.matmul(OT[:, cs], v[:, c, vcol0:vcol0 + 128], ATb[:, c, :],
                                                 start=True, stop=False), r=[v, ATb], w=[OT])
            cx.pe(lambda e, cs=cs, spb=spb: e.matmul(OT[:, cs], spb[:], qt[:, cs], start=False, stop=True),
                  r=[spb, qt], w=[OT])
            cx.dve(lambda e, c=c, s_e2=s_e2, S=S, Sn=Sn: e.scalar_tensor_tensor(Sn[:], S[:], s_e2, KVd[:, c, :], ALU.mult, ALU.add),
                   r=[S, KVd] + r_e2, w=[Sn])
        cx.act(lambda e: e.copy(oT_sb[:], OT[:]), r=[OT], w=[oT_sb])

    def run(self, h, qt, kt, v, vcol0, ebm, e2, dlast, oT_sb):
        self.pre(0, qt, kt, v, vcol0, dlast)
        self.chain(0, h, qt, v, vcol0, ebm, e2, oT_sb)


def proj_fm(cx, ps, wb, col0, ncols, hin, n):
    for c in range(8):
        cx.pe(lambda e, c=c: e.matmul(ps[0:ncols, 0:n], wb[:, c, col0:col0 + ncols], hin[:, c, 0:n],
                                      start=(c == 0), stop=(c == 7)), r=[wb, hin], w=[ps])


def stage_hgrn(cx, hT_ap, w_ap, normg_ap, lb_ap, mixT_ap, ident_ap, maskT_ap, scanm_ap, S, layer_j):
    with ExitStack() as es:
        wb = cx.sb(es, "winb", [128, 8, 4096], BF16)
        with ExitStack() as es2:
            stg = [cx.sb(es2, "wstg%d" % i, [128, 2048], F32) for i in range(2)]
            load_weight_bf16(cx, wb, w_ap, 8, 4096, stg)
            cx.barrier()
        ident = cx.sb(es, "ident", [128, 128], BF16)
        cx.dma(ident[:], ident_ap, writes=[ident])
        onesb = cx.sb(es, "onesb", [128, 128], BF16)
        cx.dve(lambda e: e.memset(onesb[:], 1.0), w=[onesb])
        scanm = cx.sb(es, "scanm", [128, MTK], F32)
        cx.dma(scanm[:], scanm_ap, writes=[scanm])
        normg = cx.sb(es, "normg", [128, 1], F32)
        cx.dma(normg[:], normg_ap.rearrange("(p o) -> p o", o=1), writes=[normg])
        lbT = cx.sb(es, "lbT", [128, 8], F32)
        omlT = cx.sb(es, "omlT", [128, 8], F32)
        if layer_j == 0:
            cx.dve(lambda e: e.memset(lbT[:], 0.0), w=[lbT])
        else:
            l0 = cx.sb(es, "l0", [128, 8], F32)
            l1 = cx.sb(es, "l1", [128, 8], F32)
            cx.dma(l0[:], lb_ap[0, :].rearrange("(h p) -> p h", p=128), writes=[l0], allow_slow_non_contiguous=True)
            cx.dma(l1[:], lb_ap[1, :].rearrange("(h p) -> p h", p=128), writes=[l1], allow_slow_non_contiguous=True)
            cx.dve(lambda e: e.tensor_tensor(l1[:], l1[:], l0[:], ALU.subtract), r=[l0, l1], w=[l1])
            cx.act(lambda e: e.activation(lbT[:], l1[:], AF.Sigmoid), r=[l1], w=[lbT])
        cx.dve(lambda e: e.tensor_scalar(omlT[:], lbT[:], -1.0, 1.0, ALU.mult, ALU.add), r=[lbT], w=[omlT])
        core = GLACore(cx, es, 8, ident, maskT_ap)
        hins = [cx.sb(es, "hin%d" % i, [128, 8, MTK], BF16) for i in range(2)]
        v = cx.sb(es, "vtm", [64, NCH, 1024], BF16)
        f32t = lambda n: cx.sb(es, n, [128, MTK], F32)
        TT = [{n: f32t(n + str(i)) for n in ("sq", "sg", "gl", "bb", "dd", "eq", "ek")} for i in range(2)]
        for i in range(2):
            TT[i]["ebm"] = cx.sb(es, "ebm%d" % i, [128, NCH], F32)
            TT[i]["e2"] = cx.sb(es, "e2%d" % i, [128, NCH], F32)
            TT[i]["qt"] = cx.sb(es, "qt%d" % i, [128, MTK], BF16)
            TT[i]["kt"] = cx.sb(es, "kt%d" % i, [128, MTK], BF16)
        oT = f32t("oT")
        sqo = cx.sb(es, "sqo", [128, MTK], BF16)
        rt = f32t("rt")
        sgp = f32t("sgp")
        mts = [cx.sb(es, "mixt%d" % i, [128, 8, MTK], BF16) for i in range(1)]
        P = [cx.ps(es, "P%d" % i, [128, 512], F32) for i in range(3)]
        pi = [0]

        def nextP():
            pi[0] += 1
            return P[pi[0] % 3]

        def macro(m, hin, mt):
            cx.dma(hin[:], dram_fm(hT_ap, 0, 8, m * MTK, (m + 1) * MTK), writes=[hin])
            k = 0
            for c in range(NCH):
                for half in range(2):
                    ps = nextP()
                    for kc in range(8):
                        cx.pe(lambda e, ps=ps, kc=kc, c=c, half=half: e.matmul(
                            ps[0:64, :], hin[:, kc, c * CH:(c + 1) * CH],
                            wb[:, kc, 2048 + half * 512:2048 + (half + 1) * 512],
                            start=(kc == 0), stop=(kc == 7)), r=[hin, wb], w=[ps])
                    if k % 2 == 0:
                        cx.act(lambda e, ps=ps, c=c, half=half: e.copy(v[:, c, half * 512:(half + 1) * 512], ps[0:64, :]),
                               r=[ps], w=[v])
                    else:
                        cx.dve(lambda e, ps=ps, c=c, half=half: e.tensor_copy(v[:, c, half * 512:(half + 1) * 512], ps[0:64, :]),
                               r=[ps], w=[v])
                    k += 1
            def A(h, par):
                T = TT[par]
                sq, sg, gl, bb, dd, eq, ek, ebm, e2, qt, kt = (T[n] for n in ("sq", "sg", "gl", "bb", "dd", "eq", "ek", "ebm", "e2", "qt", "kt"))
                pq = nextP()
                proj_fm(cx, pq, wb, h * 128, 128, hin, MTK)
                cx.act(lambda e: e.activation(sq[:], pq[:], AF.Silu), r=[pq], w=[sq])
                pf = nextP()
                proj_fm(cx, pf, wb, 1024 + h * 128, 128, hin, MTK)
                cx.act(lambda e: e.activation(sg[:], pf[:], AF.Sigmoid), r=[pf], w=[sg])
                cx.dve(lambda e: e.tensor_scalar(sg[:], sg[:], omlT[:, h:h + 1], lbT[:, h:h + 1], ALU.mult, ALU.add),
                       r=[sg, omlT, lbT], w=[sg])
                cx.act(lambda e: e.activation(gl[:], sg[:], AF.Ln), r=[sg], w=[gl])
                cx.dve(lambda e: e.tensor_tensor_scan(bb[:], scanm[:], gl[:], 0.0, ALU.mult, ALU.add),
                       r=[scanm, gl], w=[bb])
                b3 = bb[:].rearrange("p (c n) -> p c n", n=CH)
                cx.dve(lambda e: e.tensor_tensor(
                    dd[:].rearrange("p (c n) -> p c n", n=CH), b3,
                    b3[:, :, 31:32].to_broadcast([128, NCH, CH]), ALU.subtract), r=[bb], w=[dd])
                cx.act(lambda e: e.activation(eq[:], dd[:], AF.Exp), r=[dd], w=[eq])
                cx.act(lambda e: e.activation(ek[:], dd[:], AF.Exp, scale=-1.0), r=[dd], w=[ek])
                cx.act(lambda e: e.activation(ebm[:], b3[:, :, 31], AF.Exp), r=[bb], w=[ebm])
                eq3 = eq[:].rearrange("p (c n) -> p c n", n=CH)
                cx.dve(lambda e: e.tensor_tensor(e2[:], ebm[:], eq3[:, :, CH - 1], ALU.mult),
                       r=[ebm, eq], w=[e2])
                cx.dve(lambda e: e.scalar_tensor_tensor(qt[:], sq[:], 128.0 ** -0.5, eq[:], ALU.mult, ALU.mult),
                       r=[sq, eq], w=[qt])
                cx.dve(lambda e: e.tensor_scalar(sg[:], sg[:], -1.0, 1.0, ALU.mult, ALU.add), r=[sg], w=[sg])
                cx.dve(lambda e: e.tensor_tensor(kt[:], sg[:], ek[:], ALU.mult), r=[sg, ek], w=[kt])
                core.pre(par, qt, kt, v, h * 128, (eq, eq3[:, :, CH - 1]))

            def B(h, par):
                T = TT[par]
                ebm, e2, qt = T["ebm"], T["e2"], T["qt"]
                core.chain(par, h, qt, v, h * 128, (ebm, ebm.t), (e2, e2.t), oT)
                cx.act(lambda e: e.activation(sqo[:], oT[:], AF.Square), r=[oT], w=[sqo])
                pss = nextP()
                cx.pe(lambda e: e.matmul(pss[:], onesb[:], sqo[:], start=True, stop=True),
                      r=[onesb, sqo], w=[pss])
                cx.act(lambda e: e.activation(rt[:], pss[:], AF.Sqrt, bias=EPS, scale=1.0 / 128.0),
                       r=[pss], w=[rt])
                cx.dve(lambda e: e.reciprocal(rt[:], rt[:]), r=[rt], w=[rt])
                pg = nextP()
                proj_fm(cx, pg, wb, 3072 + h * 128, 128, hin, MTK)
                cx.act(lambda e: e.activation(sgp[:], pg[:], AF.Sigmoid), r=[pg], w=[sgp])
                cx.dve(lambda e: e.scalar_tensor_tensor(rt[:], oT[:], normg[:, 0:1], rt[:], ALU.mult, ALU.mult),
                       r=[oT, normg, rt], w=[rt])
                cx.dve(lambda e: e.tensor_tensor(mt[:, h, :], rt[:], sgp[:], ALU.mult),
                       r=[rt, sgp], w=[mt])

            A(0, 0)
            for h in range(8):
                if h + 1 < 8:
                    A(h + 1, (h + 1) % 2)
                B(h, h % 2)
            cx.dma(dram_fm(mixT_ap, 0, 8, m * MTK, (m + 1) * MTK), mt[:], reads=[mt], q="pool")

        for m in range(S // MTK):
            macro(m, hins[m % 2], mts[0])
    cx.barrier()


RET_GAMMA = [1.0 - 2.0 ** (-5.0 - h) for h in range(4)]


def stage_ret(cx, hT_ap, w_ap, cos_ap, sin_ap, dec_ap, mixT_ap, ident_ap, maskT_ap, S):
    with ExitStack() as es:
        wb = cx.sb(es, "winb", [128, 8, 3072], BF16)
        with ExitStack() as es2:
            stg = [cx.sb(es2, "wstg%d" % i, [128, 1536], F32) for i in range(2)]
            load_weight_bf16(cx, wb, w_ap, 8, 3072, stg)
            cx.barrier()
        ident = cx.sb(es, "ident", [128, 128], BF16)
        cx.dma(ident[:], ident_ap, writes=[ident])
        onesb = cx.sb(es, "onesb", [128, 128], BF16)
        cx.dve(lambda e: e.memset(onesb[:], 1.0), w=[onesb])
        dec = cx.sb(es, "dec", [128, 8, MTK], F32)
        cx.dma(dec[:], dec_ap.rearrange("h t p n -> p (h t) n"), writes=[dec])
        core = GLACore(cx, es, 4, ident, maskT_ap)
        hins = [cx.sb(es, "hin%d" % i, [128, 8, MTK], BF16) for i in range(2)]
        coss = [cx.sb(es, "cos%d" % i, [128, MTK], F32) for i in range(2)]
        sins = [cx.sb(es, "sin%d" % i, [128, MTK], F32) for i in range(2)]
        v = cx.sb(es, "vtm", [64, NCH, 512], BF16)
        f32t = lambda n: cx.sb(es, n, [128, MTK], F32)
        oT, mean, var, sgp = [f32t(n) for n in ("oT", "mean", "var", "sgp")]
        TT = [{"t1": f32t("t1_%d" % i), "t2": f32t("t2_%d" % i),
               "qt": cx.sb(es, "qt%d" % i, [128, MTK], BF16), "kt": cx.sb(es, "kt%d" % i, [128, MTK], BF16)} for i in range(2)]
        ob = cx.sb(es, "ob", [128, MTK], BF16)
        sqo = cx.sb(es, "sqo", [128, MTK], BF16)
        mts = [cx.sb(es, "mixt%d" % i, [128, 4, MTK], BF16) for i in range(2)]
        P = [cx.ps(es, "P%d" % i, [128, 512], F32) for i in range(3)]
        pi = [0]

        def nextP():
            pi[0] += 1
            return P[pi[0] % 3]

        def rot(h, col0, tab, out, hin, cs, sn, t1, t2):
            pa = nextP()
            proj_fm(cx, pa, wb, col0 + h * 128, 128, hin, MTK)
            cx.dve(lambda e: e.tensor_tensor(t1[:], pa[:], cs[:], ALU.mult), r=[pa, cs], w=[t1])
            pb = nextP()
            proj_fm(cx, pb, wb, 1024 + col0 + h * 128, 128, hin, MTK)
            cx.dve(lambda e: e.tensor_tensor(t2[:], pb[:], sn[:], ALU.mult), r=[pb, sn], w=[t2])
            cx.dve(lambda e: e.tensor_tensor(t1[:], t1[:], t2[:], ALU.add), r=[t1, t2], w=[t1])
            cx.dve(lambda e: e.tensor_tensor(out[:], t1[:], dec[:, tab, :], ALU.mult), r=[t1, dec], w=[out])

        def macro(m, hin, mt, cs, sn):
            cx.dma(hin[:], dram_fm(hT_ap, 0, 8, m * MTK, (m + 1) * MTK), writes=[hin])
            cx.dma(cs[:], cos_ap[:, m * MTK:(m + 1) * MTK], writes=[cs])
            cx.dma(sn[:], sin_ap[:, m * MTK:(m + 1) * MTK], writes=[sn])
            for c in range(NCH):
                ps = nextP()
                for kc in range(8):
                    cx.pe(lambda e, ps=ps, kc=kc, c=c: e.matmul(
                        ps[0:64, :], hin[:, kc, c * CH:(c + 1) * CH], wb[:, kc, 2048:2560],
                        start=(kc == 0), stop=(kc == 7)), r=[hin, wb], w=[ps])
                if c % 2 == 0:
                    cx.act(lambda e, ps=ps, c=c: e.copy(v[:, c, :], ps[0:64, :]), r=[ps], w=[v])
                else:
                    cx.dve(lambda e, ps=ps, c=c: e.tensor_copy(v[:, c, :], ps[0:64, :]), r=[ps], w=[v])
            def A(h, par):
                T = TT[par]
                g = RET_GAMMA[h]
                rot(h, 0, 2 * h, T["qt"], hin, cs, sn, T["t1"], T["t2"])
                rot(h, 512, 2 * h + 1, T["kt"], hin, cs, sn, T["t1"], T["t2"])
                core.pre(par, T["qt"], T["kt"], v, h * 128, float(g ** 32))

            def B(h, par):
                T = TT[par]
                g = RET_GAMMA[h]
                core.chain(par, h, T["qt"], v, h * 128, float(g ** 32), float(g ** 64), oT)
                cx.act(lambda e: e.copy(ob[:], oT[:]), r=[oT], w=[ob])
                cx.act(lambda e: e.activation(sqo[:], oT[:], AF.Square), r=[oT], w=[sqo])
                p1 = nextP()
                cx.pe(lambda e: e.matmul(p1[:], onesb[:], ob[:], start=True, stop=True), r=[onesb, ob], w=[p1])
                p2 = nextP()
                cx.pe(lambda e: e.matmul(p2[:], onesb[:], sqo[:], start=True, stop=True), r=[onesb, sqo], w=[p2])
                cx.act(lambda e: e.activation(mean[:], p1[:], AF.Copy, scale=1.0 / 128.0), r=[p1], w=[mean])
                cx.dve(lambda e: e.tensor_tensor(var[:], mean[:], mean[:], ALU.mult), r=[mean], w=[var])
                cx.dve(lambda e: e.scalar_tensor_tensor(var[:], p2[:], 1.0 / 128.0, var[:], ALU.mult, ALU.subtract),
                       r=[p2, var], w=[var])
                cx.act(lambda e: e.activation(var[:], var[:], AF.Sqrt, bias=1e-5, scale=1.0), r=[var], w=[var])
                cx.dve(lambda e: e.reciprocal(var[:], var[:]), r=[var], w=[var])
                cx.dve(lambda e: e.tensor_tensor(oT[:], oT[:], mean[:], ALU.subtract), r=[oT, mean], w=[oT])
                cx.dve(lambda e: e.tensor_tensor(oT[:], oT[:], var[:], ALU.mult), r=[oT, var], w=[oT])
                pg = nextP()
                proj_fm(cx, pg, wb, 2560 + h * 128, 128, hin, MTK)
                cx.act(lambda e: e.activation(sgp[:], pg[:], AF.Silu), r=[pg], w=[sgp])
                cx.dve(lambda e: e.tensor_tensor(mt[:, h, :], oT[:], sgp[:], ALU.mult), r=[oT, sgp], w=[mt])

            A(0, 0)
            for h in range(4):
                if h + 1 < 4:
                    A(h + 1, (h + 1) % 2)
                B(h, h % 2)
            cx.dma(dram_fm(mixT_ap, 0, 4, m * MTK, (m + 1) * MTK), mt[:], reads=[mt], q="pool")

        for m in range(S // MTK):
            macro(m, hins[m % 2], mts[m % 2], coss[m % 2], sins[m % 2])
    cx.barrier()


def host_consts(S):
    import ml_dtypes
    c = {}
    c["ident"] = np.eye(128, dtype=np.float32).astype(ml_dtypes.bfloat16)
    m = np.arange(64)
    c["maskT"] = (m[:, None] <= m[None, :]).astype(np.float32)
    sm = np.ones((128, MTK), np.float32)
    sm[:, ::CH] = 0
    c["scanm"] = sm
    half = 64
    inv = (10000.0 ** (-np.arange(half, dtype=np.float32) / half)).astype(np.float32)
    ang = (np.arange(S, dtype=np.float32)[:, None] * inv[None, :]).astype(np.float32)
    cos = np.cos(ang).T.astype(np.float32)
    sin = np.sin(ang).T.astype(np.float32)
    c["cos"] = np.ascontiguousarray(np.concatenate([cos, cos], 0))
    c["sin"] = np.ascontiguousarray(np.concatenate([-sin, sin], 0))
    dec = np.zeros((4, 2, 128, MTK), np.float32)
    n = (np.arange(MTK) % CH).astype(np.float64)
    for h in range(4):
        g = RET_GAMMA[h]
        dec[h, 0] = (g ** (n - 31.0))[None, :]
        dec[h, 1] = (g ** (31.0 - n) * 128.0 ** -0.5)[None, :]
    c["dec"] = dec
    return c


NEG = -30000.0
NSA_PIPE = True
NSA_HOLD = True
NEG8 = NEG * 8.0


def nsa_consts(S):
    import ml_dtypes
    bf = ml_dtypes.bfloat16
    nb = S // 128
    c = {}
    tl = np.arange(128)
    n = np.arange(256)
    cm = np.full((nb, 128, 256), NEG8, np.float32)
    for i in range(nb):
        t = i * 128 + tl
        ok = (16 * n[None, :] + 31 <= t[:, None]) & (n[None, :] < S // 16 - 1)
        cm[i][ok] = 0.0
    c["cmask"] = cm.astype(bf)
    j = np.arange(64)
    fb = np.zeros((nb, 128, 64), np.float32)
    for i in range(nb):
        bt = (i * 128 + tl) // 64
        d = bt[:, None] - j[None, :]
        forced = (j[None, :] == 0) | ((d >= 0) & (d < 2))
        fb[i] = np.where(d >= 0, np.where(forced, 1.0e4, 0.0), -1.0e30)
    c["fbias"] = fb
    cs = np.arange(256) * 16
    ce = cs + 31
    ss = np.arange(64) * 64
    se = ss + 63
    ov = ((cs[:, None] <= se[None, :]) & (ce[:, None] >= ss[None, :])).astype(np.float32)
    ov[S // 16 - 1:, :] = 0
    c["ovl"] = ov.astype(bf)
    c["causal"] = np.where(tl[None, :] <= tl[:, None], 0.0, NEG8).astype(np.float32).astype(bf)
    kr = np.arange(640) - 512
    dist = tl[:, None] - kr[None, :]
    c["wmask"] = np.where((dist >= 0) & (dist < 512), 0.0, NEG8).astype(np.float32).astype(bf)
    c["rvalid"] = (tl >= 31).astype(np.float32).reshape(128, 1)
    kk = np.arange(S)
    c["blockE"] = (kk[None, :] // 64 == np.arange(64)[:, None]).astype(np.float32).astype(bf)
    return c


def stage_nsa(cx, hT_ap, w_ap, cw, mixT_ap, ident_ap, cn, S):
    NB = S // 128
    NT = S // 128
    with ExitStack() as es:
        wb = cx.sb(es, "wnsa", [128, 8, 1304], BF16)
        ksE = [cx.sb(es, "ksE%d" % g, [128, S], BF16) for g in range(2)]
        kwT = [cx.sb(es, "kwT%d" % g, [64, S], BF16) for g in range(2)]
        vsw = cx.sb(es, "vsw", [128, NT, 256], BF16)
        kcmpT = [cx.sb(es, "kcmpT%d" % g, [64, 256], BF16) for g in range(2)]
        vcmp = cx.sb(es, "vcmp", [128, 2, 2, 64], BF16)
        ident = cx.sb(es, "ident", [128, 128], BF16)
        P = [cx.ps(es, "P%d" % i, [128, 512], F32) for i in range(4)]
        TP = [cx.ps(es, "TP%d" % i, [128, 8, 128], BF16) for i in range(2)]
        PV = [cx.ps(es, "PV%d" % i, [128, 64], F32) for i in range(1)]
        IMP = cx.ps(es, "IMP", [128, 64], F32)
        cnt = {"p": 0, "tp": 0, "pv": 0, "cp": 0, "sp": 0}

        def nP():
            cnt["p"] += 1
            return P[cnt["p"] % 2]

        def nH():
            cnt["h"] = cnt.get("h", 0) + 1
            return P[2 + cnt["h"] % 2]

        def nTP():
            cnt["tp"] += 1
            return TP[cnt["tp"] % 2]

        def nPV():
            return PV[0]

        def cp(out_ap, in_ap, r, w):
            cnt["cp"] += 1
            if cnt["cp"] % 2:
                cx.act(lambda e: e.copy(out_ap, in_ap), r=r, w=w)
            else:
                cx.dve(lambda e: e.tensor_copy(out_ap, in_ap), r=r, w=w)

        with ExitStack() as es2:
            stg = [cx.sb(es2, "wstg%d" % i, [128, 1304], F32) for i in range(2)]
            load_weight_bf16(cx, wb, w_ap, 8, 1304, stg)
            cx.dma(ident[:], ident_ap, writes=[ident])
            for g in range(2):
                cx.dma(ksE[g][64:128, :], cn["blockE"], writes=[(ksE[g].key, "E")])
            cx.barrier()
            kcT = [cx.sb(es2, "kcT%d" % g, [64, S], BF16) for g in range(2)]
            vcT = [cx.sb(es2, "vcT%d" % g, [64, S], BF16) for g in range(2)]
            hins = [cx.sb(es2, "hin%d" % i, [128, 8, MTK], BF16) for i in range(2)]

            def phaseA(m, hin):
                cx.dma(hin[:], dram_fm(hT_ap, 0, 8, m * MTK, (m + 1) * MTK), writes=[hin])
                for (dst, col) in ((kcT, 512), (vcT, 640), (ksE, 768), (kwT, 896)):
                    for g in range(2):
                        ps = nP()
                        proj_fm(cx, ps, wb, col + g * 64, 64, hin, MTK)
                        cp(dst[g][0:64, m * MTK:(m + 1) * MTK], ps[0:64, :], [ps], [dst[g]])
                for j in range(4):
                    ps = nP()
                    for kc in range(8):
                        cx.pe(lambda e, ps=ps, kc=kc, j=j: e.matmul(
                            ps[:, 0:256], hin[:, kc, j * 128:(j + 1) * 128], wb[:, kc, 1024:1280],
                            start=(kc == 0), stop=(kc == 7)), r=[hin, wb], w=[ps])
                    cp(vsw[:, m * 4 + j, :], ps[:, 0:256], [ps], [vsw])

            for m in range(S // MTK):
                phaseA(m, hins[m % 2])

            w1s = cx.sb(es2, "w1s", [64, 32, 64], F32)
            w1b = cx.sb(es2, "w1b", [64, 32, 64], BF16)
            w2s = cx.sb(es2, "w2s", [64, 64], F32)
            w2b = cx.sb(es2, "w2b", [64, 64], BF16)
            poss = cx.sb(es2, "poss", [64, 32], F32)
            posb = cx.sb(es2, "posb", [64, 32], BF16)
            cb = cx.sb(es2, "cb", [64, 1], F32)
            tt = [cx.sb(es2, "gt%d" % i, [64, 256], F32) for i in range(3)]
            glb = cx.sb(es2, "glb", [64, 256], BF16)
            for g in range(2):
                cx.dve(lambda e, g=g: e.memset(kcmpT[g][:], 0.0), w=[kcmpT[g]])
            cx.dve(lambda e: e.memset(vcmp[:], 0.0), w=[vcmp])
            cx.dve(lambda e: e.memset(glb[:], 0.0), w=[glb])
            NCMP = S // 16 - 1

            def phaseB(kind, g, src):
                pos_ap, w1_ap, w2_ap = cw["pos_" + kind], cw["w1_" + kind], cw["w2_" + kind]
                if g == 0:
                    cx.dma(w1s[:], w1_ap.rearrange("(p d) o -> d p o", d=64), writes=[w1s])
                    cx.dma(w2s[:], w2_ap, writes=[w2s])
                    cx.dma(poss[:], pos_ap.rearrange("p d -> d p"), writes=[poss], allow_slow_non_contiguous=True)
                    cx.dve(lambda e: e.tensor_copy(w1b[:], w1s[:]), r=[w1s], w=[w1b])
                    cx.dve(lambda e: e.tensor_copy(w2b[:], w2s[:]), r=[w2s], w=[w2b])
                    cx.dve(lambda e: e.tensor_copy(posb[:], poss[:]), r=[poss], w=[posb])
                    pc = nPV()
                    for p in range(32):
                        cx.pe(lambda e, p=p, pc=pc: e.matmul(pc[0:64, 0:1], w1b[:, p, :], posb[:, p:p + 1],
                                                             start=(p == 0), stop=(p == 31)), r=[w1b, posb], w=[pc])
                    cx.act(lambda e, pc=pc: e.copy(cb[:], pc[0:64, 0:1]), r=[pc], w=[cb])
                ps = nP()
                x3 = src[0:64, :].rearrange("d (n s) -> d n s", s=16)
                for p in range(32):
                    n0, r_ = (0, p) if p < 16 else (1, p - 16)
                    cx.pe(lambda e, p=p, n0=n0, r_=r_, ps=ps: e.matmul(
                        ps[0:64, 0:NCMP], w1b[:, p, :], x3[:, n0:n0 + NCMP, r_],
                        start=(p == 0), stop=(p == 31)), r=[w1b, src], w=[ps])
                t0, t1_, t2_ = tt
                N = NCMP
                cx.act(lambda e, ps=ps: e.activation(t0[:, 0:N], ps[0:64, 0:N], AF.Identity, bias=cb[:], scale=1.0),
                       r=[ps, cb], w=[t0])
                cx.dve(lambda e: e.tensor_tensor(t1_[:, 0:N], t0[:, 0:N], t0[:, 0:N], ALU.mult), r=[t0], w=[t1_])
                cx.dve(lambda e: e.tensor_scalar(t1_[:, 0:N], t1_[:, 0:N], 0.044715, 1.0, ALU.mult, ALU.add), r=[t1_], w=[t1_])
                cx.dve(lambda e: e.tensor_tensor(t1_[:, 0:N], t1_[:, 0:N], t0[:, 0:N], ALU.mult), r=[t1_, t0], w=[t1_])
                cx.act(lambda e: e.activation(t2_[:, 0:N], t1_[:, 0:N], AF.Sigmoid, scale=2.0 * math.sqrt(2.0 / math.pi)),
                       r=[t1_], w=[t2_])
                cx.dve(lambda e: e.tensor_tensor(glb[:, 0:N], t0[:, 0:N], t2_[:, 0:N], ALU.mult), r=[t0, t2_], w=[glb])
                if kind == "k":
                    po = nP()
                    cx.pe(lambda e, po=po: e.matmul(po[0:64, 0:N], w2b[:], glb[:, 0:N], start=True, stop=True),
                          r=[w2b, glb], w=[po])
                    cp(kcmpT[g][:, 0:N], po[0:64, 0:N], [po], [kcmpT[g]])
                else:
                    for kc2 in range(2):
                        po = nPV()
                        n1 = min(128, N - kc2 * 128)
                        if n1 <= 0:
                            continue
                        cx.pe(lambda e, po=po, kc2=kc2, n1=n1: e.matmul(
                            po[0:n1, :], glb[:, kc2 * 128:kc2 * 128 + n1], w2b[:], start=True, stop=True),
                            r=[glb, w2b], w=[po])
                        cp(vcmp[0:n1, kc2, g, :], po[0:n1, :], [po], [vcmp])

            for kind, srcs in (("k", kcT), ("v", vcT)):
                for g in range(2):
                    phaseB(kind, g, srcs[g])
            cx.barrier()

        ovl = cx.sb(es, "ovl", [128, 2, 64], BF16)
        cx.dma(ovl[:], cn["ovl"].rearrange("(c p) j -> p c j", p=128), writes=[ovl])
        causal = cx.sb(es, "causal", [128, 128], BF16)
        cx.dma(causal[:], cn["causal"], writes=[causal])
        wmask = cx.sb(es, "wmask", [128, 640], BF16)
        cx.dma(wmask[:], cn["wmask"], writes=[wmask])
        rvalid = cx.sb(es, "rvalid", [128, 1], F32)
        cx.dma(rvalid[:], cn["rvalid"], writes=[rvalid])
        hqs = [cx.sb(es, "hq%d" % i, [128, 8, 128], BF16) for i in range(2)]
        cms = [cx.sb(es, "cm%d" % i, [128, 256], BF16) for i in range(2)]
        fbs = [cx.sb(es, "fb%d" % i, [128, 64], F32) for i in range(2)]
        qsel = [cx.sb(es, "qsel%d" % i, [128, 4, 128], BF16) for i in range(2)]
        selw = cx.sb(es, "selw", [128, 128], BF16)
        cx.dve(lambda e: e.memset(selw[:], 0.0), w=[selw])
        pcT = [cx.sb(es, "pcT%d" % i, [128, 2, 128], BF16) for i in range(4)]
        pc32 = [cx.sb(es, "pc32_%d" % i, [128, 256], F32) for i in range(2)]
        pbs = [cx.sb(es, "pb%d" % i, [128, S], BF16) for i in range(2)]
        pTs = [cx.sb(es, "pT%d" % i, [128, NT, 128], BF16) for i in range(2)]
        acc = cx.sb(es, "acc", [128, 512], F32)
        accb = cx.sb(es, "accb", [128, 512], BF16)
        mixt = [cx.sb(es, "mixt%d" % i, [128, 4, 128], BF16) for i in range(2)]
        sms = [{n_: cx.sb(es, n_ + str(i), [128, 8 if n_[0] == "c" else 1], F32)
                for n_ in ("cmax", "crs", "mx", "rs", "rinv", "fac")} for i in range(2)]
        sc64 = cx.sb(es, "sc64", [128, 64], F32)
        top8 = cx.sb(es, "top8", [128, 8], F32)
        sel01 = cx.sb(es, "sel01", [128, 64], F32)
        SCALE = 0.125

        def softmax_item(q_ap, q_r, KT, k0, nk, maskfn, vfn, gate_ap, gate_r, acc_ap, first, normalize, keepT=None, i0=False):
            cnt["sp"] += 1
            b = cnt["sp"] % 2
            sm, pb, pT = sms[b], pbs[b], pTs[b]
            cmax, crs, mx, rs, rinv, fac = sm["cmax"], sm["crs"], sm["mx"], sm["rs"], sm["rinv"], sm["fac"]
            chunks = [(c0, min(512, nk - c0)) for c0 in range(0, nk, 512)]
            ncn = len(chunks)
            dst32 = pc32[b] if normalize else None
            held = []

            def scores(ps, c0, n_):
                mm = maskfn(c0, n_)
                cx.pe(lambda e: e.matmul(ps[:, 0:n_], q_ap, KT[0:q_ap.shape[0], k0 + c0:k0 + c0 + n_],
                                         start=True, stop=(len(mm) == 0)), r=q_r + [KT], w=[ps])
                for idx, (l_ap, r_ap, lo, hi, rd) in enumerate(mm):
                    cx.pe(lambda e, l_ap=l_ap, r_ap=r_ap, lo=lo, hi=hi, idx=idx: e.matmul(
                        ps[:, lo:hi], l_ap, r_ap, start=False, stop=(idx == len(mm) - 1)), r=rd, w=[ps])

            def expo(ps, ci, c0, n_):
                out_ap = dst32[:, c0:c0 + n_] if normalize else pb[:, c0:c0 + n_]
                wr = [dst32] if normalize else [pb]
                cx.act(lambda e: e.activation(out_ap, ps[:, 0:n_], AF.Exp, bias=mx[:], scale=SCALE,
                                              accum_out=crs[:, ci:ci + 1]), r=[ps, mx], w=wr + [crs])

            def p1():
                for ci, (c0, n_) in enumerate(chunks):
                    ps = nH() if (ncn == 1 and NSA_HOLD) else nP()
                    scores(ps, c0, n_)
                    cx.dve(lambda e, ps=ps, ci=ci, n_=n_: e.reduce_max(cmax[:, ci:ci + 1], ps[:, 0:n_], AX.X),
                           r=[ps], w=[cmax])
                    if ncn == 1 and NSA_HOLD:
                        held.append(ps)
                if ncn == 1:
                    cx.dve(lambda e: e.tensor_scalar_mul(mx[:], cmax[:, 0:1], -SCALE), r=[cmax], w=[mx])
                else:
                    cx.dve(lambda e: e.tensor_reduce(mx[:], cmax[:, 0:ncn], AX.X, ALU.max), r=[cmax], w=[mx])
                    cx.dve(lambda e: e.tensor_scalar_mul(mx[:], mx[:], -SCALE), r=[mx], w=[mx])

            def p2():
                if ncn == 1 and NSA_HOLD:
                    expo(held[0], 0, chunks[0][0], chunks[0][1])
                    cx.dve(lambda e: e.reciprocal(rinv[:], crs[:, 0:1]), r=[crs], w=[rinv])
                elif ncn == 1:
                    ps = nP()
                    scores(ps, chunks[0][0], chunks[0][1])
                    expo(ps, 0, chunks[0][0], chunks[0][1])
                    cx.dve(lambda e: e.reciprocal(rinv[:], crs[:, 0:1]), r=[crs], w=[rinv])
                else:
                    for ci, (c0, n_) in enumerate(chunks):
                        ps = nP()
                        scores(ps, c0, n_)
                        expo(ps, ci, c0, n_)
                    cx.dve(lambda e: e.reduce_sum(rs[:], crs[:, 0:ncn], AX.X), r=[crs], w=[rs])
                    cx.dve(lambda e: e.reciprocal(rinv[:], rs[:]), r=[rs], w=[rinv])
                if normalize:
                    if i0:
                        cx.dve(lambda e: e.tensor_tensor(rinv[:], rinv[:], rvalid[:], ALU.mult), r=[rinv, rvalid], w=[rinv])
                    cx.dve(lambda e: e.tensor_scalar_mul(pb[:, 0:nk], dst32[:, 0:nk], rinv[:, 0:1]), r=[dst32, rinv], w=[pb])
                dstT = keepT if keepT is not None else pT
                nkt = nk // 128
                for t0 in range(0, nkt, 8):
                    n8 = min(8, nkt - t0)
                    tp = nTP()
                    for t in range(n8):
                        cx.pe(lambda e, tp=tp, t=t, t0=t0: e.transpose(tp[:, t, :], pb[:, (t0 + t) * 128:(t0 + t + 1) * 128], ident[:]),
                              r=[pb, ident], w=[tp])
                    cp(dstT[:, t0:t0 + n8, :], tp[:, 0:n8, :], [tp], [dstT])
                po = nPV()
                for t in range(nkt):
                    v_ap, v_r = vfn(t)
                    cx.pe(lambda e, po=po, t=t, v_ap=v_ap: e.matmul(po[:], dstT[:, t, :], v_ap, start=(t == 0), stop=(t == nkt - 1)),
                          r=[dstT] + v_r, w=[po])
                if normalize:
                    sc_ap, sc_r = gate_ap, [gate_r]
                else:
                    cx.dve(lambda e: e.tensor_tensor(fac[:], rinv[:], gate_ap, ALU.mult), r=[rinv, gate_r], w=[fac])
                    sc_ap, sc_r = fac[:, 0:1], [fac]
                if first:
                    cx.dve(lambda e, po=po: e.tensor_scalar_mul(acc_ap, po[:], sc_ap), r=[po] + sc_r, w=[acc])
                else:
                    cx.dve(lambda e, po=po: e.scalar_tensor_tensor(acc_ap, po[:], sc_ap, acc_ap, ALU.mult, ALU.add),
                           r=[po, acc] + sc_r, w=[acc])

            return p1, p2

        items = []

        def block(i, hq, cm, fb, mt, gates):
            nk = 128 * (i + 1)
            kt0 = max(0, i - 4)
            nkw = 128 * (i - kt0 + 1)

            def blk_pre():
                cx.dma(hq[:], dram_fm(hT_ap, 0, 8, i * 128, (i + 1) * 128), writes=[hq])
                cx.dma(cm[:], cn["cmask"][i], writes=[cm])
                cx.dma(fb[:], cn["fbias"][i], writes=[fb])
                pg = nPV()
                for kc in range(8):
                    cx.pe(lambda e, kc=kc, pg=pg: e.matmul(pg[:, 0:24], hq[:, kc, :], wb[:, kc, 1280:1304],
                                                           start=(kc == 0), stop=(kc == 7)), r=[hq, wb], w=[pg])
                cx.act(lambda e, pg=pg: e.activation(gates[:], pg[:, 0:24], AF.Sigmoid), r=[pg], w=[gates])

            def blk_post():
                cx.act(lambda e: e.copy(accb[:], acc[:]), r=[acc], w=[accb])
                tp = nTP()
                for c in range(4):
                    cx.pe(lambda e, c=c, tp=tp: e.transpose(tp[:, c, :], accb[:, c * 128:(c + 1) * 128], ident[:]),
                          r=[accb, ident], w=[tp])
                cp(mt[:], tp[:, 0:4, :], [tp], [mt])
                cx.dma(dram_fm(mixT_ap, 4, 8, i * 128, (i + 1) * 128), mt[:], reads=[mt], q="pool")

            def group(g):
                qs = qsel[g]
                qk = (qs.key, "q")
                sk = (qs.key, "s")

                def q_pre():
                    for hp in range(4):
                        hd = g * 4 + hp
                        ps = nP()
                        proj_fm(cx, ps, wb, hd * 64, 64, hq, 128)
                        cp(qs[0:64, hp, :], ps[0:64, 0:128], [ps], [qk])

                def sel_pre():
                    for hp in range(4):
                        for kc2 in range(2):
                            cx.pe(lambda e, hp=hp, kc2=kc2: e.matmul(IMP[:], pcT[hp][:, kc2, :], ovl[:, kc2, :],
                                                                     start=(hp == 0 and kc2 == 0), stop=(hp == 3 and kc2 == 1)),
                                  r=[pcT[hp], ovl], w=[IMP])
                    cx.dve(lambda e: e.tensor_tensor(sc64[:], IMP[:], fb[:], ALU.add), r=[IMP, fb], w=[sc64])
                    cx.dve(lambda e: e.max(top8[:], sc64[:]), r=[sc64], w=[top8])
                    cx.dve(lambda e: e.tensor_scalar(sel01[:], sc64[:], top8[:, 7:8], None, ALU.is_ge), r=[sc64, top8], w=[sel01])
                    cx.dve(lambda e: e.tensor_scalar(selw[:, 64:128], sel01[:], -NEG8, NEG8, ALU.mult, ALU.add), r=[sel01], w=[selw])
                    tps = nTP()
                    cx.pe(lambda e: e.transpose(tps[:, 0, :], selw[:], ident[:]), r=[selw, ident], w=[tps])
                    cx.act(lambda e: e.copy(qs[64:128, :, :], tps[64:128, 0:1, :].to_broadcast([64, 4, 128])),
                           r=[tps], w=[sk])

                def mk_cmp(hp):
                    hd = g * 4 + hp
                    return lambda: softmax_item(
                        qs[0:64, hp, :], [qk], kcmpT[g], 0, 256,
                        lambda c0, n_: [(ident[:], cm[:, c0:c0 + n_], 0, n_, [ident, cm])],
                        lambda t: (vcmp[:, t, g, :], [vcmp]),
                        gates[:, hd:hd + 1], gates, acc[:, hd * 64:(hd + 1) * 64], True, True, keepT=pcT[hp], i0=(i == 0))

                def mk_win(hp):
                    hd = g * 4 + hp
                    return lambda: softmax_item(
                        qs[0:64, hp, :], [qk], kwT[g], kt0 * 128, nkw,
                        lambda c0, n_: [(ident[:], wmask[:, 640 - nkw + c0:640 - nkw + c0 + n_], 0, n_, [ident, wmask])],
                        lambda t: (vsw[:, kt0 + t, 128 + g * 64:128 + (g + 1) * 64], [vsw]),
                        gates[:, 16 + hd:17 + hd], gates, acc[:, hd * 64:(hd + 1) * 64], False, False)

                def mk_slc(hp):
                    hd = g * 4 + hp
                    return lambda: softmax_item(
                        qs[:, hp, :], [qk, sk], ksE[g], 0, nk,
                        lambda c0, n_: ([(ident[:], causal[:], n_ - 128, n_, [ident, causal])] if c0 + n_ == nk else []),
                        lambda t: (vsw[:, t, g * 64:(g + 1) * 64], [vsw]),
                        gates[:, 8 + hd:9 + hd], gates, acc[:, hd * 64:(hd + 1) * 64], False, False)

                lst = []
                for hp in range(4):
                    lst.append([q_pre if hp == 0 else None, mk_cmp(hp), None])
                for hp in range(4):
                    lst.append([sel_pre if hp == 1 else None, mk_win(hp), None])
                for hp in range(4):
                    lst.append([None, mk_slc(hp), None])
                return lst

            lst = group(0) + group(1)
            first_pre = lst[0][0]
            lst[0][0] = lambda: (blk_pre(), first_pre())
            lst[-1][2] = blk_post
            items.extend(lst)

        gates2 = [cx.sb(es, "gates%d" % i_, [128, 24], F32) for i_ in range(2)]
        for i in range(NB):
            block(i, hqs[i % 2], cms[i % 2], fbs[i % 2], mixt[i % 2], gates2[i % 2])
        prev = None
        for pre, mk, post in items:
            if pre is not None:
                pre()
            p1, p2 = mk()
            p1()
            if not NSA_PIPE:
                p2()
                if post is not None:
                    post()
                continue
            if prev is not None:
                prev[0]()
                if prev[1] is not None:
                    prev[1]()
            prev = (p2, post)
        if NSA_PIPE:
            prev[0]()
            if prev[1] is not None:
                prev[1]()
    cx.barrier()


def nsa2_consts(S):
    import ml_dtypes
    bf = ml_dtypes.bfloat16
    nb = S // 128
    base = nsa_consts(S)
    c = {"fbias": base["fbias"], "blockE": base["blockE"]}
    cm = base["cmask"].astype(np.float32)
    cmT = cm.transpose(0, 2, 1).reshape(nb, 2, 128, 128)
    cmT = np.broadcast_to(cmT.transpose(0, 2, 1, 3)[:, :, :, None, :], (nb, 128, 2, 4, 128))
    c["cmaskT"] = np.ascontiguousarray(cmT).astype(bf)
    kl = np.arange(128)[:, None]
    ql = np.arange(128)[None, :]
    cz = np.where(kl <= ql, 0.0, NEG8).astype(np.float32)
    w0 = np.where(kl > ql, 0.0, NEG8).astype(np.float32)
    c["causalT4"] = np.ascontiguousarray(np.broadcast_to(cz[:, None, :], (128, 4, 128))).astype(bf)
    c["wm0T4"] = np.ascontiguousarray(np.broadcast_to(w0[:, None, :], (128, 4, 128))).astype(bf)
    ov = np.ones((256, 80), np.float32)
    ov[:, 0:64] = base["ovl"].astype(np.float32)
    c["ovla"] = ov.astype(bf)
    sr = np.zeros((24, 24, 64), np.float32)
    for r in range(24):
        sr[r, r, :] = 1.0
    c["selrows"] = sr
    return c


NSA2_STOP = ""


def stage_nsa2(cx, hT_ap, w_ap, cw, mixT_ap, ident_ap, cn, S):
    NB = S // 128
    NT = S // 128
    with ExitStack() as es:
        wb = cx.sb(es, "wnsa", [128, 8, 1312], BF16)
        cx.dve(lambda e: e.memset(wb[:, :, 1304:1312], 0.0), w=[(wb.key, "pad")])
        ksE = [cx.sb(es, "ksE%d" % g, [128, S], BF16) for g in range(2)]
        kwT = [cx.sb(es, "kwT%d" % g, [64, S], BF16) for g in range(2)]
        vaug = cx.sb(es, "vaug", [128, NT, 4, 80], BF16)
        kcmpT = [cx.sb(es, "kcmpT%d" % g, [64, 256], BF16) for g in range(2)]
        vcmp = cx.sb(es, "vcmp", [128, 2, 2, 80], BF16)
        ident = cx.sb(es, "ident", [128, 128], BF16)
        onesb = cx.sb(es, "onesb", [128, 128], BF16)
        ones32 = cx.sb(es, "ones32", [128, 64], F32)
        kmx = cx.sb(es, "kmx", [128, 8], F32)
        P = [cx.ps(es, "P%d" % i, [128, 512], F32) for i in range(3)]
        OTs = [cx.ps(es, "OT%d" % i, [128, 512], F32) for i in range(2)]
        RP = cx.ps(es, "RP", [128, 4, 128], F32)
        M1 = cx.ps(es, "M1", [128, 512], F32)
        M2 = cx.ps(es, "M2", [128, 8, 128], BF16)
        cnt = {"p": 0, "cp": 0, "o": 0, "pt": 0}

        def nP():
            cnt["p"] += 1
            return P[cnt["p"] % 3]

        def nO():
            cnt["o"] += 1
            return OTs[cnt["o"] % 2]

        def cp(out_ap, in_ap, r, w):
            cnt["cp"] += 1
            if cnt["cp"] % 2:
                cx.act(lambda e: e.copy(out_ap, in_ap), r=r, w=w)
            else:
                cx.dve(lambda e: e.tensor_copy(out_ap, in_ap), r=r, w=w)

        cx.dve(lambda e: e.memset(onesb[:], 1.0), w=[onesb])
        cx.dve(lambda e: e.memset(ones32[:], 1.0), w=[ones32])
        cx.dve(lambda e: e.memset(vaug[:], 1.0), w=[vaug])
        with ExitStack() as es2:
            stg = [cx.sb(es2, "wstg%d" % i, [128, 1304], F32) for i in range(2)]
            load_weight_bf16(cx, wb, w_ap, 8, 1304, stg)
            cx.dma(ident[:], ident_ap, writes=[ident])
            for g in range(2):
                cx.dma(ksE[g][64:128, :], cn["blockE"], writes=[(ksE[g].key, "E")])
            cx.barrier()
            kcT = [cx.sb(es2, "kcT%d" % g, [64, S], BF16) for g in range(2)]
            vcT = [cx.sb(es2, "vcT%d" % g, [64, S], BF16) for g in range(2)]
            hins = [cx.sb(es2, "hin%d" % i, [128, 8, MTK], BF16) for i in range(2)]

            def phaseA(m, hin):
                cx.dma(hin[:], dram_fm(hT_ap, 0, 8, m * MTK, (m + 1) * MTK), writes=[hin])
                for (dst, col) in ((kcT, 512), (vcT, 640), (ksE, 768), (kwT, 896)):
                    for g in range(2):
                        ps = nP()
                        proj_fm(cx, ps, wb, col + g * 64, 64, hin, MTK)
                        cp(dst[g][0:64, m * MTK:(m + 1) * MTK], ps[0:64, :], [ps], [dst[g]])
                for j in range(4):
                    ps = nP()
                    for kc in range(8):
                        cx.pe(lambda e, ps=ps, kc=kc, j=j: e.matmul(
                            ps[:, 0:256], hin[:, kc, j * 128:(j + 1) * 128], wb[:, kc, 1024:1280],
                            start=(kc == 0), stop=(kc == 7)), r=[hin, wb], w=[ps])
                    cp(vaug[:, m * 4 + j, :, 0:64], ps[:, 0:256].rearrange("p (v d) -> p v d", d=64), [ps], [vaug])

            for m in range(S // MTK):
                phaseA(m, hins[m % 2])

            w1s = cx.sb(es2, "w1s", [64, 32, 64], F32)
            w1b = cx.sb(es2, "w1b", [64, 32, 64], BF16)
            w2s = cx.sb(es2, "w2s", [64, 64], F32)
            w2b = cx.sb(es2, "w2b", [64, 64], BF16)
            poss = cx.sb(es2, "poss", [64, 32], F32)
            posb = cx.sb(es2, "posb", [64, 32], BF16)
            cb = cx.sb(es2, "cb", [64, 1], F32)
            tt = [cx.sb(es2, "gt%d" % i, [64, 256], F32) for i in range(3)]
            glb = cx.sb(es2, "glb", [64, 256], BF16)
            for g in range(2):
                cx.dve(lambda e, g=g: e.memset(kcmpT[g][:], 0.0), w=[kcmpT[g]])
            cx.dve(lambda e: e.memset(vcmp[:], 0.0), w=[vcmp])
            cx.dve(lambda e: e.memset(vcmp[:, :, :, 64:80], 1.0), r=[vcmp], w=[vcmp])
            cx.dve(lambda e: e.memset(glb[:], 0.0), w=[glb])
            NCMP = S // 16 - 1

            def phaseB(kind, g, src):
                pos_ap, w1_ap, w2_ap = cw["pos_" + kind], cw["w1_" + kind], cw["w2_" + kind]
                if g == 0:
                    cx.dma(w1s[:], w1_ap.rearrange("(p d) o -> d p o", d=64), writes=[w1s])
                    cx.dma(w2s[:], w2_ap, writes=[w2s])
                    cx.dma(poss[:], pos_ap.rearrange("p d -> d p"), writes=[poss], allow_slow_non_contiguous=True)
                    cx.dve(lambda e: e.tensor_copy(w1b[:], w1s[:]), r=[w1s], w=[w1b])
                    cx.dve(lambda e: e.tensor_copy(w2b[:], w2s[:]), r=[w2s], w=[w2b])
                    cx.dve(lambda e: e.tensor_copy(posb[:], poss[:]), r=[poss], w=[posb])
                    for p in range(32):
                        cx.pe(lambda e, p=p: e.matmul(M1[0:64, 0:1], w1b[:, p, :], posb[:, p:p + 1],
                                                      start=(p == 0), stop=(p == 31)), r=[w1b, posb], w=[M1])
                    cx.act(lambda e: e.copy(cb[:], M1[0:64, 0:1]), r=[M1], w=[cb])
                ps = nP()
                x3 = src[0:64, :].rearrange("d (n s) -> d n s", s=16)
                for p in range(32):
                    n0, r_ = (0, p) if p < 16 else (1, p - 16)
                    cx.pe(lambda e, p=p, n0=n0, r_=r_, ps=ps: e.matmul(
                        ps[0:64, 0:NCMP], w1b[:, p, :], x3[:, n0:n0 + NCMP, r_],
                        start=(p == 0), stop=(p == 31)), r=[w1b, src], w=[ps])
                t0, t1_, t2_ = tt
                N = NCMP
                cx.act(lambda e, ps=ps: e.activation(t0[:, 0:N], ps[0:64, 0:N], AF.Identity, bias=cb[:], scale=1.0),
                       r=[ps, cb], w=[t0])
                cx.dve(lambda e: e.tensor_tensor(t1_[:, 0:N], t0[:, 0:N], t0[:, 0:N], ALU.mult), r=[t0], w=[t1_])
                cx.dve(lambda e: e.tensor_scalar(t1_[:, 0:N], t1_[:, 0:N], 0.044715, 1.0, ALU.mult, ALU.add), r=[t1_], w=[t1_])
                cx.dve(lambda e: e.tensor_tensor(t1_[:, 0:N], t1_[:, 0:N], t0[:, 0:N], ALU.mult), r=[t1_, t0], w=[t1_])
                cx.act(lambda e: e.activation(t2_[:, 0:N], t1_[:, 0:N], AF.Sigmoid, scale=2.0 * math.sqrt(2.0 / math.pi)),
                       r=[t1_], w=[t2_])
                cx.dve(lambda e: e.tensor_tensor(glb[:, 0:N], t0[:, 0:N], t2_[:, 0:N], ALU.mult), r=[t0, t2_], w=[glb])
                if kind == "k":
                    po = nP()
                    cx.pe(lambda e, po=po: e.matmul(po[0:64, 0:N], w2b[:], glb[:, 0:N], start=True, stop=True),
                          r=[w2b, glb], w=[po])
                    cp(kcmpT[g][:, 0:N], po[0:64, 0:N], [po], [kcmpT[g]])
                else:
                    for kc2 in range(2):
                        n1 = min(128, N - kc2 * 128)
                        if n1 <= 0:
                            continue
                        po = nP()
                        cx.pe(lambda e, po=po, kc2=kc2, n1=n1: e.matmul(
                            po[0:n1, 0:64], glb[:, kc2 * 128:kc2 * 128 + n1], w2b[:], start=True, stop=True),
                            r=[glb, w2b], w=[po])
                        cp(vcmp[0:n1, kc2, g, 0:64], po[0:n1, 0:64], [po], [vcmp])

            for kind, srcs in (("k", kcT), ("v", vcT)):
                for g in range(2):
                    phaseB(kind, g, srcs[g])

            sqk = [cx.sb(es2, "sqk%d" % i, [64, 512], BF16) for i in range(2)]
            kcm = cx.sb(es2, "kcm", [128, 8], F32)
            qi = [0]

            def kmax(src, ncols, col):
                nchunk = (ncols + 511) // 512
                for ci in range(nchunk):
                    c0 = ci * 512
                    n_ = min(512, ncols - c0)
                    sq = sqk[qi[0] % 2]
                    qi[0] += 1
                    cx.act(lambda e, sq=sq, c0=c0, n_=n_: e.activation(sq[:, 0:n_], src[0:64, c0:c0 + n_], AF.Square),
                           r=[src], w=[sq])
                    ps = nP()
                    cx.pe(lambda e, ps=ps, sq=sq, n_=n_: e.matmul(ps[:, 0:n_], onesb[0:64, :], sq[:, 0:n_], start=True, stop=True),
                          r=[onesb, sq], w=[ps])
                    cx.dve(lambda e, ps=ps, ci=ci, n_=n_: e.reduce_max(kcm[:, ci:ci + 1], ps[:, 0:n_], AX.X), r=[ps], w=[kcm])
                cx.dve(lambda e: e.tensor_reduce(kmx[:, col:col + 1], kcm[:, 0:nchunk], AX.X, ALU.max), r=[kcm], w=[kmx])

            for g in range(2):
                kmax(kcmpT[g], 256, 0 + g)
                kmax(kwT[g], S, 2 + g)
                kmax(ksE[g], S, 4 + g)
            cx.barrier()

        def cload(name, shape, dtype, src):
            t = cx.sb(es, name, shape, dtype)
            cx.dma(t[:], src, writes=[t])
            return t

        ovla = cload("ovla", [128, 2, 80], BF16, cn["ovla"].rearrange("(c p) j -> p c j", p=128))
        causalT4 = cload("causalT4", [128, 4, 128], BF16, cn["causalT4"])
        wm0T4 = cload("wm0T4", [128, 4, 128], BF16, cn["wm0T4"])
        selrows = cload("selrows", [24, 24, 64], F32, cn["selrows"])
        hqs = [cx.sb(es, "hq%d" % i, [128, 8, 128], BF16) for i in range(2)]
        cmTs = [cx.sb(es, "cmT%d" % i, [128, 2, 4, 128], BF16) for i in range(2)]
        fbs = [cx.sb(es, "fb%d" % i, [128, 64], F32) for i in range(2)]
        gatesT = [cx.sb(es, "gatesT%d" % i, [32, 128], F32) for i in range(2)]
        qsel = [cx.sb(es, "qsel%d" % i, [128, 4, 128], BF16) for i in range(2)]
        sqqs = [cx.sb(es, "sqq%d" % i, [64, 4, 128], BF16) for i in range(2)]
        qms = [cx.sb(es, "qm%d" % i, [128, 1], F32) for i in range(2)]
        negcs = [[cx.sb(es, "negc%d_%d" % (g_, i), [128, 1], F32) for i in range(3)] for g_ in range(2)]
        pending = []

        def flush():
            while pending:
                pending.pop(0)()
        selw = cx.sb(es, "selw", [128, 128], BF16)
        cx.dve(lambda e: e.memset(selw[:], 0.0), w=[selw])
        PcT = cx.sb(es, "PcT", [128, 2, 512], BF16)
        NPT = 6
        PTs = [cx.sb(es, "PT%d" % i, [128, 512], BF16) for i in range(NPT)]
        rsr = cx.sb(es, "rsr", [128, 512], F32)
        bcs = cx.sb(es, "bcs", [64, 512], F32)
        tmpo = cx.sb(es, "tmpo", [64, 512], F32)
        accT = [cx.sb(es, "accT%d" % i, [64, 8, 128], F32) for i in range(2)]
        accTb = [cx.sb(es, "accTb%d" % i, [64, 8, 128], BF16) for i in range(2)]
        rs4 = cx.sb(es, "rs4", [128, 4], F32)
        impb = cx.sb(es, "impb", [128, 64], F32)
        sc64 = cx.sb(es, "sc64", [128, 64], F32)
        top8 = cx.sb(es, "top8", [128, 8], F32)
        sel01 = cx.sb(es, "sel01", [128, 64], F32)
        SCALE = 0.125
        mix_dst = mixT_ap[4:8].rearrange("c (two d) s -> d (c two) s", two=2)

        def block(i, hq, cmT, fb, gT, acc, accb):
            kt0 = max(0, i - 4)
            cx.dma(hq[:], dram_fm(hT_ap, 0, 8, i * 128, (i + 1) * 128), writes=[hq])
            cx.dma(cmT[:], cn["cmaskT"][i], writes=[cmT])
            cx.dma(fb[:], cn["fbias"][i], writes=[fb])
            for kc in range(8):
                cx.pe(lambda e, kc=kc: e.matmul(M1[0:32, 0:128], wb[:, kc, 1280:1312], hq[:, kc, :],
                                                start=(kc == 0), stop=(kc == 7)), r=[hq, wb], w=[M1])
            cx.act(lambda e: e.activation(gT[:], M1[0:32, 0:128], AF.Sigmoid), r=[M1], w=[gT])

            def finalize(OT, g, br, first):
                accg = acc[:, g * 4:(g + 1) * 4, :]
                cx.dve(lambda e: e.tensor_scalar_max(rsr[64:65, :], OT[64:65, :], 1e-30), r=[OT], w=[rsr])
                cx.dve(lambda e: e.reciprocal(rsr[64:65, :], rsr[64:65, :]), r=[rsr], w=[rsr])
                cx.pe(lambda e: e.matmul(M1[0:64, :], ones32[64:65, 0:64], rsr[64:65, :], start=True, stop=True),
                      r=[ones32, rsr], w=[M1])
                cx.act(lambda e: e.copy(bcs[:], M1[0:64, :]), r=[M1], w=[bcs])
                cx.dve(lambda e: e.tensor_tensor(tmpo[:], OT[0:64, :], bcs[:], ALU.mult), r=[OT, bcs], w=[tmpo])
                for hp in range(4):
                    r_ = br * 8 + g * 4 + hp
                    cx.pe(lambda e, hp=hp, r_=r_: e.matmul(M1[0:64, hp * 128:(hp + 1) * 128], selrows[:, r_, :], gT[0:24, :],
                                                           start=True, stop=True), r=[selrows, gT], w=[M1])
                t3 = tmpo[:].rearrange("p (h q) -> p h q", q=128)
                m3 = M1[0:64, :].rearrange("p (h q) -> p h q", q=128)
                if first:
                    cx.dve(lambda e: e.tensor_tensor(accg, t3, m3, ALU.mult), r=[tmpo, M1], w=[acc])
                else:
                    cx.dve(lambda e: e.tensor_tensor(t3, t3, m3, ALU.mult), r=[tmpo, M1], w=[tmpo])
                    cx.dve(lambda e: e.tensor_tensor(accg, accg, t3, ALU.add), r=[acc, tmpo], w=[acc])

            def run_tiles(tiles, qrows, ncg, vfn, mfn, qsel_key):
                OT = nO()
                pend = None
                n = len(tiles)
                for t, (l_ap, l_r) in enumerate(tiles):
                    ps = nP()
                    mm = mfn(t)
                    cx.pe(lambda e, ps=ps, l_ap=l_ap, stp=(mm is None): e.matmul(ps[:], l_ap, qrows, start=True, stop=stp),
                          r=l_r + [qsel_key], w=[ps])
                    if mm is not None:
                        cx.pe(lambda e, ps=ps, mm=mm: e.matmul(ps[:], ident[:], mm[0], start=False, stop=True),
                              r=[ident] + mm[1], w=[ps])
                    cnt["pt"] += 1
                    pt = PTs[cnt["pt"] % NPT]
                    cx.act(lambda e, ps=ps, pt=pt: e.activation(pt[:], ps[:], AF.Exp, bias=ncg[:], scale=SCALE),
                           r=[ps, ncg], w=[pt])
                    if pend is not None:
                        pend()
                    v_ap, v_r = vfn(t)
                    pend = (lambda t=t, pt=pt, v_ap=v_ap, v_r=v_r: cx.pe(
                        lambda e: e.matmul(OT[0:80, :], v_ap[:, 0:80], pt[:], start=(t == 0), stop=(t == n - 1)),
                        r=[pt] + v_r, w=[OT]))
                pend()
                return OT

            def prelude(g):
                qs = qsel[g]
                qk = (qs.key, "q")
                sqq, qm, negc = sqqs[g], qms[g], negcs[g]
                for hp in range(4):
                    hd = g * 4 + hp
                    for kc in range(8):
                        cx.pe(lambda e, kc=kc, hp=hp, hd=hd: e.matmul(M1[0:64, hp * 128:(hp + 1) * 128], wb[:, kc, hd * 64:(hd + 1) * 64],
                                                                     hq[:, kc, :], start=(kc == 0), stop=(kc == 7)),
                              r=[wb, hq], w=[M1])
                cx.dve(lambda e: e.tensor_copy(qs[0:64, :, :].rearrange("p h q -> p (h q)"), M1[0:64, :]), r=[M1], w=[qk])
                cx.act(lambda e: e.activation(sqq[:].rearrange("p h q -> p (h q)"), M1[0:64, :], AF.Square), r=[M1], w=[sqq])
                ps = nP()
                cx.pe(lambda e: e.matmul(ps[:], onesb[0:64, :], sqq[:].rearrange("p h q -> p (h q)"), start=True, stop=True),
                      r=[onesb, sqq], w=[ps])
                cx.dve(lambda e: e.reduce_max(qm[:], ps[:], AX.X), r=[ps], w=[qm])
                for br in range(3):
                    nb_ = negc[br]
                    cx.dve(lambda e, nb_=nb_, br=br: e.tensor_tensor(nb_[:], qm[:], kmx[:, 2 * br + g:2 * br + g + 1], ALU.mult),
                           r=[qm, kmx], w=[nb_])
                    cx.act(lambda e, nb_=nb_: e.activation(nb_[:], nb_[:], AF.Sqrt), r=[nb_], w=[nb_])
                    cx.dve(lambda e, nb_=nb_: e.tensor_scalar_mul(nb_[:], nb_[:], -SCALE * 1.05), r=[nb_], w=[nb_])

            def group(g):
                qs = qsel[g]
                qk = (qs.key, "q")
                sk = (qs.key, "s")
                negc = negcs[g]
                global_q = qs[0:64, :, :].rearrange("p h q -> p (h q)")
                OTc = nO()
                for kc2 in range(2):
                    ps = nP()
                    cx.pe(lambda e, ps=ps, kc2=kc2: e.matmul(ps[:], kcmpT[g][:, kc2 * 128:(kc2 + 1) * 128], global_q,
                                                            start=True, stop=False), r=[kcmpT[g], qk], w=[ps])
                    cx.pe(lambda e, ps=ps, kc2=kc2: e.matmul(ps[:], ident[:], cmT[:, kc2, :, :].rearrange("p h q -> p (h q)"),
                                                            start=False, stop=True), r=[ident, cmT], w=[ps])
                    cx.act(lambda e, ps=ps, kc2=kc2: e.activation(PcT[:, kc2, :], ps[:], AF.Exp, bias=negc[0][:], scale=SCALE),
                           r=[ps, negc[0]], w=[PcT])
                for kc2 in range(2):
                    cx.pe(lambda e, kc2=kc2: e.matmul(OTc[0:80, :], vcmp[:, kc2, g, 0:80], PcT[:, kc2, :],
                                                      start=(kc2 == 0), stop=(kc2 == 1)), r=[vcmp, PcT], w=[OTc])
                for hp in range(4):
                    for kc2 in range(2):
                        cx.pe(lambda e, hp=hp, kc2=kc2: e.matmul(RP[:, hp, 0:80], PcT[:, kc2, hp * 128:(hp + 1) * 128], ovla[:, kc2, :],
                                                                 start=(kc2 == 0), stop=(kc2 == 1)), r=[PcT, ovla], w=[RP])
                cx.dve(lambda e: e.tensor_scalar_max(rs4[:], RP[:, :, 64], 1e-30), r=[RP], w=[rs4])
                cx.dve(lambda e: e.reciprocal(rs4[:], rs4[:]), r=[rs4], w=[rs4])
                cx.dve(lambda e: e.scalar_tensor_tensor(impb[:], RP[:, 0, 0:64], rs4[:, 0:1], fb[:], ALU.mult, ALU.add),
                       r=[RP, rs4, fb], w=[impb])
                for hp in range(1, 4):
                    cx.dve(lambda e, hp=hp: e.scalar_tensor_tensor(impb[:], RP[:, hp, 0:64], rs4[:, hp:hp + 1], impb[:], ALU.mult, ALU.add),
                           r=[RP, rs4, impb], w=[impb])
                cx.dve(lambda e: e.max(top8[:], impb[:]), r=[impb], w=[top8])
                cx.dve(lambda e: e.tensor_scalar(sel01[:], impb[:], top8[:, 7:8], None, ALU.is_ge), r=[impb, top8], w=[sel01])
                cx.dve(lambda e: e.tensor_scalar(selw[:, 64:128], sel01[:], -NEG8, NEG8, ALU.mult, ALU.add), r=[sel01], w=[selw])
                flush()
                wt = list(range(kt0, i + 1))

                def wmask(t):
                    r_ = wt[t] - (i - 4)
                    if r_ == 0:
                        return (wm0T4[:].rearrange("p h q -> p (h q)"), [wm0T4])
                    if r_ == 4:
                        return (causalT4[:].rearrange("p h q -> p (h q)"), [causalT4])
                    return None

                OTw = run_tiles([(kwT[g][:, kt * 128:(kt + 1) * 128], [kwT[g]]) for kt in wt], global_q, negc[1],
                                lambda t: (vaug[:, wt[t], 2 + g, :], [vaug]), wmask, qk)
                cx.pe(lambda e: e.transpose(M2[:, 0, :], selw[:], ident[:]), r=[selw, ident], w=[M2])
                cx.act(lambda e: e.copy(qs[64:128, :, :], M2[64:128, 0:1, :].to_broadcast([64, 4, 128])), r=[M2], w=[sk])
                finalize(OTc, g, 0, True)
                qfull = qs[:, :, :].rearrange("p h q -> p (h q)")
                OTs = run_tiles([(ksE[g][:, kt * 128:(kt + 1) * 128], [ksE[g], (ksE[g].key, "E"), qk]) for kt in range(i + 1)],
                                qfull, negc[2], lambda t: (vaug[:, t, g, :], [vaug]),
                                lambda t: ((causalT4[:].rearrange("p h q -> p (h q)"), [causalT4]) if t == i else None), sk)
                finalize(OTw, g, 2, False)
                pending.append(lambda: finalize(OTs, g, 1, False))

            def blk_post():
                cx.act(lambda e: e.copy(accb[:], acc[:]), r=[acc], w=[accb])
                cx.dma(mix_dst[:, :, i * 128:(i + 1) * 128], accb[:], reads=[accb], q="pool")

            prelude(0)
            prelude(1)
            group(0)
            group(1)
            pending.append(blk_post)

        for i in range(NB if NSA2_STOP != "AB" else 0):
            block(i, hqs[i % 2], cmTs[i % 2], fbs[i % 2], gatesT[i % 2], accT[i % 2], accTb[i % 2])
        flush()
    cx.barrier()


SEQ = 4096
NCORES = 8
DEPTH = 4


def build_full(S=SEQ, depth=DEPTH):
    nc = bass.Bass("TRN2", target_bir_lowering=False)

    def din(n, s, d=F32):
        return nc.dram_tensor(n, list(s), d, kind="ExternalInput").ap()

    x = din("x", [S, D])
    norm_mix_g = din("norm_mix_g", [4, D])
    norm_ffn_g = din("norm_ffn_g", [4, D])
    final_norm_g = din("final_norm_g", [D])
    w_ret = din("w_ret", [2, D, 3072])
    w_nsa = din("w_nsa", [2, D, 1304])
    even_w_out = din("even_w_out", [2, D, D])
    cws = {}
    for kind in "kv":
        cws["pos_" + kind] = din("cmp_pos_" + kind, [2, 32, 64])
        cws["w1_" + kind] = din("cmp_w1_" + kind, [2, 2048, 64])
        cws["w2_" + kind] = din("cmp_w2_" + kind, [2, 64, 64])
    odd_w_in = din("odd_w_in", [2, D, 4096])
    odd_w_out = din("odd_w_out", [2, D, D])
    hgrn_norm_g = din("hgrn_norm_g", [2, 128])
    hgrn_lb = din("hgrn_lb_logits", [2, 1024])
    ffn_w1 = din("ffn_w1", [4, D, DFF])
    ffn_w3 = din("ffn_w3", [4, D, DFF])
    ffn_w2 = din("ffn_w2", [4, DFF, D])
    ident = din("c_ident", [128, 128], BF16)
    maskT = din("c_maskT", [64, 64])
    scanm = din("c_scanm", [128, MTK])
    cos = din("c_cos", [128, S])
    sin = din("c_sin", [128, S])
    dec = din("c_dec", [4, 2, 128, MTK])
    NB = S // 128
    cn = {
        "cmaskT": din("c_cmaskT", [NB, 128, 2, 4, 128], BF16),
        "fbias": din("c_fbias", [NB, 128, 64]),
        "blockE": din("c_blockE", [64, S], BF16),
        "causalT4": din("c_causalT4", [128, 4, 128], BF16),
        "wm0T4": din("c_wm0T4", [128, 4, 128], BF16),
        "ovla": din("c_ovla", [256, 80], BF16),
        "selrows": din("c_selrows", [24, 24, 64]),
    }
    y = nc.dram_tensor("y", [S, D], F32, kind="ExternalOutput").ap()
    xs = nc.dram_tensor("xs", [S, D], F32).ap()
    hTa = nc.dram_tensor("hTa", [8, 128, S], BF16).ap()
    hTb = nc.dram_tensor("hTb", [8, 128, S], BF16).ap()
    mixT = nc.dram_tensor("mixT", [8, 128, S], BF16).ap()

    cx = Ctx(nc)
    stage_norm0(cx, x, hTb, norm_mix_g[0], ident, S)
    for layer in range(depth):
        j = layer // 2
        if layer % 2 == 0:
            stage_ret(cx, hTb, w_ret[j], cos, sin, dec, mixT, ident, maskT, S)
            cw = {k_: v_[j] for k_, v_ in cws.items()}
            stage_nsa2(cx, hTb, w_nsa[j], cw, mixT, ident, cn, S)
            w_out = even_w_out[j]
        else:
            stage_hgrn(cx, hTb, odd_w_in[j], hgrn_norm_g[j], hgrn_lb, mixT, ident, maskT, scanm, S, j)
            w_out = odd_w_out[j]
        stage_out(cx, mixT, w_out, x if layer == 0 else xs, xs, hTa, norm_ffn_g[layer], ident, S)
        last = layer == depth - 1
        stage_ffn(cx, hTa, ffn_w1[layer], ffn_w3[layer], ffn_w2[layer], xs, xs, hTb,
                  norm_mix_g[min(layer + 1, 3)], ident, S, last, gfin_ap=final_norm_g, y_ap=y)
    cx.emit()
    return nc


def host_layout(inputs, S=SEQ):
    f32 = lambda a: np.ascontiguousarray(np.asarray(a, dtype=np.float32))
    ew = f32(inputs["even_w_in"])

    def swap(w):
        return w.reshape(w.shape[0], D, 4, 2, 64)[:, :, :, ::-1, :].reshape(w.shape[0], D, 512)

    rq, rk, rv, rg = ew[:, :, 0:512], ew[:, :, 512:1024], ew[:, :, 1024:1536], ew[:, :, 1536:2048]
    nq = ew[:, :, 2048:2560]
    kc, vc, ks, vs, kw, vw = [ew[:, :, 2560 + 128 * i:2560 + 128 * (i + 1)] for i in range(6)]
    ng = ew[:, :, 3328:3352]
    shared = {
        "w_ret": np.ascontiguousarray(np.concatenate([rq, rk, swap(rq), swap(rk), rv, rg], axis=2)),
        "w_nsa": np.ascontiguousarray(np.concatenate([nq, kc, vc, ks, kw, vs, vw, ng], axis=2)),
    }
    for k_ in ("norm_mix_g", "norm_ffn_g", "final_norm_g", "even_w_out", "cmp_pos_k", "cmp_w1_k", "cmp_w2_k",
               "cmp_pos_v", "cmp_w1_v", "cmp_w2_v", "odd_w_in", "odd_w_out", "hgrn_norm_g", "hgrn_lb_logits",
               "ffn_w1", "ffn_w3", "ffn_w2"):
        shared[k_] = f32(inputs[k_])
    for k_, v_ in host_consts(S).items():
        shared["c_" + k_] = v_
    for k_, v_ in nsa2_consts(S).items():
        shared["c_" + k_] = v_
    return shared


def kernel(**inputs):
    x = np.ascontiguousarray(np.asarray(inputs["x"], dtype=np.float32))
    B, S, _ = x.shape
    shared = host_layout(inputs, S)
    nc = build_full(S)
    in_maps = []
    for b in range(B):
        m = dict(shared)
        m["x"] = np.ascontiguousarray(x[b])
        in_maps.append(m)
    res = run_bass_kernel_spmd(nc, in_maps, core_ids=list(range(B)))
    return np.stack([np.asarray(r["y"], dtype=np.float32) for r in res.results], axis=0)
```

```python
import math
from contextlib import ExitStack

import numpy as np
import concourse.bass as bass
import concourse.mybir as mybir
from concourse.bass_utils import run_bass_kernel_spmd

F32 = mybir.dt.float32
BF16 = mybir.dt.bfloat16
AF = mybir.ActivationFunctionType
ALU = mybir.AluOpType
AX = mybir.AxisListType

D = 1024
DFF = 2816
NFF = DFF // 128
EPS = 1e-6
EVEN_IN = 3352


class Buf:
    def __init__(self, t, key, psum=False):
        self.t = t
        self.key = key
        self.psum = psum

    def __getitem__(self, k):
        return self.t[k]


class Ctx:
    NDMA = 8
    ENG = ("pe", "act", "dve", "pool", "sp")

    def __init__(self, nc):
        self.nc = nc
        self.streams = {e: [] for e in self.ENG}
        self.cnt = {e: 0 for e in ("pe", "act", "dve", "pool")}
        self.dman = {q: 0 for q in ("sp", "act", "pool")}
        self.lastw = {}
        self.readers = {}
        self.known = {e: {} for e in self.ENG}
        self.all_tokens = {}
        self.nbuf = 0

    def sb(self, es, name, shape, dtype):
        self.nbuf += 1
        nm = "%s_%d" % (name, self.nbuf)
        t = es.enter_context(self.nc.sbuf_tensor(nm, list(shape), dtype))
        return Buf(t, nm)

    def ps(self, es, name, shape, dtype):
        self.nbuf += 1
        nm = "%s_%d" % (name, self.nbuf)
        t = es.enter_context(self.nc.psum_tensor(nm, list(shape), dtype))
        return Buf(t, nm, psum=True)

    @staticmethod
    def _k(x):
        return x.key if isinstance(x, Buf) else x

    def _collect(self, eng, reads, writes):
        deps = []
        for r in reads:
            k = self._k(r)
            if k in self.lastw:
                deps.append(self.lastw[k])
        for w in writes:
            k = self._k(w)
            if k in self.lastw:
                deps.append(self.lastw[k])
            deps.extend(self.readers.get(k, ()))
        return deps

    def _record(self, tok, reads, writes):
        for r in reads:
            self.readers.setdefault(self._k(r), []).append(tok)
        for w in writes:
            k = self._k(w)
            self.lastw[k] = tok
            self.readers[k] = []

    def _waits(self, eng, deps, is_pe_compute):
        waits = {}
        kn = self.known[eng]
        for (sk, v, src) in deps:
            if is_pe_compute and src == "pe":
                continue
            if kn.get(sk, 0) >= v:
                continue
            if waits.get(sk, 0) < v:
                waits[sk] = v
        for sk, v in waits.items():
            kn[sk] = v
        return list(waits.items())

    def op(self, eng, fn, reads=(), writes=()):
        ex = [r for r in reads if isinstance(r, Buf) and r.psum]
        if ex:
            writes = list(writes) + ex
        deps = self._collect(eng, reads, writes)
        waits = self._waits(eng, deps, eng == "pe")
        self.cnt[eng] += 1
        tok = (eng, self.cnt[eng], eng)
        self.streams[eng].append((waits, fn, (eng, 1)))
        self.all_tokens[eng] = tok
        self._record(tok, reads, writes)

    def dma(self, out, in_, reads=(), writes=(), q="sp", **kw):
        deps = self._collect(q, reads, writes)
        n = self.dman[q]
        slot = n % self.NDMA
        sk = ("dma", q, slot)
        if n >= self.NDMA:
            deps.append((sk, 16 * (n // self.NDMA), "dma"))
        waits = self._waits(q, deps, False)
        self.dman[q] += 1
        tok = (sk, 16 * (n // self.NDMA + 1), "dma")
        self.streams[q].append((waits, lambda e: e.dma_start(out=out, in_=in_, **kw), (sk, 16)))
        self.all_tokens[sk] = tok
        self._record(tok, reads, writes)

    def barrier(self):
        toks = list(self.all_tokens.values())
        for e in self.ENG:
            waits = self._waits(e, toks, False)
            if waits:
                self.streams[e].append((waits, None, None))

    def pe(self, fn, r=(), w=()):
        self.op("pe", fn, r, w)

    def act(self, fn, r=(), w=()):
        self.op("act", fn, r, w)

    def dve(self, fn, r=(), w=()):
        self.op("dve", fn, r, w)

    def pool(self, fn, r=(), w=()):
        self.op("pool", fn, r, w)

    def emit(self):
        nc = self.nc
        self.barrier()
        with ExitStack() as es:
            sems = {}
            for e in ("pe", "act", "dve", "pool"):
                sems[e] = es.enter_context(nc.semaphore("s_" + e))
            for q in ("sp", "act", "pool"):
                for s in range(self.NDMA):
                    sems[("dma", q, s)] = es.enter_context(nc.semaphore("d_%s%d" % (q, s)))
            block = es.enter_context(nc.Block())
            streams = self.streams

            def replay(name, eng):
                for waits, fn, inc in streams[name]:
                    for sk, v in waits:
                        eng.wait_ge(sems[sk], v)
                    if fn is not None:
                        fn(eng).then_inc(sems[inc[0]], inc[1])

            if streams["sp"]:
                @block.sync
                def _(eng):
                    replay("sp", eng)
            if streams["pe"]:
                @block.tensor
                def _(eng):
                    replay("pe", eng)
            if streams["dve"]:
                @block.vector
                def _(eng):
                    replay("dve", eng)
            if streams["act"]:
                @block.scalar
                def _(eng):
                    replay("act", eng)
            if streams["pool"]:
                @block.gpsimd
                def _(eng):
                    replay("pool", eng)


def load_weight_bf16(cx, wb, w_ap, kchunks, ncols, stage):
    step = stage[0].t.shape[1]
    i = 0
    for c in range(kchunks):
        for c0 in range(0, ncols, step):
            n = min(step, ncols - c0)
            st = stage[i % len(stage)]
            cx.dma(st[:, 0:n], w_ap[c * 128:(c + 1) * 128, c0:c0 + n], writes=[st], q=("sp" if i % 2 == 0 else "pool"))
            if (i // 2) % 2 == 0:
                cx.dve(lambda e, st=st, c=c, c0=c0, n=n: e.tensor_copy(wb[:, c, c0:c0 + n], st[:, 0:n]),
                       r=[st], w=[(wb.key, c, c0)])
            else:
                cx.act(lambda e, st=st, c=c, c0=c0, n=n: e.copy(wb[:, c, c0:c0 + n], st[:, 0:n]),
                       r=[st], w=[(wb.key, c, c0)])
            i += 1
    return wb


class NormTools:
    def __init__(self, cx, es, g_ap):
        self.cx = cx
        self.ident = cx.sb(es, "ident", [128, 128], BF16)
        self.gT = cx.sb(es, "gT", [128, 8], F32)
        self.ss = [cx.sb(es, "ss%d" % i, [128, 1], F32) for i in range(2)]
        self.rstd = [cx.sb(es, "rstd%d" % i, [128, 1], F32) for i in range(2)]
        self.junk = cx.sb(es, "junk", [128, D], BF16)
        self.hb = [cx.sb(es, "hb%d" % i, [128, D], BF16) for i in range(2)]
        self.tp = [cx.ps(es, "tp%d" % i, [128, 8, 128], BF16) for i in range(2)]
        self.i = 0
        cx.dma(self.gT[:], g_ap.rearrange("(c p) -> p c", p=128), writes=[self.gT],
               allow_slow_non_contiguous=True)

    def load_ident(self, ident_ap):
        self.cx.dma(self.ident[:], ident_ap, writes=[self.ident])

    def stats(self, xt):
        cx = self.cx
        i = self.i
        self.i += 1
        ss, rstd = self.ss[i % 2], self.rstd[i % 2]
        junk = self.junk
        cx.act(lambda e: e.activation(junk[:], xt[:], AF.Square, scale=1.0 / 32.0, accum_out=ss[:]),
               r=[xt], w=[junk, ss])
        cx.act(lambda e: e.activation(ss[:], ss[:], AF.Sqrt, bias=EPS, scale=1.0), r=[ss], w=[ss])
        cx.dve(lambda e: e.reciprocal(rstd[:], ss[:]), r=[ss], w=[rstd])
        return rstd

    def run(self, xt, hT, col0, scale_by_g=True):
        cx = self.cx
        i = self.i
        self.i += 1
        ss, rstd, hb, tp = self.ss[i % 2], self.rstd[i % 2], self.hb[i % 2], self.tp[i % 2]
        junk = self.junk
        cx.act(lambda e: e.activation(junk[:], xt[:], AF.Square, scale=1.0 / 32.0, accum_out=ss[:]),
               r=[xt], w=[junk, ss])
        cx.act(lambda e: e.activation(ss[:], ss[:], AF.Sqrt, bias=EPS, scale=1.0), r=[ss], w=[ss])
        cx.dve(lambda e: e.reciprocal(rstd[:], ss[:]), r=[ss], w=[rstd])
        cx.act(lambda e: e.activation(hb[:], xt[:], AF.Copy, scale=rstd[:]), r=[xt, rstd], w=[hb])
        for c in range(8):
            cx.pe(lambda e, c=c: e.transpose(tp[:, c, :], hb[:, c * 128:(c + 1) * 128], self.ident[:]),
                  r=[hb, self.ident], w=[tp])
        gT = self.gT
        cx.dve(lambda e: e.tensor_tensor(hT[:, :, col0:col0 + 128], tp[:],
                                         gT[:].unsqueeze(2).to_broadcast([128, 8, 128]), ALU.mult),
               r=[tp, gT], w=[hT])
        return rstd


def dram_fm(ap, c0, c1, s0, s1):
    return ap[c0:c1, :, s0:s1].rearrange("c p s -> p c s")


def stage_norm0(cx, x_ap, hT_ap, g_ap, ident_ap, S):
    with ExitStack() as es:
        nt = NormTools(cx, es, g_ap)
        nt.load_ident(ident_ap)
        xts = [cx.sb(es, "xt%d" % i, [128, D], F32) for i in range(2)]
        hTs = [cx.sb(es, "hTt%d" % i, [128, 8, 512], BF16) for i in range(2)]
        for m in range(S // 512):
            hT = hTs[m % 2]
            for j in range(4):
                t = m * 4 + j
                xt = xts[t % 2]
                cx.dma(xt[:], x_ap[t * 128:(t + 1) * 128, :], writes=[xt])
                nt.run(xt, hT, j * 128)
            cx.dma(dram_fm(hT_ap, 0, 8, m * 512, (m + 1) * 512), hT[:], reads=[hT], q="pool")
    cx.barrier()


def stage_out(cx, mixT_ap, w_ap, xin_ap, xout_ap, hT_ap, g_ap, ident_ap, S):
    with ExitStack() as es:
        wb = cx.sb(es, "woutb", [128, 8, D], BF16)
        with ExitStack() as es2:
            stg = [cx.sb(es2, "wstg%d" % i, [128, 1024], F32) for i in range(4)]
            load_weight_bf16(cx, wb, w_ap, 8, D, stg)
            cx.barrier()
        nt = NormTools(cx, es, g_ap)
        nt.load_ident(ident_ap)
        mts = [cx.sb(es, "mixt%d" % i, [128, 8, 512], BF16) for i in range(2)]
        xts = [cx.sb(es, "xt%d" % i, [128, D], F32) for i in range(2)]
        x1s = [cx.sb(es, "x1t%d" % i, [128, D], F32) for i in range(2)]
        hTs = [cx.sb(es, "hTt%d" % i, [128, 8, 512], BF16) for i in range(2)]
        yps = [cx.ps(es, "yps%d" % i, [128, 512], F32) for i in range(4)]
        for m in range(S // 512):
            mt = mts[m % 2]
            hT = hTs[m % 2]
            cx.dma(mt[:], dram_fm(mixT_ap, 0, 8, m * 512, (m + 1) * 512), writes=[mt])
            for j in range(4):
                t = m * 4 + j
                xt = xts[t % 2]
                x1 = x1s[t % 2]
                cx.dma(xt[:], xin_ap[t * 128:(t + 1) * 128, :], writes=[xt])
                for half in range(2):
                    yp = yps[(t * 2 + half) % 4]
                    for c in range(8):
                        cx.pe(lambda e, yp=yp, c=c, j=j, half=half, mt=mt: e.matmul(
                            yp[:], mt[:, c, j * 128:(j + 1) * 128], wb[:, c, half * 512:(half + 1) * 512],
                            start=(c == 0), stop=(c == 7)), r=[mt, wb], w=[yp])
                    cx.dve(lambda e, yp=yp, half=half, xt=xt, x1=x1: e.tensor_tensor(
                        x1[:, half * 512:(half + 1) * 512], yp[:], xt[:, half * 512:(half + 1) * 512], ALU.add),
                        r=[yp, xt], w=[x1])
                cx.dma(xout_ap[t * 128:(t + 1) * 128, :], x1[:], reads=[x1], q="pool")
                nt.run(x1, hT, j * 128)
            cx.dma(dram_fm(hT_ap, 0, 8, m * 512, (m + 1) * 512), hT[:], reads=[hT], q="pool")
    cx.barrier()


def stage_ffn(cx, hT_ap, w1_ap, w3_ap, w2_ap, xin_ap, xout_ap, hTout_ap, g_ap, ident_ap, S, final,
              gfin_ap=None, y_ap=None):
    MT = 256
    with ExitStack() as es:
        w1b = cx.sb(es, "w1b", [128, 8, DFF], BF16)
        w3b = cx.sb(es, "w3b", [128, 8, DFF], BF16)
        w2b = cx.sb(es, "w2b", [128, NFF, D], BF16)
        with ExitStack() as es2:
            stg = [cx.sb(es2, "wstg%d" % i, [128, 1408], F32) for i in range(4)]
            load_weight_bf16(cx, w1b, w1_ap, 8, DFF, stg)
            load_weight_bf16(cx, w3b, w3_ap, 8, DFF, stg)
            load_weight_bf16(cx, w2b, w2_ap, NFF, D, stg)
            cx.barrier()
        nt = NormTools(cx, es, g_ap)
        nt.load_ident(ident_ap)
        if final:
            gfin = cx.sb(es, "gfin", [128, D], F32)
            cx.dma(gfin[:], gfin_ap.partition_broadcast(128), writes=[gfin])
        hins = [cx.sb(es, "hin%d" % i, [128, 8, MT], BF16) for i in range(2)]
        gT = cx.sb(es, "gTff", [128, NFF, MT], BF16)
        sil = [cx.sb(es, "sil%d" % i, [128, MT], F32) for i in range(2)]
        xts = [cx.sb(es, "xt%d" % i, [128, D], F32) for i in range(2)]
        x2s = [cx.sb(es, "x2t%d" % i, [128, D], F32) for i in range(2)]
        hTs = [cx.sb(es, "hTt%d" % i, [128, 8, MT], BF16) for i in range(2)]
        ups = [cx.ps(es, "ups%d" % i, [128, 512], F32) for i in range(4)]
        yps = [cx.ps(es, "yps%d" % i, [128, 512], F32) for i in range(2)]
        nsub = MT // 128
        for m in range(S // MT):
            hin = hins[m % 2]
            hT = hTs[m % 2]
            cx.dma(hin[:], dram_fm(hT_ap, 0, 8, m * MT, (m + 1) * MT), writes=[hin])
            for f in range(NFF):
                u1 = ups[(f % 2) * 2]
                u3 = ups[(f % 2) * 2 + 1]
                sl = sil[f % 2]
                for (wb, up) in ((w1b, u1), (w3b, u3)):
                    for c in range(8):
                        cx.pe(lambda e, wb=wb, up=up, c=c, f=f, hin=hin: e.matmul(
                            up[:, 0:MT], wb[:, c, f * 128:(f + 1) * 128], hin[:, c, :],
                            start=(c == 0), stop=(c == 7)), r=[wb, hin], w=[up])
                cx.act(lambda e, u1=u1, sl=sl: e.activation(sl[:], u1[:, 0:MT], AF.Silu), r=[u1], w=[sl])
                cx.dve(lambda e, u3=u3, sl=sl, f=f: e.tensor_tensor(gT[:, f, :], u3[:, 0:MT], sl[:], ALU.mult),
                       r=[u3, sl], w=[gT])
            for j in range(nsub):
                t = m * nsub + j
                xt = xts[t % 2]
                x2 = x2s[t % 2]
                cx.dma(xt[:], xin_ap[t * 128:(t + 1) * 128, :], writes=[xt])
                for half in range(2):
                    yp = yps[half]
                    for f in range(NFF):
                        cx.pe(lambda e, yp=yp, f=f, j=j, half=half: e.matmul(
                            yp[:], gT[:, f, j * 128:(j + 1) * 128], w2b[:, f, half * 512:(half + 1) * 512],
                            start=(f == 0), stop=(f == NFF - 1)), r=[gT, w2b], w=[yp])
                    cx.dve(lambda e, yp=yp, half=half, xt=xt, x2=x2: e.tensor_tensor(
                        x2[:, half * 512:(half + 1) * 512], yp[:], xt[:, half * 512:(half + 1) * 512], ALU.add),
                        r=[yp, xt], w=[x2])
                if not final:
                    cx.dma(xout_ap[t * 128:(t + 1) * 128, :], x2[:], reads=[x2], q="pool")
                    nt.run(x2, hT, j * 128)
                else:
                    rstd = nt.stats(x2)
                    ot = xt
                    cx.dve(lambda e, x2=x2, rstd=rstd, ot=ot: e.scalar_tensor_tensor(
                        ot[:], x2[:], rstd[:], gfin[:], ALU.mult, ALU.mult), r=[x2, rstd, gfin], w=[ot])
                    cx.dma(y_ap[t * 128:(t + 1) * 128, :], ot[:], reads=[ot], q="pool")
            if not final:
                cx.dma(dram_fm(hTout_ap, 0, 8, m * MT, (m + 1) * MT), hT[:], reads=[hT], q="pool")
    cx.barrier()


DEBUG = {}
CH = 64
MTK = 512
NCH = MTK // CH


class GLACore:
    def __init__(self, cx, es, nheads, ident, maskT_ap):
        self.cx = cx
        self.ident = ident
        self.maskT = cx.sb(es, "maskT", [64, 64], F32)
        cx.dma(self.maskT[:], maskT_ap, writes=[self.maskT])
        self.S = [cx.sb(es, "S%d" % h, [128, 128], F32) for h in range(nheads)]
        self.Sx = [cx.sb(es, "Sx%d" % h, [128, 128], F32) for h in range(nheads)]
        for h in range(nheads):
            cx.dve(lambda e, h=h: e.memset(self.S[h][:], 0.0), w=[self.S[h]])
        self.ATbs = [cx.sb(es, "ATb%d" % i, [64, NCH, 64], BF16) for i in range(2)]
        self.ktms = [cx.sb(es, "ktm%d" % i, [64, NCH, 128], BF16) for i in range(2)]
        self.KVds = [cx.sb(es, "KVd%d" % i, [128, NCH, 128], F32) for i in range(2)]
        self.spb = [cx.sb(es, "spb%d" % i, [128, 128], BF16) for i in range(2)]
        self.AT = cx.ps(es, "ATp", [64, NCH, 64], F32)
        self.KTt = cx.ps(es, "KTt", [64, NCH, 128], BF16)
        self.OT = cx.ps(es, "OTp", [128, MTK], F32)
        self.KV = [cx.ps(es, "KVp%d" % i, [128, 4, 128], F32) for i in range(2)]

    def pre(self, par, qt, kt, v, vcol0, dlast):
        cx = self.cx
        AT, ATb, KTt, ktm, KV, KVd = self.AT, self.ATbs[par], self.KTt, self.ktms[par], self.KV, self.KVds[par]
        maskT, ident = self.maskT, self.ident
        for c in range(NCH):
            cs = slice(c * CH, (c + 1) * CH)
            cx.pe(lambda e, c=c, cs=cs: e.matmul(AT[:, c, :], kt[:, cs], qt[:, cs], start=True, stop=True),
                  r=[kt, qt], w=[AT])
        cx.dve(lambda e: e.tensor_tensor(ATb[:], AT[:], maskT[:].unsqueeze(1).to_broadcast([64, NCH, 64]), ALU.mult),
               r=[AT, maskT], w=[ATb])
        for c in range(NCH):
            cs = slice(c * CH, (c + 1) * CH)
            cx.pe(lambda e, c=c, cs=cs: e.transpose(KTt[:, c, :], kt[:, cs], ident[:]), r=[kt, ident], w=[KTt])
        cx.act(lambda e: e.copy(ktm[:], KTt[:]), r=[KTt], w=[ktm])
        for c in range(NCH):
            cx.pe(lambda e, c=c: e.matmul(KV[c // 4][:, c % 4, :], ktm[:, c, :], v[:, c, vcol0:vcol0 + 128],
                                          start=True, stop=True), r=[ktm, v], w=[KV[c // 4]])
        for b in range(2):
            if isinstance(dlast, float):
                cx.dve(lambda e, b=b: e.tensor_scalar_mul(KVd[:, 4 * b:4 * b + 4, :], KV[b][:], dlast),
                       r=[KV[b]], w=[KVd])
            else:
                cx.dve(lambda e, b=b: e.tensor_tensor(
                    KVd[:, 4 * b:4 * b + 4, :], KV[b][:],
                    dlast[1][:, 4 * b:4 * b + 4].unsqueeze(2).to_broadcast([128, 4, 128]), ALU.mult),
                    r=[KV[b], dlast[0]], w=[KVd])

    def chain(self, par, h, qt, v, vcol0, ebm, e2, oT_sb):
        cx = self.cx
        ATb, KVd, OT = self.ATbs[par], self.KVds[par], self.OT
        SS = (self.S[h], self.Sx[h])

        def sc(x, c):
            return (x, []) if isinstance(x, float) else (x[1][:, c:c + 1], [x[0]])

        for c in range(NCH):
            cs = slice(c * CH, (c + 1) * CH)
            spb = self.spb[c % 2]
            S, Sn = SS[c % 2], SS[(c + 1) % 2]
            s_ebm, r_ebm = sc(ebm, c)
            s_e2, r_e2 = sc(e2, c)
            cx.act(lambda e, spb=spb, s_ebm=s_ebm, S=S: e.activation(spb[:], S[:], AF.Copy, scale=s_ebm),
                   r=[S] + r_ebm, w=[spb])
            cx.pe(lambda e, c=c, cs=cs: e.matmul(OT[:, cs], v[:, c, vcol0:vcol0 + 128], ATb[:, c, :],
                                                 start=True, stop=False), r=[v, ATb], w=[OT])
            cx.pe(lambda e, cs=cs, spb=spb: e.matmul(OT[:, cs], spb[:], qt[:, cs], start=False, stop=True),
                  r=[spb, qt], w=[OT])
            cx.dve(lambda e, c=c, s_e2=s_e2, S=S, Sn=Sn: e.scalar_tensor_tensor(Sn[:], S[:], s_e2, KVd[:, c, :], ALU.mult, ALU.add),
                   r=[S, KVd] + r_e2, w=[Sn])
        cx.act(lambda e: e.copy(oT_sb[:], OT[:]), r=[OT], w=[oT_sb])

    def run(self, h, qt, kt, v, vcol0, ebm, e2, dlast, oT_sb):
        self.pre(0, qt, kt, v, vcol0, dlast)
        self.chain(0, h, qt, v, vcol0, ebm, e2, oT_sb)


def proj_fm(cx, ps, wb, col0, ncols, hin, n):
    for c in range(8):
        cx.pe(lambda e, c=c: e.matmul(ps[0:ncols, 0:n], wb[:, c, col0:col0 + ncols], hin[:, c, 0:n],
                                      start=(c == 0), stop=(c == 7)), r=[wb, hin], w=[ps])


def stage_hgrn(cx, hT_ap, w_ap, normg_ap, lb_ap, mixT_ap, ident_ap, maskT_ap, scanm_ap, S, layer_j):
    with ExitStack() as es:
        wb = cx.sb(es, "winb", [128, 8, 4096], BF16)
        with ExitStack() as es2:
            stg = [cx.sb(es2, "wstg%d" % i, [128, 2048], F32) for i in range(4)]
            load_weight_bf16(cx, wb, w_ap, 8, 4096, stg)
            cx.barrier()
        ident = cx.sb(es, "ident", [128, 128], BF16)
        cx.dma(ident[:], ident_ap, writes=[ident])
        onesb = cx.sb(es, "onesb", [128, 128], BF16)
        cx.dve(lambda e: e.memset(onesb[:], 1.0), w=[onesb])
        scanm = cx.sb(es, "scanm", [128, MTK], F32)
        cx.dma(scanm[:], scanm_ap, writes=[scanm])
        normg = cx.sb(es, "normg", [128, 1], F32)
        cx.dma(normg[:], normg_ap.rearrange("(p o) -> p o", o=1), writes=[normg])
        lbT = cx.sb(es, "lbT", [128, 8], F32)
        omlT = cx.sb(es, "omlT", [128, 8], F32)
        if layer_j == 0:
            cx.dve(lambda e: e.memset(lbT[:], 0.0), w=[lbT])
        else:
            l0 = cx.sb(es, "l0", [128, 8], F32)
            l1 = cx.sb(es, "l1", [128, 8], F32)
            cx.dma(l0[:], lb_ap[0, :].rearrange("(h p) -> p h", p=128), writes=[l0], allow_slow_non_contiguous=True)
            cx.dma(l1[:], lb_ap[1, :].rearrange("(h p) -> p h", p=128), writes=[l1], allow_slow_non_contiguous=True)
            cx.dve(lambda e: e.tensor_tensor(l1[:], l1[:], l0[:], ALU.subtract), r=[l0, l1], w=[l1])
            cx.act(lambda e: e.activation(lbT[:], l1[:], AF.Sigmoid), r=[l1], w=[lbT])
        cx.dve(lambda e: e.tensor_scalar(omlT[:], lbT[:], -1.0, 1.0, ALU.mult, ALU.add), r=[lbT], w=[omlT])
        core = GLACore(cx, es, 8, ident, maskT_ap)
        hins = [cx.sb(es, "hin%d" % i, [128, 8, MTK], BF16) for i in range(2)]
        v = cx.sb(es, "vtm", [64, NCH, 1024], BF16)
        f32t = lambda n: cx.sb(es, n, [128, MTK], F32)
        TT = [{n: f32t(n + str(i)) for n in ("sq", "sg", "gl", "bb", "dd", "eq", "ek")} for i in range(2)]
        for i in range(2):
            TT[i]["ebm"] = cx.sb(es, "ebm%d" % i, [128, NCH], F32)
            TT[i]["e2"] = cx.sb(es, "e2%d" % i, [128, NCH], F32)
            TT[i]["qt"] = cx.sb(es, "qt%d" % i, [128, MTK], BF16)
            TT[i]["kt"] = cx.sb(es, "kt%d" % i, [128, MTK], BF16)
        oT = f32t("oT")
        sqo = cx.sb(es, "sqo", [128, MTK], BF16)
        rt = f32t("rt")
        sgp = f32t("sgp")
        mts = [cx.sb(es, "mixt%d" % i, [128, 8, MTK], BF16) for i in range(1)]
        P = [cx.ps(es, "P%d" % i, [128, 512], F32) for i in range(3)]
        pi = [0]

        def nextP():
            pi[0] += 1
            return P[pi[0] % 3]

        def macro(m, hin, mt):
            cx.dma(hin[:], dram_fm(hT_ap, 0, 8, m * MTK, (m + 1) * MTK), writes=[hin])
            k = 0
            for c in range(NCH):
                for half in range(2):
                    ps = nextP()
                    for kc in range(8):
                        cx.pe(lambda e, ps=ps, kc=kc, c=c, half=half: e.matmul(
                            ps[0:64, :], hin[:, kc, c * CH:(c + 1) * CH],
                            wb[:, kc, 2048 + half * 512:2048 + (half + 1) * 512],
                            start=(kc == 0), stop=(kc == 7)), r=[hin, wb], w=[ps])
                    if k % 2 == 0:
                        cx.act(lambda e, ps=ps, c=c, half=half: e.copy(v[:, c, half * 512:(half + 1) * 512], ps[0:64, :]),
                               r=[ps], w=[v])
                    else:
                        cx.dve(lambda e, ps=ps, c=c, half=half: e.tensor_copy(v[:, c, half * 512:(half + 1) * 512], ps[0:64, :]),
                               r=[ps], w=[v])
                    k += 1
            def A(h, par):
                T = TT[par]
                sq, sg, gl, bb, dd, eq, ek, ebm, e2, qt, kt = (T[n] for n in ("sq", "sg", "gl", "bb", "dd", "eq", "ek", "ebm", "e2", "qt", "kt"))
                pq = nextP()
                proj_fm(cx, pq, wb, h * 128, 128, hin, MTK)
                cx.act(lambda e: e.activation(sq[:], pq[:], AF.Silu), r=[pq], w=[sq])
                pf = nextP()
                proj_fm(cx, pf, wb, 1024 + h * 128, 128, hin, MTK)
                cx.act(lambda e: e.activation(sg[:], pf[:], AF.Sigmoid), r=[pf], w=[sg])
                cx.dve(lambda e: e.tensor_scalar(sg[:], sg[:], omlT[:, h:h + 1], lbT[:, h:h + 1], ALU.mult, ALU.add),
                       r=[sg, omlT, lbT], w=[sg])
                cx.act(lambda e: e.activation(gl[:], sg[:], AF.Ln), r=[sg], w=[gl])
                cx.dve(lambda e: e.tensor_tensor_scan(bb[:], scanm[:], gl[:], 0.0, ALU.mult, ALU.add),
                       r=[scanm, gl], w=[bb])
                b3 = bb[:].rearrange("p (c n) -> p c n", n=CH)
                cx.dve(lambda e: e.tensor_tensor(
                    dd[:].rearrange("p (c n) -> p c n", n=CH), b3,
                    b3[:, :, 31:32].to_broadcast([128, NCH, CH]), ALU.subtract), r=[bb], w=[dd])
                cx.act(lambda e: e.activation(eq[:], dd[:], AF.Exp), r=[dd], w=[eq])
                cx.act(lambda e: e.activation(ek[:], dd[:], AF.Exp, scale=-1.0), r=[dd], w=[ek])
                cx.act(lambda e: e.activation(ebm[:], b3[:, :, 31], AF.Exp), r=[bb], w=[ebm])
                eq3 = eq[:].rearrange("p (c n) -> p c n", n=CH)
                cx.dve(lambda e: e.tensor_tensor(e2[:], ebm[:], eq3[:, :, CH - 1], ALU.mult),
                       r=[ebm, eq], w=[e2])
                cx.dve(lambda e: e.scalar_tensor_tensor(qt[:], sq[:], 128.0 ** -0.5, eq[:], ALU.mult, ALU.mult),
                       r=[sq, eq], w=[qt])
                cx.dve(lambda e: e.tensor_scalar(sg[:], sg[:], -1.0, 1.0, ALU.mult, ALU.add), r=[sg], w=[sg])
                cx.dve(lambda e: e.tensor_tensor(kt[:], sg[:], ek[:], ALU.mult), r=[sg, ek], w=[kt])
                core.pre(par, qt, kt, v, h * 128, (eq, eq3[:, :, CH - 1]))

            def B(h, par):
                T = TT[par]
                ebm, e2, qt = T["ebm"], T["e2"], T["qt"]
                core.chain(par, h, qt, v, h * 128, (ebm, ebm.t), (e2, e2.t), oT)
                cx.act(lambda e: e.activation(sqo[:], oT[:], AF.Square), r=[oT], w=[sqo])
                pss = nextP()
                cx.pe(lambda e: e.matmul(pss[:], onesb[:], sqo[:], start=True, stop=True),
                      r=[onesb, sqo], w=[pss])
                cx.act(lambda e: e.activation(rt[:], pss[:], AF.Sqrt, bias=EPS, scale=1.0 / 128.0),
                       r=[pss], w=[rt])
                cx.dve(lambda e: e.reciprocal(rt[:], rt[:]), r=[rt], w=[rt])
                pg = nextP()
                proj_fm(cx, pg, wb, 3072 + h * 128, 128, hin, MTK)
                cx.act(lambda e: e.activation(sgp[:], pg[:], AF.Sigmoid), r=[pg], w=[sgp])
                cx.dve(lambda e: e.scalar_tensor_tensor(rt[:], oT[:], normg[:, 0:1], rt[:], ALU.mult, ALU.mult),
                       r=[oT, normg, rt], w=[rt])
                cx.dve(lambda e: e.tensor_tensor(mt[:, h, :], rt[:], sgp[:], ALU.mult),
                       r=[rt, sgp], w=[mt])

            A(0, 0)
            for h in range(8):
                if h + 1 < 8:
                    A(h + 1, (h + 1) % 2)
                B(h, h % 2)
            cx.dma(dram_fm(mixT_ap, 0, 8, m * MTK, (m + 1) * MTK), mt[:], reads=[mt], q="pool")

        for m in range(S // MTK):
            macro(m, hins[m % 2], mts[0])
    cx.barrier()


RET_GAMMA = [1.0 - 2.0 ** (-5.0 - h) for h in range(4)]


def stage_ret(cx, hT_ap, w_ap, cos_ap, sin_ap, dec_ap, mixT_ap, ident_ap, maskT_ap, S):
    with ExitStack() as es:
        wb = cx.sb(es, "winb", [128, 8, 3072], BF16)
        with ExitStack() as es2:
            stg = [cx.sb(es2, "wstg%d" % i, [128, 1536], F32) for i in range(4)]
            load_weight_bf16(cx, wb, w_ap, 8, 3072, stg)
            cx.barrier()
        ident = cx.sb(es, "ident", [128, 128], BF16)
        cx.dma(ident[:], ident_ap, writes=[ident])
        onesb = cx.sb(es, "onesb", [128, 128], BF16)
        cx.dve(lambda e: e.memset(onesb[:], 1.0), w=[onesb])
        dec = cx.sb(es, "dec", [128, 8, MTK], F32)
        cx.dma(dec[:], dec_ap.rearrange("h t p n -> p (h t) n"), writes=[dec])
        core = GLACore(cx, es, 4, ident, maskT_ap)
        hins = [cx.sb(es, "hin%d" % i, [128, 8, MTK], BF16) for i in range(2)]
        coss = [cx.sb(es, "cos%d" % i, [128, MTK], F32) for i in range(2)]
        sins = [cx.sb(es, "sin%d" % i, [128, MTK], F32) for i in range(2)]
        v = cx.sb(es, "vtm", [64, NCH, 512], BF16)
        f32t = lambda n: cx.sb(es, n, [128, MTK], F32)
        oT, mean, var, sgp = [f32t(n) for n in ("oT", "mean", "var", "sgp")]
        TT = [{"t1": f32t("t1_%d" % i), "t2": f32t("t2_%d" % i),
               "qt": cx.sb(es, "qt%d" % i, [128, MTK], BF16), "kt": cx.sb(es, "kt%d" % i, [128, MTK], BF16)} for i in range(2)]
        ob = cx.sb(es, "ob", [128, MTK], BF16)
        sqo = cx.sb(es, "sqo", [128, MTK], BF16)
        mts = [cx.sb(es, "mixt%d" % i, [128, 4, MTK], BF16) for i in range(2)]
        P = [cx.ps(es, "P%d" % i, [128, 512], F32) for i in range(3)]
        pi = [0]

        def nextP():
            pi[0] += 1
            return P[pi[0] % 3]

        def rot(h, col0, tab, out, hin, cs, sn, t1, t2):
            pa = nextP()
            proj_fm(cx, pa, wb, col0 + h * 128, 128, hin, MTK)
            cx.dve(lambda e: e.tensor_tensor(t1[:], pa[:], cs[:], ALU.mult), r=[pa, cs], w=[t1])
            pb = nextP()
            proj_fm(cx, pb, wb, 1024 + col0 + h * 128, 128, hin, MTK)
            cx.dve(lambda e: e.tensor_tensor(t2[:], pb[:], sn[:], ALU.mult), r=[pb, sn], w=[t2])
            cx.dve(lambda e: e.tensor_tensor(t1[:], t1[:], t2[:], ALU.add), r=[t1, t2], w=[t1])
            cx.dve(lambda e: e.tensor_tensor(out[:], t1[:], dec[:, tab, :], ALU.mult), r=[t1, dec], w=[out])

        def macro(m, hin, mt, cs, sn):
            cx.dma(hin[:], dram_fm(hT_ap, 0, 8, m * MTK, (m + 1) * MTK), writes=[hin])
            cx.dma(cs[:], cos_ap[:, m * MTK:(m + 1) * MTK], writes=[cs])
            cx.dma(sn[:], sin_ap[:, m * MTK:(m + 1) * MTK], writes=[sn])
            for c in range(NCH):
                ps = nextP()
                for kc in range(8):
                    cx.pe(lambda e, ps=ps, kc=kc, c=c: e.matmul(
                        ps[0:64, :], hin[:, kc, c * CH:(c + 1) * CH], wb[:, kc, 2048:2560],
                        start=(kc == 0), stop=(kc == 7)), r=[hin, wb], w=[ps])
                if c % 2 == 0:
                    cx.act(lambda e, ps=ps, c=c: e.copy(v[:, c, :], ps[0:64, :]), r=[ps], w=[v])
                else:
                    cx.dve(lambda e, ps=ps, c=c: e.tensor_copy(v[:, c, :], ps[0:64, :]), r=[ps], w=[v])
            def A(h, par):
                T = TT[par]
                g = RET_GAMMA[h]
                rot(h, 0, 2 * h, T["qt"], hin, cs, sn, T["t1"], T["t2"])
                rot(h, 512, 2 * h + 1, T["kt"], hin, cs, sn, T["t1"], T["t2"])
                core.pre(par, T["qt"], T["kt"], v, h * 128, float(g ** 32))

            def B(h, par):
                T = TT[par]
                g = RET_GAMMA[h]
                core.chain(par, h, T["qt"], v, h * 128, float(g ** 32), float(g ** 64), oT)
                cx.act(lambda e: e.copy(ob[:], oT[:]), r=[oT], w=[ob])
                cx.act(lambda e: e.activation(sqo[:], oT[:], AF.Square), r=[oT], w=[sqo])
                p1 = nextP()
                cx.pe(lambda e: e.matmul(p1[:], onesb[:], ob[:], start=True, stop=True), r=[onesb, ob], w=[p1])
                p2 = nextP()
                cx.pe(lambda e: e.matmul(p2[:], onesb[:], sqo[:], start=True, stop=True), r=[onesb, sqo], w=[p2])
                cx.act(lambda e: e.activation(mean[:], p1[:], AF.Copy, scale=1.0 / 128.0), r=[p1], w=[mean])
                cx.dve(lambda e: e.tensor_tensor(var[:], mean[:], mean[:], ALU.mult), r=[mean], w=[var])
                cx.dve(lambda e: e.scalar_tensor_tensor(var[:], p2[:], 1.0 / 128.0, var[:], ALU.mult, ALU.subtract),
                       r=[p2, var], w=[var])
                cx.act(lambda e: e.activation(var[:], var[:], AF.Sqrt, bias=1e-5, scale=1.0), r=[var], w=[var])
                cx.dve(lambda e: e.reciprocal(var[:], var[:]), r=[var], w=[var])
                cx.dve(lambda e: e.tensor_tensor(oT[:], oT[:], mean[:], ALU.subtract), r=[oT, mean], w=[oT])
                cx.dve(lambda e: e.tensor_tensor(oT[:], oT[:], var[:], ALU.mult), r=[oT, var], w=[oT])
                pg = nextP()
                proj_fm(cx, pg, wb, 2560 + h * 128, 128, hin, MTK)
                cx.act(lambda e: e.activation(sgp[:], pg[:], AF.Silu), r=[pg], w=[sgp])
                cx.dve(lambda e: e.tensor_tensor(mt[:, h, :], oT[:], sgp[:], ALU.mult), r=[oT, sgp], w=[mt])

            A(0, 0)
            for h in range(4):
                if h + 1 < 4:
                    A(h + 1, (h + 1) % 2)
                B(h, h % 2)
            cx.dma(dram_fm(mixT_ap, 0, 4, m * MTK, (m + 1) * MTK), mt[:], reads=[mt], q="pool")

        for m in range(S // MTK):
            macro(m, hins[m % 2], mts[m % 2], coss[m % 2], sins[m % 2])
    cx.barrier()


def host_consts(S):
    import ml_dtypes
    c = {}
    c["ident"] = np.eye(128, dtype=np.float32).astype(ml_dtypes.bfloat16)
    m = np.arange(64)
    c["maskT"] = (m[:, None] <= m[None, :]).astype(np.float32)
    sm = np.ones((128, MTK), np.float32)
    sm[:, ::CH] = 0
    c["scanm"] = sm
    half = 64
    inv = (10000.0 ** (-np.arange(half, dtype=np.float32) / half)).astype(np.float32)
    ang = (np.arange(S, dtype=np.float32)[:, None] * inv[None, :]).astype(np.float32)
    cos = np.cos(ang).T.astype(np.float32)
    sin = np.sin(ang).T.astype(np.float32)
    c["cos"] = np.ascontiguousarray(np.concatenate([cos, cos], 0))
    c["sin"] = np.ascontiguousarray(np.concatenate([-sin, sin], 0))
    dec = np.zeros((4, 2, 128, MTK), np.float32)
    n = (np.arange(MTK) % CH).astype(np.float64)
    for h in range(4):
        g = RET_GAMMA[h]
        dec[h, 0] = (g ** (n - 31.0))[None, :]
        dec[h, 1] = (g ** (31.0 - n) * 128.0 ** -0.5)[None, :]
    c["dec"] = dec
    return c


NEG = -30000.0
NSA_PIPE = True
NSA_HOLD = True
NEG8 = NEG * 8.0


def nsa_consts(S):
    import ml_dtypes
    bf = ml_dtypes.bfloat16
    nb = S // 128
    c = {}
    tl = np.arange(128)
    n = np.arange(256)
    cm = np.full((nb, 128, 256), NEG8, np.float32)
    for i in range(nb):
        t = i * 128 + tl
        ok = (16 * n[None, :] + 31 <= t[:, None]) & (n[None, :] < S // 16 - 1)
        cm[i][ok] = 0.0
    c["cmask"] = cm.astype(bf)
    j = np.arange(64)
    fb = np.zeros((nb, 128, 64), np.float32)
    for i in range(nb):
        bt = (i * 128 + tl) // 64
        d = bt[:, None] - j[None, :]
        forced = (j[None, :] == 0) | ((d >= 0) & (d < 2))
        fb[i] = np.where(d >= 0, np.where(forced, 1.0e4, 0.0), -1.0e30)
    c["fbias"] = fb
    cs = np.arange(256) * 16
    ce = cs + 31
    ss = np.arange(64) * 64
    se = ss + 63
    ov = ((cs[:, None] <= se[None, :]) & (ce[:, None] >= ss[None, :])).astype(np.float32)
    ov[S // 16 - 1:, :] = 0
    c["ovl"] = ov.astype(bf)
    c["causal"] = np.where(tl[None, :] <= tl[:, None], 0.0, NEG8).astype(np.float32).astype(bf)
    kr = np.arange(640) - 512
    dist = tl[:, None] - kr[None, :]
    c["wmask"] = np.where((dist >= 0) & (dist < 512), 0.0, NEG8).astype(np.float32).astype(bf)
    c["rvalid"] = (tl >= 31).astype(np.float32).reshape(128, 1)
    kk = np.arange(S)
    c["blockE"] = (kk[None, :] // 64 == np.arange(64)[:, None]).astype(np.float32).astype(bf)
    return c


def stage_nsa(cx, hT_ap, w_ap, cw, mixT_ap, ident_ap, cn, S):
    NB = S // 128
    NT = S // 128
    with ExitStack() as es:
        wb = cx.sb(es, "wnsa", [128, 8, 1304], BF16)
        ksE = [cx.sb(es, "ksE%d" % g, [128, S], BF16) for g in range(2)]
        kwT = [cx.sb(es, "kwT%d" % g, [64, S], BF16) for g in range(2)]
        vsw = cx.sb(es, "vsw", [128, NT, 256], BF16)
        kcmpT = [cx.sb(es, "kcmpT%d" % g, [64, 256], BF16) for g in range(2)]
        vcmp = cx.sb(es, "vcmp", [128, 2, 2, 64], BF16)
        ident = cx.sb(es, "ident", [128, 128], BF16)
        P = [cx.ps(es, "P%d" % i, [128, 512], F32) for i in range(4)]
        TP = [cx.ps(es, "TP%d" % i, [128, 8, 128], BF16) for i in range(2)]
        PV = [cx.ps(es, "PV%d" % i, [128, 64], F32) for i in range(1)]
        IMP = cx.ps(es, "IMP", [128, 64], F32)
        cnt = {"p": 0, "tp": 0, "pv": 0, "cp": 0, "sp": 0}

        def nP():
            cnt["p"] += 1
            return P[cnt["p"] % 2]

        def nH():
            cnt["h"] = cnt.get("h", 0) + 1
            return P[2 + cnt["h"] % 2]

        def nTP():
            cnt["tp"] += 1
            return TP[cnt["tp"] % 2]

        def nPV():
            return PV[0]

        def cp(out_ap, in_ap, r, w):
            cnt["cp"] += 1
            if cnt["cp"] % 2:
                cx.act(lambda e: e.copy(out_ap, in_ap), r=r, w=w)
            else:
                cx.dve(lambda e: e.tensor_copy(out_ap, in_ap), r=r, w=w)

        with ExitStack() as es2:
            stg = [cx.sb(es2, "wstg%d" % i, [128, 1304], F32) for i in range(4)]
            load_weight_bf16(cx, wb, w_ap, 8, 1304, stg)
            cx.dma(ident[:], ident_ap, writes=[ident])
            for g in range(2):
                cx.dma(ksE[g][64:128, :], cn["blockE"], writes=[(ksE[g].key, "E")])
            cx.barrier()
            kcT = [cx.sb(es2, "kcT%d" % g, [64, S], BF16) for g in range(2)]
            vcT = [cx.sb(es2, "vcT%d" % g, [64, S], BF16) for g in range(2)]
            hins = [cx.sb(es2, "hin%d" % i, [128, 8, MTK], BF16) for i in range(2)]

            def phaseA(m, hin):
                cx.dma(hin[:], dram_fm(hT_ap, 0, 8, m * MTK, (m + 1) * MTK), writes=[hin])
                for (dst, col) in ((kcT, 512), (vcT, 640), (ksE, 768), (kwT, 896)):
                    for g in range(2):
                        ps = nP()
                        proj_fm(cx, ps, wb, col + g * 64, 64, hin, MTK)
                        cp(dst[g][0:64, m * MTK:(m + 1) * MTK], ps[0:64, :], [ps], [dst[g]])
                for j in range(4):
                    ps = nP()
                    for kc in range(8):
                        cx.pe(lambda e, ps=ps, kc=kc, j=j: e.matmul(
                            ps[:, 0:256], hin[:, kc, j * 128:(j + 1) * 128], wb[:, kc, 1024:1280],
                            start=(kc == 0), stop=(kc == 7)), r=[hin, wb], w=[ps])
                    cp(vsw[:, m * 4 + j, :], ps[:, 0:256], [ps], [vsw])

            for m in range(S // MTK):
                phaseA(m, hins[m % 2])

            w1s = cx.sb(es2, "w1s", [64, 32, 64], F32)
            w1b = cx.sb(es2, "w1b", [64, 32, 64], BF16)
            w2s = cx.sb(es2, "w2s", [64, 64], F32)
            w2b = cx.sb(es2, "w2b", [64, 64], BF16)
            poss = cx.sb(es2, "poss", [64, 32], F32)
            posb = cx.sb(es2, "posb", [64, 32], BF16)
            cb = cx.sb(es2, "cb", [64, 1], F32)
            tt = [cx.sb(es2, "gt%d" % i, [64, 256], F32) for i in range(3)]
            glb = cx.sb(es2, "glb", [64, 256], BF16)
            for g in range(2):
                cx.dve(lambda e, g=g: e.memset(kcmpT[g][:], 0.0), w=[kcmpT[g]])
            cx.dve(lambda e: e.memset(vcmp[:], 0.0), w=[vcmp])
            cx.dve(lambda e: e.memset(glb[:], 0.0), w=[glb])
            NCMP = S // 16 - 1

            def phaseB(kind, g, src):
                pos_ap, w1_ap, w2_ap = cw["pos_" + kind], cw["w1_" + kind], cw["w2_" + kind]
                if g == 0:
                    cx.dma(w1s[:], w1_ap.rearrange("(p d) o -> d p o", d=64), writes=[w1s])
                    cx.dma(w2s[:], w2_ap, writes=[w2s])
                    cx.dma(poss[:], pos_ap.rearrange("p d -> d p"), writes=[poss], allow_slow_non_contiguous=True)
                    cx.dve(lambda e: e.tensor_copy(w1b[:], w1s[:]), r=[w1s], w=[w1b])
                    cx.dve(lambda e: e.tensor_copy(w2b[:], w2s[:]), r=[w2s], w=[w2b])
                    cx.dve(lambda e: e.tensor_copy(posb[:], poss[:]), r=[poss], w=[posb])
                    pc = nPV()
                    for p in range(32):
                        cx.pe(lambda e, p=p, pc=pc: e.matmul(pc[0:64, 0:1], w1b[:, p, :], posb[:, p:p + 1],
                                                             start=(p == 0), stop=(p == 31)), r=[w1b, posb], w=[pc])
                    cx.act(lambda e, pc=pc: e.copy(cb[:], pc[0:64, 0:1]), r=[pc], w=[cb])
                ps = nP()
                x3 = src[0:64, :].rearrange("d (n s) -> d n s", s=16)
                for p in range(32):
                    n0, r_ = (0, p) if p < 16 else (1, p - 16)
                    cx.pe(lambda e, p=p, n0=n0, r_=r_, ps=ps: e.matmul(
                        ps[0:64, 0:NCMP], w1b[:, p, :], x3[:, n0:n0 + NCMP, r_],
                        start=(p == 0), stop=(p == 31)), r=[w1b, src], w=[ps])
                t0, t1_, t2_ = tt
                N = NCMP
                cx.act(lambda e, ps=ps: e.activation(t0[:, 0:N], ps[0:64, 0:N], AF.Identity, bias=cb[:], scale=1.0),
                       r=[ps, cb], w=[t0])
                cx.dve(lambda e: e.tensor_tensor(t1_[:, 0:N], t0[:, 0:N], t0[:, 0:N], ALU.mult), r=[t0], w=[t1_])
                cx.dve(lambda e: e.tensor_scalar(t1_[:, 0:N], t1_[:, 0:N], 0.044715, 1.0, ALU.mult, ALU.add), r=[t1_], w=[t1_])
                cx.dve(lambda e: e.tensor_tensor(t1_[:, 0:N], t1_[:, 0:N], t0[:, 0:N], ALU.mult), r=[t1_, t0], w=[t1_])
                cx.act(lambda e: e.activation(t2_[:, 0:N], t1_[:, 0:N], AF.Sigmoid, scale=2.0 * math.sqrt(2.0 / math.pi)),
                       r=[t1_], w=[t2_])
                cx.dve(lambda e: e.tensor_tensor(glb[:, 0:N], t0[:, 0:N], t2_[:, 0:N], ALU.mult), r=[t0, t2_], w=[glb])
                if kind == "k":
                    po = nP()
                    cx.pe(lambda e, po=po: e.matmul(po[0:64, 0:N], w2b[:], glb[:, 0:N], start=True, stop=True),
                          r=[w2b, glb], w=[po])
                    cp(kcmpT[g][:, 0:N], po[0:64, 0:N], [po], [kcmpT[g]])
                else:
                    for kc2 in range(2):
                        po = nPV()
                        n1 = min(128, N - kc2 * 128)
                        if n1 <= 0:
                            continue
                        cx.pe(lambda e, po=po, kc2=kc2, n1=n1: e.matmul(
                            po[0:n1, :], glb[:, kc2 * 128:kc2 * 128 + n1], w2b[:], start=True, stop=True),
                            r=[glb, w2b], w=[po])
                        cp(vcmp[0:n1, kc2, g, :], po[0:n1, :], [po], [vcmp])

            for kind, srcs in (("k", kcT), ("v", vcT)):
                for g in range(2):
                    phaseB(kind, g, srcs[g])
            cx.barrier()

        ovl = cx.sb(es, "ovl", [128, 2, 64], BF16)
        cx.dma(ovl[:], cn["ovl"].rearrange("(c p) j -> p c j", p=128), writes=[ovl])
        causal = cx.sb(es, "causal", [128, 128], BF16)
        cx.dma(causal[:], cn["causal"], writes=[causal])
        wmask = cx.sb(es, "wmask", [128, 640], BF16)
        cx.dma(wmask[:], cn["wmask"], writes=[wmask])
        rvalid = cx.sb(es, "rvalid", [128, 1], F32)
        cx.dma(rvalid[:], cn["rvalid"], writes=[rvalid])
        hqs = [cx.sb(es, "hq%d" % i, [128, 8, 128], BF16) for i in range(2)]
        cms = [cx.sb(es, "cm%d" % i, [128, 256], BF16) for i in range(2)]
        fbs = [cx.sb(es, "fb%d" % i, [128, 64], F32) for i in range(2)]
        qsel = [cx.sb(es, "qsel%d" % i, [128, 4, 128], BF16) for i in range(2)]
        selw = cx.sb(es, "selw", [128, 128], BF16)
        cx.dve(lambda e: e.memset(selw[:], 0.0), w=[selw])
        pcT = [cx.sb(es, "pcT%d" % i, [128, 2, 128], BF16) for i in range(4)]
        pc32 = [cx.sb(es, "pc32_%d" % i, [128, 256], F32) for i in range(2)]
        pbs = [cx.sb(es, "pb%d" % i, [128, S], BF16) for i in range(2)]
        pTs = [cx.sb(es, "pT%d" % i, [128, NT, 128], BF16) for i in range(2)]
        acc = cx.sb(es, "acc", [128, 512], F32)
        accb = cx.sb(es, "accb", [128, 512], BF16)
        mixt = [cx.sb(es, "mixt%d" % i, [128, 4, 128], BF16) for i in range(2)]
        sms = [{n_: cx.sb(es, n_ + str(i), [128, 8 if n_[0] == "c" else 1], F32)
                for n_ in ("cmax", "crs", "mx", "rs", "rinv", "fac")} for i in range(2)]
        sc64 = cx.sb(es, "sc64", [128, 64], F32)
        top8 = cx.sb(es, "top8", [128, 8], F32)
        sel01 = cx.sb(es, "sel01", [128, 64], F32)
        SCALE = 0.125

        def softmax_item(q_ap, q_r, KT, k0, nk, maskfn, vfn, gate_ap, gate_r, acc_ap, first, normalize, keepT=None, i0=False):
            cnt["sp"] += 1
            b = cnt["sp"] % 2
            sm, pb, pT = sms[b], pbs[b], pTs[b]
            cmax, crs, mx, rs, rinv, fac = sm["cmax"], sm["crs"], sm["mx"], sm["rs"], sm["rinv"], sm["fac"]
            chunks = [(c0, min(512, nk - c0)) for c0 in range(0, nk, 512)]
            ncn = len(chunks)
            dst32 = pc32[b] if normalize else None
            held = []

            def scores(ps, c0, n_):
                mm = maskfn(c0, n_)
                cx.pe(lambda e: e.matmul(ps[:, 0:n_], q_ap, KT[0:q_ap.shape[0], k0 + c0:k0 + c0 + n_],
                                         start=True, stop=(len(mm) == 0)), r=q_r + [KT], w=[ps])
                for idx, (l_ap, r_ap, lo, hi, rd) in enumerate(mm):
                    cx.pe(lambda e, l_ap=l_ap, r_ap=r_ap, lo=lo, hi=hi, idx=idx: e.matmul(
                        ps[:, lo:hi], l_ap, r_ap, start=False, stop=(idx == len(mm) - 1)), r=rd, w=[ps])

            def expo(ps, ci, c0, n_):
                out_ap = dst32[:, c0:c0 + n_] if normalize else pb[:, c0:c0 + n_]
                wr = [dst32] if normalize else [pb]
                cx.act(lambda e: e.activation(out_ap, ps[:, 0:n_], AF.Exp, bias=mx[:], scale=SCALE,
                                              accum_out=crs[:, ci:ci + 1]), r=[ps, mx], w=wr + [crs])

            def p1():
                for ci, (c0, n_) in enumerate(chunks):
                    ps = nH() if (ncn == 1 and NSA_HOLD) else nP()
                    scores(ps, c0, n_)
                    cx.dve(lambda e, ps=ps, ci=ci, n_=n_: e.reduce_max(cmax[:, ci:ci + 1], ps[:, 0:n_], AX.X),
                           r=[ps], w=[cmax])
                    if ncn == 1 and NSA_HOLD:
                        held.append(ps)
                if ncn == 1:
                    cx.dve(lambda e: e.tensor_scalar_mul(mx[:], cmax[:, 0:1], -SCALE), r=[cmax], w=[mx])
                else:
                    cx.dve(lambda e: e.tensor_reduce(mx[:], cmax[:, 0:ncn], AX.X, ALU.max), r=[cmax], w=[mx])
                    cx.dve(lambda e: e.tensor_scalar_mul(mx[:], mx[:], -SCALE), r=[mx], w=[mx])

            def p2():
                if ncn == 1 and NSA_HOLD:
                    expo(held[0], 0, chunks[0][0], chunks[0][1])
                    cx.dve(lambda e: e.reciprocal(rinv[:], crs[:, 0:1]), r=[crs], w=[rinv])
                elif ncn == 1:
                    ps = nP()
                    scores(ps, chunks[0][0], chunks[0][1])
                    expo(ps, 0, chunks[0][0], chunks[0][1])
                    cx.dve(lambda e: e.reciprocal(rinv[:], crs[:, 0:1]), r=[crs], w=[rinv])
                else:
                    for ci, (c0, n_) in enumerate(chunks):
                        ps = nP()
                        scores(ps, c0, n_)
                        expo(ps, ci, c0, n_)
                    cx.dve(lambda e: e.reduce_sum(rs[:], crs[:, 0:ncn], AX.X), r=[crs], w=[rs])
                    cx.dve(lambda e: e.reciprocal(rinv[:], rs[:]), r=[rs], w=[rinv])
                if normalize:
                    if i0:
                        cx.dve(lambda e: e.tensor_tensor(rinv[:], rinv[:], rvalid[:], ALU.mult), r=[rinv, rvalid], w=[rinv])
                    cx.dve(lambda e: e.tensor_scalar_mul(pb[:, 0:nk], dst32[:, 0:nk], rinv[:, 0:1]), r=[dst32, rinv], w=[pb])
                dstT = keepT if keepT is not None else pT
                nkt = nk // 128
                for t0 in range(0, nkt, 8):
                    n8 = min(8, nkt - t0)
                    tp = nTP()
                    for t in range(n8):
                        cx.pe(lambda e, tp=tp, t=t, t0=t0: e.transpose(tp[:, t, :], pb[:, (t0 + t) * 128:(t0 + t + 1) * 128], ident[:]),
                              r=[pb, ident], w=[tp])
                    cp(dstT[:, t0:t0 + n8, :], tp[:, 0:n8, :], [tp], [dstT])
                po = nPV()
                for t in range(nkt):
                    v_ap, v_r = vfn(t)
                    cx.pe(lambda e, po=po, t=t, v_ap=v_ap: e.matmul(po[:], dstT[:, t, :], v_ap, start=(t == 0), stop=(t == nkt - 1)),
                          r=[dstT] + v_r, w=[po])
                if normalize:
                    sc_ap, sc_r = gate_ap, [gate_r]
                else:
                    cx.dve(lambda e: e.tensor_tensor(fac[:], rinv[:], gate_ap, ALU.mult), r=[rinv, gate_r], w=[fac])
                    sc_ap, sc_r = fac[:, 0:1], [fac]
                if first:
                    cx.dve(lambda e, po=po: e.tensor_scalar_mul(acc_ap, po[:], sc_ap), r=[po] + sc_r, w=[acc])
                else:
                    cx.dve(lambda e, po=po: e.scalar_tensor_tensor(acc_ap, po[:], sc_ap, acc_ap, ALU.mult, ALU.add),
                           r=[po, acc] + sc_r, w=[acc])

            return p1, p2

        items = []

        def block(i, hq, cm, fb, mt, gates):
            nk = 128 * (i + 1)
            kt0 = max(0, i - 4)
            nkw = 128 * (i - kt0 + 1)

            def blk_pre():
                cx.dma(hq[:], dram_fm(hT_ap, 0, 8, i * 128, (i + 1) * 128), writes=[hq])
                cx.dma(cm[:], cn["cmask"][i], writes=[cm])
                cx.dma(fb[:], cn["fbias"][i], writes=[fb])
                pg = nPV()
                for kc in range(8):
                    cx.pe(lambda e, kc=kc, pg=pg: e.matmul(pg[:, 0:24], hq[:, kc, :], wb[:, kc, 1280:1304],
                                                           start=(kc == 0), stop=(kc == 7)), r=[hq, wb], w=[pg])
                cx.act(lambda e, pg=pg: e.activation(gates[:], pg[:, 0:24], AF.Sigmoid), r=[pg], w=[gates])

            def blk_post():
                cx.act(lambda e: e.copy(accb[:], acc[:]), r=[acc], w=[accb])
                tp = nTP()
                for c in range(4):
                    cx.pe(lambda e, c=c, tp=tp: e.transpose(tp[:, c, :], accb[:, c * 128:(c + 1) * 128], ident[:]),
                          r=[accb, ident], w=[tp])
                cp(mt[:], tp[:, 0:4, :], [tp], [mt])
                cx.dma(dram_fm(mixT_ap, 4, 8, i * 128, (i + 1) * 128), mt[:], reads=[mt], q="pool")

            def group(g):
                qs = qsel[g]
                qk = (qs.key, "q")
                sk = (qs.key, "s")

                def q_pre():
                    for hp in range(4):
                        hd = g * 4 + hp
                        ps = nP()
                        proj_fm(cx, ps, wb, hd * 64, 64, hq, 128)
                        cp(qs[0:64, hp, :], ps[0:64, 0:128], [ps], [qk])

                def sel_pre():
                    for hp in range(4):
                        for kc2 in range(2):
                            cx.pe(lambda e, hp=hp, kc2=kc2: e.matmul(IMP[:], pcT[hp][:, kc2, :], ovl[:, kc2, :],
                                                                     start=(hp == 0 and kc2 == 0), stop=(hp == 3 and kc2 == 1)),
                                  r=[pcT[hp], ovl], w=[IMP])
                    cx.dve(lambda e: e.tensor_tensor(sc64[:], IMP[:], fb[:], ALU.add), r=[IMP, fb], w=[sc64])
                    cx.dve(lambda e: e.max(top8[:], sc64[:]), r=[sc64], w=[top8])
                    cx.dve(lambda e: e.tensor_scalar(sel01[:], sc64[:], top8[:, 7:8], None, ALU.is_ge), r=[sc64, top8], w=[sel01])
                    cx.dve(lambda e: e.tensor_scalar(selw[:, 64:128], sel01[:], -NEG8, NEG8, ALU.mult, ALU.add), r=[sel01], w=[selw])
                    tps = nTP()
                    cx.pe(lambda e: e.transpose(tps[:, 0, :], selw[:], ident[:]), r=[selw, ident], w=[tps])
                    cx.act(lambda e: e.copy(qs[64:128, :, :], tps[64:128, 0:1, :].to_broadcast([64, 4, 128])),
                           r=[tps], w=[sk])

                def mk_cmp(hp):
                    hd = g * 4 + hp
                    return lambda: softmax_item(
                        qs[0:64, hp, :], [qk], kcmpT[g], 0, 256,
                        lambda c0, n_: [(ident[:], cm[:, c0:c0 + n_], 0, n_, [ident, cm])],
                        lambda t: (vcmp[:, t, g, :], [vcmp]),
                        gates[:, hd:hd + 1], gates, acc[:, hd * 64:(hd + 1) * 64], True, True, keepT=pcT[hp], i0=(i == 0))

                def mk_win(hp):
                    hd = g * 4 + hp
                    return lambda: softmax_item(
                        qs[0:64, hp, :], [qk], kwT[g], kt0 * 128, nkw,
                        lambda c0, n_: [(ident[:], wmask[:, 640 - nkw + c0:640 - nkw + c0 + n_], 0, n_, [ident, wmask])],
                        lambda t: (vsw[:, kt0 + t, 128 + g * 64:128 + (g + 1) * 64], [vsw]),
                        gates[:, 16 + hd:17 + hd], gates, acc[:, hd * 64:(hd + 1) * 64], False, False)

                def mk_slc(hp):
                    hd = g * 4 + hp
                    return lambda: softmax_item(
                        qs[:, hp, :], [qk, sk], ksE[g], 0, nk,
                        lambda c0, n_: ([(ident[:], causal[:], n_ - 128, n_, [ident, causal])] if c0 + n_ == nk else []),
                        lambda t: (vsw[:, t, g * 64:(g + 1) * 64], [vsw]),
                        gates[:, 8 + hd:9 + hd], gates, acc[:, hd * 64:(hd + 1) * 64], False, False)

                lst = []
                for hp in range(4):
                    lst.append([q_pre if hp == 0 else None, mk_cmp(hp), None])
                for hp in range(4):
                    lst.append([sel_pre if hp == 1 else None, mk_win(hp), None])
                for hp in range(4):
                    lst.append([None, mk_slc(hp), None])
                return lst

            lst = group(0) + group(1)
            first_pre = lst[0][0]
            lst[0][0] = lambda: (blk_pre(), first_pre())
            lst[-1][2] = blk_post
            items.extend(lst)

        gates2 = [cx.sb(es, "gates%d" % i_, [128, 24], F32) for i_ in range(2)]
        for i in range(NB):
            block(i, hqs[i % 2], cms[i % 2], fbs[i % 2], mixt[i % 2], gates2[i % 2])
        prev = None
        for pre, mk, post in items:
            if pre is not None:
                pre()
            p1, p2 = mk()
            p1()
            if not NSA_PIPE:
                p2()
                if post is not None:
                    post()
                continue
            if prev is not None:
                prev[0]()
                if prev[1] is not None:
                    prev[1]()
            prev = (p2, post)
        if NSA_PIPE:
            prev[0]()
            if prev[1] is not None:
                prev[1]()
    cx.barrier()


def nsa2_consts(S):
    import ml_dtypes
    bf = ml_dtypes.bfloat16
    nb = S // 128
    base = nsa_consts(S)
    c = {"fbias": base["fbias"], "blockE": base["blockE"]}
    cm = base["cmask"].astype(np.float32)
    cmT = cm.transpose(0, 2, 1).reshape(nb, 2, 128, 128)
    cmT = np.broadcast_to(cmT.transpose(0, 2, 1, 3)[:, :, :, None, :], (nb, 128, 2, 4, 128))
    c["cmaskT"] = np.ascontiguousarray(cmT).astype(bf)
    kl = np.arange(128)[:, None]
    ql = np.arange(128)[None, :]
    cz = np.where(kl <= ql, 0.0, NEG8).astype(np.float32)
    w0 = np.where(kl > ql, 0.0, NEG8).astype(np.float32)
    c["causalT4"] = np.ascontiguousarray(np.broadcast_to(cz[:, None, :], (128, 4, 128))).astype(bf)
    c["wm0T4"] = np.ascontiguousarray(np.broadcast_to(w0[:, None, :], (128, 4, 128))).astype(bf)
    ov = np.ones((256, 80), np.float32)
    ov[:, 0:64] = base["ovl"].astype(np.float32)
    c["ovla"] = ov.astype(bf)
    sr = np.zeros((24, 24, 64), np.float32)
    for r in range(24):
        sr[r, r, :] = 1.0
    c["selrows"] = sr
    return c


NSA2_STOP = ""


def stage_nsa2(cx, hT_ap, w_ap, cw, mixT_ap, ident_ap, cn, S):
    NB = S // 128
    NT = S // 128
    with ExitStack() as es:
        wb = cx.sb(es, "wnsa", [128, 8, 1312], BF16)
        cx.dve(lambda e: e.memset(wb[:, :, 1304:1312], 0.0), w=[(wb.key, "pad")])
        ksE = [cx.sb(es, "ksE%d" % g, [128, S], BF16) for g in range(2)]
        kwT = [cx.sb(es, "kwT%d" % g, [64, S], BF16) for g in range(2)]
        vaug = cx.sb(es, "vaug", [128, NT, 4, 80], BF16)
        kcmpT = [cx.sb(es, "kcmpT%d" % g, [64, 256], BF16) for g in range(2)]
        vcmp = cx.sb(es, "vcmp", [128, 2, 2, 80], BF16)
        ident = cx.sb(es, "ident", [128, 128], BF16)
        onesb = cx.sb(es, "onesb", [128, 128], BF16)
        ones32 = cx.sb(es, "ones32", [128, 64], F32)
        kmx = cx.sb(es, "kmx", [128, 8], F32)
        P = [cx.ps(es, "P%d" % i, [128, 512], F32) for i in range(3)]
        OTs = [cx.ps(es, "OT%d" % i, [128, 512], F32) for i in range(2)]
        RP = cx.ps(es, "RP", [128, 4, 128], F32)
        M1 = cx.ps(es, "M1", [128, 512], F32)
        M2 = cx.ps(es, "M2", [128, 8, 128], BF16)
        cnt = {"p": 0, "cp": 0, "o": 0, "pt": 0}

        def nP():
            cnt["p"] += 1
            return P[cnt["p"] % 3]

        def nO():
            cnt["o"] += 1
            return OTs[cnt["o"] % 2]

        def cp(out_ap, in_ap, r, w):
            cnt["cp"] += 1
            if cnt["cp"] % 2:
                cx.act(lambda e: e.copy(out_ap, in_ap), r=r, w=w)
            else:
                cx.dve(lambda e: e.tensor_copy(out_ap, in_ap), r=r, w=w)

        cx.dve(lambda e: e.memset(onesb[:], 1.0), w=[onesb])
        cx.dve(lambda e: e.memset(ones32[:], 1.0), w=[ones32])
        cx.dve(lambda e: e.memset(vaug[:], 1.0), w=[vaug])
        with ExitStack() as es2:
            stg = [cx.sb(es2, "wstg%d" % i, [128, 1304], F32) for i in range(4)]
            load_weight_bf16(cx, wb, w_ap, 8, 1304, stg)
            cx.dma(ident[:], ident_ap, writes=[ident])
            for g in range(2):
                cx.dma(ksE[g][64:128, :], cn["blockE"], writes=[(ksE[g].key, "E")])
            cx.barrier()
            kcT = [cx.sb(es2, "kcT%d" % g, [64, S], BF16) for g in range(2)]
            vcT = [cx.sb(es2, "vcT%d" % g, [64, S], BF16) for g in range(2)]
            hins = [cx.sb(es2, "hin%d" % i, [128, 8, MTK], BF16) for i in range(2)]

            def phaseA(m, hin):
                cx.dma(hin[:], dram_fm(hT_ap, 0, 8, m * MTK, (m + 1) * MTK), writes=[hin])
                for (dst, col) in ((kcT, 512), (vcT, 640), (ksE, 768), (kwT, 896)):
                    for g in range(2):
                        ps = nP()
                        proj_fm(cx, ps, wb, col + g * 64, 64, hin, MTK)
                        cp(dst[g][0:64, m * MTK:(m + 1) * MTK], ps[0:64, :], [ps], [dst[g]])
                for j in range(4):
                    ps = nP()
                    for kc in range(8):
                        cx.pe(lambda e, ps=ps, kc=kc, j=j: e.matmul(
                            ps[:, 0:256], hin[:, kc, j * 128:(j + 1) * 128], wb[:, kc, 1024:1280],
                            start=(kc == 0), stop=(kc == 7)), r=[hin, wb], w=[ps])
                    cp(vaug[:, m * 4 + j, :, 0:64], ps[:, 0:256].rearrange("p (v d) -> p v d", d=64), [ps], [vaug])

            for m in range(S // MTK):
                phaseA(m, hins[m % 2])

            w1s = cx.sb(es2, "w1s", [64, 32, 64], F32)
            w1b = cx.sb(es2, "w1b", [64, 32, 64], BF16)
            w2s = cx.sb(es2, "w2s", [64, 64], F32)
            w2b = cx.sb(es2, "w2b", [64, 64], BF16)
            poss = cx.sb(es2, "poss", [64, 32], F32)
            posb = cx.sb(es2, "posb", [64, 32], BF16)
            cb = cx.sb(es2, "cb", [64, 1], F32)
            tt = [cx.sb(es2, "gt%d" % i, [64, 256], F32) for i in range(3)]
            glb = cx.sb(es2, "glb", [64, 256], BF16)
            for g in range(2):
                cx.dve(lambda e, g=g: e.memset(kcmpT[g][:], 0.0), w=[kcmpT[g]])
            cx.dve(lambda e: e.memset(vcmp[:], 0.0), w=[vcmp])
            cx.dve(lambda e: e.memset(vcmp[:, :, :, 64:80], 1.0), r=[vcmp], w=[vcmp])
            cx.dve(lambda e: e.memset(glb[:], 0.0), w=[glb])
            NCMP = S // 16 - 1

            def phaseB(kind, g, src):
                pos_ap, w1_ap, w2_ap = cw["pos_" + kind], cw["w1_" + kind], cw["w2_" + kind]
                if g == 0:
                    cx.dma(w1s[:], w1_ap.rearrange("(p d) o -> d p o", d=64), writes=[w1s])
                    cx.dma(w2s[:], w2_ap, writes=[w2s])
                    cx.dma(poss[:], pos_ap.rearrange("p d -> d p"), writes=[poss], allow_slow_non_contiguous=True)
                    cx.dve(lambda e: e.tensor_copy(w1b[:], w1s[:]), r=[w1s], w=[w1b])
                    cx.dve(lambda e: e.tensor_copy(w2b[:], w2s[:]), r=[w2s], w=[w2b])
                    cx.dve(lambda e: e.tensor_copy(posb[:], poss[:]), r=[poss], w=[posb])
                    for p in range(32):
                        cx.pe(lambda e, p=p: e.matmul(M1[0:64, 0:1], w1b[:, p, :], posb[:, p:p + 1],
                                                      start=(p == 0), stop=(p == 31)), r=[w1b, posb], w=[M1])
                    cx.act(lambda e: e.copy(cb[:], M1[0:64, 0:1]), r=[M1], w=[cb])
                ps = nP()
                x3 = src[0:64, :].rearrange("d (n s) -> d n s", s=16)
                for p in range(32):
                    n0, r_ = (0, p) if p < 16 else (1, p - 16)
                    cx.pe(lambda e, p=p, n0=n0, r_=r_, ps=ps: e.matmul(
                        ps[0:64, 0:NCMP], w1b[:, p, :], x3[:, n0:n0 + NCMP, r_],
                        start=(p == 0), stop=(p == 31)), r=[w1b, src], w=[ps])
                t0, t1_, t2_ = tt
                N = NCMP
                cx.act(lambda e, ps=ps: e.activation(t0[:, 0:N], ps[0:64, 0:N], AF.Identity, bias=cb[:], scale=1.0),
                       r=[ps, cb], w=[t0])
                cx.dve(lambda e: e.tensor_tensor(t1_[:, 0:N], t0[:, 0:N], t0[:, 0:N], ALU.mult), r=[t0], w=[t1_])
                cx.dve(lambda e: e.tensor_scalar(t1_[:, 0:N], t1_[:, 0:N], 0.044715, 1.0, ALU.mult, ALU.add), r=[t1_], w=[t1_])
                cx.dve(lambda e: e.tensor_tensor(t1_[:, 0:N], t1_[:, 0:N], t0[:, 0:N], ALU.mult), r=[t1_, t0], w=[t1_])
                cx.act(lambda e: e.activation(t2_[:, 0:N], t1_[:, 0:N], AF.Sigmoid, scale=2.0 * math.sqrt(2.0 / math.pi)),
                       r=[t1_], w=[t2_])
                cx.dve(lambda e: e.tensor_tensor(glb[:, 0:N], t0[:, 0:N], t2_[:, 0:N], ALU.mult), r=[t0, t2_], w=[glb])
                if kind == "k":
                    po = nP()
                    cx.pe(lambda e, po=po: e.matmul(po[0:64, 0:N], w2b[:], glb[:, 0:N], start=True, stop=True),
                          r=[w2b, glb], w=[po])
                    cp(kcmpT[g][:, 0:N], po[0:64, 0:N], [po], [kcmpT[g]])
                else:
                    for kc2 in range(2):
                        n1 = min(128, N - kc2 * 128)
                        if n1 <= 0:
                            continue
                        po = nP()
                        cx.pe(lambda e, po=po, kc2=kc2, n1=n1: e.matmul(
                            po[0:n1, 0:64], glb[:, kc2 * 128:kc2 * 128 + n1], w2b[:], start=True, stop=True),
                            r=[glb, w2b], w=[po])
                        cp(vcmp[0:n1, kc2, g, 0:64], po[0:n1, 0:64], [po], [vcmp])

            for kind, srcs in (("k", kcT), ("v", vcT)):
                for g in range(2):
                    phaseB(kind, g, srcs[g])

            sqk = [cx.sb(es2, "sqk%d" % i, [64, 512], BF16) for i in range(2)]
            kcm = cx.sb(es2, "kcm", [128, 8], F32)
            qi = [0]

            def kmax(src, ncols, col):
                nchunk = (ncols + 511) // 512
                for ci in range(nchunk):
                    c0 = ci * 512
                    n_ = min(512, ncols - c0)
                    sq = sqk[qi[0] % 2]
                    qi[0] += 1
                    cx.act(lambda e, sq=sq, c0=c0, n_=n_: e.activation(sq[:, 0:n_], src[0:64, c0:c0 + n_], AF.Square),
                           r=[src], w=[sq])
                    ps = nP()
                    cx.pe(lambda e, ps=ps, sq=sq, n_=n_: e.matmul(ps[:, 0:n_], onesb[0:64, :], sq[:, 0:n_], start=True, stop=True),
                          r=[onesb, sq], w=[ps])
                    cx.dve(lambda e, ps=ps, ci=ci, n_=n_: e.reduce_max(kcm[:, ci:ci + 1], ps[:, 0:n_], AX.X), r=[ps], w=[kcm])
                cx.dve(lambda e: e.tensor_reduce(kmx[:, col:col + 1], kcm[:, 0:nchunk], AX.X, ALU.max), r=[kcm], w=[kmx])

            for g in range(2):
                kmax(kcmpT[g], 256, 0 + g)
                kmax(kwT[g], S, 2 + g)
                kmax(ksE[g], S, 4 + g)
            cx.barrier()

        def cload(name, shape, dtype, src):
            t = cx.sb(es, name, shape, dtype)
            cx.dma(t[:], src, writes=[t])
            return t

        ovla = cload("ovla", [128, 2, 80], BF16, cn["ovla"].rearrange("(c p) j -> p c j", p=128))
        causalT4 = cload("causalT4", [128, 4, 128], BF16, cn["causalT4"])
        wm0T4 = cload("wm0T4", [128, 4, 128], BF16, cn["wm0T4"])
        selrows = cload("selrows", [24, 24, 64], F32, cn["selrows"])
        hqs = [cx.sb(es, "hq%d" % i, [128, 8, 128], BF16) for i in range(2)]
        cmTs = [cx.sb(es, "cmT%d" % i, [128, 2, 4, 128], BF16) for i in range(2)]
        fbs = [cx.sb(es, "fb%d" % i, [128, 64], F32) for i in range(2)]
        gatesT = [cx.sb(es, "gatesT%d" % i, [32, 128], F32) for i in range(2)]
        qsel = [cx.sb(es, "qsel%d" % i, [128, 4, 128], BF16) for i in range(2)]
        sqqs = [cx.sb(es, "sqq%d" % i, [64, 4, 128], BF16) for i in range(2)]
        qms = [cx.sb(es, "qm%d" % i, [128, 1], F32) for i in range(2)]
        negcs = [[cx.sb(es, "negc%d_%d" % (g_, i), [128, 1], F32) for i in range(3)] for g_ in range(2)]
        pending = []

        def flush():
            while pending:
                pending.pop(0)()
        selw = cx.sb(es, "selw", [128, 128], BF16)
        cx.dve(lambda e: e.memset(selw[:], 0.0), w=[selw])
        PcT = cx.sb(es, "PcT", [128, 2, 512], BF16)
        NPT = 6
        PTs = [cx.sb(es, "PT%d" % i, [128, 512], BF16) for i in range(NPT)]
        rsr = cx.sb(es, "rsr", [128, 512], F32)
        bcs = cx.sb(es, "bcs", [64, 512], F32)
        tmpo = cx.sb(es, "tmpo", [64, 512], F32)
        accT = [cx.sb(es, "accT%d" % i, [64, 8, 128], F32) for i in range(2)]
        accTb = [cx.sb(es, "accTb%d" % i, [64, 8, 128], BF16) for i in range(2)]
        rs4 = cx.sb(es, "rs4", [128, 4], F32)
        impb = cx.sb(es, "impb", [128, 64], F32)
        sc64 = cx.sb(es, "sc64", [128, 64], F32)
        top8 = cx.sb(es, "top8", [128, 8], F32)
        sel01 = cx.sb(es, "sel01", [128, 64], F32)
        SCALE = 0.125
        mix_dst = mixT_ap[4:8].rearrange("c (two d) s -> d (c two) s", two=2)

        def block(i, hq, cmT, fb, gT, acc, accb):
            kt0 = max(0, i - 4)
            cx.dma(hq[:], dram_fm(hT_ap, 0, 8, i * 128, (i + 1) * 128), writes=[hq])
            cx.dma(cmT[:], cn["cmaskT"][i], writes=[cmT])
            cx.dma(fb[:], cn["fbias"][i], writes=[fb])
            for kc in range(8):
                cx.pe(lambda e, kc=kc: e.matmul(M1[0:32, 0:128], wb[:, kc, 1280:1312], hq[:, kc, :],
                                                start=(kc == 0), stop=(kc == 7)), r=[hq, wb], w=[M1])
            cx.act(lambda e: e.activation(gT[:], M1[0:32, 0:128], AF.Sigmoid), r=[M1], w=[gT])

            def finalize(OT, g, br, first):
                accg = acc[:, g * 4:(g + 1) * 4, :]
                cx.dve(lambda e: e.tensor_scalar_max(rsr[64:65, :], OT[64:65, :], 1e-30), r=[OT], w=[rsr])
                cx.dve(lambda e: e.reciprocal(rsr[64:65, :], rsr[64:65, :]), r=[rsr], w=[rsr])
                cx.pe(lambda e: e.matmul(M1[0:64, :], ones32[64:65, 0:64], rsr[64:65, :], start=True, stop=True),
                      r=[ones32, rsr], w=[M1])
                cx.act(lambda e: e.copy(bcs[:], M1[0:64, :]), r=[M1], w=[bcs])
                cx.dve(lambda e: e.tensor_tensor(tmpo[:], OT[0:64, :], bcs[:], ALU.mult), r=[OT, bcs], w=[tmpo])
                for hp in range(4):
                    r_ = br * 8 + g * 4 + hp
                    cx.pe(lambda e, hp=hp, r_=r_: e.matmul(M1[0:64, hp * 128:(hp + 1) * 128], selrows[:, r_, :], gT[0:24, :],
                                                           start=True, stop=True), r=[selrows, gT], w=[M1])
                t3 = tmpo[:].rearrange("p (h q) -> p h q", q=128)
                m3 = M1[0:64, :].rearrange("p (h q) -> p h q", q=128)
                if first:
                    cx.dve(lambda e: e.tensor_tensor(accg, t3, m3, ALU.mult), r=[tmpo, M1], w=[acc])
                else:
                    cx.dve(lambda e: e.tensor_tensor(t3, t3, m3, ALU.mult), r=[tmpo, M1], w=[tmpo])
                    cx.dve(lambda e: e.tensor_tensor(accg, accg, t3, ALU.add), r=[acc, tmpo], w=[acc])

            def run_tiles(tiles, qrows, ncg, vfn, mfn, qsel_key):
                OT = nO()
                pend = None
                n = len(tiles)
                for t, (l_ap, l_r) in enumerate(tiles):
                    ps = nP()
                    mm = mfn(t)
                    cx.pe(lambda e, ps=ps, l_ap=l_ap, stp=(mm is None): e.matmul(ps[:], l_ap, qrows, start=True, stop=stp),
                          r=l_r + [qsel_key], w=[ps])
                    if mm is not None:
                        cx.pe(lambda e, ps=ps, mm=mm: e.matmul(ps[:], ident[:], mm[0], start=False, stop=True),
                              r=[ident] + mm[1], w=[ps])
                    cnt["pt"] += 1
                    pt = PTs[cnt["pt"] % NPT]
                    cx.act(lambda e, ps=ps, pt=pt: e.activation(pt[:], ps[:], AF.Exp, bias=ncg[:], scale=SCALE),
                           r=[ps, ncg], w=[pt])
                    if pend is not None:
                        pend()
                    v_ap, v_r = vfn(t)
                    pend = (lambda t=t, pt=pt, v_ap=v_ap, v_r=v_r: cx.pe(
                        lambda e: e.matmul(OT[0:80, :], v_ap[:, 0:80], pt[:], start=(t == 0), stop=(t == n - 1)),
                        r=[pt] + v_r, w=[OT]))
                pend()
                return OT

            def prelude(g):
                qs = qsel[g]
                qk = (qs.key, "q")
                sqq, qm, negc = sqqs[g], qms[g], negcs[g]
                for hp in range(4):
                    hd = g * 4 + hp
                    for kc in range(8):
                        cx.pe(lambda e, kc=kc, hp=hp, hd=hd: e.matmul(M1[0:64, hp * 128:(hp + 1) * 128], wb[:, kc, hd * 64:(hd + 1) * 64],
                                                                     hq[:, kc, :], start=(kc == 0), stop=(kc == 7)),
                              r=[wb, hq], w=[M1])
                cx.dve(lambda e: e.tensor_copy(qs[0:64, :, :].rearrange("p h q -> p (h q)"), M1[0:64, :]), r=[M1], w=[qk])
                cx.act(lambda e: e.activation(sqq[:].rearrange("p h q -> p (h q)"), M1[0:64, :], AF.Square), r=[M1], w=[sqq])
                ps = nP()
                cx.pe(lambda e: e.matmul(ps[:], onesb[0:64, :], sqq[:].rearrange("p h q -> p (h q)"), start=True, stop=True),
                      r=[onesb, sqq], w=[ps])
                cx.dve(lambda e: e.reduce_max(qm[:], ps[:], AX.X), r=[ps], w=[qm])
                for br in range(3):
                    nb_ = negc[br]
                    cx.dve(lambda e, nb_=nb_, br=br: e.tensor_tensor(nb_[:], qm[:], kmx[:, 2 * br + g:2 * br + g + 1], ALU.mult),
                           r=[qm, kmx], w=[nb_])
                    cx.act(lambda e, nb_=nb_: e.activation(nb_[:], nb_[:], AF.Sqrt), r=[nb_], w=[nb_])
                    cx.dve(lambda e, nb_=nb_: e.tensor_scalar_mul(nb_[:], nb_[:], -SCALE * 1.05), r=[nb_], w=[nb_])

            def group(g):
                qs = qsel[g]
                qk = (qs.key, "q")
                sk = (qs.key, "s")
                negc = negcs[g]
                global_q = qs[0:64, :, :].rearrange("p h q -> p (h q)")
                OTc = nO()
                for kc2 in range(2):
                    ps = nP()
                    cx.pe(lambda e, ps=ps, kc2=kc2: e.matmul(ps[:], kcmpT[g][:, kc2 * 128:(kc2 + 1) * 128], global_q,
                                                            start=True, stop=False), r=[kcmpT[g], qk], w=[ps])
                    cx.pe(lambda e, ps=ps, kc2=kc2: e.matmul(ps[:], ident[:], cmT[:, kc2, :, :].rearrange("p h q -> p (h q)"),
                                                            start=False, stop=True), r=[ident, cmT], w=[ps])
                    cx.act(lambda e, ps=ps, kc2=kc2: e.activation(PcT[:, kc2, :], ps[:], AF.Exp, bias=negc[0][:], scale=SCALE),
                           r=[ps, negc[0]], w=[PcT])
                for kc2 in range(2):
                    cx.pe(lambda e, kc2=kc2: e.matmul(OTc[0:80, :], vcmp[:, kc2, g, 0:80], PcT[:, kc2, :],
                                                      start=(kc2 == 0), stop=(kc2 == 1)), r=[vcmp, PcT], w=[OTc])
                for hp in range(4):
                    for kc2 in range(2):
                        cx.pe(lambda e, hp=hp, kc2=kc2: e.matmul(RP[:, hp, 0:80], PcT[:, kc2, hp * 128:(hp + 1) * 128], ovla[:, kc2, :],
                                                                 start=(kc2 == 0), stop=(kc2 == 1)), r=[PcT, ovla], w=[RP])
                cx.dve(lambda e: e.tensor_scalar_max(rs4[:], RP[:, :, 64], 1e-30), r=[RP], w=[rs4])
                cx.dve(lambda e: e.reciprocal(rs4[:], rs4[:]), r=[rs4], w=[rs4])
                cx.dve(lambda e: e.scalar_tensor_tensor(impb[:], RP[:, 0, 0:64], rs4[:, 0:1], fb[:], ALU.mult, ALU.add),
                       r=[RP, rs4, fb], w=[impb])
                for hp in range(1, 4):
                    cx.dve(lambda e, hp=hp: e.scalar_tensor_tensor(impb[:], RP[:, hp, 0:64], rs4[:, hp:hp + 1], impb[:], ALU.mult, ALU.add),
                           r=[RP, rs4, impb], w=[impb])
                cx.dve(lambda e: e.max(top8[:], impb[:]), r=[impb], w=[top8])
                cx.dve(lambda e: e.tensor_scalar(sel01[:], impb[:], top8[:, 7:8], None, ALU.is_ge), r=[impb, top8], w=[sel01])
                cx.dve(lambda e: e.tensor_scalar(selw[:, 64:128], sel01[:], -NEG8, NEG8, ALU.mult, ALU.add), r=[sel01], w=[selw])
                flush()
                wt = list(range(kt0, i + 1))

                def wmask(t):
                    r_ = wt[t] - (i - 4)
                    if r_ == 0:
                        return (wm0T4[:].rearrange("p h q -> p (h q)"), [wm0T4])
                    if r_ == 4:
                        return (causalT4[:].rearrange("p h q -> p (h q)"), [causalT4])
                    return None

                OTw = run_tiles([(kwT[g][:, kt * 128:(kt + 1) * 128], [kwT[g]]) for kt in wt], global_q, negc[1],
                                lambda t: (vaug[:, wt[t], 2 + g, :], [vaug]), wmask, qk)
                cx.pe(lambda e: e.transpose(M2[:, 0, :], selw[:], ident[:]), r=[selw, ident], w=[M2])
                cx.act(lambda e: e.copy(qs[64:128, :, :], M2[64:128, 0:1, :].to_broadcast([64, 4, 128])), r=[M2], w=[sk])
                finalize(OTc, g, 0, True)
                qfull = qs[:, :, :].rearrange("p h q -> p (h q)")
                OTs = run_tiles([(ksE[g][:, kt * 128:(kt + 1) * 128], [ksE[g], (ksE[g].key, "E"), qk]) for kt in range(i + 1)],
                                qfull, negc[2], lambda t: (vaug[:, t, g, :], [vaug]),
                                lambda t: ((causalT4[:].rearrange("p h q -> p (h q)"), [causalT4]) if t == i else None), sk)
                finalize(OTw, g, 2, False)
                pending.append(lambda: finalize(OTs, g, 1, False))

            def blk_post():
                cx.act(lambda e: e.copy(accb[:], acc[:]), r=[acc], w=[accb])
                cx.dma(mix_dst[:, :, i * 128:(i + 1) * 128], accb[:], reads=[accb], q="pool")

            prelude(0)
            prelude(1)
            group(0)
            group(1)
            pending.append(blk_post)

        for i in range(NB if NSA2_STOP != "AB" else 0):
            block(i, hqs[i % 2], cmTs[i % 2], fbs[i % 2], gatesT[i % 2], accT[i % 2], accTb[i % 2])
        flush()
    cx.barrier()


SEQ = 4096
NCORES = 8
DEPTH = 4


def build_full(S=SEQ, depth=DEPTH):
    nc = bass.Bass("TRN2", target_bir_lowering=False)

    def din(n, s, d=F32):
        return nc.dram_tensor(n, list(s), d, kind="ExternalInput").ap()

    x = din("x", [S, D])
    norm_mix_g = din("norm_mix_g", [4, D])
    norm_ffn_g = din("norm_ffn_g", [4, D])
    final_norm_g = din("final_norm_g", [D])
    w_ret = din("w_ret", [2, D, 3072])
    w_nsa = din("w_nsa", [2, D, 1304])
    even_w_out = din("even_w_out", [2, D, D])
    cws = {}
    for kind in "kv":
        cws["pos_" + kind] = din("cmp_pos_" + kind, [2, 32, 64])
        cws["w1_" + kind] = din("cmp_w1_" + kind, [2, 2048, 64])
        cws["w2_" + kind] = din("cmp_w2_" + kind, [2, 64, 64])
    odd_w_in = din("odd_w_in", [2, D, 4096])
    odd_w_out = din("odd_w_out", [2, D, D])
    hgrn_norm_g = din("hgrn_norm_g", [2, 128])
    hgrn_lb = din("hgrn_lb_logits", [2, 1024])
    ffn_w1 = din("ffn_w1", [4, D, DFF])
    ffn_w3 = din("ffn_w3", [4, D, DFF])
    ffn_w2 = din("ffn_w2", [4, DFF, D])
    ident = din("c_ident", [128, 128], BF16)
    maskT = din("c_maskT", [64, 64])
    scanm = din("c_scanm", [128, MTK])
    cos = din("c_cos", [128, S])
    sin = din("c_sin", [128, S])
    dec = din("c_dec", [4, 2, 128, MTK])
    NB = S // 128
    cn = {
        "cmaskT": din("c_cmaskT", [NB, 128, 2, 4, 128], BF16),
        "fbias": din("c_fbias", [NB, 128, 64]),
        "blockE": din("c_blockE", [64, S], BF16),
        "causalT4": din("c_causalT4", [128, 4, 128], BF16),
        "wm0T4": din("c_wm0T4", [128, 4, 128], BF16),
        "ovla": din("c_ovla", [256, 80], BF16),
        "selrows": din("c_selrows", [24, 24, 64]),
    }
    y = nc.dram_tensor("y", [S, D], F32, kind="ExternalOutput").ap()
    xs = nc.dram_tensor("xs", [S, D], F32).ap()
    hTa = nc.dram_tensor("hTa", [8, 128, S], BF16).ap()
    hTb = nc.dram_tensor("hTb", [8, 128, S], BF16).ap()
    mixT = nc.dram_tensor("mixT", [8, 128, S], BF16).ap()

    cx = Ctx(nc)
    stage_norm0(cx, x, hTb, norm_mix_g[0], ident, S)
    for layer in range(depth):
        j = layer // 2
        if layer % 2 == 0:
            stage_ret(cx, hTb, w_ret[j], cos, sin, dec, mixT, ident, maskT, S)
            cw = {k_: v_[j] for k_, v_ in cws.items()}
            stage_nsa2(cx, hTb, w_nsa[j], cw, mixT, ident, cn, S)
            w_out = even_w_out[j]
        else:
            stage_hgrn(cx, hTb, odd_w_in[j], hgrn_norm_g[j], hgrn_lb, mixT, ident, maskT, scanm, S, j)
            w_out = odd_w_out[j]
        stage_out(cx, mixT, w_out, x if layer == 0 else xs, xs, hTa, norm_ffn_g[layer], ident, S)
        last = layer == depth - 1
        stage_ffn(cx, hTa, ffn_w1[layer], ffn_w3[layer], ffn_w2[layer], xs, xs, hTb,
                  norm_mix_g[min(layer + 1, 3)], ident, S, last, gfin_ap=final_norm_g, y_ap=y)
    cx.emit()
    return nc


def host_layout(inputs, S=SEQ):
    f32 = lambda a: np.ascontiguousarray(np.asarray(a, dtype=np.float32))
    ew = f32(inputs["even_w_in"])

    def swap(w):
        return w.reshape(w.shape[0], D, 4, 2, 64)[:, :, :, ::-1, :].reshape(w.shape[0], D, 512)

    rq, rk, rv, rg = ew[:, :, 0:512], ew[:, :, 512:1024], ew[:, :, 1024:1536], ew[:, :, 1536:2048]
    nq = ew[:, :, 2048:2560]
    kc, vc, ks, vs, kw, vw = [ew[:, :, 2560 + 128 * i:2560 + 128 * (i + 1)] for i in range(6)]
    ng = ew[:, :, 3328:3352]
    shared = {
        "w_ret": np.ascontiguousarray(np.concatenate([rq, rk, swap(rq), swap(rk), rv, rg], axis=2)),
        "w_nsa": np.ascontiguousarray(np.concatenate([nq, kc, vc, ks, kw, vs, vw, ng], axis=2)),
    }
    for k_ in ("norm_mix_g", "norm_ffn_g", "final_norm_g", "even_w_out", "cmp_pos_k", "cmp_w1_k", "cmp_w2_k",
               "cmp_pos_v", "cmp_w1_v", "cmp_w2_v", "odd_w_in", "odd_w_out", "hgrn_norm_g", "hgrn_lb_logits",
               "ffn_w1", "ffn_w3", "ffn_w2"):
        shared[k_] = f32(inputs[k_])
    for k_, v_ in host_consts(S).items():
        shared["c_" + k_] = v_
    for k_, v_ in nsa2_consts(S).items():
        shared["c_" + k_] = v_
    return shared


def kernel(**inputs):
    x = np.ascontiguousarray(np.asarray(inputs["x"], dtype=np.float32))
    B, S, _ = x.shape
    shared = host_layout(inputs, S)
    nc = build_full(S)
    in_maps = []
    for b in range(B):
        m = dict(shared)
        m["x"] = np.ascontiguousarray(x[b])
        in_maps.append(m)
    res = run_bass_kernel_spmd(nc, in_maps, core_ids=list(range(B)))
    return np.stack([np.asarray(r["y"], dtype=np.float32) for r in res.results], axis=0)
```
